# Optimizing a Trainium2 kernel written in Bass

```python
import math
import jax
import jax.numpy as jnp
from jax import lax
import numpy as np

D_MODEL = 2048
BATCH = 8
SEQ = 2048
DEPTH = 2

MIX_W = 1024
N_BRANCH = 3
HG_HEADS = 8
HG_DK = 128
HG_DV = 128
HG_CHUNK = 16
F_FLOOR = 1e-30
HY_W = 1024
HY_SHORT = 3
HY_EMB = 33
HY_BANDS = (HY_EMB - 1) // 2
HY_ORDER = 64
HY_INNER = 2
HY_FAST_DECAY = 0.3
HY_SLOW_DECAY = 1.5
HY_TARGET = 1e-2
HY_FILTER_SCALE = 0.05
ATT_HEADS = 16
ATT_KV_HEADS = 2
ATT_DH = 64
WINDOW = 128
ATT_BLOCK = 128
REL_BUCKETS = 32
REL_MAX_DIST = 128
MASK_VALUE = -1e30
N_EXPERTS = 32
TOP_K = 4
D_FF = 2048
SWIGLU_ALPHA = 1.702
SWIGLU_LIMIT = 7.0
MOE_BLOCK = 128
LN_EPS = 1e-5
RMS_EPS = 1e-6
DEEPNORM_ALPHA = (2 * DEPTH) ** 0.25
DEEPNORM_BETA = (8 * DEPTH) ** -0.25

HG_KW = HG_HEADS * HG_DK
HG_VW = HG_HEADS * HG_DV
ATT_QW = ATT_HEADS * ATT_DH
ATT_KVW = ATT_KV_HEADS * ATT_DH
IN_SIZES = (HG_KW, HG_KW, HG_KW, HG_VW, HG_VW, 3 * HY_W, ATT_QW, ATT_KVW, ATT_KVW, N_BRANCH * D_MODEL)
IN_COLS = sum(IN_SIZES)
IN_SPLITS = tuple(sum(IN_SIZES[:i + 1]) for i in range(len(IN_SIZES) - 1))

kernel_name = 'hybrid_hgrn2_hyena_swa_moe_encoder'


def layer_norm(x, g, b):
    xf = x.astype(jnp.float32)
    mu = jnp.mean(xf, axis=-1, keepdims=True)
    var = jnp.mean(jnp.square(xf - mu), axis=-1, keepdims=True)
    return ((xf - mu) * lax.rsqrt(var + LN_EPS) * g + b).astype(x.dtype)


def hgrn2_lower_bound(lb_table, layer):
    p = jax.nn.softmax(lb_table.astype(jnp.float32), axis=0)
    return jnp.cumsum(p, axis=0)[layer] - p[0]


def hgrn2_forget(z, lb):
    z = z.astype(jnp.float32)
    f = lb + (1.0 - lb) * jax.nn.sigmoid(z)
    return jnp.log(jnp.maximum(f, F_FLOOR)), (1.0 - lb) * jax.nn.sigmoid(-z)


def gla_chunked(q, k, v, log_f):
    B, L, H, DK = q.shape
    DV = v.shape[-1]
    N = L // HG_CHUNK
    q, k, v, log_f = (a.reshape(B, N, HG_CHUNK, H, a.shape[-1]) for a in (q, k, v, log_f))
    cum = jnp.cumsum(log_f, axis=2)
    last = cum[:, :, -1]
    past = jnp.tril(jnp.ones((HG_CHUNK, HG_CHUNK), bool))[None, None, :, :, None, None]
    dlog = cum[:, :, :, None] - cum[:, :, None, :]
    decay = jnp.where(past, jnp.exp(jnp.minimum(dlog, 0.0)), 0.0)
    scores = jnp.einsum('bntshk,bnshk->bnhts', q[:, :, :, None] * decay, k)
    o_intra = jnp.einsum('bnhts,bnshv->bnthv', scores, v)
    chunk_kv = jnp.einsum('bnshk,bnshv->bnhkv', k * jnp.exp(last[:, :, None] - cum), v)

    def carry_state(S, inp):
        dec, kv = inp
        return dec[..., None] * S + kv, S

    _, S_prev = lax.scan(carry_state, jnp.zeros((B, H, DK, DV), q.dtype),
                         (jnp.moveaxis(jnp.exp(last), 1, 0), jnp.moveaxis(chunk_kv, 1, 0)))
    S_prev = jnp.moveaxis(S_prev, 0, 1)
    o_inter = jnp.einsum('bnthk,bnhkv->bnthv', q * jnp.exp(cum), S_prev)
    return (o_intra + o_inter).reshape(B, L, H, DV)


def hgrn2_mixer(q, f_fwd, f_bwd, i, og, lb, norm_g):
    B, L, _ = q.shape
    heads = lambda a, d: a.reshape(B, L, HG_HEADS, d).astype(jnp.float32)
    qh = jax.nn.silu(heads(q, HG_DK)) * HG_DK ** -0.5
    vh = heads(i, HG_DV)
    lb_f, lb_b = jnp.split(lb, 2)
    logf_f, kf = hgrn2_forget(heads(f_fwd, HG_DK), lb_f.reshape(HG_HEADS, HG_DK))
    logf_b, kb = hgrn2_forget(heads(f_bwd, HG_DK), lb_b.reshape(HG_HEADS, HG_DK))
    flip = lambda a: jnp.flip(a, axis=1)
    o_f = gla_chunked(qh, kf, vh, logf_f)
    o_b = flip(gla_chunked(flip(qh), flip(kb), flip(vh), flip(logf_b)))
    o = o_f + o_b
    o = o * lax.rsqrt(jnp.mean(jnp.square(o), axis=-1, keepdims=True) + RMS_EPS) \
        * norm_g.astype(jnp.float32).reshape(HG_HEADS, HG_DV)
    o = o.reshape(B, L, HG_VW) * jax.nn.silu(og.astype(jnp.float32))
    return o.astype(q.dtype)


def short_conv(x, w, b):
    C = x.shape[-1]
    pad = HY_SHORT // 2
    y = lax.conv_general_dilated(x, w[:, None, :].astype(x.dtype), window_strides=(1,),
                                 padding=[(pad, pad)], dimension_numbers=('NWC', 'WIO', 'NWC'),
                                 feature_group_count=C)
    return y + b


def hyena_filters(L, w1, b1, w2, b2, freq, w3):
    t = jnp.linspace(0.0, 1.0, L, dtype=jnp.float32)[:, None]
    w = 2.0 * math.pi * jnp.arange(L, dtype=jnp.float32)[:, None] / L
    f = jnp.linspace(1e-4, HY_BANDS - 1, HY_BANDS, dtype=jnp.float32)[None]
    z = jnp.concatenate([t, jnp.cos(f * w), -jnp.sin(f * w)], axis=-1)
    h = jnp.sin(freq * (z @ w1 + b1))
    for j in range(HY_INNER):
        h = jnp.sin(freq * (h @ w2[j] + b2[j]))
    h = (h @ w3).astype(jnp.float32)
    max_decay = math.log(HY_TARGET) / HY_FAST_DECAY
    min_decay = math.log(HY_TARGET) / HY_SLOW_DECAY
    deltas = jnp.linspace(min_decay, max_decay, HY_W, dtype=jnp.float32)
    window = jnp.exp(-t * jnp.abs(deltas))
    h = h * jnp.tile(window, (1, 2))
    return h[:, :HY_W], h[:, HY_W:]


def bidirectional_long_conv(u, h_fwd, h_bwd):
    B, L, C = u.shape
    kern = jnp.concatenate([h_fwd, jnp.zeros((1, C), jnp.float32), h_bwd[:0:-1]], axis=0)
    U = jnp.fft.rfft(u, n=2 * L, axis=1)
    K = jnp.fft.rfft(kern, n=2 * L, axis=0)
    return jnp.fft.irfft(U * K[None], n=2 * L, axis=1)[:, :L]


def hyena_mixer(proj, conv_w, conv_b, w1, b1, w2, b2, freq, w3, skip):
    L = proj.shape[1]
    x0, x1, v = jnp.split(short_conv(proj, conv_w, conv_b), 3, axis=-1)
    h_f, h_b = hyena_filters(L, w1, b1, w2, b2, freq, w3)
    u = (x1 * v).astype(jnp.float32)
    y = bidirectional_long_conv(u, h_f, h_b) + u * skip.astype(jnp.float32)
    return (x0.astype(jnp.float32) * y).astype(proj.dtype)


def t5_relative_bucket(rel):
    half = REL_BUCKETS // 2
    max_exact = half // 2
    bucket = (rel > 0).astype(jnp.int32) * half
    n = jnp.abs(rel)
    n_safe = jnp.maximum(n, 1).astype(jnp.float32)
    large = max_exact + (jnp.log(n_safe / max_exact) / math.log(REL_MAX_DIST / max_exact)
                         * (half - max_exact)).astype(jnp.int32)
    large = jnp.clip(large, 0, half - 1)
    return bucket + jnp.where(n < max_exact, n, large)


def window_attention(q, k, v, sink, rel_bias):
    B, L, _ = q.shape
    G = ATT_HEADS // ATT_KV_HEADS
    W = ATT_BLOCK
    nb = L // W
    qb = q.reshape(B, nb, W, ATT_KV_HEADS, G, ATT_DH)

    def band(a):
        a = jnp.pad(a.reshape(B, L, ATT_KV_HEADS, ATT_DH), ((0, 0), (W, W), (0, 0), (0, 0)))
        a = a.reshape(B, nb + 2, W, ATT_KV_HEADS, ATT_DH)
        return jnp.concatenate([a[:, :-2], a[:, 1:-1], a[:, 2:]], axis=2)

    kb, vb = band(k), band(v)
    s = jnp.einsum('bnqhgd,bnshd->bnhgqs', qb, kb).astype(jnp.float32) * ATT_DH ** -0.5
    kofs = jnp.arange(3 * W, dtype=jnp.int32)[None, :] - W
    rel = kofs - jnp.arange(W, dtype=jnp.int32)[:, None]
    bias = jnp.transpose(rel_bias[t5_relative_bucket(rel)], (2, 0, 1))
    bias = bias.reshape(ATT_KV_HEADS, G, W, 3 * W).astype(jnp.float32)
    kpos = jnp.arange(nb, dtype=jnp.int32)[:, None] * W + kofs
    valid = (jnp.abs(rel) <= WINDOW)[None] & ((kpos >= 0) & (kpos < L))[:, None, :]
    s = jnp.where(valid[None, :, None, None], s + bias, MASK_VALUE)
    sk = sink.astype(jnp.float32).reshape(ATT_KV_HEADS, G)[:, :, None]
    m = jnp.maximum(jnp.max(s, axis=-1), sk)
    p = jnp.exp(s - m[..., None])
    denom = jnp.sum(p, axis=-1) + jnp.exp(sk - m)
    o = jnp.einsum('bnhgqs,bnshd->bnqhgd', p, vb.astype(jnp.float32))
    o = o / jnp.moveaxis(denom, -1, 2)[..., None]
    return o.reshape(B, L, ATT_QW).astype(q.dtype)


def mixer_sublayer(h, w_in, lb, hg_norm_g, conv_w, conv_b, fw1, fb1, fw2, fb2, ffreq, fw3, skip,
                   sink, rel_bias, w_branch, w_out):
    B, L, D = h.shape
    proj = h @ w_in
    hq, hff, hfb, hi, hog, hy, aq, ak, av, gates = jnp.split(proj, IN_SPLITS, axis=-1)
    o_hg = hgrn2_mixer(hq, hff, hfb, hi, hog, lb, hg_norm_g)
    o_hy = hyena_mixer(hy, conv_w, conv_b, fw1, fb1, fw2, fb2, ffreq, fw3, skip)
    o_at = window_attention(aq, ak, av, sink, rel_bias)
    branches = jnp.stack([o_hg, o_hy, o_at], axis=2)
    merged = jnp.einsum('blnc,ncd->blnd', branches, w_branch)
    g = jax.nn.sigmoid(gates.reshape(B, L, N_BRANCH, D).astype(jnp.float32))
    y = jnp.sum(g * merged.astype(jnp.float32), axis=2).astype(h.dtype)
    return y @ w_out


def routed_moe(h, layer, router_w, router_b, w_gate_up, b_gate_up, w_down, b_down):
    B, L, D = h.shape
    T = B * L
    TK = T * TOP_K
    xf = h.reshape(T, D)
    logits = (xf @ router_w[layer] + router_b[layer]).astype(jnp.float32)
    top_val, top_idx = lax.top_k(logits, TOP_K)
    gate = jax.nn.softmax(top_val, axis=-1)
    expert = top_idx.reshape(TK).astype(jnp.int32)
    token = jnp.arange(TK, dtype=jnp.int32) // TOP_K
    weight = gate.reshape(TK)
    order = jnp.argsort(expert)
    s_expert, s_token, s_weight = expert[order], token[order], weight[order]
    counts = jnp.zeros((N_EXPERTS,), jnp.int32).at[expert].add(1)
    padded = (counts + MOE_BLOCK - 1) // MOE_BLOCK * MOE_BLOCK
    p_end = jnp.cumsum(padded)
    p_start = p_end - padded
    s_start = jnp.cumsum(counts) - counts
    dest = p_start[s_expert] + jnp.arange(TK, dtype=jnp.int32) - s_start[s_expert]
    n_blocks = -(-TK // MOE_BLOCK) + N_EXPERTS
    P = n_blocks * MOE_BLOCK
    tok_pad = jnp.full((P,), T, jnp.int32).at[dest].set(s_token)
    w_pad = jnp.zeros((P,), jnp.float32).at[dest].set(s_weight)
    block_start = jnp.arange(n_blocks, dtype=jnp.int32) * MOE_BLOCK
    block_expert = jnp.minimum(jnp.searchsorted(p_end, block_start, side='right'),
                               N_EXPERTS - 1).astype(jnp.int32)
    x_pad = jnp.concatenate([xf, jnp.zeros((1, D), xf.dtype)], axis=0)
    x_blocks = x_pad[tok_pad].reshape(n_blocks, MOE_BLOCK, D)

    def expert_block(args):
        xb, e = args
        gu = xb @ w_gate_up[layer, e] + b_gate_up[layer, e]
        g, u = jnp.split(gu, 2, axis=-1)
        g = jnp.minimum(g, SWIGLU_LIMIT)
        u = jnp.clip(u, -SWIGLU_LIMIT, SWIGLU_LIMIT)
        act = (u + 1.0) * g * jax.nn.sigmoid(SWIGLU_ALPHA * g)
        return act @ w_down[layer, e] + b_down[layer, e]

    y_blocks = lax.map(expert_block, (x_blocks, block_expert))
    y = y_blocks.reshape(P, D).astype(jnp.float32) * w_pad[:, None]
    y = jax.ops.segment_sum(y, tok_pad, num_segments=T + 1)[:T]
    return y.reshape(B, L, D).astype(h.dtype)


def setup_inputs(seed: int = 0) -> dict:
    key = jax.random.key(seed)
    ks = iter(jax.random.split(key, 32))
    nrm = lambda shape, scale: jax.random.normal(next(ks), shape, jnp.float32) * scale
    return {
        'x': nrm((BATCH, SEQ, D_MODEL), 1.0),
        'ln_in_g': 1.0 + nrm((D_MODEL,), 0.02),
        'ln_in_b': nrm((D_MODEL,), 0.02),
        'w_in': nrm((DEPTH, D_MODEL, IN_COLS), D_MODEL ** -0.5),
        'hg_lower_bound': 1.0 + nrm((DEPTH, 2 * HG_KW), 0.1),
        'hg_norm_g': 1.0 + nrm((DEPTH, HG_VW), 0.02),
        'hy_conv_w': nrm((DEPTH, HY_SHORT, 3 * HY_W), HY_SHORT ** -0.5),
        'hy_conv_b': nrm((DEPTH, 3 * HY_W), 0.02),
        'hy_filt_w1': nrm((DEPTH, HY_EMB, HY_ORDER), HY_EMB ** -0.5),
        'hy_filt_b1': nrm((DEPTH, HY_ORDER), 0.1),
        'hy_filt_w2': nrm((DEPTH, HY_INNER, HY_ORDER, HY_ORDER), HY_ORDER ** -0.5),
        'hy_filt_b2': nrm((DEPTH, HY_INNER, HY_ORDER), 0.1),
        'hy_filt_freq': 1.0 + nrm((DEPTH, HY_ORDER), 0.1),
        'hy_filt_w3': nrm((DEPTH, HY_ORDER, 2 * HY_W), HY_FILTER_SCALE * HY_ORDER ** -0.5),
        'hy_skip': nrm((DEPTH, HY_W), 1.0),
        'att_sink': nrm((DEPTH, ATT_HEADS), 1.0),
        'rel_bias': nrm((REL_BUCKETS, ATT_HEADS), 0.1),
        'w_branch': nrm((DEPTH, N_BRANCH, MIX_W, D_MODEL), MIX_W ** -0.5),
        'w_out': nrm((DEPTH, D_MODEL, D_MODEL), DEEPNORM_BETA * D_MODEL ** -0.5),
        'ln_mix_g': 1.0 + nrm((DEPTH, D_MODEL), 0.02),
        'ln_mix_b': nrm((DEPTH, D_MODEL), 0.02),
        'router_w': nrm((DEPTH, D_MODEL, N_EXPERTS), D_MODEL ** -0.5),
        'router_b': nrm((DEPTH, N_EXPERTS), 0.01),
        'w_gate_up': nrm((DEPTH, N_EXPERTS, D_MODEL, 2 * D_FF), D_MODEL ** -0.5),
        'b_gate_up': nrm((DEPTH, N_EXPERTS, 2 * D_FF), 0.01),
        'w_down': nrm((DEPTH, N_EXPERTS, D_FF, D_MODEL), DEEPNORM_BETA * D_FF ** -0.5),
        'b_down': nrm((DEPTH, N_EXPERTS, D_MODEL), 0.01),
        'ln_moe_g': 1.0 + nrm((DEPTH, D_MODEL), 0.02),
        'ln_moe_b': nrm((DEPTH, D_MODEL), 0.02),
    }


def reference(x, ln_in_g, ln_in_b, w_in, hg_lower_bound, hg_norm_g, hy_conv_w, hy_conv_b,
              hy_filt_w1, hy_filt_b1, hy_filt_w2, hy_filt_b2, hy_filt_freq, hy_filt_w3, hy_skip,
              att_sink, rel_bias, w_branch, w_out, ln_mix_g, ln_mix_b, router_w, router_b,
              w_gate_up, b_gate_up, w_down, b_down, ln_moe_g, ln_moe_b):
    h = layer_norm(x, ln_in_g, ln_in_b)
    for layer in range(DEPTH):
        lb = hgrn2_lower_bound(hg_lower_bound, layer)
        mix = mixer_sublayer(h, w_in[layer], lb, hg_norm_g[layer], hy_conv_w[layer], hy_conv_b[layer],
                             hy_filt_w1[layer], hy_filt_b1[layer], hy_filt_w2[layer], hy_filt_b2[layer],
                             hy_filt_freq[layer], hy_filt_w3[layer], hy_skip[layer], att_sink[layer],
                             rel_bias, w_branch[layer], w_out[layer])
        h = layer_norm(DEEPNORM_ALPHA * h + mix, ln_mix_g[layer], ln_mix_b[layer])
        ffn = routed_moe(h, layer, router_w, router_b, w_gate_up, b_gate_up, w_down, b_down)
        h = layer_norm(DEEPNORM_ALPHA * h + ffn, ln_moe_g[layer], ln_moe_b[layer])
    return h
```

```python
import math
from contextlib import ExitStack
import numpy as np
import concourse.bass as bass
import concourse.mybir as mybir
from concourse.bass_utils import run_bass_kernel_spmd

F32 = mybir.dt.float32
BF16 = mybir.dt.bfloat16
AF = mybir.ActivationFunctionType
ALU = mybir.AluOpType
AX = mybir.AxisListType

D = 2048
L = 2048
DEPTH = 2
IN_COLS = 15616
NT = L // 128
KC = D // 128
LN_EPS = 1e-5
ALPHA = (2 * DEPTH) ** 0.25
N_EXP = 32
DFF = 2048

C_HQ, C_HFF, C_HFB, C_HI, C_HOG, C_HY, C_AQ, C_AK, C_AV, C_G = (
    0, 1024, 2048, 3072, 4096, 5120, 8192, 9216, 9344, 9472)


class Buf:
    __slots__ = ("name", "w", "r", "excl")

    def __init__(self, name="", excl=False):
        self.name = name
        self.w = None
        self.r = {}
        self.excl = excl


class Trk:
    NDS = 24

    def __init__(self, nc, stack):
        self.nc = nc
        self.E = {"pe": nc.tensor, "dve": nc.vector, "act": nc.scalar, "pool": nc.gpsimd,
                  "sp": nc.sync}
        self.sem = {}
        self.cnt = {}
        for e in ("pe", "dve", "act", "pool"):
            self.sem[e] = stack.enter_context(nc.semaphore("s_" + e))
            self.cnt[e] = 0
        for i in range(self.NDS):
            self.sem[("d", i)] = stack.enter_context(nc.semaphore("d%d" % i))
            self.cnt[("d", i)] = 0
        self.seen = {e: {} for e in self.E}
        self.dnext = 0
        self.n_ins = 0
        self.n_wait = 0

    def _need(self, reads, writes, e=None):
        need = {}
        for b in reads:
            if b.w is not None:
                k, v = b.w
                if need.get(k, 0) < v:
                    need[k] = v
            if b.excl:
                for k, v in b.r.items():
                    if k != e and need.get(k, 0) < v:
                        need[k] = v
        for b in writes:
            if b.w is not None:
                k, v = b.w
                if need.get(k, 0) < v:
                    need[k] = v
            for k, v in b.r.items():
                if need.get(k, 0) < v:
                    need[k] = v
        return need

    def _wait(self, e, need):
        seen = self.seen[e]
        for k, v in need.items():
            if k == e and e == "pe":
                continue
            if seen.get(k, 0) >= v:
                continue
            self.E[e].wait_ge(self.sem[k], v)
            seen[k] = v
            self.n_wait += 1

    def _mark(self, ev, reads, writes):
        k, v = ev
        for b in reads:
            if b.r.get(k, 0) < v:
                b.r[k] = v
        for b in writes:
            b.w = ev
            b.r = {}

    def op(self, e, fn, reads=(), writes=()):
        self._wait(e, self._need(reads, writes, e))
        self.cnt[e] += 1
        ins = fn(self.E[e])
        ins.then_inc(self.sem[e], 1)
        self._mark((e, self.cnt[e]), reads, writes)
        self.n_ins += 1
        return ins

    def dma(self, q, out, in_, reads=(), writes=()):
        need = self._need(reads, writes)
        i = self.dnext
        self.dnext = (self.dnext + 1) % self.NDS
        k = ("d", i)
        if self.cnt[k] > 0:
            need[k] = max(need.get(k, 0), self.cnt[k])
        self._wait(q, need)
        ins = self.E[q].dma_start(out=out, in_=in_)
        self.cnt[k] += 16
        ins.then_inc(self.sem[k], 16)
        self._mark((k, self.cnt[k]), reads, writes)
        self.n_ins += 1
        return ins

    def barrier(self):
        need = {k: v for k, v in self.cnt.items() if v > 0}
        for e in self.E:
            self._wait(e, dict(need))

    def drain(self, e, bufs):
        self._wait(e, self._need((), bufs))


class Rot:
    def __init__(self, tiles, excl=False):
        self.tiles = tiles
        self.bufs = [Buf(excl=excl) for _ in tiles]
        self.i = 0

    def next(self):
        i = self.i
        self.i = (i + 1) % len(self.tiles)
        return self.tiles[i], self.bufs[i]


class Ctx:
    pass


_uc = [0]


def _u(name):
    _uc[0] += 1
    return "%s_%d" % (name, _uc[0])


LAST_INPUT_NAMES = set()
LAST_TRK = None


class WPool:
    def __init__(self, c, S, name, kc, ncols, n_stage=2, n_bf=2):
        self.c = c
        self.stage = Rot([S("%s_st%d" % (name, i), [128, kc, ncols], F32) for i in range(n_stage)])
        self.bf = Rot([S("%s_bf%d" % (name, i), [128, kc, ncols], BF16) for i in range(n_bf)])

    def load(self, w_ap, kc, ncols):
        t = self.c.t
        st, stb = self.stage.next()
        t.dma("sp", st[:, 0:kc, 0:ncols], w_ap.rearrange("(kc p) c -> p kc c", p=128), writes=[stb])
        bf, bfb = self.bf.next()
        t.op("pool", lambda e: e.tensor_copy(bf[:, 0:kc, 0:ncols], st[:, 0:kc, 0:ncols]), reads=[stb], writes=[bfb])
        return bf, bfb


def gemm_fm(c, w_ap, n_cols, xT, xT_buf, k_chunks, n_tok, epilogue, wp, blk=512):
    t = c.t
    blocks = [(c0, min(blk, n_cols - c0)) for c0 in range(0, n_cols, blk)]
    nxt = wp.load(w_ap[:, blocks[0][0]:blocks[0][0] + blocks[0][1]], k_chunks, blocks[0][1])
    for bi, (c0, cw) in enumerate(blocks):
        wt, wb = nxt
        if bi + 1 < len(blocks):
            n0, nw = blocks[bi + 1]
            nxt = wp.load(w_ap[:, n0:n0 + nw], k_chunks, nw)
        for cg in range(cw // 128):
            for tt in range(n_tok // 512):
                ps, pb = c.psum.next()
                for kc in range(k_chunks):
                    t.op("pe", lambda e, kc=kc: e.matmul(
                        ps[:, :], wt[:, kc, cg * 128:(cg + 1) * 128],
                        xT[:, kc, tt * 512:(tt + 1) * 512],
                        start=(kc == 0), stop=(kc == k_chunks - 1)),
                        reads=[wb, xT_buf], writes=[pb])
                epilogue(ps, pb, (c0 // 128) + cg, tt)


def gemm_tm(c, w_ap, n_cols, xT, xT_buf, k_chunks, n_tok, epilogue, wp, blk=512):
    t = c.t
    blocks = [(c0, min(blk, n_cols - c0)) for c0 in range(0, n_cols, blk)]
    nxt = wp.load(w_ap[:, blocks[0][0]:blocks[0][0] + blocks[0][1]], k_chunks, blocks[0][1])
    for bi, (c0, cw) in enumerate(blocks):
        wt, wb = nxt
        if bi + 1 < len(blocks):
            n0, nw = blocks[bi + 1]
            nxt = wp.load(w_ap[:, n0:n0 + nw], k_chunks, nw)
        for tt in range(n_tok // 128):
            ps, pb = c.psum.next()
            for kc in range(k_chunks):
                t.op("pe", lambda e, kc=kc: e.matmul(
                    ps[:, 0:cw], xT[:, kc, tt * 128:(tt + 1) * 128], wt[:, kc, 0:cw],
                    start=(kc == 0), stop=(kc == k_chunks - 1)),
                    reads=[wb, xT_buf], writes=[pb])
            epilogue(ps, pb, tt, c0, cw)


_evac_flip = [0]


def evac(c, out_ap, in_ap, reads, writes):
    _evac_flip[0] ^= 1
    if _evac_flip[0]:
        c.t.op("act", lambda e: e.copy(out_ap, in_ap), reads=reads, writes=writes)
    else:
        c.t.op("dve", lambda e: e.tensor_copy(out_ap, in_ap), reads=reads, writes=writes)


def phase_ln(c, src, res, g_ap, b_ap, h_out, xT_out, final_out=None, router=None):
    t, nc = c.t, c.nc
    V = lambda fn, r=(), w=(): t.op("dve", fn, reads=r, writes=w)
    A = lambda fn, r=(), w=(): t.op("act", fn, reads=r, writes=w)
    PE = lambda fn, r=(), w=(): t.op("pe", fn, reads=r, writes=w)
    src_ap, src_buf = src
    res_ap, res_buf = res if res is not None else (None, None)
    h_out_ap, h_out_buf = h_out if h_out is not None else (None, None)
    with ExitStack() as st:
        S = lambda name, shape, dt=F32: st.enter_context(nc.sbuf_tensor(_u(name), shape, dt))
        gt = S("ln_g", [128, D]); gb_ = Buf()
        bt = S("ln_b", [128, D]); bb_ = Buf()
        t.dma("sp", gt[:, :], g_ap.partition_broadcast(128), writes=[gb_])
        t.dma("sp", bt[:, :], b_ap.partition_broadcast(128), writes=[bb_])
        xin = Rot([S("ln_x%d" % i, [128, D]) for i in range(2)])
        rin = Rot([S("ln_r%d" % i, [128, D]) for i in range(2)])
        yo = Rot([S("ln_y%d" % i, [128, D]) for i in range(2)])
        xts = Rot([S("ln_xt%d" % i, [128, 4, 128]) for i in range(5)])
        xtb = Rot([S("ln_xb%d" % i, [128, 4, 128], BF16) for i in range(3)])
        stats = S("ln_st", [128, 4 * 6]); sb_ = Buf()
        mv = S("ln_mv", [128, 2]); mvb = Buf()
        rstd = S("ln_rstd", [128, 1]); rsb = Buf()
        if router is not None:
            rw = S("ln_rw", [128, KC, N_EXP]); rwb = Buf()
            t.dma("sp", rw[:, :, :], c.router_w[router].rearrange("(k p) e -> p k e", p=128), writes=[rwb])
            rbias = S("ln_rb", [128, N_EXP])
            t.dma("sp", rbias[:, :], c.router_b[router].partition_broadcast(128), writes=[rwb])
            lg = Rot([S("ln_lg%d" % i, [128, N_EXP]) for i in range(2)])
            ex_ = Rot([S("ln_ex%d" % i, [128, N_EXP]) for i in range(2)])
            m8 = Rot([S("ln_m8%d" % i, [128, 12]) for i in range(2)])
            gT_ = Rot([S("ln_gT%d" % i, [N_EXP, 128]) for i in range(2)])
            ybf = Rot([S("ln_ybf%d" % i, [128, D], BF16) for i in range(2)])
        for tt in range(NT):
            rows = slice(tt * 128, (tt + 1) * 128)
            x, xb = xin.next()
            t.dma("sp", x[:, :], src_ap[rows, :], reads=[src_buf], writes=[xb])
            if res_ap is not None:
                r, rb = rin.next()
                t.dma("sp", r[:, :], res_ap[rows, :], reads=[res_buf], writes=[rb])
                V(lambda e: e.scalar_tensor_tensor(out=x[:, :], in0=r[:, :], scalar=float(ALPHA), in1=x[:, :],
                                                   op0=ALU.mult, op1=ALU.add), [rb, xb], [xb])
            for j in range(4):
                V(lambda e: e.bn_stats(stats[:, j * 6:(j + 1) * 6], x[:, j * 512:(j + 1) * 512]), [xb], [sb_])
            V(lambda e: e.bn_aggr(mv[:, :], stats[:, :]), [sb_], [mvb])
            A(lambda e: e.activation(rstd[:, :], mv[:, 1:2], AF.Sqrt, bias=c.eps_ln[:, :], scale=1.0),
              [mvb, c.cbuf], [rsb])
            V(lambda e: e.reciprocal(rstd[:, :], rstd[:, :]), [rsb], [rsb])
            y, yb = yo.next()
            V(lambda e: e.tensor_scalar(y[:, :], x[:, :], mv[:, 0:1], rstd[:, 0:1], op0=ALU.subtract, op1=ALU.mult),
              [xb, mvb, rsb], [yb])
            t.op("pool", lambda e: e.tensor_tensor(y[:, :], y[:, :], gt[:, :], op=ALU.mult), reads=[yb, gb_], writes=[yb])
            t.op("pool", lambda e: e.tensor_tensor(y[:, :], y[:, :], bt[:, :], op=ALU.add), reads=[yb, bb_], writes=[yb])
            if h_out_ap is not None:
                t.dma("sp", h_out_ap[rows, :], y[:, :], reads=[yb], writes=[h_out_buf])
            if router is not None:
                yb16, yb16b = ybf.next()
                A(lambda e: e.copy(yb16[:, :], y[:, :]), [yb], [yb16b])
                t.dma("sp", c.d_xtm[0][rows, :], yb16[:, :], reads=[yb16b], writes=[c.d_xtm[1]])
            if final_out is not None:
                t.dma("sp", final_out[rows, :], y[:, :], reads=[yb], writes=[c.out_buf])
            if xT_out is not None:
                if router is not None:
                    pl, plb = c.psacc.next()
                for g4 in range(KC // 4):
                    ps, pb = c.psum.next()
                    for j in range(4):
                        fc = g4 * 4 + j
                        PE(lambda e: e.transpose(ps[:, j * 128:(j + 1) * 128], y[:, fc * 128:(fc + 1) * 128],
                                                 c.ident[:, :]), [yb, c.ident_buf], [pb])
                    xb_, xbb_ = xtb.next()
                    A(lambda e: e.copy(xb_[:, :, :], ps[:, :].rearrange("p (j q) -> p j q", j=4)), [pb], [xbb_])
                    t.dma("sp", xT_out[0][g4 * 512:(g4 + 1) * 512, rows].rearrange("(j p) q -> p j q", p=128),
                          xb_[:, :, :], reads=[xbb_], writes=[xT_out[1]])
                    if router is not None:
                        xs, xsb = xts.next()
                        V(lambda e: e.tensor_copy(xs[:, :, :], ps[:, :].rearrange("p (j q) -> p j q", j=4)), [pb], [xsb])
                    if router is not None:
                        for j in range(4):
                            fc = g4 * 4 + j
                            PE(lambda e: e.matmul(pl[:, 0:N_EXP], xs[:, j, :], rw[:, fc, :],
                                                  start=(fc == 0), stop=(fc == KC - 1)), [xsb, rwb], [plb])
                if router is not None:
                    l_, lb_ = lg.next()
                    V(lambda e: e.tensor_tensor(l_[:, :], pl[:, 0:N_EXP], rbias[:, :], op=ALU.add), [plb, rwb], [lb_])
                    m_, mb_ = m8.next()
                    V(lambda e: e.max(m_[:, 0:8], l_[:, :]), [lb_], [mb_])
                    V(lambda e: e.tensor_scalar(m_[:, 8:9], m_[:, 0:1], -1.0, None, op0=ALU.mult), [mb_], [mb_])
                    e_, eb_ = ex_.next()
                    A(lambda e: e.activation(e_[:, :], l_[:, :], AF.Exp, bias=m_[:, 8:9], scale=1.0), [lb_, mb_], [eb_])
                    V(lambda e: e.scalar_tensor_tensor(out=e_[:, :], in0=l_[:, :], scalar=m_[:, 3:4], in1=e_[:, :],
                                                       op0=ALU.is_ge, op1=ALU.mult), [lb_, mb_, eb_], [eb_])
                    V(lambda e: e.reduce_sum(m_[:, 9:10], e_[:, :], axis=AX.X), [eb_], [mb_])
                    V(lambda e: e.reciprocal(m_[:, 10:11], m_[:, 9:10]), [mb_], [mb_])
                    V(lambda e: e.tensor_scalar(e_[:, :], e_[:, :], m_[:, 10:11], None, op0=ALU.mult), [eb_, mb_], [eb_])
                    t.dma("sp", c.d_gate[0][rows, :], e_[:, :], reads=[eb_], writes=[c.d_gate[1]])
                    ps, pb = c.psum.next()
                    PE(lambda e: e.transpose(ps[0:N_EXP, 0:128], e_[:, :], c.ident[:, :]), [eb_, c.ident_buf], [pb])
                    g_, gb2 = gT_.next()
                    A(lambda e: e.copy(g_[:, :], ps[0:N_EXP, 0:128]), [pb], [gb2])
                    t.dma("sp", c.d_gateT[0][:, rows], g_[:, :], reads=[gb2], writes=[c.d_gateT[1]])
        t.barrier()


def phase_proj(c, layer, only=None):
    t, nc = c.t, c.nc
    w = c.w_in[layer]
    with ExitStack() as st:
        S = lambda name, shape, dt=F32: st.enter_context(nc.sbuf_tensor(_u(name), shape, dt))
        hT = S("hT", [128, KC, L], BF16); hT_buf = Buf("hT")
        t.dma("sp", hT[:, :, :], c.d_xT[0].rearrange("(k p) q -> p k q", p=128), reads=[c.d_xT[1]], writes=[hT_buf])
        wrot = WPool(c, S, "pj_w", KC, 512)
        stg = Rot([S("pj_s%d" % i, [128, L]) for i in range(2)])
        stg_tm = Rot([S("pj_t%d" % i, [128, 512]) for i in range(3)])
        fm = [(C_HQ, 1024, c.d_hqT), (C_HFF, 1024, c.d_hffT), (C_HFB, 1024, c.d_hfbT),
              (C_HOG, 1024, c.d_hogT), (C_HY, 3072, c.d_hyT), (C_AQ, 1024, c.d_aqT),
              (C_AK, 128, c.d_akT)]
        if only is not None:
            fm = fm[only[0]:only[1]]
        for c0, n, (dst, dbuf) in fm:
            cur = {}

            def epi(ps, pb, g, tt, dst=dst, dbuf=dbuf, cur=cur):
                if tt == 0:
                    cur["s"] = stg.next()
                s, sb = cur["s"]
                evac(c, s[:, tt * 512:(tt + 1) * 512], ps[:, :], [pb], [sb])
                if tt == L // 512 - 1:
                    t.dma("sp", dst[g * 128:(g + 1) * 128, :], s[:, :], reads=[sb], writes=[dbuf])

            gemm_fm(c, w[:, c0:c0 + n], n, hT, hT_buf, KC, L, epi, wrot)
        if only is None or len(only) > 4:
            cur = {}

            def epig(ps, pb, g, tt, cur=cur):
                if tt == 0:
                    cur["s"] = stg.next()
                s, sb = cur["s"]
                t.op("act", lambda e: e.activation(s[:, tt * 512:(tt + 1) * 512], ps[:, :], AF.Sigmoid),
                     reads=[pb], writes=[sb])
                if tt == L // 512 - 1:
                    t.dma("sp", c.d_gT[0][g * 128:(g + 1) * 128, :], s[:, :], reads=[sb],
                          writes=[c.d_gT[1]])

            gemm_fm(c, w[:, C_G:C_G + 3 * D], 3 * D, hT, hT_buf, KC, L, epig, wrot)
        tm = [(C_HFF, 2048, c.d_hf), (C_HI, 1024, c.d_hi), (C_AV, 128, c.d_av)]
        if only is not None:
            tm = tm[only[2]:only[3]]
        for c0, n, (dst, dbuf) in tm:
            def epi2(ps, pb, tt, cc0, cw, dst=dst, dbuf=dbuf):
                s, sb = stg_tm.next()
                evac(c, s[:, 0:cw], ps[:, 0:cw], [pb], [sb])
                t.dma("sp", dst[tt * 128:(tt + 1) * 128, cc0:cc0 + cw], s[:, 0:cw],
                      reads=[sb], writes=[dbuf])

            gemm_tm(c, w[:, c0:c0 + n], n, hT, hT_buf, KC, L, epi2, wrot)
        t.barrier()


def host_consts():
    cs = {}
    cs["ident"] = np.eye(128, dtype=np.float32)
    M1, M2, M3, M4, M5 = hg_masks()
    cs["hg_M1"], cs["hg_M2"], cs["hg_M3"], cs["hg_M4"], cs["hg_M5"] = M1, M2, M3, M4, M5
    cs.update(hy_consts())
    cs.update(att_consts())
    cs.update(moe_consts())
    return cs


def build(layer_list=(0, 1), stop=None, dbg=(), only=None):
    nc = bass.Bass("TRN2", target_bir_lowering=False)
    c = Ctx()
    c.nc = nc
    import os as _os
    c.moe_nexp = int(_os.environ.get("MOE_NEXP", N_EXP))
    c.moe_stop = _os.environ.get("MOE_STOP", "")
    c.moe_dense = _os.environ.get("MOE_DENSE", "0") == "1"
    c.moe_epi = int(_os.environ.get("MOE_EPI", 6))
    INPUT_NAMES = []

    def ein(name, shape):
        INPUT_NAMES.append(name)
        return nc.dram_tensor(name, list(shape), F32, kind="ExternalInput").ap()
    c.x = ein("x", [L, D])
    c.ln_in_g = ein("ln_in_g", [D]); c.ln_in_b = ein("ln_in_b", [D])
    c.w_in = ein("w_in", [DEPTH, D, IN_COLS])
    c.hg_lb = ein("hg_lower_bound", [DEPTH, 2048])
    c.hg_norm_g = ein("hg_norm_g", [DEPTH, 1024])
    c.hy_conv_w = ein("hy_conv_w", [DEPTH, 3, 3072]); c.hy_conv_b = ein("hy_conv_b", [DEPTH, 3072])
    c.hy_w1 = ein("hy_filt_w1", [DEPTH, 33, 64]); c.hy_b1 = ein("hy_filt_b1", [DEPTH, 64])
    c.hy_w2 = ein("hy_filt_w2", [DEPTH, 2, 64, 64]); c.hy_b2 = ein("hy_filt_b2", [DEPTH, 2, 64])
    c.hy_freq = ein("hy_filt_freq", [DEPTH, 64]); c.hy_w3 = ein("hy_filt_w3", [DEPTH, 64, 2048])
    c.hy_skip = ein("hy_skip", [DEPTH, 1024])
    c.att_sink = ein("att_sink", [DEPTH, 16]); c.rel_bias = ein("rel_bias", [32, 16])
    c.w_branch = ein("w_branch", [DEPTH, 3, 1024, D]); c.w_out = ein("w_out", [DEPTH, D, D])
    c.ln_mix_g = ein("ln_mix_g", [DEPTH, D]); c.ln_mix_b = ein("ln_mix_b", [DEPTH, D])
    c.router_w = ein("router_w", [DEPTH, D, N_EXP]); c.router_b = ein("router_b", [DEPTH, N_EXP])
    c.w_gate_up = [ein("w_gate_up%d" % l, [c.moe_nexp, 16, 128, KC * 256]) if l in layer_list else None for l in range(DEPTH)]
    c.w_down = [ein("w_down%d" % l, [c.moe_nexp, 8, 128, KC * 256]) if l in layer_list else None for l in range(DEPTH)]
    c.b_gate_up = ein("b_gate_up", [DEPTH, N_EXP, 2 * DFF]); c.b_down = ein("b_down", [DEPTH, N_EXP, D])
    c.ln_moe_g = ein("ln_moe_g", [DEPTH, D]); c.ln_moe_b = ein("ln_moe_b", [DEPTH, D])
    consts = host_consts()
    global LAST_INPUT_NAMES
    c.k = {k: ein("k_" + k, v.shape) for k, v in consts.items()}
    LAST_INPUT_NAMES = set(INPUT_NAMES)
    c.out = nc.dram_tensor("out", [L, D], F32, kind="ExternalOutput").ap()
    c.out_buf = Buf()

    def scratch(name, shape, dt=F32):
        kind = "ExternalOutput" if name in dbg else "Internal"
        return (nc.dram_tensor(name, list(shape), dt, kind=kind).ap(), Buf(name))

    c.d_h = scratch("d_h", [L, D]); c.d_h1 = scratch("d_h1", [L, D])
    c.d_gT = scratch("d_gT", [3 * D, L])
    c.d_hqT = scratch("d_hqT", [1024, L]); c.d_hffT = scratch("d_hffT", [1024, L])
    c.d_hfbT = scratch("d_hfbT", [1024, L]); c.d_hogT = scratch("d_hogT", [1024, L])
    c.d_hyT = scratch("d_hyT", [3072, L]); c.d_aqT = scratch("d_aqT", [1024, L])
    c.d_akT = scratch("d_akT", [128, L])
    c.d_hf = scratch("d_hf", [L, 2048]); c.d_hi = scratch("d_hi", [L, 1024])
    c.d_av = scratch("d_av", [L, 128])
    c.d_ohgT = scratch("d_ohgT", [1024, L], BF16)
    c.d_ohyT = scratch("d_ohyT", [1024, L], BF16)
    c.d_oatT = scratch("d_oatT", [1024, L], BF16)
    c.d_mix = scratch("d_mix", [L, D])
    c.d_xT = scratch("d_xT", [D, L], BF16); c.d_ffn = scratch("d_ffn", [L, D])
    c.d_gate = scratch("d_gate", [L, N_EXP]); c.d_gateT = scratch("d_gateT", [N_EXP, L])
    c.d_xtm = scratch("d_xtm", [L, D], BF16)
    c.d_yT = scratch("d_yT", [D, L], BF16)
    c.d_Kr = scratch("d_Kr", [L, 1024]); c.d_Ki = scratch("d_Ki", [L, 1024])
    c.d_Yr = scratch("d_Yr", [L, 1024]); c.d_Yi = scratch("d_Yi", [L, 1024])
    c.d_uT = scratch("d_uT", [1024, L]); c.d_x0c = scratch("d_x0c", [1024, L])

    with ExitStack() as st:
        global LAST_TRK
        c.t = t = LAST_TRK = Trk(nc, st)
        S = lambda name, shape, dt=F32: st.enter_context(nc.sbuf_tensor(_u(name), shape, dt))
        banks = [st.enter_context(nc.psum_tensor("ps%d" % i, [128, 512], F32)) for i in range(8)]
        c.psum = Rot(banks[0:6], excl=True)
        c.psacc = Rot(banks[6:8], excl=True)
        c.ident = S("ident", [128, 128]); c.ident_buf = Buf()
        t.dma("sp", c.ident[:, :], c.k["ident"][:, :], writes=[c.ident_buf])
        c.rowtmp = Rot([S("rowtmp%d" % i, [128, 128]) for i in range(2)])
        c.cbuf = Buf("consts")
        c.eps_ln = S("eps_ln", [128, 1])
        t.op("pool", lambda e: e.memset(c.eps_ln[:, :], float(LN_EPS)), writes=[c.cbuf])

        def finish():
            for k in list(t.cnt):
                if isinstance(k, tuple) and t.cnt[k] > 0:
                    t._wait("sp", {k: t.cnt[k]})
            for e in ("pe", "dve", "act", "pool"):
                if t.cnt[e] > 0:
                    t._wait("sp", {e: t.cnt[e]})

        pending = dict(src=(c.x, Buf("x")), res=None, g=c.ln_in_g, b=c.ln_in_b)
        for layer in layer_list:
            phase_ln(c, pending["src"], pending["res"], pending["g"], pending["b"], c.d_h, c.d_xT)
            if stop == "ln0":
                finish(); return nc
            phase_proj(c, layer, only)
            if stop == "proj":
                finish(); return nc
            if "nohgrn" not in dbg:
                phase_hgrn(c, layer)
            if stop == "hgrn":
                finish(); return nc
            if "nohyena" not in dbg:
                phase_hyena(c, layer)
            if stop == "hyena":
                finish(); return nc
            if "noattn" not in dbg:
                phase_attn(c, layer)
            if stop == "attn":
                finish(); return nc
            phase_merge(c, layer)
            if stop == "merge":
                finish(); return nc
            phase_ln(c, c.d_mix, c.d_h, c.ln_mix_g[layer], c.ln_mix_b[layer], c.d_h1, c.d_xT, router=layer)
            if stop == "ln1":
                finish(); return nc
            (phase_moe if c.moe_dense else phase_moe_sparse)(c, layer)
            if stop == "moe":
                finish(); return nc
            pending = dict(src=c.d_ffn, res=c.d_h1, g=c.ln_moe_g[layer], b=c.ln_moe_b[layer])
        phase_ln(c, pending["src"], pending["res"], pending["g"], pending["b"], None, None, final_out=c.out)
        finish()
    return nc


def load_cols(c, dst, dst_buf, vec_ap, n, col0=0):
    t = c.t
    rows, rb = c.rowtmp.next()
    t.dma("sp", rows[0:n, :], vec_ap.rearrange("(j p) -> j p", p=128), writes=[rb])
    ps, pb = c.psum.next()
    t.op("pe", lambda e: e.transpose(ps[:, 0:n], rows[0:n, :], c.ident[0:n, 0:n]),
         reads=[rb, c.ident_buf], writes=[pb])
    t.op("dve", lambda e: e.tensor_copy(dst[:, col0:col0 + n], ps[:, 0:n]), reads=[pb],
         writes=[dst_buf])


def hg_masks():
    s = np.arange(128)
    same = (s[:, None] // 32) == (s[None, :] // 32)
    M1 = (same & (s[:, None] <= s[None, :])).astype(np.float32)
    M3 = (same & (s[:, None] > s[None, :])).astype(np.float32)
    M5 = (s[:, None] // 32 == np.arange(4)[None, :]).astype(np.float32)
    return M1, M1.T.copy(), M3, M3.T.copy(), M5


def phase_hgrn(c, layer):
    t, nc = c.t, c.nc
    V = lambda fn, r=(), w=(): t.op("dve", fn, reads=r, writes=w)
    A = lambda fn, r=(), w=(): t.op("act", fn, reads=r, writes=w)
    G = lambda fn, r=(), w=(): t.op("pool", fn, reads=r, writes=w)
    PE = lambda fn, r=(), w=(): t.op("pe", fn, reads=r, writes=w)
    QS = 128.0 ** -0.5
    with ExitStack() as st:
        S = lambda name, shape, dt=F32: st.enter_context(nc.sbuf_tensor(_u(name), shape, dt))
        kb = Buf("hgk")
        Ms = []
        for nm in ("M1", "M2", "M3", "M4"):
            m = S("hg_" + nm, [128, 128])
            t.dma("sp", m[:, :], c.k["hg_" + nm][:, :], writes=[kb])
            Ms.append(m)
        M1, M2, M3, M4 = Ms
        M5 = S("hg_M5", [128, 4])
        t.dma("sp", M5[:, :], c.k["hg_M5"][:, :], writes=[kb])
        ones = S("hg_ones", [128, 128])
        G(lambda e: e.memset(ones[:, :], 1.0), w=[kb])
        eps = S("hg_eps", [128, 1])
        G(lambda e: e.memset(eps[:, :], 1e-6), w=[kb])
        lbT = S("hg_lbT", [128, 16]); omlT = S("hg_omlT", [128, 16]); nomlT = S("hg_nomlT", [128, 16])
        lbtm = S("hg_lbtm", [128, 2048]); omltm = S("hg_omltm", [128, 2048])
        gT = S("hg_gT", [128, 8])
        lbb = Buf("lb")
        load_cols(c, gT, lbb, c.hg_norm_g[layer], 8)
        if layer == 0:
            G(lambda e: e.memset(lbT[:, :], 0.0), w=[lbb])
            G(lambda e: e.memset(lbtm[:, :], 0.0), w=[lbb])
        else:
            x0T = S("hg_x0T", [128, 16])
            load_cols(c, x0T, lbb, c.hg_lb[0], 16)
            load_cols(c, lbT, lbb, c.hg_lb[1], 16)
            V(lambda e: e.tensor_tensor(lbT[:, :], lbT[:, :], x0T[:, :], op=ALU.subtract), [lbb], [lbb])
            A(lambda e: e.activation(lbT[:, :], lbT[:, :], AF.Sigmoid), [lbb], [lbb])
            t.dma("sp", omltm[:, :], c.hg_lb[0].partition_broadcast(128), writes=[lbb])
            t.dma("sp", lbtm[:, :], c.hg_lb[1].partition_broadcast(128), writes=[lbb])
            G(lambda e: e.tensor_tensor(lbtm[:, :], lbtm[:, :], omltm[:, :], op=ALU.subtract), [lbb], [lbb])
            A(lambda e: e.activation(lbtm[:, :], lbtm[:, :], AF.Sigmoid), [lbb], [lbb])
        V(lambda e: e.tensor_scalar(omlT[:, :], lbT[:, :], -1.0, 1.0, op0=ALU.mult, op1=ALU.add), [lbb], [lbb])
        V(lambda e: e.tensor_scalar(nomlT[:, :], omlT[:, :], -1.0, None, op0=ALU.mult), [lbb], [lbb])
        V(lambda e: e.tensor_scalar(omltm[:, :], lbtm[:, :], -1.0, 1.0, op0=ALU.mult, op1=ALU.add), [lbb], [lbb])

        ztm = [S("hg_ztm%d" % d, [128, NT, 128]) for d in range(2)]; ztm_b = [Buf() for _ in range(2)]
        lgf = [S("hg_lgf%d" % d, [128, NT, 128]) for d in range(2)]; lgf_b = [Buf() for _ in range(2)]
        vt = S("hg_v", [128, NT, 128]); vt_b = Buf()
        qs = S("hg_qs", [128, L]); qs_b = Buf()
        qh = [S("hg_qh%d" % d, [128, L]) for d in range(2)]; qh_b = [Buf() for _ in range(2)]
        kt = [S("hg_kt%d" % d, [128, L]) for d in range(2)]; kt_b = [Buf() for _ in range(2)]
        og = S("hg_og", [128, L]); og_b = Buf()
        Dd = [S("hg_D%d" % d, [128, 64]) for d in range(2)]; Dd_b = [Buf() for _ in range(2)]
        St = [S("hg_S%d" % d, [128, 64, 128]) for d in range(2)]; St_b = [Buf() for _ in range(2)]
        ex = Rot([S("hg_ex%d" % i, [128, 512]) for i in range(3)])
        scm = Rot([S("hg_scm%d" % i, [128, 128]) for i in range(4)])
        vmr = Rot([S("hg_vm%d" % i, [128, 4, 128]) for i in range(2)])
        sq = Rot([S("hg_sq%d" % i, [128, 512]) for i in range(2)])
        rs = Rot([S("hg_rs%d" % i, [128, 512]) for i in range(2)])
        ost = Rot([S("hg_o%d" % i, [128, 512]) for i in range(2)])
        ostb = Rot([S("hg_ob%d" % i, [128, 512], BF16) for i in range(2)])
        Mpre = [M1, M2]
        Mex = [M3, M4]

        for hd in range(8):
            hs = slice(hd * 128, (hd + 1) * 128)
            for d in range(2):
                t.dma("sp", ztm[d][:, :, :],
                      c.d_hf[0][:, d * 1024 + hd * 128:d * 1024 + (hd + 1) * 128].rearrange(
                          "(n p) k -> p n k", p=128), reads=[c.d_hf[1]], writes=[ztm_b[d]])
            t.dma("sp", vt[:, :, :], c.d_hi[0][:, hs].rearrange("(n p) k -> p n k", p=128),
                  reads=[c.d_hi[1]], writes=[vt_b])
            t.dma("sp", qs[:, :], c.d_hqT[0][hs, :], reads=[c.d_hqT[1]], writes=[qs_b])
            t.dma("sp", kt[0][:, :], c.d_hffT[0][hs, :], reads=[c.d_hffT[1]], writes=[kt_b[0]])
            t.dma("sp", kt[1][:, :], c.d_hfbT[0][hs, :], reads=[c.d_hfbT[1]], writes=[kt_b[1]])
            t.dma("sp", og[:, :], c.d_hogT[0][hs, :], reads=[c.d_hogT[1]], writes=[og_b])
            for d in range(2):
                z = ztm[d]; zb = ztm_b[d]; lg = lgf[d]; lb_ = lgf_b[d]
                col = d * 1024 + hd * 128
                oml_bc = omltm[:, col:col + 128].unsqueeze(1).to_broadcast([128, NT, 128])
                lb_bc = lbtm[:, col:col + 128].unsqueeze(1).to_broadcast([128, NT, 128])
                A(lambda e: e.activation(z[:, :, :], z[:, :, :], AF.Sigmoid), [zb], [zb])
                G(lambda e: e.tensor_tensor(lg[:, :, :], z[:, :, :], oml_bc, op=ALU.mult), [zb, lbb], [lb_])
                G(lambda e: e.tensor_tensor(lg[:, :, :], lg[:, :, :], lb_bc, op=ALU.add), [lb_, lbb], [lb_])
                V(lambda e: e.tensor_scalar(lg[:, :, :], lg[:, :, :], 1e-30, None, op0=ALU.max), [lb_], [lb_])
                V(lambda e: e.tensor_scalar(z[:, :, :], z[:, :, :], -1.0, 1.0, op0=ALU.mult, op1=ALU.add), [zb], [zb])
                G(lambda e: e.tensor_tensor(z[:, :, :], z[:, :, :], oml_bc, op=ALU.mult), [zb, lbb], [zb])
            for d in range(2):
                lg = lgf[d]; lb_ = lgf_b[d]
                A(lambda e: e.activation(lg[:, :, :], lg[:, :, :], AF.Ln), [lb_], [lb_])
            A(lambda e: e.activation(qs[:, :], qs[:, :], AF.Silu), [qs_b], [qs_b])
            A(lambda e: e.activation(og[:, :], og[:, :], AF.Silu), [og_b], [og_b])
            for d in range(2):
                j = d * 8 + hd
                A(lambda e: e.activation(kt[d][:, :], kt[d][:, :], AF.Sigmoid), [kt_b[d]], [kt_b[d]])
                V(lambda e: e.tensor_scalar(kt[d][:, :], kt[d][:, :], nomlT[:, j:j + 1], omlT[:, j:j + 1],
                                            op0=ALU.mult, op1=ALU.add), [kt_b[d], lbb], [kt_b[d]])
            for d in range(2):
                lg = lgf[d]; lb_ = lgf_b[d]; z = ztm[d]; zb = ztm_b[d]
                ps, pb = c.psum.next()
                for n in range(NT):
                    PE(lambda e, n=n: e.matmul(ps[:, n * 4:(n + 1) * 4], lg[:, n, :], M5[:, :],
                                               start=True, stop=True), [lb_, kb], [pb])
                A(lambda e: e.activation(Dd[d][:, :], ps[:, 0:64], AF.Exp), [pb], [Dd_b[d]])
                for g4 in range(4):
                    ps, pb = c.psum.next()
                    for jn in range(4):
                        n = g4 * 4 + jn
                        PE(lambda e, n=n, jn=jn: e.matmul(ps[:, jn * 128:(jn + 1) * 128], Mex[d][:, :],
                                                          lg[:, n, :], start=True, stop=True),
                           [lb_, kb], [pb])
                    x_, xb_ = ex.next()
                    A(lambda e: e.activation(x_[:, :], ps[:, :], AF.Exp), [pb], [xb_])
                    V(lambda e, g4=g4: e.tensor_tensor(
                        z[:, g4 * 4:(g4 + 1) * 4, :], z[:, g4 * 4:(g4 + 1) * 4, :],
                        x_[:, :].rearrange("p (a b) -> p a b", a=4), op=ALU.mult), [zb, xb_], [zb])
                for g4 in range(4):
                    ps, pb = c.psum.next()
                    for jn in range(4):
                        n = g4 * 4 + jn
                        PE(lambda e, n=n, jn=jn: e.matmul(ps[:, jn * 128:(jn + 1) * 128], lg[:, n, :],
                                                          Mpre[d][:, :], start=True, stop=True),
                           [lb_, kb], [pb])
                    cs = slice(g4 * 512, (g4 + 1) * 512)
                    x1, xb1 = ex.next()
                    A(lambda e: e.activation(x1[:, :], ps[:, :], AF.Exp), [pb], [xb1])
                    V(lambda e, cs=cs: e.scalar_tensor_tensor(
                        out=qh[d][:, cs], in0=x1[:, :], scalar=float(QS), in1=qs[:, cs],
                        op0=ALU.mult, op1=ALU.mult), [xb1, qs_b], [qh_b[d]])
                    x2, xb2 = ex.next()
                    A(lambda e: e.activation(x2[:, :], ps[:, :], AF.Exp, scale=-1.0), [pb], [xb2])
                    V(lambda e, cs=cs: e.tensor_tensor(kt[d][:, cs], kt[d][:, cs], x2[:, :], op=ALU.mult),
                      [xb2, kt_b[d]], [kt_b[d]])
            for d in range(2):
                z = ztm[d]; zb = ztm_b[d]
                tiles = range(NT) if d == 0 else range(NT - 1, -1, -1)
                first = True
                for n in tiles:
                    vm, vmb = vmr.next()
                    G(lambda e, n=n: e.tensor_tensor(
                        vm[:, :, :], vt[:, n:n + 1, :].to_broadcast([128, 4, 128]),
                        M5[:, :].unsqueeze(2).to_broadcast([128, 4, 128]), op=ALU.mult),
                      [vt_b, kb], [vmb])
                    ps, pb = c.psum.next()
                    PE(lambda e, n=n: e.matmul(ps[:, :], z[:, n, :],
                                               vm[:, :, :].rearrange("p a b -> p (a b)"),
                                               start=True, stop=True), [zb, vmb], [pb])
                    chunks = range(4) if d == 0 else range(3, -1, -1)
                    for jc in chunks:
                        j = n * 4 + jc
                        if first:
                            V(lambda e, j=j: e.memset(St[d][:, j, :], 0.0), [], [St_b[d]])
                            first = False
                        jn_ = j + 1 if d == 0 else j - 1
                        if jn_ < 0 or jn_ > 63:
                            continue
                        V(lambda e, j=j, jn_=jn_, jc=jc: e.scalar_tensor_tensor(
                            out=St[d][:, jn_, :], in0=St[d][:, j, :], scalar=Dd[d][:, j:j + 1],
                            in1=ps[:, jc * 128:(jc + 1) * 128], op0=ALU.mult, op1=ALU.add),
                          [St_b[d], Dd_b[d], pb], [St_b[d]])
            def scores(n):
                res = []
                for d in range(2):
                    ps, pb = c.psum.next()
                    ts = slice(n * 128, (n + 1) * 128)
                    PE(lambda e, d=d, ts=ts: e.matmul(ps[:, 0:128], kt[d][:, ts], qh[d][:, ts],
                                                      start=True, stop=True),
                       [kt_b[d], qh_b[d]], [pb])
                    sm, smb = scm.next()
                    V(lambda e, d=d: e.tensor_tensor(sm[:, :], ps[:, 0:128], Mpre[d][:, :], op=ALU.mult),
                      [pb, kb], [smb])
                    res.append((sm, smb))
                return res

            nxt = scores(0)
            for g4 in range(4):
                pso, pbo = c.psacc.next()
                for jn in range(4):
                    n = g4 * 4 + jn
                    cur = nxt
                    if n + 1 < NT:
                        nxt = scores(n + 1)
                    oc = slice(jn * 128, (jn + 1) * 128)
                    PE(lambda e, n=n, oc=oc: e.matmul(pso[:, oc], vt[:, n, :], cur[0][0][:, :],
                                                      start=True, stop=False), [vt_b, cur[0][1]], [pbo])
                    PE(lambda e, n=n, oc=oc: e.matmul(pso[:, oc], vt[:, n, :], cur[1][0][:, :],
                                                      start=False, stop=False), [vt_b, cur[1][1]], [pbo])
                    for d in range(2):
                        for jc in range(4):
                            j = n * 4 + jc
                            last = (d == 1 and jc == 3)
                            PE(lambda e, d=d, j=j, jc=jc, last=last, jn=jn: e.matmul(
                                pso[:, jn * 128 + jc * 32:jn * 128 + (jc + 1) * 32], St[d][:, j, :],
                                qh[d][:, j * 32:(j + 1) * 32], start=False, stop=last),
                               [St_b[d], qh_b[d]], [pbo])
                cs = slice(g4 * 512, (g4 + 1) * 512)
                s_, sb_ = sq.next()
                A(lambda e: e.activation(s_[:, :], pso[:, :], AF.Square), [pbo], [sb_])
                ps2, pb2 = c.psum.next()
                PE(lambda e: e.matmul(ps2[:, :], ones[:, :], s_[:, :], start=True, stop=True),
                   [sb_, kb], [pb2])
                r_, rb_ = rs.next()
                A(lambda e: e.activation(r_[:, :], ps2[:, :], AF.Sqrt, bias=eps[:, :], scale=1.0 / 128.0),
                  [pb2, kb], [rb_])
                V(lambda e: e.reciprocal(r_[:, :], r_[:, :]), [rb_], [rb_])
                o_, ob_ = ost.next()
                V(lambda e: e.tensor_tensor(o_[:, :], pso[:, :], r_[:, :], op=ALU.mult), [pbo, rb_], [ob_])
                o2_, ob2_ = ostb.next()
                V(lambda e, cs=cs: e.scalar_tensor_tensor(
                    out=o2_[:, :], in0=o_[:, :], scalar=gT[:, hd:hd + 1], in1=og[:, cs],
                    op0=ALU.mult, op1=ALU.mult), [ob_, lbb, og_b], [ob2_])
                t.dma("sp", c.d_ohgT[0][hs, cs], o2_[:, :], reads=[ob2_], writes=[c.d_ohgT[1]])
        t.barrier()


def hy_consts():
    N = 2 * L
    t = np.arange(L, dtype=np.float64)
    ang = 2.0 * np.pi * np.outer(t, t) / N
    A = np.cos(ang)
    B = -np.sin(ang)
    B[:, 0] = (-1.0) ** t
    tl = np.linspace(0.0, 1.0, L, dtype=np.float32)[:, None]
    w = (2.0 * np.float32(math.pi) * np.arange(L, dtype=np.float32)[:, None] / np.float32(L)).astype(np.float32)
    f = np.linspace(1e-4, 15, 16, dtype=np.float32)[None]
    z = np.concatenate([tl, np.cos(f * w), -np.sin(f * w)], axis=-1).astype(np.float32)
    max_decay = math.log(1e-2) / 0.3
    min_decay = math.log(1e-2) / 1.5
    deltas = np.linspace(min_decay, max_decay, 1024, dtype=np.float32)
    win = np.exp(-tl * np.abs(deltas)[None, :]).astype(np.float32)
    return dict(hy_A=A.astype(np.float32), hy_B=B.astype(np.float32),
                hy_BT=np.ascontiguousarray(B.T).astype(np.float32),
                hy_zT=np.ascontiguousarray(z.T), hy_win=win)


def phase_hyena(c, layer):
    t, nc = c.t, c.nc
    V = lambda fn, r=(), w=(): t.op("dve", fn, reads=r, writes=w)
    A = lambda fn, r=(), w=(): t.op("act", fn, reads=r, writes=w)
    G = lambda fn, r=(), w=(): t.op("pool", fn, reads=r, writes=w)
    PE = lambda fn, r=(), w=(): t.op("pe", fn, reads=r, writes=w)
    PI = math.pi
    kA, kB, kBT = c.k["hy_A"], c.k["hy_B"], c.k["hy_BT"]
    with ExitStack() as st0:
        S0 = lambda name, shape, dt=F32: st0.enter_context(nc.sbuf_tensor(_u(name), shape, dt))
        with ExitStack() as st:
            S = lambda name, shape, dt=F32: st.enter_context(nc.sbuf_tensor(_u(name), shape, dt))
            cb = Buf("hyc")
            P = S("hy_P", [128, NT, 1024]); Pb = Buf()
            Q = S("hy_Q", [128, NT, 1024]); Qb = Buf()
            st_in = ExitStack()
            S = lambda name, shape, dt=F32: st_in.enter_context(nc.sbuf_tensor(_u(name), shape, dt))
            zT = S("hy_zT", [33, L]); t.dma("sp", zT[:, :], c.k["hy_zT"][:, :], writes=[cb])
            w1 = S("hy_w1", [33, 64]); t.dma("sp", w1[:, :], c.hy_w1[layer], writes=[cb])
            w2 = S("hy_w2", [64, 2, 64])
            t.dma("sp", w2[:, :, :], c.hy_w2[layer].rearrange("j k m -> k j m"), writes=[cb])
            w3 = S("hy_w3", [64, 2048]); t.dma("sp", w3[:, :], c.hy_w3[layer], writes=[cb])
            fr = S("hy_fr", [64, 4])
            bb = S("hy_bb", [64, 3])
            t.dma("sp", fr[:, 0:1], c.hy_freq[layer].rearrange("(p o) -> p o", o=1), writes=[cb])
            t.dma("sp", bb[:, 0:1], c.hy_b1[layer].rearrange("(p o) -> p o", o=1), writes=[cb])
            for j in range(2):
                t.dma("sp", bb[:, 1 + j:2 + j], c.hy_b2[layer][j].rearrange("(p o) -> p o", o=1), writes=[cb])
            V(lambda e: e.tensor_scalar(fr[:, 1:4], bb[:, 0:3], fr[:, 0:1], None, op0=ALU.mult), [cb], [cb])
            frs = S("hy_frs", [64, 4])
            V(lambda e: e.tensor_scalar(frs[:, 0:1], fr[:, 0:1], 1.0 / (2.0 * PI), None, op0=ALU.mult), [cb], [cb])
            V(lambda e: e.tensor_scalar(frs[:, 1:4], fr[:, 1:4], 1.0 / (2.0 * PI), 8.5, op0=ALU.mult, op1=ALU.add), [cb], [cb])
            qi = S("hy_qi", [64, 512], mybir.dt.int32); qib = Buf()
            qf = S("hy_qf", [64, 512]); qfb = Buf()
            negpi = S("hy_negpi", [64, 1])
            G(lambda e: e.memset(negpi[:, :], -PI), w=[cb])
            hA = S("hy_hA", [64, L]); hAb = Buf()
            hB = S("hy_hB", [64, L]); hBb = Buf()

            def sin_layer(lhsT, src, srcb, dst, dstb, li):
                for tt in range(4):
                    cs = slice(tt * 512, (tt + 1) * 512)
                    ps, pb = c.psum.next()
                    kk = lhsT.shape[0]
                    PE(lambda e: e.matmul(ps[0:64, :], lhsT, src[0:kk, cs], start=True, stop=True),
                       [cb, srcb], [pb])
                    V(lambda e: e.tensor_scalar(dst[:, cs], ps[0:64, :], frs[:, 0:1], frs[:, li:li + 1],
                                                op0=ALU.mult, op1=ALU.add), [pb, cb], [dstb])
                    V(lambda e: e.tensor_copy(qi[:, :], dst[:, cs]), [dstb], [qib])
                    V(lambda e: e.tensor_copy(qf[:, :], qi[:, :]), [qib], [qfb])
                    V(lambda e: e.tensor_tensor(dst[:, cs], dst[:, cs], qf[:, :], op=ALU.subtract), [dstb, qfb], [dstb])
                    V(lambda e: e.scalar_tensor_tensor(out=dst[:, cs], in0=dst[:, cs], scalar=0.0, in1=dst[:, cs],
                                                       op0=ALU.is_lt, op1=ALU.add), [dstb], [dstb])
                    A(lambda e: e.activation(dst[:, cs], dst[:, cs], AF.Sin, bias=negpi[:, :], scale=2.0 * PI),
                      [dstb, cb], [dstb])

            sin_layer(w1[:, :], zT, cb, hA, hAb, 1)
            sin_layer(w2[:, 0, :], hA, hAb, hB, hBb, 2)
            sin_layer(w2[:, 1, :], hB, hBb, hA, hAb, 3)
            winr = Rot([S("hy_win%d" % i, [128, 1024]) for i in range(2)])
            for n in range(NT):
                wn, wnb = winr.next()
                t.dma("sp", wn[:, :], c.k["hy_win"][n * 128:(n + 1) * 128, :], writes=[wnb])
                for q4 in range(4):
                    ps, pb = c.psum.next()
                    PE(lambda e: e.matmul(ps[:, :], hA[:, n * 128:(n + 1) * 128],
                                          w3[:, q4 * 512:(q4 + 1) * 512], start=True, stop=True),
                       [hAb, cb], [pb])
                    dst, dstb = (P, Pb) if q4 < 2 else (Q, Qb)
                    cc = slice((q4 % 2) * 512, (q4 % 2 + 1) * 512)
                    V(lambda e: e.tensor_tensor(dst[:, n, cc], ps[:, :], wn[:, cc], op=ALU.mult),
                      [pb, wnb], [dstb])
            G(lambda e: e.memset(Q[0:1, 0, :], 0.0), [], [Qb])
            t.barrier()
            st_in.close()
            S = lambda name, shape, dt=F32: st.enter_context(nc.sbuf_tensor(_u(name), shape, dt))
            tabA = Rot([S("hy_tA%d" % i, [128, 16, 128]) for i in range(2)])
            tabB = Rot([S("hy_tB%d" % i, [128, 16, 128]) for i in range(2)])
            stg = Rot([S("hy_stg%d" % i, [128, 512]) for i in range(4)])
            for n in range(NT):
                eng = V if n % 2 == 0 else G
                eng(lambda e: e.tensor_tensor(P[:, n, :], P[:, n, :], Q[:, n, :], op=ALU.subtract), [Pb, Qb], [Pb])
                V(lambda e: e.scalar_tensor_tensor(out=Q[:, n, :], in0=Q[:, n, :], scalar=2.0, in1=P[:, n, :],
                                                     op0=ALU.mult, op1=ALU.add), [Pb, Qb], [Qb])
            for fc in range(16):
                ta, tab_ = tabA.next(); tb, tbb_ = tabB.next()
                t.dma("sp", ta[:, :, :], kA[:, fc * 128:(fc + 1) * 128].rearrange("(k p) f -> p k f", p=128), writes=[tab_])
                t.dma("sp", tb[:, :, :], kB[:, fc * 128:(fc + 1) * 128].rearrange("(k p) f -> p k f", p=128), writes=[tbb_])
                for hh in range(2):
                    cc = slice(hh * 512, (hh + 1) * 512)
                    ps, pb = c.psum.next()
                    for k in range(16):
                        PE(lambda e: e.matmul(ps[:, :], ta[:, k, :], Q[:, k, cc], start=(k == 0), stop=(k == 15)),
                           [tab_, Qb], [pb])
                    s1, s1b = stg.next()
                    A(lambda e: e.copy(s1[:, :], ps[:, :]), [pb], [s1b])
                    t.dma("sp", c.d_Kr[0][fc * 128:(fc + 1) * 128, cc], s1[:, :], reads=[s1b], writes=[c.d_Kr[1]])
                    ps, pb = c.psum.next()
                    for k in range(16):
                        PE(lambda e: e.matmul(ps[:, :], tb[:, k, :], P[:, k, cc], start=(k == 0), stop=(k == 15)),
                           [tbb_, Pb], [pb])
                    s2, s2b = stg.next()
                    V(lambda e: e.tensor_copy(s2[:, :], ps[:, :]), [pb], [s2b])
                    if fc == 0:
                        ps3, pb3 = c.psum.next()
                        for k in range(16):
                            PE(lambda e: e.matmul(ps3[:, :], tb[:, k, :], Q[:, k, cc], start=(k == 0), stop=(k == 15)),
                               [tbb_, Qb], [pb3])
                        V(lambda e: e.tensor_copy(s2[0:1, :], ps3[0:1, :]), [pb3, s2b], [s2b])
                    t.dma("sp", c.d_Ki[0][fc * 128:(fc + 1) * 128, cc], s2[:, :], reads=[s2b], writes=[c.d_Ki[1]])
            t.barrier()
        with ExitStack() as st:
            S = lambda name, shape, dt=F32: st.enter_context(nc.sbuf_tensor(_u(name), shape, dt))
            cb = Buf("hyc2")
            cw = S("hy_cw", [128, 3, 24])
            for k in range(3):
                load_cols(c, cw[:, k, :], cb, c.hy_conv_w[layer][k], 24)
            cbias = S("hy_cb", [128, 24]); load_cols(c, cbias, cb, c.hy_conv_b[layer], 24)
            skip = S("hy_skip", [128, 8]); load_cols(c, skip, cb, c.hy_skip[layer], 8)
            utm = S("hy_utm", [128, NT, 1024]); utmb = Buf()
            tabA = Rot([S("hy_tA%d" % i, [128, 16, 128]) for i in range(2)])
            tabB = Rot([S("hy_tB%d" % i, [128, 16, 128]) for i in range(2)])
            stg = Rot([S("hy_stg%d" % i, [128, 512]) for i in range(4)])
            raw = Rot([S("hy_raw%d" % i, [128, L]) for i in range(2)])
            cv = Rot([S("hy_cv%d" % i, [128, L]) for i in range(3)])

            def conv(chunk):
                r, rb = raw.next()
                t.dma("sp", r[:, :], c.d_hyT[0][chunk * 128:(chunk + 1) * 128, :], reads=[c.d_hyT[1]], writes=[rb])
                y, yb = cv.next()
                V(lambda e: e.tensor_scalar(y[:, :], r[:, :], cw[:, 1, chunk:chunk + 1], cbias[:, chunk:chunk + 1],
                                            op0=ALU.mult, op1=ALU.add), [rb, cb], [yb])
                V(lambda e: e.scalar_tensor_tensor(out=y[:, 1:L], in0=r[:, 0:L - 1], scalar=cw[:, 0, chunk:chunk + 1],
                                                   in1=y[:, 1:L], op0=ALU.mult, op1=ALU.add), [rb, cb, yb], [yb])
                V(lambda e: e.scalar_tensor_tensor(out=y[:, 0:L - 1], in0=r[:, 1:L], scalar=cw[:, 2, chunk:chunk + 1],
                                                   in1=y[:, 0:L - 1], op0=ALU.mult, op1=ALU.add), [rb, cb, yb], [yb])
                return y, yb

            for ch in range(8):
                x0, x0b = conv(ch)
                t.dma("sp", c.d_x0c[0][ch * 128:(ch + 1) * 128, :], x0[:, :], reads=[x0b], writes=[c.d_x0c[1]])
                x1, x1b = conv(8 + ch)
                vv, vvb = conv(16 + ch)
                G(lambda e: e.tensor_tensor(x1[:, :], x1[:, :], vv[:, :], op=ALU.mult), [x1b, vvb], [x1b])
                t.dma("sp", c.d_uT[0][ch * 128:(ch + 1) * 128, :], x1[:, :], reads=[x1b], writes=[c.d_uT[1]])
                for g4 in range(4):
                    ps, pb = c.psum.next()
                    for jn in range(4):
                        n = g4 * 4 + jn
                        PE(lambda e: e.transpose(ps[:, jn * 128:(jn + 1) * 128], x1[:, n * 128:(n + 1) * 128],
                                                 c.ident[:, :]), [x1b, c.ident_buf], [pb])
                    evac(c, utm[:, g4 * 4:(g4 + 1) * 4, ch * 128:(ch + 1) * 128],
                         ps[:, :].rearrange("p (a b) -> p a b", a=4), [pb], [utmb])
            kr = Rot([S("hy_kr%d" % i, [128, 1024]) for i in range(2)])
            ki = Rot([S("hy_ki%d" % i, [128, 1024]) for i in range(2)])
            tmp = Rot([S("hy_tmp%d" % i, [128, 512]) for i in range(4)])
            for fc in range(16):
                ta, tab_ = tabA.next(); tb, tbb_ = tabB.next()
                t.dma("sp", ta[:, :, :], kA[:, fc * 128:(fc + 1) * 128].rearrange("(k p) f -> p k f", p=128), writes=[tab_])
                t.dma("sp", tb[:, :, :], kB[:, fc * 128:(fc + 1) * 128].rearrange("(k p) f -> p k f", p=128), writes=[tbb_])
                krt, krb = kr.next(); kit, kib = ki.next()
                t.dma("sp", krt[:, :], c.d_Kr[0][fc * 128:(fc + 1) * 128, :], reads=[c.d_Kr[1]], writes=[krb])
                t.dma("sp", kit[:, :], c.d_Ki[0][fc * 128:(fc + 1) * 128, :], reads=[c.d_Ki[1]], writes=[kib])
                for hh in range(2):
                    cc = slice(hh * 512, (hh + 1) * 512)
                    psr, pbr = c.psum.next()
                    for k in range(16):
                        PE(lambda e: e.matmul(psr[:, :], ta[:, k, :], utm[:, k, cc], start=(k == 0), stop=(k == 15)),
                           [tab_, utmb], [pbr])
                    psi, pbi = c.psum.next()
                    for k in range(16):
                        PE(lambda e: e.matmul(psi[:, :], tb[:, k, :], utm[:, k, cc], start=(k == 0), stop=(k == 15)),
                           [tbb_, utmb], [pbi])
                    ur, urb = tmp.next(); ui, uib = tmp.next()
                    A(lambda e: e.copy(ur[:, :], psr[:, :]), [pbr], [urb])
                    A(lambda e: e.copy(ui[:, :], psi[:, :]), [pbi], [uib])
                    yr, yrb = stg.next(); yi, yib = stg.next()
                    t1, t1b = tmp.next(); t2, t2b = tmp.next()
                    V(lambda e: e.tensor_tensor(yr[:, :], ur[:, :], krt[:, cc], op=ALU.mult), [urb, krb], [yrb])
                    G(lambda e: e.tensor_tensor(t1[:, :], ui[:, :], kit[:, cc], op=ALU.mult), [uib, kib], [t1b])
                    V(lambda e: e.tensor_tensor(yi[:, :], ur[:, :], kit[:, cc], op=ALU.mult), [urb, kib], [yib])
                    G(lambda e: e.tensor_tensor(t2[:, :], ui[:, :], krt[:, cc], op=ALU.mult), [uib, krb], [t2b])
                    V(lambda e: e.tensor_tensor(yr[:, :], yr[:, :], t1[:, :], op=ALU.subtract), [yrb, t1b], [yrb])
                    V(lambda e: e.tensor_tensor(yi[:, :], yi[:, :], t2[:, :], op=ALU.add), [yib, t2b], [yib])
                    if fc == 0:
                        V(lambda e: e.scalar_tensor_tensor(out=yr[0:1, :], in0=ur[0:1, :], scalar=0.5, in1=krt[0:1, cc],
                                                           op0=ALU.mult, op1=ALU.mult), [urb, krb, yrb], [yrb])
                        V(lambda e: e.scalar_tensor_tensor(out=yi[0:1, :], in0=ui[0:1, :], scalar=0.5, in1=kit[0:1, cc],
                                                           op0=ALU.mult, op1=ALU.mult), [uib, kib, yib], [yib])
                    t.dma("sp", c.d_Yr[0][fc * 128:(fc + 1) * 128, cc], yr[:, :], reads=[yrb], writes=[c.d_Yr[1]])
                    t.dma("sp", c.d_Yi[0][fc * 128:(fc + 1) * 128, cc], yi[:, :], reads=[yib], writes=[c.d_Yi[1]])
            t.barrier()
        with ExitStack() as st:
            S = lambda name, shape, dt=F32: st.enter_context(nc.sbuf_tensor(_u(name), shape, dt))
            cb = Buf("hyc3")
            skip = S("hy_skip2", [128, 8]); load_cols(c, skip, cb, c.hy_skip[layer], 8)
            itA = Rot([S("hy_itA%d" % i, [128, 16, 512]) for i in range(2)])
            itB = Rot([S("hy_itB%d" % i, [128, 16, 512]) for i in range(2)])
            yrr = Rot([S("hy_yr%d" % i, [128, 16, 128]) for i in range(2)])
            yir = Rot([S("hy_yi%d" % i, [128, 16, 128]) for i in range(2)])
            usr = Rot([S("hy_us%d" % i, [128, L]) for i in range(2)])
            x0r = Rot([S("hy_x0%d" % i, [128, L]) for i in range(2)])
            outr = Rot([S("hy_out%d" % i, [128, 512]) for i in range(3)])
            outb = Rot([S("hy_outb%d" % i, [128, 512], BF16) for i in range(3)])
            for ch in range(8):
                rows = slice(ch * 128, (ch + 1) * 128)
                yrt, yrb = yrr.next(); yit, yib = yir.next()
                t.dma("sp", yrt[:, :, :], c.d_Yr[0][:, rows].rearrange("(k p) m -> p k m", p=128), reads=[c.d_Yr[1]], writes=[yrb])
                t.dma("sp", yit[:, :, :], c.d_Yi[0][:, rows].rearrange("(k p) m -> p k m", p=128), reads=[c.d_Yi[1]], writes=[yib])
                us, usb = usr.next(); x0, x0b = x0r.next()
                t.dma("sp", us[:, :], c.d_uT[0][rows, :], reads=[c.d_uT[1]], writes=[usb])
                t.dma("sp", x0[:, :], c.d_x0c[0][rows, :], reads=[c.d_x0c[1]], writes=[x0b])
                G(lambda e: e.tensor_scalar(us[:, :], us[:, :], skip[:, ch:ch + 1], None, op0=ALU.mult), [usb, cb], [usb])
                for tt in range(4):
                    cs = slice(tt * 512, (tt + 1) * 512)
                    ia, iab = itA.next(); ib, ibb = itB.next()
                    t.dma("sp", ia[:, :, :], kA[:, cs].rearrange("(k p) q -> p k q", p=128), writes=[iab])
                    t.dma("sp", ib[:, :, :], kBT[:, cs].rearrange("(k p) q -> p k q", p=128), writes=[ibb])
                    ps, pb = c.psum.next()
                    for k in range(16):
                        PE(lambda e: e.matmul(ps[:, :], yrt[:, k, :], ia[:, k, :], start=(k == 0), stop=False),
                           [yrb, iab], [pb])
                    for k in range(16):
                        PE(lambda e: e.matmul(ps[:, :], yit[:, k, :], ib[:, k, :], start=False, stop=(k == 15)),
                           [yib, ibb], [pb])
                    o_, ob_ = outr.next()
                    V(lambda e: e.scalar_tensor_tensor(out=o_[:, :], in0=ps[:, :], scalar=2.0 / (2 * L), in1=us[:, cs],
                                                       op0=ALU.mult, op1=ALU.add), [pb, usb], [ob_])
                    o2_, ob2_ = outb.next()
                    G(lambda e: e.tensor_tensor(o2_[:, :], o_[:, :], x0[:, cs], op=ALU.mult), [ob_, x0b], [ob2_])
                    t.dma("sp", c.d_ohyT[0][rows, cs], o2_[:, :], reads=[ob2_], writes=[c.d_ohyT[1]])
            t.barrier()


def att_consts():
    W = 128
    kofs = np.arange(3 * W)[None, :] - W
    rel = kofs - np.arange(W)[:, None]
    half, max_exact = 16, 8
    bucket = (rel > 0).astype(np.int32) * half
    n = np.abs(rel)
    n_safe = np.maximum(n, 1).astype(np.float32)
    large = max_exact + (np.log(n_safe / np.float32(max_exact)) / np.float32(math.log(128 / max_exact))
                         * np.float32(half - max_exact)).astype(np.int32)
    large = np.clip(large, 0, half - 1)
    bucket = bucket + np.where(n < max_exact, n, large)
    onehot = (bucket[None, :, :] == np.arange(32)[:, None, None]).astype(np.float32)
    maskadd = np.where(np.abs(rel) <= 128, 0.0, -1e30).astype(np.float32)
    return dict(at_onehot=onehot, at_mask=maskadd)


def phase_attn(c, layer):
    t, nc = c.t, c.nc
    V = lambda fn, r=(), w=(): t.op("dve", fn, reads=r, writes=w)
    A = lambda fn, r=(), w=(): t.op("act", fn, reads=r, writes=w)
    G = lambda fn, r=(), w=(): t.op("pool", fn, reads=r, writes=w)
    PE = lambda fn, r=(), w=(): t.op("pe", fn, reads=r, writes=w)
    with ExitStack() as st:
        S = lambda name, shape, dt=F32: st.enter_context(nc.sbuf_tensor(_u(name), shape, dt))
        cb = Buf("atc")
        biasM = S("at_bias", [128, 16, 384])
        rbb = S("at_rbb", [128, 512])
        t.dma("sp", rbb[:, :], c.rel_bias.rearrange("b h -> (b h)").partition_broadcast(128), writes=[cb])
        sinkb = S("at_sink", [128, 16])
        t.dma("sp", sinkb[:, :], c.att_sink[layer].partition_broadcast(128), writes=[cb])
        mk = S("at_mk", [128, 384])
        t.dma("sp", mk[:, :], c.k["at_mask"][:, :], writes=[cb])
        V(lambda e: e.tensor_copy(biasM[:, :, :], mk[:, :].unsqueeze(1).to_broadcast([128, 16, 384])), [cb], [cb])
        ohr = Rot([S("at_oh%d" % i, [128, 384]) for i in range(2)])
        for b in range(32):
            oh, ohb = ohr.next()
            t.dma("sp", oh[:, :], c.k["at_onehot"][b], writes=[ohb])
            for h in range(16):
                V(lambda e: e.scalar_tensor_tensor(out=biasM[:, h, :], in0=oh[:, :], scalar=rbb[:, b * 16 + h:b * 16 + h + 1],
                                                   in1=biasM[:, h, :], op0=ALU.mult, op1=ALU.add), [ohb, cb], [cb])
        kd = []
        for g in range(2):
            k_ = S("at_kd%d" % g, [128, L])
            for half in range(2):
                t.dma("sp", k_[half * 64:(half + 1) * 64, :], c.d_akT[0][g * 64:(g + 1) * 64, :],
                      reads=[c.d_akT[1]], writes=[cb])
            kd.append(k_)
        vx = {}
        for g in range(2):
            for hh in range(2):
                v_ = S("at_v%d%d" % (g, hh), [128, NT, 128])
                G(lambda e: e.memset(v_[:, :, :], 0.0), [], [cb])
                t.dma("sp", v_[:, :, hh * 64:(hh + 1) * 64],
                      c.d_av[0][:, g * 64:(g + 1) * 64].rearrange("(n p) d -> p n d", p=128),
                      reads=[c.d_av[1]], writes=[cb])
                vx[(g, hh)] = v_
        qr = Rot([S("at_q%d" % i, [128, L]) for i in range(2)])
        otr = Rot([S("at_o%d" % i, [128, L], BF16) for i in range(2)])
        sr = Rot([S("at_s%d" % i, [128, 384]) for i in range(3)])
        pr = Rot([S("at_p%d" % i, [128, 384]) for i in range(3)])
        ptr = Rot([S("at_pt%d" % i, [128, 384]) for i in range(3)])
        smr = Rot([S("at_sm%d" % i, [128, 8]) for i in range(4)])
        for j in range(8):
            q, qb = qr.next()
            t.dma("sp", q[:, :], c.d_aqT[0][j * 128:(j + 1) * 128, :], reads=[c.d_aqT[1]], writes=[qb])
            ot, otb = otr.next()
            for n in range(NT):
                klo = max(0, (n - 1) * 128); khi = min(L, (n + 2) * 128)
                bo = klo - (n - 1) * 128; wdt = khi - klo
                nkb = wdt // 128
                oacc, oab = c.psacc.next()
                for hh in range(2):
                    h = 2 * j + hh; g = h // 8; po = hh * 64
                    ps, pb = c.psum.next()
                    PE(lambda e: e.matmul(ps[:, 0:wdt], q[po:po + 64, n * 128:(n + 1) * 128],
                                          kd[g][po:po + 64, klo:khi], start=True, stop=True), [qb, cb], [pb])
                    s_, sb_ = sr.next()
                    V(lambda e: e.scalar_tensor_tensor(out=s_[:, 0:wdt], in0=ps[:, 0:wdt], scalar=0.125,
                                                       in1=biasM[:, h, bo:bo + wdt], op0=ALU.mult, op1=ALU.add),
                      [pb, cb], [sb_])
                    sm, smb = smr.next()
                    V(lambda e: e.reduce_max(sm[:, 0:1], s_[:, 0:wdt], axis=AX.X), [sb_], [smb])
                    V(lambda e: e.tensor_tensor(sm[:, 0:1], sm[:, 0:1], sinkb[:, h:h + 1], op=ALU.max), [smb, cb], [smb])
                    V(lambda e: e.tensor_scalar(sm[:, 1:2], sm[:, 0:1], -1.0, None, op0=ALU.mult), [smb], [smb])
                    p_, pb_ = pr.next()
                    A(lambda e: e.activation(p_[:, 0:wdt], s_[:, 0:wdt], AF.Exp, bias=sm[:, 1:2], scale=1.0,
                                             accum_out=sm[:, 2:3]), [sb_, smb], [pb_, smb])
                    A(lambda e: e.activation(sm[:, 3:4], sinkb[:, h:h + 1], AF.Exp, bias=sm[:, 1:2], scale=1.0),
                      [smb, cb], [smb])
                    V(lambda e: e.tensor_tensor(sm[:, 4:5], sm[:, 2:3], sm[:, 3:4], op=ALU.add), [smb], [smb])
                    V(lambda e: e.reciprocal(sm[:, 5:6], sm[:, 4:5]), [smb], [smb])
                    G(lambda e: e.tensor_scalar(p_[:, 0:wdt], p_[:, 0:wdt], sm[:, 5:6], None, op0=ALU.mult),
                      [pb_, smb], [pb_])
                    ps2, pb2 = c.psum.next()
                    for kb in range(nkb):
                        PE(lambda e: e.transpose(ps2[:, kb * 128:(kb + 1) * 128], p_[:, kb * 128:(kb + 1) * 128],
                                                 c.ident[:, :]), [pb_, c.ident_buf], [pb2])
                    pt, ptb = ptr.next()
                    A(lambda e: e.copy(pt[:, 0:wdt], ps2[:, 0:wdt]), [pb2], [ptb])
                    for kb in range(nkb):
                        kt_ = klo // 128 + kb
                        PE(lambda e: e.matmul(oacc[:, 0:128], vx[(g, hh)][:, kt_, :], pt[:, kb * 128:(kb + 1) * 128],
                                              start=(hh == 0 and kb == 0), stop=(hh == 1 and kb == nkb - 1)),
                           [cb, ptb], [oab])
                evac(c, ot[:, n * 128:(n + 1) * 128], oacc[:, 0:128], [oab], [otb])
            t.dma("sp", c.d_oatT[0][j * 128:(j + 1) * 128, :], ot[:, :], reads=[otb], writes=[c.d_oatT[1]])
        t.barrier()


def phase_merge(c, layer):
    t, nc = c.t, c.nc
    V = lambda fn, r=(), w=(): t.op("dve", fn, reads=r, writes=w)
    G = lambda fn, r=(), w=(): t.op("pool", fn, reads=r, writes=w)
    PE = lambda fn, r=(), w=(): t.op("pe", fn, reads=r, writes=w)
    with ExitStack() as st:
        S = lambda name, shape, dt=F32: st.enter_context(nc.sbuf_tensor(_u(name), shape, dt))
        otr = Rot([S("mg_o%d" % i, [128, 8, L], BF16) for i in range(2)])
        acc = S("mg_acc", [128, 4, L]); accb = Buf()
        accbf = Rot([S("mg_accb%d" % i, [128, L], BF16) for i in range(2)])
        gr = Rot([S("mg_g%d" % i, [128, L]) for i in range(2)])
        wrot = WPool(c, S, "mg_w", 8, 512)
        tmpr = Rot([S("mg_t%d" % i, [128, 512]) for i in range(3)])
        srcs = (c.d_ohgT, c.d_ohyT, c.d_oatT)
        for blk in range(4):
            for n in range(3):
                wt, wb = wrot.load(c.w_branch[layer][n][:, blk * 512:(blk + 1) * 512], 8, 512)
                o_, ob_ = otr.next()
                t.dma("sp", o_[:, :, :], srcs[n][0].rearrange("(k p) q -> p k q", p=128),
                      reads=[srcs[n][1]], writes=[ob_])
                for cg in range(4):
                    dg = blk * 4 + cg
                    g_, gb_ = gr.next()
                    t.dma("sp", g_[:, :], c.d_gT[0][n * D + dg * 128:n * D + (dg + 1) * 128, :],
                          reads=[c.d_gT[1]], writes=[gb_])
                    for tt in range(4):
                        cs = slice(tt * 512, (tt + 1) * 512)
                        ps, pb = c.psum.next()
                        for kc in range(8):
                            PE(lambda e: e.matmul(ps[:, :], wt[:, kc, cg * 128:(cg + 1) * 128], o_[:, kc, cs],
                                                  start=(kc == 0), stop=(kc == 7)), [wb, ob_], [pb])
                        if n == 0:
                            V(lambda e: e.tensor_tensor(acc[:, cg, cs], ps[:, :], g_[:, cs], op=ALU.mult),
                              [pb, gb_], [accb])
                        else:
                            tm_, tmb_ = tmpr.next()
                            V(lambda e: e.tensor_tensor(tm_[:, :], ps[:, :], g_[:, cs], op=ALU.mult),
                              [pb, gb_], [tmb_])
                            G(lambda e: e.tensor_tensor(acc[:, cg, cs], acc[:, cg, cs], tm_[:, :], op=ALU.add),
                              [accb, tmb_], [accb])
                    if n == 2:
                        ab_, abb_ = accbf.next()
                        t.op("act", lambda e: e.copy(ab_[:, :], acc[:, cg, :]), reads=[accb], writes=[abb_])
                        t.dma("sp", c.d_yT[0][dg * 128:(dg + 1) * 128, :], ab_[:, :], reads=[abb_],
                              writes=[c.d_yT[1]])
        t.barrier()
    with ExitStack() as st:
        S = lambda name, shape, dt=F32: st.enter_context(nc.sbuf_tensor(_u(name), shape, dt))
        yT = S("mg_yT", [128, KC, L], BF16); yTb = Buf()
        t.dma("sp", yT[:, :, :], c.d_yT[0].rearrange("(k p) q -> p k q", p=128), reads=[c.d_yT[1]], writes=[yTb])
        wrot = WPool(c, S, "mg_wo", KC, 512)
        stg_tm = Rot([S("mg_s%d" % i, [128, 512]) for i in range(3)])

        def epi2(ps, pb, tt, cc0, cw):
            s, sb = stg_tm.next()
            evac(c, s[:, 0:cw], ps[:, 0:cw], [pb], [sb])
            t.dma("sp", c.d_mix[0][tt * 128:(tt + 1) * 128, cc0:cc0 + cw], s[:, 0:cw],
                  reads=[sb], writes=[c.d_mix[1]])

        gemm_tm(c, c.w_out[layer], D, yT, yTb, KC, L, epi2, wrot)
        t.barrier()


SIG7 = 1.0 / (1.0 + math.exp(-1.702 * 7.0))


def phase_moe(c, layer):
    t, nc = c.t, c.nc
    V = lambda fn, r=(), w=(): t.op("dve", fn, reads=r, writes=w)
    A = lambda fn, r=(), w=(): t.op("act", fn, reads=r, writes=w)
    G = lambda fn, r=(), w=(): t.op("pool", fn, reads=r, writes=w)
    PE = lambda fn, r=(), w=(): t.op("pe", fn, reads=r, writes=w)
    HT = L // 2
    NTH = HT // 128
    with ExitStack() as st:
        S = lambda name, shape, dt=F32: st.enter_context(nc.sbuf_tensor(_u(name), shape, dt))
        gs_ = Rot([S("me_g%d" % i, [128, 512]) for i in range(2)])
        sg_ = Rot([S("me_s%d" % i, [128, 512]) for i in range(2)])
        us_ = Rot([S("me_u%d" % i, [128, 512]) for i in range(2)])
        xh = S("me_x", [128, KC, HT], BF16); xhb = Buf()
        yacc = S("me_y", [128, NTH, D]); yab = Buf()
        act2 = S("me_a", [128, KC * HT], BF16); actb = Buf()
        actT = act2[:, :].rearrange("p (k q) -> p k q", k=KC)
        bd = S("me_bd", [N_EXP, D]); bdb = Buf()
        gateT = S("me_gT", [N_EXP, HT]); gateTb = Buf()
        wstage = Rot([S("me_ws%d" % i, [128, KC, 256]) for i in range(2)])
        wbf = Rot([S("me_wb%d" % i, [128, KC, 256], BF16) for i in range(2)])
        gate = S("me_gate", [128, NTH, N_EXP]); gateb = Buf()
        c7 = S("me_c7", [128, 1])
        G(lambda e: e.memset(c7[:, :], 7.0), [], [gateb])
        bgu = Rot([S("me_bgu%d" % i, [128, 32]) for i in range(2)])
        bgs = Rot([S("me_bgs%d" % i, [128, 16]) for i in range(2)])
        items = []
        for ex in range(c.moe_nexp):
            items += [(ex, "gu", fc) for fc in range(16)]
            items += [(ex, "dn", db) for db in range(8)]
        import os as _os2
        items = items[:int(_os2.environ.get("MOE_ITEMS", len(items)))]

        def load_item(it):
            ex, kind, j = it
            stg, stgb = wstage.next()
            if kind == "gu":
                for two in range(2):
                    src = c.w_gate_up[layer][ex][:, two * DFF + j * 128:two * DFF + (j + 1) * 128].rearrange(
                        "(kc p) c -> p kc c", p=128)
                    t.dma("sp", stg[:, :, two * 128:(two + 1) * 128], src, writes=[stgb])
            else:
                src = c.w_down[layer][ex][:, j * 256:(j + 1) * 256].rearrange("(kc p) c -> p kc c", p=128)
                t.dma("sp", stg[:, :, :], src, writes=[stgb])
            bf, bfb = wbf.next()
            G(lambda e: e.tensor_copy(bf[:, :, :], stg[:, :, :]), [stgb], [bfb])
            return bf, bfb

        for hf in range(2):
            t0 = hf * HT
            t.dma("sp", xh[:, :, :], c.d_xT[0][:, t0:t0 + HT].rearrange("(k p) q -> p k q", p=128),
                  reads=[c.d_xT[1]], writes=[xhb])
            t.dma("sp", gate[:, :, :], c.d_gate[0][t0:t0 + HT, :].rearrange("(n p) e -> p n e", p=128),
                  reads=[c.d_gate[1]], writes=[gateb])
            t.dma("sp", gateT[:, :], c.d_gateT[0][:, t0:t0 + HT], reads=[c.d_gateT[1]], writes=[gateTb])
            t.dma("sp", bd[:, :], c.b_down[layer], writes=[bdb])
            import os as _os
            _lv = int(_os.environ.get("MOE_INIT", 9))
            if _lv == 0:
                t.barrier(); return
            for tt in range(NTH if _lv in (2, 9) else 1):
                for cb4 in range(4):
                    ps, pb = c.psum.next()
                    PE(lambda e: e.matmul(ps[:, :], gateT[:, tt * 128:(tt + 1) * 128], bd[:, cb4 * 512:(cb4 + 1) * 512],
                                          start=True, stop=True), [gateTb, bdb], [pb])
                    if _lv != 3:
                        evac(c, yacc[:, tt, cb4 * 512:(cb4 + 1) * 512], ps[:, :], [pb], [yab])
            if _lv in (3, 4):
                t.barrier(); return
            nxt = load_item(items[0])
            bg = bgb = bs = bsb = None
            for ii, (ex, kind, j) in enumerate(items):
                wt, wtb = nxt
                if ii + 1 < len(items):
                    nxt = load_item(items[ii + 1])
                if kind == "gu":
                    fc = j
                    if fc == 0:
                        bg, bgb = bgu.next()
                        load_cols(c, bg, bgb, c.b_gate_up[layer][ex], 32)
                        bs, bsb = bgs.next()
                        V(lambda e: e.tensor_tensor(bs[:, :], bg[:, 0:16], c7[:, 0:1].to_broadcast([128, 16]), op=ALU.mult),
                          [bgb, gateb], [bsb])
                        V(lambda e: e.tensor_scalar(bs[:, :], bs[:, :], 1.702 / 7.0, None, op0=ALU.mult), [bsb], [bsb])
                    wv = wt[:, :, :].rearrange("p k (two f) -> p k two f", two=2)
                    for tt in range(HT // 512):
                        cs = slice(tt * 512, (tt + 1) * 512)
                        psg, pbg = c.psum.next()
                        for kc in range(KC):
                            PE(lambda e: e.matmul(psg[:, :], wv[:, kc, 0, :], xh[:, kc, cs],
                                                  start=(kc == 0), stop=(kc == KC - 1)), [wtb, xhb], [pbg])
                        psu, pbu = c.psum.next()
                        for kc in range(KC):
                            PE(lambda e: e.matmul(psu[:, :], wv[:, kc, 1, :], xh[:, kc, cs],
                                                  start=(kc == 0), stop=(kc == KC - 1)), [wtb, xhb], [pbu])
                        g1, g1b = gs_.next(); s1, s1b = sg_.next(); u1, u1b = us_.next()
                        A(lambda e: e.activation(s1[:, :], psg[:, :], AF.Sigmoid, bias=bs[:, fc:fc + 1], scale=1.702),
                          [pbg, bsb], [s1b])
                        if c.moe_epi == 1:
                            continue
                        if c.moe_epi == 24:
                            V(lambda e: e.tensor_copy(g1[:, :], psu[:, :]), [pbu], [g1b])
                            continue
                        if c.moe_epi == 25:
                            A(lambda e: e.copy(g1[:, :], psu[:, :]), [pbu], [g1b])
                            continue
                        if c.moe_epi == 26:
                            V(lambda e: e.tensor_copy(g1[:, :], psg[:, :]), [pbg, s1b], [g1b])
                            continue
                        if c.moe_epi == 21:
                            V(lambda e: e.tensor_copy(g1[:, :], psg[:, :]), [pbg], [g1b])
                            continue
                        if c.moe_epi == 7:
                            A(lambda e: e.activation(g1[:, :], psg[:, :], AF.Identity, bias=bg[:, fc:fc + 1], scale=1.0),
                              [pbg, bgb], [g1b])
                            A(lambda e: e.activation(u1[:, :], psu[:, :], AF.Identity, bias=bg[:, 16 + fc:17 + fc], scale=1.0),
                              [pbu, bgb], [u1b])
                            V(lambda e: e.tensor_scalar(g1[:, :], g1[:, :], 7.0, None, op0=ALU.min), [g1b], [g1b])
                            V(lambda e: e.tensor_scalar(u1[:, :], u1[:, :], 7.0, None, op0=ALU.min), [u1b], [u1b])
                        else:
                            V(lambda e: e.tensor_scalar(g1[:, :], psg[:, :], bg[:, fc:fc + 1], c7[:, 0:1], op0=ALU.add, op1=ALU.min),
                              [pbg, bgb, gateb], [g1b])
                            V(lambda e: e.tensor_scalar(u1[:, :], psu[:, :], bg[:, 16 + fc:17 + fc], c7[:, 0:1], op0=ALU.add, op1=ALU.min),
                              [pbu, bgb, gateb], [u1b])
                        V(lambda e: e.scalar_tensor_tensor(out=g1[:, :], in0=s1[:, :], scalar=float(SIG7), in1=g1[:, :],
                                                           op0=ALU.min, op1=ALU.mult), [s1b, g1b], [g1b])
                        V(lambda e: e.tensor_scalar(u1[:, :], u1[:, :], -7.0, 1.0, op0=ALU.max, op1=ALU.add), [u1b], [u1b])
                        V(lambda e: e.tensor_tensor(actT[:, fc, cs], g1[:, :], u1[:, :], op=ALU.mult), [g1b, u1b], [actb])
                else:
                    db = j
                    for tt in range(NTH):
                        ps, pb = c.psum.next()
                        for kc in range(KC):
                            PE(lambda e: e.matmul(ps[:, 0:256], actT[:, kc, tt * 128:(tt + 1) * 128], wt[:, kc, :],
                                                  start=(kc == 0), stop=(kc == KC - 1)), [actb, wtb], [pb])
                        V(lambda e: e.scalar_tensor_tensor(
                            out=yacc[:, tt, db * 256:(db + 1) * 256], in0=ps[:, 0:256], scalar=gate[:, tt, ex:ex + 1],
                            in1=yacc[:, tt, db * 256:(db + 1) * 256], op0=ALU.mult, op1=ALU.add),
                          [pb, gateb, yab], [yab])
            t.dma("sp", c.d_ffn[0][t0:t0 + HT, :].rearrange("(n p) d -> p n d", p=128), yacc[:, :, :],
                  reads=[yab], writes=[c.d_ffn[1]])
        t.barrier()


def relayout_gu(w):
    e = w.shape[0]
    v = w.reshape(e, KC, 128, 2, 16, 128)
    v = v.transpose(0, 4, 2, 1, 3, 5)
    return np.ascontiguousarray(v).reshape(e, 16, 128, KC * 256)


def relayout_dn(w):
    e = w.shape[0]
    v = w.reshape(e, KC, 128, 8, 256)
    v = v.transpose(0, 3, 2, 1, 4)
    return np.ascontiguousarray(v).reshape(e, 8, 128, KC * 256)


def kernel(**inputs):
    n = 8
    nc = build()
    consts = host_consts()
    shared = {}
    for name in LAST_INPUT_NAMES:
        if name == "x":
            continue
        if name.startswith("k_"):
            shared[name] = consts[name[2:]]
        elif name.startswith("w_gate_up"):
            shared[name] = relayout_gu(np.asarray(inputs["w_gate_up"], dtype=np.float32)[int(name[-1])])
        elif name.startswith("w_down"):
            shared[name] = relayout_dn(np.asarray(inputs["w_down"], dtype=np.float32)[int(name[-1])])
        else:
            shared[name] = np.ascontiguousarray(np.asarray(inputs[name], dtype=np.float32))
    x = np.asarray(inputs["x"], dtype=np.float32)
    in_maps = []
    for b in range(n):
        m = dict(shared)
        m["x"] = np.ascontiguousarray(x[b])
        in_maps.append(m)
    res = run_bass_kernel_spmd(nc, in_maps, core_ids=list(range(n)))
    return np.stack([np.asarray(res.results[b]["out"], dtype=np.float32) for b in range(n)], axis=0)


CAP = 256


def moe_consts():
    s = np.arange(128)
    ustrict = (s[:, None] < s[None, :]).astype(np.float32)
    sele = np.zeros((N_EXP, N_EXP, 128), np.float32)
    for e in range(N_EXP):
        sele[e, e, :] = 1.0
    iota_c = np.tile(np.arange(CAP, dtype=np.float32)[None, :], (128, 1))
    pidx = (np.arange(128, dtype=np.float32)[:, None] + 128.0 * np.arange(CAP // 128, dtype=np.float32)[None, :])
    return dict(me_ustrict=ustrict, me_sele=sele, me_iota=iota_c, me_pidx=np.ascontiguousarray(pidx))


def phase_moe_sparse(c, layer):
    t, nc = c.t, c.nc
    V = lambda fn, r=(), w=(): t.op("dve", fn, reads=r, writes=w)
    A = lambda fn, r=(), w=(): t.op("act", fn, reads=r, writes=w)
    G = lambda fn, r=(), w=(): t.op("pool", fn, reads=r, writes=w)
    PE = lambda fn, r=(), w=(): t.op("pe", fn, reads=r, writes=w)
    HT = L // 2
    NTH = HT // 128
    NCC = CAP // 128
    with ExitStack() as st:
        S = lambda name, shape, dt=F32: st.enter_context(nc.sbuf_tensor(_u(name), shape, dt))
        gs_ = Rot([S("ms_g%d" % i, [128, CAP]) for i in range(2)])
        sg_ = Rot([S("ms_s%d" % i, [128, CAP]) for i in range(2)])
        us_ = Rot([S("ms_u%d" % i, [128, CAP]) for i in range(2)])
        xtm = S("ms_x", [128, NTH, D], BF16); xtmb = Buf()
        yacc = S("ms_y", [128, NTH, D]); yab = Buf()
        xeT = S("ms_xe", [128, KC, CAP], BF16); xeb = Buf()
        actT = S("ms_a", [128, KC, CAP], BF16); actb = Buf()
        ye = S("ms_ye", [128, NCC, D], BF16); yeb = Buf()
        selr = Rot([S("ms_sel%d" % i, [128, NTH, CAP], BF16) for i in range(2)])
        seltr = Rot([S("ms_selT%d" % i, [128, NCC, HT], BF16) for i in range(2)])
        wstage = Rot([S("ms_ws%d" % i, [128, KC, 256]) for i in range(2)])
        wbf = Rot([S("ms_wb%d" % i, [128, KC, 256], BF16) for i in range(2)])
        gate = S("ms_gate", [128, NTH, N_EXP]); gateb = Buf()
        rk = S("ms_rk", [128, NTH, N_EXP]); rkb = Buf()
        msk = S("ms_msk", [128, NTH, N_EXP]); mskb = Buf()
        rkT = S("ms_rkT", [N_EXP, HT]); rkTb = Buf()
        cb = Buf("msc")
        iota_c = S("ms_iota", [128, CAP]); t.dma("sp", iota_c[:, :], c.k["me_iota"][:, :], writes=[cb])
        pidx = S("ms_pidx", [128, NCC]); t.dma("sp", pidx[:, :], c.k["me_pidx"][:, :], writes=[cb])
        ustr = S("ms_us", [128, 128]); t.dma("sp", ustr[:, :], c.k["me_ustrict"][:, :], writes=[cb])
        ones = S("ms_ones", [128, 128]); G(lambda e: e.memset(ones[:, :], 1.0), [], [cb])
        c7 = S("ms_c7", [128, 1]); G(lambda e: e.memset(c7[:, :], 7.0), [], [cb])
        seler = Rot([S("ms_sele%d" % i, [N_EXP, 128]) for i in range(2)])
        bgu = Rot([S("ms_bgu%d" % i, [128, 32]) for i in range(2)])
        bgs = Rot([S("ms_bgs%d" % i, [128, 16]) for i in range(2)])
        items = []
        for ex in range(c.moe_nexp):
            items += [(ex, "gu", fc) for fc in range(16)]
            items += [(ex, "dn", db) for db in range(8)]

        def load_item(it):
            ex, kind, j = it
            stg, stgb = wstage.next()
            src = (c.w_gate_up if kind == "gu" else c.w_down)[layer][ex, j]
            t.dma("sp", stg[:, :, :].rearrange("p k c -> p (k c)"), src, writes=[stgb])
            bf, bfb = wbf.next()
            G(lambda e: e.tensor_copy(bf[:, :, :], stg[:, :, :]), [stgb], [bfb])
            return bf, bfb

        for hf in range(2):
            t0 = hf * HT
            t.dma("sp", xtm[:, :, :], c.d_xtm[0][t0:t0 + HT, :].rearrange("(n p) d -> p n d", p=128),
                  reads=[c.d_xtm[1]], writes=[xtmb])
            t.dma("sp", gate[:, :, :], c.d_gate[0][t0:t0 + HT, :].rearrange("(n p) e -> p n e", p=128),
                  reads=[c.d_gate[1]], writes=[gateb])
            (sa, sab), (sb_, sbb) = wstage.next(), wstage.next()
            bd = sa[0:N_EXP, 0:8, :].rearrange("p k c -> p (k c)")
            gateT = sb_[0:N_EXP, 0:4, :].rearrange("p k c -> p (k c)")
            t.dma("sp", gateT, c.d_gateT[0][:, t0:t0 + HT], reads=[c.d_gateT[1]], writes=[sbb])
            t.dma("sp", bd, c.b_down[layer], writes=[sab])
            for tt in range(NTH):
                for cb4 in range(4):
                    ps, pb = c.psum.next()
                    PE(lambda e: e.matmul(ps[:, :], gateT[:, tt * 128:(tt + 1) * 128], bd[:, cb4 * 512:(cb4 + 1) * 512],
                                          start=True, stop=True), [sab, sbb], [pb])
                    evac(c, yacc[:, tt, cb4 * 512:(cb4 + 1) * 512], ps[:, :], [pb], [yab])
            V(lambda e: e.tensor_scalar(msk[:, :, :], gate[:, :, :], 0.0, None, op0=ALU.is_gt), [gateb], [mskb])
            for n in range(NTH):
                ps, pb = c.psum.next()
                for m in range(n):
                    PE(lambda e: e.matmul(ps[:, 0:N_EXP], ones[:, :], msk[:, m, :], start=(m == 0), stop=False),
                       [cb, mskb], [pb])
                PE(lambda e: e.matmul(ps[:, 0:N_EXP], ustr[:, :], msk[:, n, :], start=(n == 0), stop=True),
                   [cb, mskb], [pb])
                V(lambda e: e.tensor_tensor(rk[:, n, :], ps[:, 0:N_EXP], msk[:, n, :], op=ALU.mult), [pb, mskb], [rkb])
                V(lambda e: e.tensor_tensor(rk[:, n, :], rk[:, n, :], msk[:, n, :], op=ALU.add), [rkb, mskb], [rkb])
                V(lambda e: e.tensor_scalar(rk[:, n, :], rk[:, n, :], -1.0, None, op0=ALU.add), [rkb], [rkb])
                ps2, pb2 = c.psum.next()
                PE(lambda e: e.transpose(ps2[0:N_EXP, 0:128], rk[:, n, :], c.ident[:, :]), [rkb, c.ident_buf], [pb2])
                A(lambda e: e.copy(rkT[:, n * 128:(n + 1) * 128], ps2[0:N_EXP, 0:128]), [pb2], [rkTb])
            nxt = load_item(items[0])
            bg = bgb = bs = bsb = None
            sel = selb = selT = selTb = None
            for ii, (ex, kind, j) in enumerate(items):
                wt, wtb = nxt
                if ii + 1 < len(items):
                    nxt = load_item(items[ii + 1])
                if kind == "gu":
                    fc = j
                    if fc == 0:
                        bg, bgb = bgu.next()
                        load_cols(c, bg, bgb, c.b_gate_up[layer][ex], 32)
                        bs, bsb = bgs.next()
                        V(lambda e: e.tensor_scalar(bs[:, :], bg[:, 0:16], 1.702, None, op0=ALU.mult), [bgb], [bsb])
                        sel, selb = selr.next()
                        for n in range(NTH):
                            V(lambda e: e.tensor_scalar(sel[:, n, :], iota_c[:, :], rk[:, n, ex:ex + 1], None,
                                                        op0=ALU.is_equal), [cb, rkb], [selb])
                        se, seb = seler.next()
                        t.dma("sp", se[:, :], c.k["me_sele"][ex], writes=[seb])
                        selT, selTb = seltr.next()
                        for th in range(HT // 512):
                            psb, pbb = c.psum.next()
                            PE(lambda e: e.matmul(psb[:, :], se[:, :], rkT[:, th * 512:(th + 1) * 512], start=True, stop=True),
                               [seb, rkTb], [pbb])
                            for cc in range(NCC):
                                V(lambda e: e.tensor_scalar(selT[:, cc, th * 512:(th + 1) * 512], psb[:, :], pidx[:, cc:cc + 1],
                                                            None, op0=ALU.is_equal), [pbb, cb], [selTb])
                        for kc in range(KC):
                            psx, pbx = c.psum.next()
                            for n in range(NTH):
                                PE(lambda e: e.matmul(psx[:, 0:CAP], xtm[:, n, kc * 128:(kc + 1) * 128], sel[:, n, :],
                                                      start=(n == 0), stop=(n == NTH - 1)), [xtmb, selb], [pbx])
                            evac(c, xeT[:, kc, :], psx[:, 0:CAP], [pbx], [xeb])
                    psg, pbg = c.psum.next()
                    for kc in range(KC):
                        PE(lambda e: e.matmul(psg[:, 0:CAP], wt[:, kc, 0:128], xeT[:, kc, :],
                                              start=(kc == 0), stop=(kc == KC - 1)), [wtb, xeb], [pbg])
                    psu, pbu = c.psum.next()
                    for kc in range(KC):
                        PE(lambda e: e.matmul(psu[:, 0:CAP], wt[:, kc, 128:256], xeT[:, kc, :],
                                              start=(kc == 0), stop=(kc == KC - 1)), [wtb, xeb], [pbu])
                    g1, g1b = gs_.next(); s1, s1b = sg_.next(); u1, u1b = us_.next()
                    A(lambda e: e.activation(s1[:, :], psg[:, 0:CAP], AF.Sigmoid, bias=bs[:, fc:fc + 1], scale=1.702),
                      [pbg, bsb], [s1b])
                    V(lambda e: e.tensor_scalar(g1[:, :], psg[:, 0:CAP], bg[:, fc:fc + 1], c7[:, 0:1], op0=ALU.add, op1=ALU.min),
                      [pbg, bgb, cb], [g1b])
                    V(lambda e: e.tensor_scalar(u1[:, :], psu[:, 0:CAP], bg[:, 16 + fc:17 + fc], c7[:, 0:1], op0=ALU.add, op1=ALU.min),
                      [pbu, bgb, cb], [u1b])
                    V(lambda e: e.scalar_tensor_tensor(out=g1[:, :], in0=s1[:, :], scalar=float(SIG7), in1=g1[:, :],
                                                       op0=ALU.min, op1=ALU.mult), [s1b, g1b], [g1b])
                    V(lambda e: e.tensor_scalar(u1[:, :], u1[:, :], -7.0, 1.0, op0=ALU.max, op1=ALU.add), [u1b], [u1b])
                    V(lambda e: e.tensor_tensor(actT[:, fc, :], g1[:, :], u1[:, :], op=ALU.mult), [g1b, u1b], [actb])
                else:
                    db = j
                    for cc in range(NCC):
                        ps, pb = c.psum.next()
                        for kc in range(KC):
                            PE(lambda e: e.matmul(ps[:, 0:256], actT[:, kc, cc * 128:(cc + 1) * 128], wt[:, kc, :],
                                                  start=(kc == 0), stop=(kc == KC - 1)), [actb, wtb], [pb])
                        A(lambda e: e.copy(ye[:, cc, db * 256:(db + 1) * 256], ps[:, 0:256]), [pb], [yeb])
                    if db == 7:
                        for n in range(NTH):
                            for b4 in range(4):
                                ps, pb = c.psum.next()
                                for cc in range(NCC):
                                    PE(lambda e: e.matmul(ps[:, :], selT[:, cc, n * 128:(n + 1) * 128],
                                                          ye[:, cc, b4 * 512:(b4 + 1) * 512],
                                                          start=(cc == 0), stop=(cc == NCC - 1)), [selTb, yeb], [pb])
                                V(lambda e: e.scalar_tensor_tensor(
                                    out=yacc[:, n, b4 * 512:(b4 + 1) * 512], in0=ps[:, :], scalar=gate[:, n, ex:ex + 1],
                                    in1=yacc[:, n, b4 * 512:(b4 + 1) * 512], op0=ALU.mult, op1=ALU.add),
                                  [pb, gateb, yab], [yab])
            t.dma("sp", c.d_ffn[0][t0:t0 + HT, :].rearrange("(n p) d -> p n d", p=128), yacc[:, :, :],
                  reads=[yab], writes=[c.d_ffn[1]])
        t.barrier()
```

```python
import math
from contextlib import ExitStack
import numpy as np
import concourse.bass as bass
import concourse.mybir as mybir
from concourse.bass_utils import run_bass_kernel_spmd

F32 = mybir.dt.float32
BF16 = mybir.dt.bfloat16
AF = mybir.ActivationFunctionType
ALU = mybir.AluOpType
AX = mybir.AxisListType

D = 2048
L = 2048
DEPTH = 2
IN_COLS = 15616
NT = L // 128
KC = D // 128
LN_EPS = 1e-5
ALPHA = (2 * DEPTH) ** 0.25
N_EXP = 32
DFF = 2048

C_HQ, C_HFF, C_HFB, C_HI, C_HOG, C_HY, C_AQ, C_AK, C_AV, C_G = (
    0, 1024, 2048, 3072, 4096, 5120, 8192, 9216, 9344, 9472)


class Buf:
    __slots__ = ("name", "w", "r", "excl")

    def __init__(self, name="", excl=False):
        self.name = name
        self.w = None
        self.r = {}
        self.excl = excl


class Trk:
    NDS = 24

    def __init__(self, nc, stack):
        self.nc = nc
        self.E = {"pe": nc.tensor, "dve": nc.vector, "act": nc.scalar, "pool": nc.gpsimd,
                  "sp": nc.sync}
        self.sem = {}
        self.cnt = {}
        for e in ("pe", "dve", "act", "pool"):
            self.sem[e] = stack.enter_context(nc.semaphore("s_" + e))
            self.cnt[e] = 0
        for i in range(self.NDS):
            self.sem[("d", i)] = stack.enter_context(nc.semaphore("d%d" % i))
            self.cnt[("d", i)] = 0
        self.seen = {e: {} for e in self.E}
        self.dnext = 0
        self.n_ins = 0
        self.n_wait = 0

    def _need(self, reads, writes, e=None):
        need = {}
        for b in reads:
            if b.w is not None:
                k, v = b.w
                if need.get(k, 0) < v:
                    need[k] = v
            if b.excl:
                for k, v in b.r.items():
                    if k != e and need.get(k, 0) < v:
                        need[k] = v
        for b in writes:
            if b.w is not None:
                k, v = b.w
                if need.get(k, 0) < v:
                    need[k] = v
            for k, v in b.r.items():
                if need.get(k, 0) < v:
                    need[k] = v
        return need

    def _wait(self, e, need):
        seen = self.seen[e]
        for k, v in need.items():
            if k == e and e == "pe":
                continue
            if seen.get(k, 0) >= v:
                continue
            self.E[e].wait_ge(self.sem[k], v)
            seen[k] = v
            self.n_wait += 1

    def _mark(self, ev, reads, writes):
        k, v = ev
        for b in reads:
            if b.r.get(k, 0) < v:
                b.r[k] = v
        for b in writes:
            b.w = ev
            b.r = {}

    def op(self, e, fn, reads=(), writes=()):
        self._wait(e, self._need(reads, writes, e))
        self.cnt[e] += 1
        ins = fn(self.E[e])
        ins.then_inc(self.sem[e], 1)
        self._mark((e, self.cnt[e]), reads, writes)
        self.n_ins += 1
        return ins

    def dma(self, q, out, in_, reads=(), writes=()):
        need = self._need(reads, writes)
        i = self.dnext
        self.dnext = (self.dnext + 1) % self.NDS
        k = ("d", i)
        if self.cnt[k] > 0:
            need[k] = max(need.get(k, 0), self.cnt[k])
        self._wait(q, need)
        ins = self.E[q].dma_start(out=out, in_=in_)
        self.cnt[k] += 16
        ins.then_inc(self.sem[k], 16)
        self._mark((k, self.cnt[k]), reads, writes)
        self.n_ins += 1
        return ins

    def barrier(self):
        need = {k: v for k, v in self.cnt.items() if v > 0}
        for e in self.E:
            self._wait(e, dict(need))

    def drain(self, e, bufs):
        self._wait(e, self._need((), bufs))


class Rot:
    def __init__(self, tiles, excl=False):
        self.tiles = tiles
        self.bufs = [Buf(excl=excl) for _ in tiles]
        self.i = 0

    def next(self):
        i = self.i
        self.i = (i + 1) % len(self.tiles)
        return self.tiles[i], self.bufs[i]


class Ctx:
    pass


_uc = [0]


def _u(name):
    _uc[0] += 1
    return "%s_%d" % (name, _uc[0])


LAST_INPUT_NAMES = set()
LAST_TRK = None


class WPool:
    def __init__(self, c, S, name, kc, ncols, n_stage=2, n_bf=2):
        self.c = c
        self.stage = Rot([S("%s_st%d" % (name, i), [128, kc, ncols], F32) for i in range(n_stage)])
        self.bf = Rot([S("%s_bf%d" % (name, i), [128, kc, ncols], BF16) for i in range(n_bf)])

    def load(self, w_ap, kc, ncols):
        t = self.c.t
        st, stb = self.stage.next()
        t.dma("sp", st[:, 0:kc, 0:ncols], w_ap.rearrange("(kc p) c -> p kc c", p=128), writes=[stb])
        bf, bfb = self.bf.next()
        t.op("pool", lambda e: e.tensor_copy(bf[:, 0:kc, 0:ncols], st[:, 0:kc, 0:ncols]), reads=[stb], writes=[bfb])
        return bf, bfb


def gemm_fm(c, w_ap, n_cols, xT, xT_buf, k_chunks, n_tok, epilogue, wp, blk=512):
    t = c.t
    blocks = [(c0, min(blk, n_cols - c0)) for c0 in range(0, n_cols, blk)]
    nxt = wp.load(w_ap[:, blocks[0][0]:blocks[0][0] + blocks[0][1]], k_chunks, blocks[0][1])
    for bi, (c0, cw) in enumerate(blocks):
        wt, wb = nxt
        if bi + 1 < len(blocks):
            n0, nw = blocks[bi + 1]
            nxt = wp.load(w_ap[:, n0:n0 + nw], k_chunks, nw)
        for cg in range(cw // 128):
            for tt in range(n_tok // 512):
                ps, pb = c.psum.next()
                for kc in range(k_chunks):
                    t.op("pe", lambda e, kc=kc: e.matmul(
                        ps[:, :], wt[:, kc, cg * 128:(cg + 1) * 128],
                        xT[:, kc, tt * 512:(tt + 1) * 512],
                        start=(kc == 0), stop=(kc == k_chunks - 1)),
                        reads=[wb, xT_buf], writes=[pb])
                epilogue(ps, pb, (c0 // 128) + cg, tt)


def gemm_tm(c, w_ap, n_cols, xT, xT_buf, k_chunks, n_tok, epilogue, wp, blk=512):
    t = c.t
    blocks = [(c0, min(blk, n_cols - c0)) for c0 in range(0, n_cols, blk)]
    nxt = wp.load(w_ap[:, blocks[0][0]:blocks[0][0] + blocks[0][1]], k_chunks, blocks[0][1])
    for bi, (c0, cw) in enumerate(blocks):
        wt, wb = nxt
        if bi + 1 < len(blocks):
            n0, nw = blocks[bi + 1]
            nxt = wp.load(w_ap[:, n0:n0 + nw], k_chunks, nw)
        for tt in range(n_tok // 128):
            ps, pb = c.psum.next()
            for kc in range(k_chunks):
                t.op("pe", lambda e, kc=kc: e.matmul(
                    ps[:, 0:cw], xT[:, kc, tt * 128:(tt + 1) * 128], wt[:, kc, 0:cw],
                    start=(kc == 0), stop=(kc == k_chunks - 1)),
                    reads=[wb, xT_buf], writes=[pb])
            epilogue(ps, pb, tt, c0, cw)


_evac_flip = [0]


def evac(c, out_ap, in_ap, reads, writes):
    _evac_flip[0] ^= 1
    if _evac_flip[0]:
        c.t.op("act", lambda e: e.copy(out_ap, in_ap), reads=reads, writes=writes)
    else:
        c.t.op("dve", lambda e: e.tensor_copy(out_ap, in_ap), reads=reads, writes=writes)


def phase_ln(c, src, res, g_ap, b_ap, h_out, xT_out, final_out=None, router=None):
    t, nc = c.t, c.nc
    V = lambda fn, r=(), w=(): t.op("dve", fn, reads=r, writes=w)
    A = lambda fn, r=(), w=(): t.op("act", fn, reads=r, writes=w)
    PE = lambda fn, r=(), w=(): t.op("pe", fn, reads=r, writes=w)
    src_ap, src_buf = src
    res_ap, res_buf = res if res is not None else (None, None)
    h_out_ap, h_out_buf = h_out if h_out is not None else (None, None)
    with ExitStack() as st:
        S = lambda name, shape, dt=F32: st.enter_context(nc.sbuf_tensor(_u(name), shape, dt))
        gt = S("ln_g", [128, D]); gb_ = Buf()
        bt = S("ln_b", [128, D]); bb_ = Buf()
        t.dma("sp", gt[:, :], g_ap.partition_broadcast(128), writes=[gb_])
        t.dma("sp", bt[:, :], b_ap.partition_broadcast(128), writes=[bb_])
        xin = Rot([S("ln_x%d" % i, [128, D]) for i in range(2)])
        rin = Rot([S("ln_r%d" % i, [128, D]) for i in range(2)])
        yo = Rot([S("ln_y%d" % i, [128, D]) for i in range(2)])
        xts = Rot([S("ln_xt%d" % i, [128, 4, 128]) for i in range(5)])
        xtb = Rot([S("ln_xb%d" % i, [128, 4, 128], BF16) for i in range(3)])
        stats = S("ln_st", [128, 4 * 6]); sb_ = Buf()
        mv = S("ln_mv", [128, 2]); mvb = Buf()
        rstd = S("ln_rstd", [128, 1]); rsb = Buf()
        if router is not None:
            rw = S("ln_rw", [128, KC, N_EXP]); rwb = Buf()
            t.dma("sp", rw[:, :, :], c.router_w[router].rearrange("(k p) e -> p k e", p=128), writes=[rwb])
            rbias = S("ln_rb", [128, N_EXP])
            t.dma("sp", rbias[:, :], c.router_b[router].partition_broadcast(128), writes=[rwb])
            lg = Rot([S("ln_lg%d" % i, [128, N_EXP]) for i in range(2)])
            ex_ = Rot([S("ln_ex%d" % i, [128, N_EXP]) for i in range(2)])
            m8 = Rot([S("ln_m8%d" % i, [128, 12]) for i in range(2)])
            gT_ = Rot([S("ln_gT%d" % i, [N_EXP, 128]) for i in range(2)])
            ybf = Rot([S("ln_ybf%d" % i, [128, D], BF16) for i in range(2)])
        for tt in range(NT):
            rows = slice(tt * 128, (tt + 1) * 128)
            x, xb = xin.next()
            t.dma("sp", x[:, :], src_ap[rows, :], reads=[src_buf], writes=[xb])
            if res_ap is not None:
                r, rb = rin.next()
                t.dma("sp", r[:, :], res_ap[rows, :], reads=[res_buf], writes=[rb])
                V(lambda e: e.scalar_tensor_tensor(out=x[:, :], in0=r[:, :], scalar=float(ALPHA), in1=x[:, :],
                                                   op0=ALU.mult, op1=ALU.add), [rb, xb], [xb])
            for j in range(4):
                V(lambda e: e.bn_stats(stats[:, j * 6:(j + 1) * 6], x[:, j * 512:(j + 1) * 512]), [xb], [sb_])
            V(lambda e: e.bn_aggr(mv[:, :], stats[:, :]), [sb_], [mvb])
            A(lambda e: e.activation(rstd[:, :], mv[:, 1:2], AF.Sqrt, bias=c.eps_ln[:, :], scale=1.0),
              [mvb, c.cbuf], [rsb])
            V(lambda e: e.reciprocal(rstd[:, :], rstd[:, :]), [rsb], [rsb])
            y, yb = yo.next()
            V(lambda e: e.tensor_scalar(y[:, :], x[:, :], mv[:, 0:1], rstd[:, 0:1], op0=ALU.subtract, op1=ALU.mult),
              [xb, mvb, rsb], [yb])
            t.op("pool", lambda e: e.tensor_tensor(y[:, :], y[:, :], gt[:, :], op=ALU.mult), reads=[yb, gb_], writes=[yb])
            t.op("pool", lambda e: e.tensor_tensor(y[:, :], y[:, :], bt[:, :], op=ALU.add), reads=[yb, bb_], writes=[yb])
            if h_out_ap is not None:
                t.dma("sp", h_out_ap[rows, :], y[:, :], reads=[yb], writes=[h_out_buf])
            if router is not None:
                yb16, yb16b = ybf.next()
                A(lambda e: e.copy(yb16[:, :], y[:, :]), [yb], [yb16b])
                t.dma("sp", c.d_xtm[0][rows, :], yb16[:, :], reads=[yb16b], writes=[c.d_xtm[1]])
            if final_out is not None:
                t.dma("sp", final_out[rows, :], y[:, :], reads=[yb], writes=[c.out_buf])
            if xT_out is not None:
                if router is not None:
                    pl, plb = c.psacc.next()
                for g4 in range(KC // 4):
                    ps, pb = c.psum.next()
                    for j in range(4):
                        fc = g4 * 4 + j
                        PE(lambda e: e.transpose(ps[:, j * 128:(j + 1) * 128], y[:, fc * 128:(fc + 1) * 128],
                                                 c.ident[:, :]), [yb, c.ident_buf], [pb])
                    xb_, xbb_ = xtb.next()
                    A(lambda e: e.copy(xb_[:, :, :], ps[:, :].rearrange("p (j q) -> p j q", j=4)), [pb], [xbb_])
                    t.dma("sp", xT_out[0][g4 * 512:(g4 + 1) * 512, rows].rearrange("(j p) q -> p j q", p=128),
                          xb_[:, :, :], reads=[xbb_], writes=[xT_out[1]])
                    if router is not None:
                        xs, xsb = xts.next()
                        V(lambda e: e.tensor_copy(xs[:, :, :], ps[:, :].rearrange("p (j q) -> p j q", j=4)), [pb], [xsb])
                    if router is not None:
                        for j in range(4):
                            fc = g4 * 4 + j
                            PE(lambda e: e.matmul(pl[:, 0:N_EXP], xs[:, j, :], rw[:, fc, :],
                                                  start=(fc == 0), stop=(fc == KC - 1)), [xsb, rwb], [plb])
                if router is not None:
                    l_, lb_ = lg.next()
                    V(lambda e: e.tensor_tensor(l_[:, :], pl[:, 0:N_EXP], rbias[:, :], op=ALU.add), [plb, rwb], [lb_])
                    m_, mb_ = m8.next()
                    V(lambda e: e.max(m_[:, 0:8], l_[:, :]), [lb_], [mb_])
                    V(lambda e: e.tensor_scalar(m_[:, 8:9], m_[:, 0:1], -1.0, None, op0=ALU.mult), [mb_], [mb_])
                    e_, eb_ = ex_.next()
                    A(lambda e: e.activation(e_[:, :], l_[:, :], AF.Exp, bias=m_[:, 8:9], scale=1.0), [lb_, mb_], [eb_])
                    V(lambda e: e.scalar_tensor_tensor(out=e_[:, :], in0=l_[:, :], scalar=m_[:, 3:4], in1=e_[:, :],
                                                       op0=ALU.is_ge, op1=ALU.mult), [lb_, mb_, eb_], [eb_])
                    V(lambda e: e.reduce_sum(m_[:, 9:10], e_[:, :], axis=AX.X), [eb_], [mb_])
                    V(lambda e: e.reciprocal(m_[:, 10:11], m_[:, 9:10]), [mb_], [mb_])
                    V(lambda e: e.tensor_scalar(e_[:, :], e_[:, :], m_[:, 10:11], None, op0=ALU.mult), [eb_, mb_], [eb_])
                    t.dma("sp", c.d_gate[0][rows, :], e_[:, :], reads=[eb_], writes=[c.d_gate[1]])
                    ps, pb = c.psum.next()
                    PE(lambda e: e.transpose(ps[0:N_EXP, 0:128], e_[:, :], c.ident[:, :]), [eb_, c.ident_buf], [pb])
                    g_, gb2 = gT_.next()
                    A(lambda e: e.copy(g_[:, :], ps[0:N_EXP, 0:128]), [pb], [gb2])
                    t.dma("sp", c.d_gateT[0][:, rows], g_[:, :], reads=[gb2], writes=[c.d_gateT[1]])
        t.barrier()


def phase_proj(c, layer, only=None):
    t, nc = c.t, c.nc
    w = c.w_in[layer]
    with ExitStack() as st:
        S = lambda name, shape, dt=F32: st.enter_context(nc.sbuf_tensor(_u(name), shape, dt))
        hT = S("hT", [128, KC, L], BF16); hT_buf = Buf("hT")
        t.dma("sp", hT[:, :, :], c.d_xT[0].rearrange("(k p) q -> p k q", p=128), reads=[c.d_xT[1]], writes=[hT_buf])
        wrot = WPool(c, S, "pj_w", KC, 512)
        stg = Rot([S("pj_s%d" % i, [128, L]) for i in range(2)])
        stg_tm = Rot([S("pj_t%d" % i, [128, 512]) for i in range(3)])
        fm = [(C_HQ, 1024, c.d_hqT), (C_HFF, 1024, c.d_hffT), (C_HFB, 1024, c.d_hfbT),
              (C_HOG, 1024, c.d_hogT), (C_HY, 3072, c.d_hyT), (C_AQ, 1024, c.d_aqT),
              (C_AK, 128, c.d_akT)]
        if only is not None:
            fm = fm[only[0]:only[1]]
        for c0, n, (dst, dbuf) in fm:
            cur = {}

            def epi(ps, pb, g, tt, dst=dst, dbuf=dbuf, cur=cur):
                if tt == 0:
                    cur["s"] = stg.next()
                s, sb = cur["s"]
                evac(c, s[:, tt * 512:(tt + 1) * 512], ps[:, :], [pb], [sb])
                if tt == L // 512 - 1:
                    t.dma("sp", dst[g * 128:(g + 1) * 128, :], s[:, :], reads=[sb], writes=[dbuf])

            gemm_fm(c, w[:, c0:c0 + n], n, hT, hT_buf, KC, L, epi, wrot)
        if only is None or len(only) > 4:
            cur = {}

            def epig(ps, pb, g, tt, cur=cur):
                if tt == 0:
                    cur["s"] = stg.next()
                s, sb = cur["s"]
                t.op("act", lambda e: e.activation(s[:, tt * 512:(tt + 1) * 512], ps[:, :], AF.Sigmoid),
                     reads=[pb], writes=[sb])
                if tt == L // 512 - 1:
                    t.dma("sp", c.d_gT[0][g * 128:(g + 1) * 128, :], s[:, :], reads=[sb],
                          writes=[c.d_gT[1]])

            gemm_fm(c, w[:, C_G:C_G + 3 * D], 3 * D, hT, hT_buf, KC, L, epig, wrot)
        tm = [(C_HFF, 2048, c.d_hf), (C_HI, 1024, c.d_hi), (C_AV, 128, c.d_av)]
        if only is not None:
            tm = tm[only[2]:only[3]]
        for c0, n, (dst, dbuf) in tm:
            def epi2(ps, pb, tt, cc0, cw, dst=dst, dbuf=dbuf):
                s, sb = stg_tm.next()
                evac(c, s[:, 0:cw], ps[:, 0:cw], [pb], [sb])
                t.dma("sp", dst[tt * 128:(tt + 1) * 128, cc0:cc0 + cw], s[:, 0:cw],
                      reads=[sb], writes=[dbuf])

            gemm_tm(c, w[:, c0:c0 + n], n, hT, hT_buf, KC, L, epi2, wrot)
        t.barrier()


def host_consts():
    cs = {}
    cs["ident"] = np.eye(128, dtype=np.float32)
    M1, M2, M3, M4, M5 = hg_masks()
    cs["hg_M1"], cs["hg_M2"], cs["hg_M3"], cs["hg_M4"], cs["hg_M5"] = M1, M2, M3, M4, M5
    cs.update(hy_consts())
    cs.update(att_consts())
    cs.update(moe_consts())
    return cs


def build(layer_list=(0, 1), stop=None, dbg=(), only=None):
    nc = bass.Bass("TRN2", target_bir_lowering=False)
    c = Ctx()
    c.nc = nc
    import os as _os
    c.moe_nexp = int(_os.environ.get("MOE_NEXP", N_EXP))
    c.moe_stop = _os.environ.get("MOE_STOP", "")
    c.moe_dense = _os.environ.get("MOE_DENSE", "0") == "1"
    c.moe_epi = int(_os.environ.get("MOE_EPI", 6))
    INPUT_NAMES = []

    def ein(name, shape):
        INPUT_NAMES.append(name)
        return nc.dram_tensor(name, list(shape), F32, kind="ExternalInput").ap()
    c.x = ein("x", [L, D])
    c.ln_in_g = ein("ln_in_g", [D]); c.ln_in_b = ein("ln_in_b", [D])
    c.w_in = ein("w_in", [DEPTH, D, IN_COLS])
    c.hg_lb = ein("hg_lower_bound", [DEPTH, 2048])
    c.hg_norm_g = ein("hg_norm_g", [DEPTH, 1024])
    c.hy_conv_w = ein("hy_conv_w", [DEPTH, 3, 3072]); c.hy_conv_b = ein("hy_conv_b", [DEPTH, 3072])
    c.hy_w1 = ein("hy_filt_w1", [DEPTH, 33, 64]); c.hy_b1 = ein("hy_filt_b1", [DEPTH, 64])
    c.hy_w2 = ein("hy_filt_w2", [DEPTH, 2, 64, 64]); c.hy_b2 = ein("hy_filt_b2", [DEPTH, 2, 64])
    c.hy_freq = ein("hy_filt_freq", [DEPTH, 64]); c.hy_w3 = ein("hy_filt_w3", [DEPTH, 64, 2048])
    c.hy_skip = ein("hy_skip", [DEPTH, 1024])
    c.att_sink = ein("att_sink", [DEPTH, 16]); c.rel_bias = ein("rel_bias", [32, 16])
    c.w_branch = ein("w_branch", [DEPTH, 3, 1024, D]); c.w_out = ein("w_out", [DEPTH, D, D])
    c.ln_mix_g = ein("ln_mix_g", [DEPTH, D]); c.ln_mix_b = ein("ln_mix_b", [DEPTH, D])
    c.router_w = ein("router_w", [DEPTH, D, N_EXP]); c.router_b = ein("router_b", [DEPTH, N_EXP])
    c.w_gate_up = [ein("w_gate_up%d" % l, [c.moe_nexp, 16, 128, KC * 256]) if l in layer_list else None for l in range(DEPTH)]
    c.w_down = [ein("w_down%d" % l, [c.moe_nexp, 8, 128, KC * 256]) if l in layer_list else None for l in range(DEPTH)]
    c.b_gate_up = ein("b_gate_up", [DEPTH, N_EXP, 2 * DFF]); c.b_down = ein("b_down", [DEPTH, N_EXP, D])
    c.ln_moe_g = ein("ln_moe_g", [DEPTH, D]); c.ln_moe_b = ein("ln_moe_b", [DEPTH, D])
    consts = host_consts()
    global LAST_INPUT_NAMES
    c.k = {k: ein("k_" + k, v.shape) for k, v in consts.items()}
    LAST_INPUT_NAMES = set(INPUT_NAMES)
    c.out = nc.dram_tensor("out", [L, D], F32, kind="ExternalOutput").ap()
    c.out_buf = Buf()

    def scratch(name, shape, dt=F32):
        kind = "ExternalOutput" if name in dbg else "Internal"
        return (nc.dram_tensor(name, list(shape), dt, kind=kind).ap(), Buf(name))

    c.d_h = scratch("d_h", [L, D]); c.d_h1 = scratch("d_h1", [L, D])
    c.d_gT = scratch("d_gT", [3 * D, L])
    c.d_hqT = scratch("d_hqT", [1024, L]); c.d_hffT = scratch("d_hffT", [1024, L])
    c.d_hfbT = scratch("d_hfbT", [1024, L]); c.d_hogT = scratch("d_hogT", [1024, L])
    c.d_hyT = scratch("d_hyT", [3072, L]); c.d_aqT = scratch("d_aqT", [1024, L])
    c.d_akT = scratch("d_akT", [128, L])
    c.d_hf = scratch("d_hf", [L, 2048]); c.d_hi = scratch("d_hi", [L, 1024])
    c.d_av = scratch("d_av", [L, 128])
    c.d_ohgT = scratch("d_ohgT", [1024, L], BF16)
    c.d_ohyT = scratch("d_ohyT", [1024, L], BF16)
    c.d_oatT = scratch("d_oatT", [1024, L], BF16)
    c.d_mix = scratch("d_mix", [L, D])
    c.d_xT = scratch("d_xT", [D, L], BF16); c.d_ffn = scratch("d_ffn", [L, D])
    c.d_gate = scratch("d_gate", [L, N_EXP]); c.d_gateT = scratch("d_gateT", [N_EXP, L])
    c.d_xtm = scratch("d_xtm", [L, D], BF16)
    c.d_yT = scratch("d_yT", [D, L], BF16)
    c.d_Kr = scratch("d_Kr", [L, 1024]); c.d_Ki = scratch("d_Ki", [L, 1024])
    c.d_Yr = scratch("d_Yr", [L, 1024]); c.d_Yi = scratch("d_Yi", [L, 1024])
    c.d_uT = scratch("d_uT", [1024, L]); c.d_x0c = scratch("d_x0c", [1024, L])

    with ExitStack() as st:
        global LAST_TRK
        c.t = t = LAST_TRK = Trk(nc, st)
        S = lambda name, shape, dt=F32: st.enter_context(nc.sbuf_tensor(_u(name), shape, dt))
        banks = [st.enter_context(nc.psum_tensor("ps%d" % i, [128, 512], F32)) for i in range(8)]
        c.psum = Rot(banks[0:6], excl=True)
        c.psacc = Rot(banks[6:8], excl=True)
        c.ident = S("ident", [128, 128]); c.ident_buf = Buf()
        t.dma("sp", c.ident[:, :], c.k["ident"][:, :], writes=[c.ident_buf])
        c.rowtmp = Rot([S("rowtmp%d" % i, [128, 128]) for i in range(2)])
        c.cbuf = Buf("consts")
        c.eps_ln = S("eps_ln", [128, 1])
        t.op("pool", lambda e: e.memset(c.eps_ln[:, :], float(LN_EPS)), writes=[c.cbuf])

        def finish():
            for k in list(t.cnt):
                if isinstance(k, tuple) and t.cnt[k] > 0:
                    t._wait("sp", {k: t.cnt[k]})
            for e in ("pe", "dve", "act", "pool"):
                if t.cnt[e] > 0:
                    t._wait("sp", {e: t.cnt[e]})

        pending = dict(src=(c.x, Buf("x")), res=None, g=c.ln_in_g, b=c.ln_in_b)
        for layer in layer_list:
            phase_ln(c, pending["src"], pending["res"], pending["g"], pending["b"], c.d_h, c.d_xT)
            if stop == "ln0":
                finish(); return nc
            phase_proj(c, layer, only)
            if stop == "proj":
                finish(); return nc
            if "nohgrn" not in dbg:
                phase_hgrn(c, layer)
            if stop == "hgrn":
                finish(); return nc
            if "nohyena" not in dbg:
                phase_hyena(c, layer)
            if stop == "hyena":
                finish(); return nc
            if "noattn" not in dbg:
                phase_attn(c, layer)
            if stop == "attn":
                finish(); return nc
            phase_merge(c, layer)
            if stop == "merge":
                finish(); return nc
            phase_ln(c, c.d_mix, c.d_h, c.ln_mix_g[layer], c.ln_mix_b[layer], c.d_h1, c.d_xT, router=layer)
            if stop == "ln1":
                finish(); return nc
            (phase_moe if c.moe_dense else phase_moe_sparse)(c, layer)
            if stop == "moe":
                finish(); return nc
            pending = dict(src=c.d_ffn, res=c.d_h1, g=c.ln_moe_g[layer], b=c.ln_moe_b[layer])
        phase_ln(c, pending["src"], pending["res"], pending["g"], pending["b"], None, None, final_out=c.out)
        finish()
    return nc


def load_cols(c, dst, dst_buf, vec_ap, n, col0=0):
    t = c.t
    rows, rb = c.rowtmp.next()
    t.dma("sp", rows[0:n, :], vec_ap.rearrange("(j p) -> j p", p=128), writes=[rb])
    ps, pb = c.psum.next()
    t.op("pe", lambda e: e.transpose(ps[:, 0:n], rows[0:n, :], c.ident[0:n, 0:n]),
         reads=[rb, c.ident_buf], writes=[pb])
    t.op("dve", lambda e: e.tensor_copy(dst[:, col0:col0 + n], ps[:, 0:n]), reads=[pb],
         writes=[dst_buf])


def hg_masks():
    s = np.arange(128)
    same = (s[:, None] // 32) == (s[None, :] // 32)
    M1 = (same & (s[:, None] <= s[None, :])).astype(np.float32)
    M3 = (same & (s[:, None] > s[None, :])).astype(np.float32)
    M5 = (s[:, None] // 32 == np.arange(4)[None, :]).astype(np.float32)
    return M1, M1.T.copy(), M3, M3.T.copy(), M5


def phase_hgrn(c, layer):
    t, nc = c.t, c.nc
    V = lambda fn, r=(), w=(): t.op("dve", fn, reads=r, writes=w)
    A = lambda fn, r=(), w=(): t.op("act", fn, reads=r, writes=w)
    G = lambda fn, r=(), w=(): t.op("pool", fn, reads=r, writes=w)
    PE = lambda fn, r=(), w=(): t.op("pe", fn, reads=r, writes=w)
    QS = 128.0 ** -0.5
    with ExitStack() as st:
        S = lambda name, shape, dt=F32: st.enter_context(nc.sbuf_tensor(_u(name), shape, dt))
        kb = Buf("hgk")
        Ms = []
        for nm in ("M1", "M2", "M3", "M4"):
            m = S("hg_" + nm, [128, 128])
            t.dma("sp", m[:, :], c.k["hg_" + nm][:, :], writes=[kb])
            Ms.append(m)
        M1, M2, M3, M4 = Ms
        M5 = S("hg_M5", [128, 4])
        t.dma("sp", M5[:, :], c.k["hg_M5"][:, :], writes=[kb])
        ones = S("hg_ones", [128, 128])
        G(lambda e: e.memset(ones[:, :], 1.0), w=[kb])
        eps = S("hg_eps", [128, 1])
        G(lambda e: e.memset(eps[:, :], 1e-6), w=[kb])
        lbT = S("hg_lbT", [128, 16]); omlT = S("hg_omlT", [128, 16]); nomlT = S("hg_nomlT", [128, 16])
        lbtm = S("hg_lbtm", [128, 2048]); omltm = S("hg_omltm", [128, 2048])
        gT = S("hg_gT", [128, 8])
        lbb = Buf("lb")
        load_cols(c, gT, lbb, c.hg_norm_g[layer], 8)
        if layer == 0:
            G(lambda e: e.memset(lbT[:, :], 0.0), w=[lbb])
            G(lambda e: e.memset(lbtm[:, :], 0.0), w=[lbb])
        else:
            x0T = S("hg_x0T", [128, 16])
            load_cols(c, x0T, lbb, c.hg_lb[0], 16)
            load_cols(c, lbT, lbb, c.hg_lb[1], 16)
            V(lambda e: e.tensor_tensor(lbT[:, :], lbT[:, :], x0T[:, :], op=ALU.subtract), [lbb], [lbb])
            A(lambda e: e.activation(lbT[:, :], lbT[:, :], AF.Sigmoid), [lbb], [lbb])
            t.dma("sp", omltm[:, :], c.hg_lb[0].partition_broadcast(128), writes=[lbb])
            t.dma("sp", lbtm[:, :], c.hg_lb[1].partition_broadcast(128), writes=[lbb])
            G(lambda e: e.tensor_tensor(lbtm[:, :], lbtm[:, :], omltm[:, :], op=ALU.subtract), [lbb], [lbb])
            A(lambda e: e.activation(lbtm[:, :], lbtm[:, :], AF.Sigmoid), [lbb], [lbb])
        V(lambda e: e.tensor_scalar(omlT[:, :], lbT[:, :], -1.0, 1.0, op0=ALU.mult, op1=ALU.add), [lbb], [lbb])
        V(lambda e: e.tensor_scalar(nomlT[:, :], omlT[:, :], -1.0, None, op0=ALU.mult), [lbb], [lbb])
        V(lambda e: e.tensor_scalar(omltm[:, :], lbtm[:, :], -1.0, 1.0, op0=ALU.mult, op1=ALU.add), [lbb], [lbb])

        ztm = [S("hg_ztm%d" % d, [128, NT, 128]) for d in range(2)]; ztm_b = [Buf() for _ in range(2)]
        lgf = [S("hg_lgf%d" % d, [128, NT, 128]) for d in range(2)]; lgf_b = [Buf() for _ in range(2)]
        vt = S("hg_v", [128, NT, 128]); vt_b = Buf()
        qs = S("hg_qs", [128, L]); qs_b = Buf()
        qh = [S("hg_qh%d" % d, [128, L]) for d in range(2)]; qh_b = [Buf() for _ in range(2)]
        kt = [S("hg_kt%d" % d, [128, L]) for d in range(2)]; kt_b = [Buf() for _ in range(2)]
        og = S("hg_og", [128, L]); og_b = Buf()
        Dd = [S("hg_D%d" % d, [128, 64]) for d in range(2)]; Dd_b = [Buf() for _ in range(2)]
        St = [S("hg_S%d" % d, [128, 64, 128]) for d in range(2)]; St_b = [Buf() for _ in range(2)]
        ex = Rot([S("hg_ex%d" % i, [128, 512]) for i in range(3)])
        scm = Rot([S("hg_scm%d" % i, [128, 128]) for i in range(4)])
        vmr = Rot([S("hg_vm%d" % i, [128, 4, 128]) for i in range(2)])
        sq = Rot([S("hg_sq%d" % i, [128, 512]) for i in range(2)])
        rs = Rot([S("hg_rs%d" % i, [128, 512]) for i in range(2)])
        ost = Rot([S("hg_o%d" % i, [128, 512]) for i in range(2)])
        ostb = Rot([S("hg_ob%d" % i, [128, 512], BF16) for i in range(2)])
        Mpre = [M1, M2]
        Mex = [M3, M4]

        for hd in range(8):
            hs = slice(hd * 128, (hd + 1) * 128)
            for d in range(2):
                t.dma("sp", ztm[d][:, :, :],
                      c.d_hf[0][:, d * 1024 + hd * 128:d * 1024 + (hd + 1) * 128].rearrange(
                          "(n p) k -> p n k", p=128), reads=[c.d_hf[1]], writes=[ztm_b[d]])
            t.dma("sp", vt[:, :, :], c.d_hi[0][:, hs].rearrange("(n p) k -> p n k", p=128),
                  reads=[c.d_hi[1]], writes=[vt_b])
            t.dma("sp", qs[:, :], c.d_hqT[0][hs, :], reads=[c.d_hqT[1]], writes=[qs_b])
            t.dma("sp", kt[0][:, :], c.d_hffT[0][hs, :], reads=[c.d_hffT[1]], writes=[kt_b[0]])
            t.dma("sp", kt[1][:, :], c.d_hfbT[0][hs, :], reads=[c.d_hfbT[1]], writes=[kt_b[1]])
            t.dma("sp", og[:, :], c.d_hogT[0][hs, :], reads=[c.d_hogT[1]], writes=[og_b])
            for d in range(2):
                z = ztm[d]; zb = ztm_b[d]; lg = lgf[d]; lb_ = lgf_b[d]
                col = d * 1024 + hd * 128
                oml_bc = omltm[:, col:col + 128].unsqueeze(1).to_broadcast([128, NT, 128])
                lb_bc = lbtm[:, col:col + 128].unsqueeze(1).to_broadcast([128, NT, 128])
                A(lambda e: e.activation(z[:, :, :], z[:, :, :], AF.Sigmoid), [zb], [zb])
                G(lambda e: e.tensor_tensor(lg[:, :, :], z[:, :, :], oml_bc, op=ALU.mult), [zb, lbb], [lb_])
                G(lambda e: e.tensor_tensor(lg[:, :, :], lg[:, :, :], lb_bc, op=ALU.add), [lb_, lbb], [lb_])
                V(lambda e: e.tensor_scalar(lg[:, :, :], lg[:, :, :], 1e-30, None, op0=ALU.max), [lb_], [lb_])
                V(lambda e: e.tensor_scalar(z[:, :, :], z[:, :, :], -1.0, 1.0, op0=ALU.mult, op1=ALU.add), [zb], [zb])
                G(lambda e: e.tensor_tensor(z[:, :, :], z[:, :, :], oml_bc, op=ALU.mult), [zb, lbb], [zb])
            for d in range(2):
                lg = lgf[d]; lb_ = lgf_b[d]
                A(lambda e: e.activation(lg[:, :, :], lg[:, :, :], AF.Ln), [lb_], [lb_])
            A(lambda e: e.activation(qs[:, :], qs[:, :], AF.Silu), [qs_b], [qs_b])
            A(lambda e: e.activation(og[:, :], og[:, :], AF.Silu), [og_b], [og_b])
            for d in range(2):
                j = d * 8 + hd
                A(lambda e: e.activation(kt[d][:, :], kt[d][:, :], AF.Sigmoid), [kt_b[d]], [kt_b[d]])
                V(lambda e: e.tensor_scalar(kt[d][:, :], kt[d][:, :], nomlT[:, j:j + 1], omlT[:, j:j + 1],
                                            op0=ALU.mult, op1=ALU.add), [kt_b[d], lbb], [kt_b[d]])
            for d in range(2):
                lg = lgf[d]; lb_ = lgf_b[d]; z = ztm[d]; zb = ztm_b[d]
                ps, pb = c.psum.next()
                for n in range(NT):
                    PE(lambda e, n=n: e.matmul(ps[:, n * 4:(n + 1) * 4], lg[:, n, :], M5[:, :],
                                               start=True, stop=True), [lb_, kb], [pb])
                A(lambda e: e.activation(Dd[d][:, :], ps[:, 0:64], AF.Exp), [pb], [Dd_b[d]])
                for g4 in range(4):
                    ps, pb = c.psum.next()
                    for jn in range(4):
                        n = g4 * 4 + jn
                        PE(lambda e, n=n, jn=jn: e.matmul(ps[:, jn * 128:(jn + 1) * 128], Mex[d][:, :],
                                                          lg[:, n, :], start=True, stop=True),
                           [lb_, kb], [pb])
                    x_, xb_ = ex.next()
                    A(lambda e: e.activation(x_[:, :], ps[:, :], AF.Exp), [pb], [xb_])
                    V(lambda e, g4=g4: e.tensor_tensor(
                        z[:, g4 * 4:(g4 + 1) * 4, :], z[:, g4 * 4:(g4 + 1) * 4, :],
                        x_[:, :].rearrange("p (a b) -> p a b", a=4), op=ALU.mult), [zb, xb_], [zb])
                for g4 in range(4):
                    ps, pb = c.psum.next()
                    for jn in range(4):
                        n = g4 * 4 + jn
                        PE(lambda e, n=n, jn=jn: e.matmul(ps[:, jn * 128:(jn + 1) * 128], lg[:, n, :],
                                                          Mpre[d][:, :], start=True, stop=True),
                           [lb_, kb], [pb])
                    cs = slice(g4 * 512, (g4 + 1) * 512)
                    x1, xb1 = ex.next()
                    A(lambda e: e.activation(x1[:, :], ps[:, :], AF.Exp), [pb], [xb1])
                    V(lambda e, cs=cs: e.scalar_tensor_tensor(
                        out=qh[d][:, cs], in0=x1[:, :], scalar=float(QS), in1=qs[:, cs],
                        op0=ALU.mult, op1=ALU.mult), [xb1, qs_b], [qh_b[d]])
                    x2, xb2 = ex.next()
                    A(lambda e: e.activation(x2[:, :], ps[:, :], AF.Exp, scale=-1.0), [pb], [xb2])
                    V(lambda e, cs=cs: e.tensor_tensor(kt[d][:, cs], kt[d][:, cs], x2[:, :], op=ALU.mult),
                      [xb2, kt_b[d]], [kt_b[d]])
            for d in range(2):
                z = ztm[d]; zb = ztm_b[d]
                tiles = range(NT) if d == 0 else range(NT - 1, -1, -1)
                first = True
                for n in tiles:
                    vm, vmb = vmr.next()
                    G(lambda e, n=n: e.tensor_tensor(
                        vm[:, :, :], vt[:, n:n + 1, :].to_broadcast([128, 4, 128]),
                        M5[:, :].unsqueeze(2).to_broadcast([128, 4, 128]), op=ALU.mult),
                      [vt_b, kb], [vmb])
                    ps, pb = c.psum.next()
                    PE(lambda e, n=n: e.matmul(ps[:, :], z[:, n, :],
                                               vm[:, :, :].rearrange("p a b -> p (a b)"),
                                               start=True, stop=True), [zb, vmb], [pb])
                    chunks = range(4) if d == 0 else range(3, -1, -1)
                    for jc in chunks:
                        j = n * 4 + jc
                        if first:
                            V(lambda e, j=j: e.memset(St[d][:, j, :], 0.0), [], [St_b[d]])
                            first = False
                        jn_ = j + 1 if d == 0 else j - 1
                        if jn_ < 0 or jn_ > 63:
                            continue
                        V(lambda e, j=j, jn_=jn_, jc=jc: e.scalar_tensor_tensor(
                            out=St[d][:, jn_, :], in0=St[d][:, j, :], scalar=Dd[d][:, j:j + 1],
                            in1=ps[:, jc * 128:(jc + 1) * 128], op0=ALU.mult, op1=ALU.add),
                          [St_b[d], Dd_b[d], pb], [St_b[d]])
            def scores(n):
                res = []
                for d in range(2):
                    ps, pb = c.psum.next()
                    ts = slice(n * 128, (n + 1) * 128)
                    PE(lambda e, d=d, ts=ts: e.matmul(ps[:, 0:128], kt[d][:, ts], qh[d][:, ts],
                                                      start=True, stop=True),
                       [kt_b[d], qh_b[d]], [pb])
                    sm, smb = scm.next()
                    V(lambda e, d=d: e.tensor_tensor(sm[:, :], ps[:, 0:128], Mpre[d][:, :], op=ALU.mult),
                      [pb, kb], [smb])
                    res.append((sm, smb))
                return res

            nxt = scores(0)
            for g4 in range(4):
                pso, pbo = c.psacc.next()
                for jn in range(4):
                    n = g4 * 4 + jn
                    cur = nxt
                    if n + 1 < NT:
                        nxt = scores(n + 1)
                    oc = slice(jn * 128, (jn + 1) * 128)
                    PE(lambda e, n=n, oc=oc: e.matmul(pso[:, oc], vt[:, n, :], cur[0][0][:, :],
                                                      start=True, stop=False), [vt_b, cur[0][1]], [pbo])
                    PE(lambda e, n=n, oc=oc: e.matmul(pso[:, oc], vt[:, n, :], cur[1][0][:, :],
                                                      start=False, stop=False), [vt_b, cur[1][1]], [pbo])
                    for d in range(2):
                        for jc in range(4):
                            j = n * 4 + jc
                            last = (d == 1 and jc == 3)
                            PE(lambda e, d=d, j=j, jc=jc, last=last, jn=jn: e.matmul(
                                pso[:, jn * 128 + jc * 32:jn * 128 + (jc + 1) * 32], St[d][:, j, :],
                                qh[d][:, j * 32:(j + 1) * 32], start=False, stop=last),
                               [St_b[d], qh_b[d]], [pbo])
                cs = slice(g4 * 512, (g4 + 1) * 512)
                s_, sb_ = sq.next()
                A(lambda e: e.activation(s_[:, :], pso[:, :], AF.Square), [pbo], [sb_])
                ps2, pb2 = c.psum.next()
                PE(lambda e: e.matmul(ps2[:, :], ones[:, :], s_[:, :], start=True, stop=True),
                   [sb_, kb], [pb2])
                r_, rb_ = rs.next()
                A(lambda e: e.activation(r_[:, :], ps2[:, :], AF.Sqrt, bias=eps[:, :], scale=1.0 / 128.0),
                  [pb2, kb], [rb_])
                V(lambda e: e.reciprocal(r_[:, :], r_[:, :]), [rb_], [rb_])
                o_, ob_ = ost.next()
                V(lambda e: e.tensor_tensor(o_[:, :], pso[:, :], r_[:, :], op=ALU.mult), [pbo, rb_], [ob_])
                o2_, ob2_ = ostb.next()
                V(lambda e, cs=cs: e.scalar_tensor_tensor(
                    out=o2_[:, :], in0=o_[:, :], scalar=gT[:, hd:hd + 1], in1=og[:, cs],
                    op0=ALU.mult, op1=ALU.mult), [ob_, lbb, og_b], [ob2_])
                t.dma("sp", c.d_ohgT[0][hs, cs], o2_[:, :], reads=[ob2_], writes=[c.d_ohgT[1]])
        t.barrier()


def hy_consts():
    N = 2 * L
    t = np.arange(L, dtype=np.float64)
    ang = 2.0 * np.pi * np.outer(t, t) / N
    A = np.cos(ang)
    B = -np.sin(ang)
    B[:, 0] = (-1.0) ** t
    tl = np.linspace(0.0, 1.0, L, dtype=np.float32)[:, None]
    w = (2.0 * np.float32(math.pi) * np.arange(L, dtype=np.float32)[:, None] / np.float32(L)).astype(np.float32)
    f = np.linspace(1e-4, 15, 16, dtype=np.float32)[None]
    z = np.concatenate([tl, np.cos(f * w), -np.sin(f * w)], axis=-1).astype(np.float32)
    max_decay = math.log(1e-2) / 0.3
    min_decay = math.log(1e-2) / 1.5
    deltas = np.linspace(min_decay, max_decay, 1024, dtype=np.float32)
    win = np.exp(-tl * np.abs(deltas)[None, :]).astype(np.float32)
    return dict(hy_A=A.astype(np.float32), hy_B=B.astype(np.float32),
                hy_BT=np.ascontiguousarray(B.T).astype(np.float32),
                hy_zT=np.ascontiguousarray(z.T), hy_win=win)


def phase_hyena(c, layer):
    t, nc = c.t, c.nc
    V = lambda fn, r=(), w=(): t.op("dve", fn, reads=r, writes=w)
    A = lambda fn, r=(), w=(): t.op("act", fn, reads=r, writes=w)
    G = lambda fn, r=(), w=(): t.op("pool", fn, reads=r, writes=w)
    PE = lambda fn, r=(), w=(): t.op("pe", fn, reads=r, writes=w)
    PI = math.pi
    kA, kB, kBT = c.k["hy_A"], c.k["hy_B"], c.k["hy_BT"]
    with ExitStack() as st0:
        S0 = lambda name, shape, dt=F32: st0.enter_context(nc.sbuf_tensor(_u(name), shape, dt))
        with ExitStack() as st:
            S = lambda name, shape, dt=F32: st.enter_context(nc.sbuf_tensor(_u(name), shape, dt))
            cb = Buf("hyc")
            P = S("hy_P", [128, NT, 1024]); Pb = Buf()
            Q = S("hy_Q", [128, NT, 1024]); Qb = Buf()
            st_in = ExitStack()
            S = lambda name, shape, dt=F32: st_in.enter_context(nc.sbuf_tensor(_u(name), shape, dt))
            zT = S("hy_zT", [33, L]); t.dma("sp", zT[:, :], c.k["hy_zT"][:, :], writes=[cb])
            w1 = S("hy_w1", [33, 64]); t.dma("sp", w1[:, :], c.hy_w1[layer], writes=[cb])
            w2 = S("hy_w2", [64, 2, 64])
            t.dma("sp", w2[:, :, :], c.hy_w2[layer].rearrange("j k m -> k j m"), writes=[cb])
            w3 = S("hy_w3", [64, 2048]); t.dma("sp", w3[:, :], c.hy_w3[layer], writes=[cb])
            fr = S("hy_fr", [64, 4])
            bb = S("hy_bb", [64, 3])
            t.dma("sp", fr[:, 0:1], c.hy_freq[layer].rearrange("(p o) -> p o", o=1), writes=[cb])
            t.dma("sp", bb[:, 0:1], c.hy_b1[layer].rearrange("(p o) -> p o", o=1), writes=[cb])
            for j in range(2):
                t.dma("sp", bb[:, 1 + j:2 + j], c.hy_b2[layer][j].rearrange("(p o) -> p o", o=1), writes=[cb])
            V(lambda e: e.tensor_scalar(fr[:, 1:4], bb[:, 0:3], fr[:, 0:1], None, op0=ALU.mult), [cb], [cb])
            frs = S("hy_frs", [64, 4])
            V(lambda e: e.tensor_scalar(frs[:, 0:1], fr[:, 0:1], 1.0 / (2.0 * PI), None, op0=ALU.mult), [cb], [cb])
            V(lambda e: e.tensor_scalar(frs[:, 1:4], fr[:, 1:4], 1.0 / (2.0 * PI), 8.5, op0=ALU.mult, op1=ALU.add), [cb], [cb])
            qi = S("hy_qi", [64, 512], mybir.dt.int32); qib = Buf()
            qf = S("hy_qf", [64, 512]); qfb = Buf()
            negpi = S("hy_negpi", [64, 1])
            G(lambda e: e.memset(negpi[:, :], -PI), w=[cb])
            hA = S("hy_hA", [64, L]); hAb = Buf()
            hB = S("hy_hB", [64, L]); hBb = Buf()

            def sin_layer(lhsT, src, srcb, dst, dstb, li):
                for tt in range(4):
                    cs = slice(tt * 512, (tt + 1) * 512)
                    ps, pb = c.psum.next()
                    kk = lhsT.shape[0]
                    PE(lambda e: e.matmul(ps[0:64, :], lhsT, src[0:kk, cs], start=True, stop=True),
                       [cb, srcb], [pb])
                    V(lambda e: e.tensor_scalar(dst[:, cs], ps[0:64, :], frs[:, 0:1], frs[:, li:li + 1],
                                                op0=ALU.mult, op1=ALU.add), [pb, cb], [dstb])
                    V(lambda e: e.tensor_copy(qi[:, :], dst[:, cs]), [dstb], [qib])
                    V(lambda e: e.tensor_copy(qf[:, :], qi[:, :]), [qib], [qfb])
                    V(lambda e: e.tensor_tensor(dst[:, cs], dst[:, cs], qf[:, :], op=ALU.subtract), [dstb, qfb], [dstb])
                    V(lambda e: e.scalar_tensor_tensor(out=dst[:, cs], in0=dst[:, cs], scalar=0.0, in1=dst[:, cs],
                                                       op0=ALU.is_lt, op1=ALU.add), [dstb], [dstb])
                    A(lambda e: e.activation(dst[:, cs], dst[:, cs], AF.Sin, bias=negpi[:, :], scale=2.0 * PI),
                      [dstb, cb], [dstb])

            sin_layer(w1[:, :], zT, cb, hA, hAb, 1)
            sin_layer(w2[:, 0, :], hA, hAb, hB, hBb, 2)
            sin_layer(w2[:, 1, :], hB, hBb, hA, hAb, 3)
            winr = Rot([S("hy_win%d" % i, [128, 1024]) for i in range(2)])
            for n in range(NT):
                wn, wnb = winr.next()
                t.dma("sp", wn[:, :], c.k["hy_win"][n * 128:(n + 1) * 128, :], writes=[wnb])
                for q4 in range(4):
                    ps, pb = c.psum.next()
                    PE(lambda e: e.matmul(ps[:, :], hA[:, n * 128:(n + 1) * 128],
                                          w3[:, q4 * 512:(q4 + 1) * 512], start=True, stop=True),
                       [hAb, cb], [pb])
                    dst, dstb = (P, Pb) if q4 < 2 else (Q, Qb)
                    cc = slice((q4 % 2) * 512, (q4 % 2 + 1) * 512)
                    V(lambda e: e.tensor_tensor(dst[:, n, cc], ps[:, :], wn[:, cc], op=ALU.mult),
                      [pb, wnb], [dstb])
            G(lambda e: e.memset(Q[0:1, 0, :], 0.0), [], [Qb])
            t.barrier()
            st_in.close()
            S = lambda name, shape, dt=F32: st.enter_context(nc.sbuf_tensor(_u(name), shape, dt))
            tabA = Rot([S("hy_tA%d" % i, [128, 16, 128]) for i in range(2)])
            tabB = Rot([S("hy_tB%d" % i, [128, 16, 128]) for i in range(2)])
            stg = Rot([S("hy_stg%d" % i, [128, 512]) for i in range(4)])
            for n in range(NT):
                eng = V if n % 2 == 0 else G
                eng(lambda e: e.tensor_tensor(P[:, n, :], P[:, n, :], Q[:, n, :], op=ALU.subtract), [Pb, Qb], [Pb])
                V(lambda e: e.scalar_tensor_tensor(out=Q[:, n, :], in0=Q[:, n, :], scalar=2.0, in1=P[:, n, :],
                                                     op0=ALU.mult, op1=ALU.add), [Pb, Qb], [Qb])
            for fc in range(16):
                ta, tab_ = tabA.next(); tb, tbb_ = tabB.next()
                t.dma("sp", ta[:, :, :], kA[:, fc * 128:(fc + 1) * 128].rearrange("(k p) f -> p k f", p=128), writes=[tab_])
                t.dma("sp", tb[:, :, :], kB[:, fc * 128:(fc + 1) * 128].rearrange("(k p) f -> p k f", p=128), writes=[tbb_])
                for hh in range(2):
                    cc = slice(hh * 512, (hh + 1) * 512)
                    ps, pb = c.psum.next()
                    for k in range(16):
                        PE(lambda e: e.matmul(ps[:, :], ta[:, k, :], Q[:, k, cc], start=(k == 0), stop=(k == 15)),
                           [tab_, Qb], [pb])
                    s1, s1b = stg.next()
                    A(lambda e: e.copy(s1[:, :], ps[:, :]), [pb], [s1b])
                    t.dma("sp", c.d_Kr[0][fc * 128:(fc + 1) * 128, cc], s1[:, :], reads=[s1b], writes=[c.d_Kr[1]])
                    ps, pb = c.psum.next()
                    for k in range(16):
                        PE(lambda e: e.matmul(ps[:, :], tb[:, k, :], P[:, k, cc], start=(k == 0), stop=(k == 15)),
                           [tbb_, Pb], [pb])
                    s2, s2b = stg.next()
                    V(lambda e: e.tensor_copy(s2[:, :], ps[:, :]), [pb], [s2b])
                    if fc == 0:
                        ps3, pb3 = c.psum.next()
                        for k in range(16):
                            PE(lambda e: e.matmul(ps3[:, :], tb[:, k, :], Q[:, k, cc], start=(k == 0), stop=(k == 15)),
                               [tbb_, Qb], [pb3])
                        V(lambda e: e.tensor_copy(s2[0:1, :], ps3[0:1, :]), [pb3, s2b], [s2b])
                    t.dma("sp", c.d_Ki[0][fc * 128:(fc + 1) * 128, cc], s2[:, :], reads=[s2b], writes=[c.d_Ki[1]])
            t.barrier()
        with ExitStack() as st:
            S = lambda name, shape, dt=F32: st.enter_context(nc.sbuf_tensor(_u(name), shape, dt))
            cb = Buf("hyc2")
            cw = S("hy_cw", [128, 3, 24])
            for k in range(3):
                load_cols(c, cw[:, k, :], cb, c.hy_conv_w[layer][k], 24)
            cbias = S("hy_cb", [128, 24]); load_cols(c, cbias, cb, c.hy_conv_b[layer], 24)
            skip = S("hy_skip", [128, 8]); load_cols(c, skip, cb, c.hy_skip[layer], 8)
            utm = S("hy_utm", [128, NT, 1024]); utmb = Buf()
            tabA = Rot([S("hy_tA%d" % i, [128, 16, 128]) for i in range(2)])
            tabB = Rot([S("hy_tB%d" % i, [128, 16, 128]) for i in range(2)])
            stg = Rot([S("hy_stg%d" % i, [128, 512]) for i in range(4)])
            raw = Rot([S("hy_raw%d" % i, [128, L]) for i in range(2)])
            cv = Rot([S("hy_cv%d" % i, [128, L]) for i in range(3)])

            def conv(chunk):
                r, rb = raw.next()
                t.dma("sp", r[:, :], c.d_hyT[0][chunk * 128:(chunk + 1) * 128, :], reads=[c.d_hyT[1]], writes=[rb])
                y, yb = cv.next()
                V(lambda e: e.tensor_scalar(y[:, :], r[:, :], cw[:, 1, chunk:chunk + 1], cbias[:, chunk:chunk + 1],
                                            op0=ALU.mult, op1=ALU.add), [rb, cb], [yb])
                V(lambda e: e.scalar_tensor_tensor(out=y[:, 1:L], in0=r[:, 0:L - 1], scalar=cw[:, 0, chunk:chunk + 1],
                                                   in1=y[:, 1:L], op0=ALU.mult, op1=ALU.add), [rb, cb, yb], [yb])
                V(lambda e: e.scalar_tensor_tensor(out=y[:, 0:L - 1], in0=r[:, 1:L], scalar=cw[:, 2, chunk:chunk + 1],
                                                   in1=y[:, 0:L - 1], op0=ALU.mult, op1=ALU.add), [rb, cb, yb], [yb])
                return y, yb

            for ch in range(8):
                x0, x0b = conv(ch)
                t.dma("sp", c.d_x0c[0][ch * 128:(ch + 1) * 128, :], x0[:, :], reads=[x0b], writes=[c.d_x0c[1]])
                x1, x1b = conv(8 + ch)
                vv, vvb = conv(16 + ch)
                G(lambda e: e.tensor_tensor(x1[:, :], x1[:, :], vv[:, :], op=ALU.mult), [x1b, vvb], [x1b])
                t.dma("sp", c.d_uT[0][ch * 128:(ch + 1) * 128, :], x1[:, :], reads=[x1b], writes=[c.d_uT[1]])
                for g4 in range(4):
                    ps, pb = c.psum.next()
                    for jn in range(4):
                        n = g4 * 4 + jn
                        PE(lambda e: e.transpose(ps[:, jn * 128:(jn + 1) * 128], x1[:, n * 128:(n + 1) * 128],
                                                 c.ident[:, :]), [x1b, c.ident_buf], [pb])
                    evac(c, utm[:, g4 * 4:(g4 + 1) * 4, ch * 128:(ch + 1) * 128],
                         ps[:, :].rearrange("p (a b) -> p a b", a=4), [pb], [utmb])
            kr = Rot([S("hy_kr%d" % i, [128, 1024]) for i in range(2)])
            ki = Rot([S("hy_ki%d" % i, [128, 1024]) for i in range(2)])
            tmp = Rot([S("hy_tmp%d" % i, [128, 512]) for i in range(4)])
            for fc in range(16):
                ta, tab_ = tabA.next(); tb, tbb_ = tabB.next()
                t.dma("sp", ta[:, :, :], kA[:, fc * 128:(fc + 1) * 128].rearrange("(k p) f -> p k f", p=128), writes=[tab_])
                t.dma("sp", tb[:, :, :], kB[:, fc * 128:(fc + 1) * 128].rearrange("(k p) f -> p k f", p=128), writes=[tbb_])
                krt, krb = kr.next(); kit, kib = ki.next()
                t.dma("sp", krt[:, :], c.d_Kr[0][fc * 128:(fc + 1) * 128, :], reads=[c.d_Kr[1]], writes=[krb])
                t.dma("sp", kit[:, :], c.d_Ki[0][fc * 128:(fc + 1) * 128, :], reads=[c.d_Ki[1]], writes=[kib])
                for hh in range(2):
                    cc = slice(hh * 512, (hh + 1) * 512)
                    psr, pbr = c.psum.next()
                    for k in range(16):
                        PE(lambda e: e.matmul(psr[:, :], ta[:, k, :], utm[:, k, cc], start=(k == 0), stop=(k == 15)),
                           [tab_, utmb], [pbr])
                    psi, pbi = c.psum.next()
                    for k in range(16):
                        PE(lambda e: e.matmul(psi[:, :], tb[:, k, :], utm[:, k, cc], start=(k == 0), stop=(k == 15)),
                           [tbb_, utmb], [pbi])
                    ur, urb = tmp.next(); ui, uib = tmp.next()
                    A(lambda e: e.copy(ur[:, :], psr[:, :]), [pbr], [urb])
                    A(lambda e: e.copy(ui[:, :], psi[:, :]), [pbi], [uib])
                    yr, yrb = stg.next(); yi, yib = stg.next()
                    t1, t1b = tmp.next(); t2, t2b = tmp.next()
                    V(lambda e: e.tensor_tensor(yr[:, :], ur[:, :], krt[:, cc], op=ALU.mult), [urb, krb], [yrb])
                    G(lambda e: e.tensor_tensor(t1[:, :], ui[:, :], kit[:, cc], op=ALU.mult), [uib, kib], [t1b])
                    V(lambda e: e.tensor_tensor(yi[:, :], ur[:, :], kit[:, cc], op=ALU.mult), [urb, kib], [yib])
                    G(lambda e: e.tensor_tensor(t2[:, :], ui[:, :], krt[:, cc], op=ALU.mult), [uib, krb], [t2b])
                    V(lambda e: e.tensor_tensor(yr[:, :], yr[:, :], t1[:, :], op=ALU.subtract), [yrb, t1b], [yrb])
                    V(lambda e: e.tensor_tensor(yi[:, :], yi[:, :], t2[:, :], op=ALU.add), [yib, t2b], [yib])
                    if fc == 0:
                        V(lambda e: e.scalar_tensor_tensor(out=yr[0:1, :], in0=ur[0:1, :], scalar=0.5, in1=krt[0:1, cc],
                                                           op0=ALU.mult, op1=ALU.mult), [urb, krb, yrb], [yrb])
                        V(lambda e: e.scalar_tensor_tensor(out=yi[0:1, :], in0=ui[0:1, :], scalar=0.5, in1=kit[0:1, cc],
                                                           op0=ALU.mult, op1=ALU.mult), [uib, kib, yib], [yib])
                    t.dma("sp", c.d_Yr[0][fc * 128:(fc + 1) * 128, cc], yr[:, :], reads=[yrb], writes=[c.d_Yr[1]])
                    t.dma("sp", c.d_Yi[0][fc * 128:(fc + 1) * 128, cc], yi[:, :], reads=[yib], writes=[c.d_Yi[1]])
            t.barrier()
        with ExitStack() as st:
            S = lambda name, shape, dt=F32: st.enter_context(nc.sbuf_tensor(_u(name), shape, dt))
            cb = Buf("hyc3")
            skip = S("hy_skip2", [128, 8]); load_cols(c, skip, cb, c.hy_skip[layer], 8)
            itA = Rot([S("hy_itA%d" % i, [128, 16, 512]) for i in range(2)])
            itB = Rot([S("hy_itB%d" % i, [128, 16, 512]) for i in range(2)])
            yrr = Rot([S("hy_yr%d" % i, [128, 16, 128]) for i in range(2)])
            yir = Rot([S("hy_yi%d" % i, [128, 16, 128]) for i in range(2)])
            usr = Rot([S("hy_us%d" % i, [128, L]) for i in range(2)])
            x0r = Rot([S("hy_x0%d" % i, [128, L]) for i in range(2)])
            outr = Rot([S("hy_out%d" % i, [128, 512]) for i in range(3)])
            outb = Rot([S("hy_outb%d" % i, [128, 512], BF16) for i in range(3)])
            for ch in range(8):
                rows = slice(ch * 128, (ch + 1) * 128)
                yrt, yrb = yrr.next(); yit, yib = yir.next()
                t.dma("sp", yrt[:, :, :], c.d_Yr[0][:, rows].rearrange("(k p) m -> p k m", p=128), reads=[c.d_Yr[1]], writes=[yrb])
                t.dma("sp", yit[:, :, :], c.d_Yi[0][:, rows].rearrange("(k p) m -> p k m", p=128), reads=[c.d_Yi[1]], writes=[yib])
                us, usb = usr.next(); x0, x0b = x0r.next()
                t.dma("sp", us[:, :], c.d_uT[0][rows, :], reads=[c.d_uT[1]], writes=[usb])
                t.dma("sp", x0[:, :], c.d_x0c[0][rows, :], reads=[c.d_x0c[1]], writes=[x0b])
                G(lambda e: e.tensor_scalar(us[:, :], us[:, :], skip[:, ch:ch + 1], None, op0=ALU.mult), [usb, cb], [usb])
                for tt in range(4):
                    cs = slice(tt * 512, (tt + 1) * 512)
                    ia, iab = itA.next(); ib, ibb = itB.next()
                    t.dma("sp", ia[:, :, :], kA[:, cs].rearrange("(k p) q -> p k q", p=128), writes=[iab])
                    t.dma("sp", ib[:, :, :], kBT[:, cs].rearrange("(k p) q -> p k q", p=128), writes=[ibb])
                    ps, pb = c.psum.next()
                    for k in range(16):
                        PE(lambda e: e.matmul(ps[:, :], yrt[:, k, :], ia[:, k, :], start=(k == 0), stop=False),
                           [yrb, iab], [pb])
                    for k in range(16):
                        PE(lambda e: e.matmul(ps[:, :], yit[:, k, :], ib[:, k, :], start=False, stop=(k == 15)),
                           [yib, ibb], [pb])
                    o_, ob_ = outr.next()
                    V(lambda e: e.scalar_tensor_tensor(out=o_[:, :], in0=ps[:, :], scalar=2.0 / (2 * L), in1=us[:, cs],
                                                       op0=ALU.mult, op1=ALU.add), [pb, usb], [ob_])
                    o2_, ob2_ = outb.next()
                    G(lambda e: e.tensor_tensor(o2_[:, :], o_[:, :], x0[:, cs], op=ALU.mult), [ob_, x0b], [ob2_])
                    t.dma("sp", c.d_ohyT[0][rows, cs], o2_[:, :], reads=[ob2_], writes=[c.d_ohyT[1]])
            t.barrier()


def att_consts():
    W = 128
    kofs = np.arange(3 * W)[None, :] - W
    rel = kofs - np.arange(W)[:, None]
    half, max_exact = 16, 8
    bucket = (rel > 0).astype(np.int32) * half
    n = np.abs(rel)
    n_safe = np.maximum(n, 1).astype(np.float32)
    large = max_exact + (np.log(n_safe / np.float32(max_exact)) / np.float32(math.log(128 / max_exact))
                         * np.float32(half - max_exact)).astype(np.int32)
    large = np.clip(large, 0, half - 1)
    bucket = bucket + np.where(n < max_exact, n, large)
    onehot = (bucket[None, :, :] == np.arange(32)[:, None, None]).astype(np.float32)
    maskadd = np.where(np.abs(rel) <= 128, 0.0, -1e30).astype(np.float32)
    return dict(at_onehot=onehot, at_mask=maskadd)


def phase_attn(c, layer):
    t, nc = c.t, c.nc
    V = lambda fn, r=(), w=(): t.op("dve", fn, reads=r, writes=w)
    A = lambda fn, r=(), w=(): t.op("act", fn, reads=r, writes=w)
    G = lambda fn, r=(), w=(): t.op("pool", fn, reads=r, writes=w)
    PE = lambda fn, r=(), w=(): t.op("pe", fn, reads=r, writes=w)
    with ExitStack() as st:
        S = lambda name, shape, dt=F32: st.enter_context(nc.sbuf_tensor(_u(name), shape, dt))
        cb = Buf("atc")
        biasM = S("at_bias", [128, 16, 384])
        rbb = S("at_rbb", [128, 512])
        t.dma("sp", rbb[:, :], c.rel_bias.rearrange("b h -> (b h)").partition_broadcast(128), writes=[cb])
        sinkb = S("at_sink", [128, 16])
        t.dma("sp", sinkb[:, :], c.att_sink[layer].partition_broadcast(128), writes=[cb])
        mk = S("at_mk", [128, 384])
        t.dma("sp", mk[:, :], c.k["at_mask"][:, :], writes=[cb])
        V(lambda e: e.tensor_copy(biasM[:, :, :], mk[:, :].unsqueeze(1).to_broadcast([128, 16, 384])), [cb], [cb])
        ohr = Rot([S("at_oh%d" % i, [128, 384]) for i in range(2)])
        for b in range(32):
            oh, ohb = ohr.next()
            t.dma("sp", oh[:, :], c.k["at_onehot"][b], writes=[ohb])
            for h in range(16):
                V(lambda e: e.scalar_tensor_tensor(out=biasM[:, h, :], in0=oh[:, :], scalar=rbb[:, b * 16 + h:b * 16 + h + 1],
                                                   in1=biasM[:, h, :], op0=ALU.mult, op1=ALU.add), [ohb, cb], [cb])
        kd = []
        for g in range(2):
            k_ = S("at_kd%d" % g, [128, L])
            for half in range(2):
                t.dma("sp", k_[half * 64:(half + 1) * 64, :], c.d_akT[0][g * 64:(g + 1) * 64, :],
                      reads=[c.d_akT[1]], writes=[cb])
            kd.append(k_)
        vx = {}
        for g in range(2):
            for hh in range(2):
                v_ = S("at_v%d%d" % (g, hh), [128, NT, 128])
                G(lambda e: e.memset(v_[:, :, :], 0.0), [], [cb])
                t.dma("sp", v_[:, :, hh * 64:(hh + 1) * 64],
                      c.d_av[0][:, g * 64:(g + 1) * 64].rearrange("(n p) d -> p n d", p=128),
                      reads=[c.d_av[1]], writes=[cb])
                vx[(g, hh)] = v_
        qr = Rot([S("at_q%d" % i, [128, L]) for i in range(2)])
        otr = Rot([S("at_o%d" % i, [128, L], BF16) for i in range(2)])
        sr = Rot([S("at_s%d" % i, [128, 384]) for i in range(3)])
        pr = Rot([S("at_p%d" % i, [128, 384]) for i in range(3)])
        ptr = Rot([S("at_pt%d" % i, [128, 384]) for i in range(3)])
        smr = Rot([S("at_sm%d" % i, [128, 8]) for i in range(4)])
        for j in range(8):
            q, qb = qr.next()
            t.dma("sp", q[:, :], c.d_aqT[0][j * 128:(j + 1) * 128, :], reads=[c.d_aqT[1]], writes=[qb])
            ot, otb = otr.next()
            for n in range(NT):
                klo = max(0, (n - 1) * 128); khi = min(L, (n + 2) * 128)
                bo = klo - (n - 1) * 128; wdt = khi - klo
                nkb = wdt // 128
                oacc, oab = c.psacc.next()
                for hh in range(2):
                    h = 2 * j + hh; g = h // 8; po = hh * 64
                    ps, pb = c.psum.next()
                    PE(lambda e: e.matmul(ps[:, 0:wdt], q[po:po + 64, n * 128:(n + 1) * 128],
                                          kd[g][po:po + 64, klo:khi], start=True, stop=True), [qb, cb], [pb])
                    s_, sb_ = sr.next()
                    V(lambda e: e.scalar_tensor_tensor(out=s_[:, 0:wdt], in0=ps[:, 0:wdt], scalar=0.125,
                                                       in1=biasM[:, h, bo:bo + wdt], op0=ALU.mult, op1=ALU.add),
                      [pb, cb], [sb_])
                    sm, smb = smr.next()
                    V(lambda e: e.reduce_max(sm[:, 0:1], s_[:, 0:wdt], axis=AX.X), [sb_], [smb])
                    V(lambda e: e.tensor_tensor(sm[:, 0:1], sm[:, 0:1], sinkb[:, h:h + 1], op=ALU.max), [smb, cb], [smb])
                    V(lambda e: e.tensor_scalar(sm[:, 1:2], sm[:, 0:1], -1.0, None, op0=ALU.mult), [smb], [smb])
                    p_, pb_ = pr.next()
                    A(lambda e: e.activation(p_[:, 0:wdt], s_[:, 0:wdt], AF.Exp, bias=sm[:, 1:2], scale=1.0,
                                             accum_out=sm[:, 2:3]), [sb_, smb], [pb_, smb])
                    A(lambda e: e.activation(sm[:, 3:4], sinkb[:, h:h + 1], AF.Exp, bias=sm[:, 1:2], scale=1.0),
                      [smb, cb], [smb])
                    V(lambda e: e.tensor_tensor(sm[:, 4:5], sm[:, 2:3], sm[:, 3:4], op=ALU.add), [smb], [smb])
                    V(lambda e: e.reciprocal(sm[:, 5:6], sm[:, 4:5]), [smb], [smb])
                    G(lambda e: e.tensor_scalar(p_[:, 0:wdt], p_[:, 0:wdt], sm[:, 5:6], None, op0=ALU.mult),
                      [pb_, smb], [pb_])
                    ps2, pb2 = c.psum.next()
                    for kb in range(nkb):
                        PE(lambda e: e.transpose(ps2[:, kb * 128:(kb + 1) * 128], p_[:, kb * 128:(kb + 1) * 128],
                                                 c.ident[:, :]), [pb_, c.ident_buf], [pb2])
                    pt, ptb = ptr.next()
                    A(lambda e: e.copy(pt[:, 0:wdt], ps2[:, 0:wdt]), [pb2], [ptb])
                    for kb in range(nkb):
                        kt_ = klo // 128 + kb
                        PE(lambda e: e.matmul(oacc[:, 0:128], vx[(g, hh)][:, kt_, :], pt[:, kb * 128:(kb + 1) * 128],
                                              start=(hh == 0 and kb == 0), stop=(hh == 1 and kb == nkb - 1)),
                           [cb, ptb], [oab])
                evac(c, ot[:, n * 128:(n + 1) * 128], oacc[:, 0:128], [oab], [otb])
            t.dma("sp", c.d_oatT[0][j * 128:(j + 1) * 128, :], ot[:, :], reads=[otb], writes=[c.d_oatT[1]])
        t.barrier()


def phase_merge(c, layer):
    t, nc = c.t, c.nc
    V = lambda fn, r=(), w=(): t.op("dve", fn, reads=r, writes=w)
    G = lambda fn, r=(), w=(): t.op("pool", fn, reads=r, writes=w)
    PE = lambda fn, r=(), w=(): t.op("pe", fn, reads=r, writes=w)
    with ExitStack() as st:
        S = lambda name, shape, dt=F32: st.enter_context(nc.sbuf_tensor(_u(name), shape, dt))
        otr = Rot([S("mg_o%d" % i, [128, 8, L], BF16) for i in range(2)])
        acc = S("mg_acc", [128, 4, L]); accb = Buf()
        accbf = Rot([S("mg_accb%d" % i, [128, L], BF16) for i in range(2)])
        gr = Rot([S("mg_g%d" % i, [128, L]) for i in range(2)])
        wrot = WPool(c, S, "mg_w", 8, 512)
        tmpr = Rot([S("mg_t%d" % i, [128, 512]) for i in range(3)])
        srcs = (c.d_ohgT, c.d_ohyT, c.d_oatT)
        for blk in range(4):
            for n in range(3):
                wt, wb = wrot.load(c.w_branch[layer][n][:, blk * 512:(blk + 1) * 512], 8, 512)
                o_, ob_ = otr.next()
                t.dma("sp", o_[:, :, :], srcs[n][0].rearrange("(k p) q -> p k q", p=128),
                      reads=[srcs[n][1]], writes=[ob_])
                for cg in range(4):
                    dg = blk * 4 + cg
                    g_, gb_ = gr.next()
                    t.dma("sp", g_[:, :], c.d_gT[0][n * D + dg * 128:n * D + (dg + 1) * 128, :],
                          reads=[c.d_gT[1]], writes=[gb_])
                    for tt in range(4):
                        cs = slice(tt * 512, (tt + 1) * 512)
                        ps, pb = c.psum.next()
                        for kc in range(8):
                            PE(lambda e: e.matmul(ps[:, :], wt[:, kc, cg * 128:(cg + 1) * 128], o_[:, kc, cs],
                                                  start=(kc == 0), stop=(kc == 7)), [wb, ob_], [pb])
                        if n == 0:
                            V(lambda e: e.tensor_tensor(acc[:, cg, cs], ps[:, :], g_[:, cs], op=ALU.mult),
                              [pb, gb_], [accb])
                        else:
                            tm_, tmb_ = tmpr.next()
                            V(lambda e: e.tensor_tensor(tm_[:, :], ps[:, :], g_[:, cs], op=ALU.mult),
                              [pb, gb_], [tmb_])
                            G(lambda e: e.tensor_tensor(acc[:, cg, cs], acc[:, cg, cs], tm_[:, :], op=ALU.add),
                              [accb, tmb_], [accb])
                    if n == 2:
                        ab_, abb_ = accbf.next()
                        t.op("act", lambda e: e.copy(ab_[:, :], acc[:, cg, :]), reads=[accb], writes=[abb_])
                        t.dma("sp", c.d_yT[0][dg * 128:(dg + 1) * 128, :], ab_[:, :], reads=[abb_],
                              writes=[c.d_yT[1]])
        t.barrier()
    with ExitStack() as st:
        S = lambda name, shape, dt=F32: st.enter_context(nc.sbuf_tensor(_u(name), shape, dt))
        yT = S("mg_yT", [128, KC, L], BF16); yTb = Buf()
        t.dma("sp", yT[:, :, :], c.d_yT[0].rearrange("(k p) q -> p k q", p=128), reads=[c.d_yT[1]], writes=[yTb])
        wrot = WPool(c, S, "mg_wo", KC, 512)
        stg_tm = Rot([S("mg_s%d" % i, [128, 512]) for i in range(3)])

        def epi2(ps, pb, tt, cc0, cw):
            s, sb = stg_tm.next()
            evac(c, s[:, 0:cw], ps[:, 0:cw], [pb], [sb])
            t.dma("sp", c.d_mix[0][tt * 128:(tt + 1) * 128, cc0:cc0 + cw], s[:, 0:cw],
                  reads=[sb], writes=[c.d_mix[1]])

        gemm_tm(c, c.w_out[layer], D, yT, yTb, KC, L, epi2, wrot)
        t.barrier()


SIG7 = 1.0 / (1.0 + math.exp(-1.702 * 7.0))


def phase_moe(c, layer):
    t, nc = c.t, c.nc
    V = lambda fn, r=(), w=(): t.op("dve", fn, reads=r, writes=w)
    A = lambda fn, r=(), w=(): t.op("act", fn, reads=r, writes=w)
    G = lambda fn, r=(), w=(): t.op("pool", fn, reads=r, writes=w)
    PE = lambda fn, r=(), w=(): t.op("pe", fn, reads=r, writes=w)
    HT = L // 2
    NTH = HT // 128
    with ExitStack() as st:
        S = lambda name, shape, dt=F32: st.enter_context(nc.sbuf_tensor(_u(name), shape, dt))
        gs_ = Rot([S("me_g%d" % i, [128, 512]) for i in range(2)])
        sg_ = Rot([S("me_s%d" % i, [128, 512]) for i in range(2)])
        us_ = Rot([S("me_u%d" % i, [128, 512]) for i in range(2)])
        xh = S("me_x", [128, KC, HT], BF16); xhb = Buf()
        yacc = S("me_y", [128, NTH, D]); yab = Buf()
        act2 = S("me_a", [128, KC * HT], BF16); actb = Buf()
        actT = act2[:, :].rearrange("p (k q) -> p k q", k=KC)
        bd = S("me_bd", [N_EXP, D]); bdb = Buf()
        gateT = S("me_gT", [N_EXP, HT]); gateTb = Buf()
        wstage = Rot([S("me_ws%d" % i, [128, KC, 256]) for i in range(2)])
        wbf = Rot([S("me_wb%d" % i, [128, KC, 256], BF16) for i in range(2)])
        gate = S("me_gate", [128, NTH, N_EXP]); gateb = Buf()
        c7 = S("me_c7", [128, 1])
        G(lambda e: e.memset(c7[:, :], 7.0), [], [gateb])
        bgu = Rot([S("me_bgu%d" % i, [128, 32]) for i in range(2)])
        bgs = Rot([S("me_bgs%d" % i, [128, 16]) for i in range(2)])
        items = []
        for ex in range(c.moe_nexp):
            items += [(ex, "gu", fc) for fc in range(16)]
            items += [(ex, "dn", db) for db in range(8)]
        import os as _os2
        items = items[:int(_os2.environ.get("MOE_ITEMS", len(items)))]

        def load_item(it):
            ex, kind, j = it
            stg, stgb = wstage.next()
            if kind == "gu":
                for two in range(2):
                    src = c.w_gate_up[layer][ex][:, two * DFF + j * 128:two * DFF + (j + 1) * 128].rearrange(
                        "(kc p) c -> p kc c", p=128)
                    t.dma("sp", stg[:, :, two * 128:(two + 1) * 128], src, writes=[stgb])
            else:
                src = c.w_down[layer][ex][:, j * 256:(j + 1) * 256].rearrange("(kc p) c -> p kc c", p=128)
                t.dma("sp", stg[:, :, :], src, writes=[stgb])
            bf, bfb = wbf.next()
            G(lambda e: e.tensor_copy(bf[:, :, :], stg[:, :, :]), [stgb], [bfb])
            return bf, bfb

        for hf in range(2):
            t0 = hf * HT
            t.dma("sp", xh[:, :, :], c.d_xT[0][:, t0:t0 + HT].rearrange("(k p) q -> p k q", p=128),
                  reads=[c.d_xT[1]], writes=[xhb])
            t.dma("sp", gate[:, :, :], c.d_gate[0][t0:t0 + HT, :].rearrange("(n p) e -> p n e", p=128),
                  reads=[c.d_gate[1]], writes=[gateb])
            t.dma("sp", gateT[:, :], c.d_gateT[0][:, t0:t0 + HT], reads=[c.d_gateT[1]], writes=[gateTb])
            t.dma("sp", bd[:, :], c.b_down[layer], writes=[bdb])
            import os as _os
            _lv = int(_os.environ.get("MOE_INIT", 9))
            if _lv == 0:
                t.barrier(); return
            for tt in range(NTH if _lv in (2, 9) else 1):
                for cb4 in range(4):
                    ps, pb = c.psum.next()
                    PE(lambda e: e.matmul(ps[:, :], gateT[:, tt * 128:(tt + 1) * 128], bd[:, cb4 * 512:(cb4 + 1) * 512],
                                          start=True, stop=True), [gateTb, bdb], [pb])
                    if _lv != 3:
                        evac(c, yacc[:, tt, cb4 * 512:(cb4 + 1) * 512], ps[:, :], [pb], [yab])
            if _lv in (3, 4):
                t.barrier(); return
            nxt = load_item(items[0])
            bg = bgb = bs = bsb = None
            for ii, (ex, kind, j) in enumerate(items):
                wt, wtb = nxt
                if ii + 1 < len(items):
                    nxt = load_item(items[ii + 1])
                if kind == "gu":
                    fc = j
                    if fc == 0:
                        bg, bgb = bgu.next()
                        load_cols(c, bg, bgb, c.b_gate_up[layer][ex], 32)
                        bs, bsb = bgs.next()
                        V(lambda e: e.tensor_tensor(bs[:, :], bg[:, 0:16], c7[:, 0:1].to_broadcast([128, 16]), op=ALU.mult),
                          [bgb, gateb], [bsb])
                        V(lambda e: e.tensor_scalar(bs[:, :], bs[:, :], 1.702 / 7.0, None, op0=ALU.mult), [bsb], [bsb])
                    wv = wt[:, :, :].rearrange("p k (two f) -> p k two f", two=2)
                    for tt in range(HT // 512):
                        cs = slice(tt * 512, (tt + 1) * 512)
                        psg, pbg = c.psum.next()
                        for kc in range(KC):
                            PE(lambda e: e.matmul(psg[:, :], wv[:, kc, 0, :], xh[:, kc, cs],
                                                  start=(kc == 0), stop=(kc == KC - 1)), [wtb, xhb], [pbg])
                        psu, pbu = c.psum.next()
                        for kc in range(KC):
                            PE(lambda e: e.matmul(psu[:, :], wv[:, kc, 1, :], xh[:, kc, cs],
                                                  start=(kc == 0), stop=(kc == KC - 1)), [wtb, xhb], [pbu])
                        g1, g1b = gs_.next(); s1, s1b = sg_.next(); u1, u1b = us_.next()
                        A(lambda e: e.activation(s1[:, :], psg[:, :], AF.Sigmoid, bias=bs[:, fc:fc + 1], scale=1.702),
                          [pbg, bsb], [s1b])
                        if c.moe_epi == 1:
                            continue
                        if c.moe_epi == 24:
                            V(lambda e: e.tensor_copy(g1[:, :], psu[:, :]), [pbu], [g1b])
                            continue
                        if c.moe_epi == 25:
                            A(lambda e: e.copy(g1[:, :], psu[:, :]), [pbu], [g1b])
                            continue
                        if c.moe_epi == 26:
                            V(lambda e: e.tensor_copy(g1[:, :], psg[:, :]), [pbg, s1b], [g1b])
                            continue
                        if c.moe_epi == 21:
                            V(lambda e: e.tensor_copy(g1[:, :], psg[:, :]), [pbg], [g1b])
                            continue
                        if c.moe_epi == 7:
                            A(lambda e: e.activation(g1[:, :], psg[:, :], AF.Identity, bias=bg[:, fc:fc + 1], scale=1.0),
                              [pbg, bgb], [g1b])
                            A(lambda e: e.activation(u1[:, :], psu[:, :], AF.Identity, bias=bg[:, 16 + fc:17 + fc], scale=1.0),
                              [pbu, bgb], [u1b])
                            V(lambda e: e.tensor_scalar(g1[:, :], g1[:, :], 7.0, None, op0=ALU.min), [g1b], [g1b])
                            V(lambda e: e.tensor_scalar(u1[:, :], u1[:, :], 7.0, None, op0=ALU.min), [u1b], [u1b])
                        else:
                            V(lambda e: e.tensor_scalar(g1[:, :], psg[:, :], bg[:, fc:fc + 1], c7[:, 0:1], op0=ALU.add, op1=ALU.min),
                              [pbg, bgb, gateb], [g1b])
                            V(lambda e: e.tensor_scalar(u1[:, :], psu[:, :], bg[:, 16 + fc:17 + fc], c7[:, 0:1], op0=ALU.add, op1=ALU.min),
                              [pbu, bgb, gateb], [u1b])
                        V(lambda e: e.scalar_tensor_tensor(out=g1[:, :], in0=s1[:, :], scalar=float(SIG7), in1=g1[:, :],
                                                           op0=ALU.min, op1=ALU.mult), [s1b, g1b], [g1b])
                        V(lambda e: e.tensor_scalar(u1[:, :], u1[:, :], -7.0, 1.0, op0=ALU.max, op1=ALU.add), [u1b], [u1b])
                        V(lambda e: e.tensor_tensor(actT[:, fc, cs], g1[:, :], u1[:, :], op=ALU.mult), [g1b, u1b], [actb])
                else:
                    db = j
                    for tt in range(NTH):
                        ps, pb = c.psum.next()
                        for kc in range(KC):
                            PE(lambda e: e.matmul(ps[:, 0:256], actT[:, kc, tt * 128:(tt + 1) * 128], wt[:, kc, :],
                                                  start=(kc == 0), stop=(kc == KC - 1)), [actb, wtb], [pb])
                        V(lambda e: e.scalar_tensor_tensor(
                            out=yacc[:, tt, db * 256:(db + 1) * 256], in0=ps[:, 0:256], scalar=gate[:, tt, ex:ex + 1],
                            in1=yacc[:, tt, db * 256:(db + 1) * 256], op0=ALU.mult, op1=ALU.add),
                          [pb, gateb, yab], [yab])
            t.dma("sp", c.d_ffn[0][t0:t0 + HT, :].rearrange("(n p) d -> p n d", p=128), yacc[:, :, :],
                  reads=[yab], writes=[c.d_ffn[1]])
        t.barrier()


def relayout_gu(w):
    e = w.shape[0]
    v = w.reshape(e, KC, 128, 2, 16, 128)
    v = v.transpose(0, 4, 2, 1, 3, 5)
    return np.ascontiguousarray(v).reshape(e, 16, 128, KC * 256)


def relayout_dn(w):
    e = w.shape[0]
    v = w.reshape(e, KC, 128, 8, 256)
    v = v.transpose(0, 3, 2, 1, 4)
    return np.ascontiguousarray(v).reshape(e, 8, 128, KC * 256)


def kernel(**inputs):
    n = 8
    nc = build()
    consts = host_consts()
    shared = {}
    for name in LAST_INPUT_NAMES:
        if name == "x":
            continue
        if name.startswith("k_"):
            shared[name] = consts[name[2:]]
        elif name.startswith("w_gate_up"):
            shared[name] = relayout_gu(np.asarray(inputs["w_gate_up"], dtype=np.float32)[int(name[-1])])
        elif name.startswith("w_down"):
            shared[name] = relayout_dn(np.asarray(inputs["w_down"], dtype=np.float32)[int(name[-1])])
        else:
            shared[name] = np.ascontiguousarray(np.asarray(inputs[name], dtype=np.float32))
    x = np.asarray(inputs["x"], dtype=np.float32)
    in_maps = []
    for b in range(n):
        m = dict(shared)
        m["x"] = np.ascontiguousarray(x[b])
        in_maps.append(m)
    res = run_bass_kernel_spmd(nc, in_maps, core_ids=list(range(n)))
    return np.stack([np.asarray(res.results[b]["out"], dtype=np.float32) for b in range(n)], axis=0)


CAP = 256


def moe_consts():
    s = np.arange(128)
    ustrict = (s[:, None] < s[None, :]).astype(np.float32)
    sele = np.zeros((N_EXP, N_EXP, 128), np.float32)
    for e in range(N_EXP):
        sele[e, e, :] = 1.0
    iota_c = np.tile(np.arange(CAP, dtype=np.float32)[None, :], (128, 1))
    pidx = (np.arange(128, dtype=np.float32)[:, None] + 128.0 * np.arange(CAP // 128, dtype=np.float32)[None, :])
    return dict(me_ustrict=ustrict, me_sele=sele, me_iota=iota_c, me_pidx=np.ascontiguousarray(pidx))


def phase_moe_sparse(c, layer):
    t, nc = c.t, c.nc
    V = lambda fn, r=(), w=(): t.op("dve", fn, reads=r, writes=w)
    A = lambda fn, r=(), w=(): t.op("act", fn, reads=r, writes=w)
    G = lambda fn, r=(), w=(): t.op("pool", fn, reads=r, writes=w)
    PE = lambda fn, r=(), w=(): t.op("pe", fn, reads=r, writes=w)
    HT = L // 2
    NTH = HT // 128
    NCC = CAP // 128
    with ExitStack() as st:
        S = lambda name, shape, dt=F32: st.enter_context(nc.sbuf_tensor(_u(name), shape, dt))
        gs_ = Rot([S("ms_g%d" % i, [128, CAP]) for i in range(2)])
        sg_ = Rot([S("ms_s%d" % i, [128, CAP]) for i in range(2)])
        us_ = Rot([S("ms_u%d" % i, [128, CAP]) for i in range(2)])
        xtm = S("ms_x", [128, NTH, D], BF16); xtmb = Buf()
        yacc = S("ms_y", [128, NTH, D]); yab = Buf()
        xeT = S("ms_xe", [128, KC, CAP], BF16); xeb = Buf()
        actT = S("ms_a", [128, KC, CAP], BF16); actb = Buf()
        ye = S("ms_ye", [128, NCC, D], BF16); yeb = Buf()
        selr = Rot([S("ms_sel%d" % i, [128, NTH, CAP], BF16) for i in range(2)])
        seltr = Rot([S("ms_selT%d" % i, [128, NCC, HT], BF16) for i in range(2)])
        wstage = Rot([S("ms_ws%d" % i, [128, KC, 256]) for i in range(2)])
        wbf = Rot([S("ms_wb%d" % i, [128, KC, 256], BF16) for i in range(2)])
        gate = S("ms_gate", [128, NTH, N_EXP]); gateb = Buf()
        rk = S("ms_rk", [128, NTH, N_EXP]); rkb = Buf()
        msk = S("ms_msk", [128, NTH, N_EXP]); mskb = Buf()
        rkT = S("ms_rkT", [N_EXP, HT]); rkTb = Buf()
        cb = Buf("msc")
        iota_c = S("ms_iota", [128, CAP]); t.dma("sp", iota_c[:, :], c.k["me_iota"][:, :], writes=[cb])
        pidx = S("ms_pidx", [128, NCC]); t.dma("sp", pidx[:, :], c.k["me_pidx"][:, :], writes=[cb])
        ustr = S("ms_us", [128, 128]); t.dma("sp", ustr[:, :], c.k["me_ustrict"][:, :], writes=[cb])
        ones = S("ms_ones", [128, 128]); G(lambda e: e.memset(ones[:, :], 1.0), [], [cb])
        c7 = S("ms_c7", [128, 1]); G(lambda e: e.memset(c7[:, :], 7.0), [], [cb])
        seler = Rot([S("ms_sele%d" % i, [N_EXP, 128]) for i in range(2)])
        bgu = Rot([S("ms_bgu%d" % i, [128, 32]) for i in range(2)])
        bgs = Rot([S("ms_bgs%d" % i, [128, 16]) for i in range(2)])
        items = []
        for ex in range(c.moe_nexp):
            items += [(ex, "gu", fc) for fc in range(16)]
            items += [(ex, "dn", db) for db in range(8)]

        def load_item(it):
            ex, kind, j = it
            stg, stgb = wstage.next()
            src = (c.w_gate_up if kind == "gu" else c.w_down)[layer][ex, j]
            t.dma("sp", stg[:, :, :].rearrange("p k c -> p (k c)"), src, writes=[stgb])
            bf, bfb = wbf.next()
            A(lambda e: e.copy(bf[:, 0:12, :], stg[:, 0:12, :]), [stgb], [bfb])
            G(lambda e: e.tensor_copy(bf[:, 12:16, :], stg[:, 12:16, :]), [stgb], [bfb])
            return bf, bfb

        for hf in range(2):
            t0 = hf * HT
            t.dma("sp", xtm[:, :, :], c.d_xtm[0][t0:t0 + HT, :].rearrange("(n p) d -> p n d", p=128),
                  reads=[c.d_xtm[1]], writes=[xtmb])
            t.dma("sp", gate[:, :, :], c.d_gate[0][t0:t0 + HT, :].rearrange("(n p) e -> p n e", p=128),
                  reads=[c.d_gate[1]], writes=[gateb])
            (sa, sab), (sb_, sbb) = wstage.next(), wstage.next()
            bd = sa[0:N_EXP, 0:8, :].rearrange("p k c -> p (k c)")
            gateT = sb_[0:N_EXP, 0:4, :].rearrange("p k c -> p (k c)")
            t.dma("sp", gateT, c.d_gateT[0][:, t0:t0 + HT], reads=[c.d_gateT[1]], writes=[sbb])
            t.dma("sp", bd, c.b_down[layer], writes=[sab])
            for tt in range(NTH):
                for cb4 in range(4):
                    ps, pb = c.psum.next()
                    PE(lambda e: e.matmul(ps[:, :], gateT[:, tt * 128:(tt + 1) * 128], bd[:, cb4 * 512:(cb4 + 1) * 512],
                                          start=True, stop=True), [sab, sbb], [pb])
                    evac(c, yacc[:, tt, cb4 * 512:(cb4 + 1) * 512], ps[:, :], [pb], [yab])
            V(lambda e: e.tensor_scalar(msk[:, :, :], gate[:, :, :], 0.0, None, op0=ALU.is_gt), [gateb], [mskb])
            for n in range(NTH):
                ps, pb = c.psum.next()
                for m in range(n):
                    PE(lambda e: e.matmul(ps[:, 0:N_EXP], ones[:, :], msk[:, m, :], start=(m == 0), stop=False),
                       [cb, mskb], [pb])
                PE(lambda e: e.matmul(ps[:, 0:N_EXP], ustr[:, :], msk[:, n, :], start=(n == 0), stop=True),
                   [cb, mskb], [pb])
                V(lambda e: e.tensor_tensor(rk[:, n, :], ps[:, 0:N_EXP], msk[:, n, :], op=ALU.mult), [pb, mskb], [rkb])
                V(lambda e: e.tensor_tensor(rk[:, n, :], rk[:, n, :], msk[:, n, :], op=ALU.add), [rkb, mskb], [rkb])
                V(lambda e: e.tensor_scalar(rk[:, n, :], rk[:, n, :], -1.0, None, op0=ALU.add), [rkb], [rkb])
                ps2, pb2 = c.psum.next()
                PE(lambda e: e.transpose(ps2[0:N_EXP, 0:128], rk[:, n, :], c.ident[:, :]), [rkb, c.ident_buf], [pb2])
                A(lambda e: e.copy(rkT[:, n * 128:(n + 1) * 128], ps2[0:N_EXP, 0:128]), [pb2], [rkTb])
            nxt = load_item(items[0])
            bg = bgb = bs = bsb = None
            sel = selb = selT = selTb = None
            for ii, (ex, kind, j) in enumerate(items):
                wt, wtb = nxt
                if ii + 1 < len(items):
                    nxt = load_item(items[ii + 1])
                if kind == "gu":
                    fc = j
                    if fc == 0:
                        bg, bgb = bgu.next()
                        load_cols(c, bg, bgb, c.b_gate_up[layer][ex], 32)
                        bs, bsb = bgs.next()
                        V(lambda e: e.tensor_scalar(bs[:, :], bg[:, 0:16], 1.702, None, op0=ALU.mult), [bgb], [bsb])
                        sel, selb = selr.next()
                        for n in range(NTH):
                            V(lambda e: e.tensor_scalar(sel[:, n, :], iota_c[:, :], rk[:, n, ex:ex + 1], None,
                                                        op0=ALU.is_equal), [cb, rkb], [selb])
                        se, seb = seler.next()
                        t.dma("sp", se[:, :], c.k["me_sele"][ex], writes=[seb])
                        selT, selTb = seltr.next()
                        for th in range(HT // 512):
                            psb, pbb = c.psum.next()
                            PE(lambda e: e.matmul(psb[:, :], se[:, :], rkT[:, th * 512:(th + 1) * 512], start=True, stop=True),
                               [seb, rkTb], [pbb])
                            for cc in range(NCC):
                                V(lambda e: e.tensor_scalar(selT[:, cc, th * 512:(th + 1) * 512], psb[:, :], pidx[:, cc:cc + 1],
                                                            None, op0=ALU.is_equal), [pbb, cb], [selTb])
                        for kc in range(KC):
                            psx, pbx = c.psum.next()
                            for n in range(NTH):
                                PE(lambda e: e.matmul(psx[:, 0:CAP], xtm[:, n, kc * 128:(kc + 1) * 128], sel[:, n, :],
                                                      start=(n == 0), stop=(n == NTH - 1)), [xtmb, selb], [pbx])
                            evac(c, xeT[:, kc, :], psx[:, 0:CAP], [pbx], [xeb])
                    psg, pbg = c.psum.next()
                    for kc in range(KC):
                        PE(lambda e: e.matmul(psg[:, 0:CAP], wt[:, kc, 0:128], xeT[:, kc, :],
                                              start=(kc == 0), stop=(kc == KC - 1)), [wtb, xeb], [pbg])
                    psu, pbu = c.psum.next()
                    for kc in range(KC):
                        PE(lambda e: e.matmul(psu[:, 0:CAP], wt[:, kc, 128:256], xeT[:, kc, :],
                                              start=(kc == 0), stop=(kc == KC - 1)), [wtb, xeb], [pbu])
                    g1, g1b = gs_.next(); s1, s1b = sg_.next(); u1, u1b = us_.next()
                    A(lambda e: e.activation(s1[:, :], psg[:, 0:CAP], AF.Sigmoid, bias=bs[:, fc:fc + 1], scale=1.702),
                      [pbg, bsb], [s1b])
                    V(lambda e: e.tensor_scalar(g1[:, :], psg[:, 0:CAP], bg[:, fc:fc + 1], c7[:, 0:1], op0=ALU.add, op1=ALU.min),
                      [pbg, bgb, cb], [g1b])
                    V(lambda e: e.tensor_scalar(u1[:, :], psu[:, 0:CAP], bg[:, 16 + fc:17 + fc], c7[:, 0:1], op0=ALU.add, op1=ALU.min),
                      [pbu, bgb, cb], [u1b])
                    V(lambda e: e.scalar_tensor_tensor(out=g1[:, :], in0=s1[:, :], scalar=float(SIG7), in1=g1[:, :],
                                                       op0=ALU.min, op1=ALU.mult), [s1b, g1b], [g1b])
                    V(lambda e: e.tensor_scalar(u1[:, :], u1[:, :], -7.0, 1.0, op0=ALU.max, op1=ALU.add), [u1b], [u1b])
                    V(lambda e: e.tensor_tensor(actT[:, fc, :], g1[:, :], u1[:, :], op=ALU.mult), [g1b, u1b], [actb])
                else:
                    db = j
                    for cc in range(NCC):
                        ps, pb = c.psum.next()
                        for kc in range(KC):
                            PE(lambda e: e.matmul(ps[:, 0:256], actT[:, kc, cc * 128:(cc + 1) * 128], wt[:, kc, :],
                                                  start=(kc == 0), stop=(kc == KC - 1)), [actb, wtb], [pb])
                        A(lambda e: e.copy(ye[:, cc, db * 256:(db + 1) * 256], ps[:, 0:256]), [pb], [yeb])
                    if db == 7:
                        for n in range(NTH):
                            for b4 in range(4):
                                ps, pb = c.psum.next()
                                for cc in range(NCC):
                                    PE(lambda e: e.matmul(ps[:, :], selT[:, cc, n * 128:(n + 1) * 128],
                                                          ye[:, cc, b4 * 512:(b4 + 1) * 512],
                                                          start=(cc == 0), stop=(cc == NCC - 1)), [selTb, yeb], [pb])
                                V(lambda e: e.scalar_tensor_tensor(
                                    out=yacc[:, n, b4 * 512:(b4 + 1) * 512], in0=ps[:, :], scalar=gate[:, n, ex:ex + 1],
                                    in1=yacc[:, n, b4 * 512:(b4 + 1) * 512], op0=ALU.mult, op1=ALU.add),
                                  [pb, gateb, yab], [yab])
            t.dma("sp", c.d_ffn[0][t0:t0 + HT, :].rearrange("(n p) d -> p n d", p=128), yacc[:, :, :],
                  reads=[yab], writes=[c.d_ffn[1]])
        t.barrier()
```

```python
import math
from contextlib import ExitStack
import numpy as np
import concourse.bass as bass
import concourse.mybir as mybir
from concourse.bass_utils import run_bass_kernel_spmd

F32 = mybir.dt.float32
BF16 = mybir.dt.bfloat16
AF = mybir.ActivationFunctionType
ALU = mybir.AluOpType
AX = mybir.AxisListType

D = 2048
L = 2048
DEPTH = 2
IN_COLS = 15616
NT = L // 128
KC = D // 128
LN_EPS = 1e-5
ALPHA = (2 * DEPTH) ** 0.25
N_EXP = 32
DFF = 2048

C_HQ, C_HFF, C_HFB, C_HI, C_HOG, C_HY, C_AQ, C_AK, C_AV, C_G = (
    0, 1024, 2048, 3072, 4096, 5120, 8192, 9216, 9344, 9472)


class Buf:
    __slots__ = ("name", "w", "r", "excl")

    def __init__(self, name="", excl=False):
        self.name = name
        self.w = None
        self.r = {}
        self.excl = excl


class Trk:
    NDS = 24

    def __init__(self, nc, stack):
        self.nc = nc
        self.E = {"pe": nc.tensor, "dve": nc.vector, "act": nc.scalar, "pool": nc.gpsimd,
                  "sp": nc.sync}
        self.sem = {}
        self.cnt = {}
        for e in ("pe", "dve", "act", "pool"):
            self.sem[e] = stack.enter_context(nc.semaphore("s_" + e))
            self.cnt[e] = 0
        for i in range(self.NDS):
            self.sem[("d", i)] = stack.enter_context(nc.semaphore("d%d" % i))
            self.cnt[("d", i)] = 0
        self.seen = {e: {} for e in self.E}
        self.dnext = 0
        self.n_ins = 0
        self.n_wait = 0

    def _need(self, reads, writes, e=None):
        need = {}
        for b in reads:
            if b.w is not None:
                k, v = b.w
                if need.get(k, 0) < v:
                    need[k] = v
            if b.excl:
                for k, v in b.r.items():
                    if k != e and need.get(k, 0) < v:
                        need[k] = v
        for b in writes:
            if b.w is not None:
                k, v = b.w
                if need.get(k, 0) < v:
                    need[k] = v
            for k, v in b.r.items():
                if need.get(k, 0) < v:
                    need[k] = v
        return need

    def _wait(self, e, need):
        seen = self.seen[e]
        for k, v in need.items():
            if k == e and e == "pe":
                continue
            if seen.get(k, 0) >= v:
                continue
            self.E[e].wait_ge(self.sem[k], v)
            seen[k] = v
            self.n_wait += 1

    def _mark(self, ev, reads, writes):
        k, v = ev
        for b in reads:
            if b.r.get(k, 0) < v:
                b.r[k] = v
        for b in writes:
            b.w = ev
            b.r = {}

    def op(self, e, fn, reads=(), writes=()):
        self._wait(e, self._need(reads, writes, e))
        self.cnt[e] += 1
        ins = fn(self.E[e])
        ins.then_inc(self.sem[e], 1)
        self._mark((e, self.cnt[e]), reads, writes)
        self.n_ins += 1
        return ins

    def dma(self, q, out, in_, reads=(), writes=()):
        need = self._need(reads, writes)
        i = self.dnext
        self.dnext = (self.dnext + 1) % self.NDS
        k = ("d", i)
        if self.cnt[k] > 0:
            need[k] = max(need.get(k, 0), self.cnt[k])
        self._wait(q, need)
        ins = self.E[q].dma_start(out=out, in_=in_)
        self.cnt[k] += 16
        ins.then_inc(self.sem[k], 16)
        self._mark((k, self.cnt[k]), reads, writes)
        self.n_ins += 1
        return ins

    def barrier(self):
        need = {k: v for k, v in self.cnt.items() if v > 0}
        for e in self.E:
            self._wait(e, dict(need))

    def drain(self, e, bufs):
        self._wait(e, self._need((), bufs))


class Rot:
    def __init__(self, tiles, excl=False):
        self.tiles = tiles
        self.bufs = [Buf(excl=excl) for _ in tiles]
        self.i = 0

    def next(self):
        i = self.i
        self.i = (i + 1) % len(self.tiles)
        return self.tiles[i], self.bufs[i]


class Ctx:
    pass


_uc = [0]


def _u(name):
    _uc[0] += 1
    return "%s_%d" % (name, _uc[0])


LAST_INPUT_NAMES = set()
LAST_TRK = None


class WPool:
    def __init__(self, c, S, name, kc, ncols, n_stage=2, n_bf=2):
        self.c = c
        self.stage = Rot([S("%s_st%d" % (name, i), [128, kc, ncols], F32) for i in range(n_stage)])
        self.bf = Rot([S("%s_bf%d" % (name, i), [128, kc, ncols], BF16) for i in range(n_bf)])

    def load(self, w_ap, kc, ncols):
        t = self.c.t
        st, stb = self.stage.next()
        t.dma("sp", st[:, 0:kc, 0:ncols], w_ap.rearrange("(kc p) c -> p kc c", p=128), writes=[stb])
        bf, bfb = self.bf.next()
        t.op("pool", lambda e: e.tensor_copy(bf[:, 0:kc, 0:ncols], st[:, 0:kc, 0:ncols]), reads=[stb], writes=[bfb])
        return bf, bfb


def gemm_fm(c, w_ap, n_cols, xT, xT_buf, k_chunks, n_tok, epilogue, wp, blk=512):
    t = c.t
    blocks = [(c0, min(blk, n_cols - c0)) for c0 in range(0, n_cols, blk)]
    nxt = wp.load(w_ap[:, blocks[0][0]:blocks[0][0] + blocks[0][1]], k_chunks, blocks[0][1])
    for bi, (c0, cw) in enumerate(blocks):
        wt, wb = nxt
        if bi + 1 < len(blocks):
            n0, nw = blocks[bi + 1]
            nxt = wp.load(w_ap[:, n0:n0 + nw], k_chunks, nw)
        for cg in range(cw // 128):
            for tt in range(n_tok // 512):
                ps, pb = c.psum.next()
                for kc in range(k_chunks):
                    t.op("pe", lambda e, kc=kc: e.matmul(
                        ps[:, :], wt[:, kc, cg * 128:(cg + 1) * 128],
                        xT[:, kc, tt * 512:(tt + 1) * 512],
                        start=(kc == 0), stop=(kc == k_chunks - 1)),
                        reads=[wb, xT_buf], writes=[pb])
                epilogue(ps, pb, (c0 // 128) + cg, tt)


def gemm_tm(c, w_ap, n_cols, xT, xT_buf, k_chunks, n_tok, epilogue, wp, blk=512):
    t = c.t
    blocks = [(c0, min(blk, n_cols - c0)) for c0 in range(0, n_cols, blk)]
    nxt = wp.load(w_ap[:, blocks[0][0]:blocks[0][0] + blocks[0][1]], k_chunks, blocks[0][1])
    for bi, (c0, cw) in enumerate(blocks):
        wt, wb = nxt
        if bi + 1 < len(blocks):
            n0, nw = blocks[bi + 1]
            nxt = wp.load(w_ap[:, n0:n0 + nw], k_chunks, nw)
        for tt in range(n_tok // 128):
            ps, pb = c.psum.next()
            for kc in range(k_chunks):
                t.op("pe", lambda e, kc=kc: e.matmul(
                    ps[:, 0:cw], xT[:, kc, tt * 128:(tt + 1) * 128], wt[:, kc, 0:cw],
                    start=(kc == 0), stop=(kc == k_chunks - 1)),
                    reads=[wb, xT_buf], writes=[pb])
            epilogue(ps, pb, tt, c0, cw)


_evac_flip = [0]


def evac(c, out_ap, in_ap, reads, writes):
    _evac_flip[0] ^= 1
    if _evac_flip[0]:
        c.t.op("act", lambda e: e.copy(out_ap, in_ap), reads=reads, writes=writes)
    else:
        c.t.op("dve", lambda e: e.tensor_copy(out_ap, in_ap), reads=reads, writes=writes)


def phase_ln(c, src, res, g_ap, b_ap, h_out, xT_out, final_out=None, router=None):
    t, nc = c.t, c.nc
    V = lambda fn, r=(), w=(): t.op("dve", fn, reads=r, writes=w)
    A = lambda fn, r=(), w=(): t.op("act", fn, reads=r, writes=w)
    PE = lambda fn, r=(), w=(): t.op("pe", fn, reads=r, writes=w)
    src_ap, src_buf = src
    res_ap, res_buf = res if res is not None else (None, None)
    h_out_ap, h_out_buf = h_out if h_out is not None else (None, None)
    with ExitStack() as st:
        S = lambda name, shape, dt=F32: st.enter_context(nc.sbuf_tensor(_u(name), shape, dt))
        gt = S("ln_g", [128, D]); gb_ = Buf()
        bt = S("ln_b", [128, D]); bb_ = Buf()
        t.dma("sp", gt[:, :], g_ap.partition_broadcast(128), writes=[gb_])
        t.dma("sp", bt[:, :], b_ap.partition_broadcast(128), writes=[bb_])
        xin = Rot([S("ln_x%d" % i, [128, D]) for i in range(2)])
        rin = Rot([S("ln_r%d" % i, [128, D]) for i in range(2)])
        yo = Rot([S("ln_y%d" % i, [128, D]) for i in range(2)])
        xts = Rot([S("ln_xt%d" % i, [128, 4, 128]) for i in range(5)])
        xtb = Rot([S("ln_xb%d" % i, [128, 4, 128], BF16) for i in range(3)])
        stats = S("ln_st", [128, 4 * 6]); sb_ = Buf()
        mv = S("ln_mv", [128, 2]); mvb = Buf()
        rstd = S("ln_rstd", [128, 1]); rsb = Buf()
        if router is not None:
            rw = S("ln_rw", [128, KC, N_EXP]); rwb = Buf()
            t.dma("sp", rw[:, :, :], c.router_w[router].rearrange("(k p) e -> p k e", p=128), writes=[rwb])
            rbias = S("ln_rb", [128, N_EXP])
            t.dma("sp", rbias[:, :], c.router_b[router].partition_broadcast(128), writes=[rwb])
            lg = Rot([S("ln_lg%d" % i, [128, N_EXP]) for i in range(2)])
            ex_ = Rot([S("ln_ex%d" % i, [128, N_EXP]) for i in range(2)])
            m8 = Rot([S("ln_m8%d" % i, [128, 12]) for i in range(2)])
            gT_ = Rot([S("ln_gT%d" % i, [N_EXP, 128]) for i in range(2)])
            ybf = Rot([S("ln_ybf%d" % i, [128, D], BF16) for i in range(2)])
        for tt in range(NT):
            rows = slice(tt * 128, (tt + 1) * 128)
            x, xb = xin.next()
            t.dma("sp", x[:, :], src_ap[rows, :], reads=[src_buf], writes=[xb])
            if res_ap is not None:
                r, rb = rin.next()
                t.dma("sp", r[:, :], res_ap[rows, :], reads=[res_buf], writes=[rb])
                V(lambda e: e.scalar_tensor_tensor(out=x[:, :], in0=r[:, :], scalar=float(ALPHA), in1=x[:, :],
                                                   op0=ALU.mult, op1=ALU.add), [rb, xb], [xb])
            for j in range(4):
                V(lambda e: e.bn_stats(stats[:, j * 6:(j + 1) * 6], x[:, j * 512:(j + 1) * 512]), [xb], [sb_])
            V(lambda e: e.bn_aggr(mv[:, :], stats[:, :]), [sb_], [mvb])
            A(lambda e: e.activation(rstd[:, :], mv[:, 1:2], AF.Sqrt, bias=c.eps_ln[:, :], scale=1.0),
              [mvb, c.cbuf], [rsb])
            V(lambda e: e.reciprocal(rstd[:, :], rstd[:, :]), [rsb], [rsb])
            y, yb = yo.next()
            V(lambda e: e.tensor_scalar(y[:, :], x[:, :], mv[:, 0:1], rstd[:, 0:1], op0=ALU.subtract, op1=ALU.mult),
              [xb, mvb, rsb], [yb])
            t.op("pool", lambda e: e.tensor_tensor(y[:, :], y[:, :], gt[:, :], op=ALU.mult), reads=[yb, gb_], writes=[yb])
            t.op("pool", lambda e: e.tensor_tensor(y[:, :], y[:, :], bt[:, :], op=ALU.add), reads=[yb, bb_], writes=[yb])
            if h_out_ap is not None:
                t.dma("sp", h_out_ap[rows, :], y[:, :], reads=[yb], writes=[h_out_buf])
            if router is not None:
                yb16, yb16b = ybf.next()
                A(lambda e: e.copy(yb16[:, :], y[:, :]), [yb], [yb16b])
                t.dma("sp", c.d_xtm[0][rows, :], yb16[:, :], reads=[yb16b], writes=[c.d_xtm[1]])
            if final_out is not None:
                t.dma("sp", final_out[rows, :], y[:, :], reads=[yb], writes=[c.out_buf])
            if xT_out is not None:
                if router is not None:
                    pl, plb = c.psacc.next()
                for g4 in range(KC // 4):
                    ps, pb = c.psum.next()
                    for j in range(4):
                        fc = g4 * 4 + j
                        PE(lambda e: e.transpose(ps[:, j * 128:(j + 1) * 128], y[:, fc * 128:(fc + 1) * 128],
                                                 c.ident[:, :]), [yb, c.ident_buf], [pb])
                    xb_, xbb_ = xtb.next()
                    A(lambda e: e.copy(xb_[:, :, :], ps[:, :].rearrange("p (j q) -> p j q", j=4)), [pb], [xbb_])
                    t.dma("sp", xT_out[0][g4 * 512:(g4 + 1) * 512, rows].rearrange("(j p) q -> p j q", p=128),
                          xb_[:, :, :], reads=[xbb_], writes=[xT_out[1]])
                    if router is not None:
                        xs, xsb = xts.next()
                        V(lambda e: e.tensor_copy(xs[:, :, :], ps[:, :].rearrange("p (j q) -> p j q", j=4)), [pb], [xsb])
                    if router is not None:
                        for j in range(4):
                            fc = g4 * 4 + j
                            PE(lambda e: e.matmul(pl[:, 0:N_EXP], xs[:, j, :], rw[:, fc, :],
                                                  start=(fc == 0), stop=(fc == KC - 1)), [xsb, rwb], [plb])
                if router is not None:
                    l_, lb_ = lg.next()
                    V(lambda e: e.tensor_tensor(l_[:, :], pl[:, 0:N_EXP], rbias[:, :], op=ALU.add), [plb, rwb], [lb_])
                    m_, mb_ = m8.next()
                    V(lambda e: e.max(m_[:, 0:8], l_[:, :]), [lb_], [mb_])
                    V(lambda e: e.tensor_scalar(m_[:, 8:9], m_[:, 0:1], -1.0, None, op0=ALU.mult), [mb_], [mb_])
                    e_, eb_ = ex_.next()
                    A(lambda e: e.activation(e_[:, :], l_[:, :], AF.Exp, bias=m_[:, 8:9], scale=1.0), [lb_, mb_], [eb_])
                    V(lambda e: e.scalar_tensor_tensor(out=e_[:, :], in0=l_[:, :], scalar=m_[:, 3:4], in1=e_[:, :],
                                                       op0=ALU.is_ge, op1=ALU.mult), [lb_, mb_, eb_], [eb_])
                    V(lambda e: e.reduce_sum(m_[:, 9:10], e_[:, :], axis=AX.X), [eb_], [mb_])
                    V(lambda e: e.reciprocal(m_[:, 10:11], m_[:, 9:10]), [mb_], [mb_])
                    V(lambda e: e.tensor_scalar(e_[:, :], e_[:, :], m_[:, 10:11], None, op0=ALU.mult), [eb_, mb_], [eb_])
                    t.dma("sp", c.d_gate[0][rows, :], e_[:, :], reads=[eb_], writes=[c.d_gate[1]])
                    ps, pb = c.psum.next()
                    PE(lambda e: e.transpose(ps[0:N_EXP, 0:128], e_[:, :], c.ident[:, :]), [eb_, c.ident_buf], [pb])
                    g_, gb2 = gT_.next()
                    A(lambda e: e.copy(g_[:, :], ps[0:N_EXP, 0:128]), [pb], [gb2])
                    t.dma("sp", c.d_gateT[0][:, rows], g_[:, :], reads=[gb2], writes=[c.d_gateT[1]])
        t.barrier()


def phase_proj(c, layer, only=None):
    t, nc = c.t, c.nc
    w = c.w_in[layer]
    with ExitStack() as st:
        S = lambda name, shape, dt=F32: st.enter_context(nc.sbuf_tensor(_u(name), shape, dt))
        hT = S("hT", [128, KC, L], BF16); hT_buf = Buf("hT")
        t.dma("sp", hT[:, :, :], c.d_xT[0].rearrange("(k p) q -> p k q", p=128), reads=[c.d_xT[1]], writes=[hT_buf])
        wrot = WPool(c, S, "pj_w", KC, 512)
        stg = Rot([S("pj_s%d" % i, [128, L]) for i in range(2)])
        stg_tm = Rot([S("pj_t%d" % i, [128, 512]) for i in range(3)])
        fm = [(C_HQ, 1024, c.d_hqT), (C_HFF, 1024, c.d_hffT), (C_HFB, 1024, c.d_hfbT),
              (C_HOG, 1024, c.d_hogT), (C_HY, 3072, c.d_hyT), (C_AQ, 1024, c.d_aqT),
              (C_AK, 128, c.d_akT)]
        if only is not None:
            fm = fm[only[0]:only[1]]
        for c0, n, (dst, dbuf) in fm:
            cur = {}

            def epi(ps, pb, g, tt, dst=dst, dbuf=dbuf, cur=cur):
                if tt == 0:
                    cur["s"] = stg.next()
                s, sb = cur["s"]
                evac(c, s[:, tt * 512:(tt + 1) * 512], ps[:, :], [pb], [sb])
                if tt == L // 512 - 1:
                    t.dma("sp", dst[g * 128:(g + 1) * 128, :], s[:, :], reads=[sb], writes=[dbuf])

            gemm_fm(c, w[:, c0:c0 + n], n, hT, hT_buf, KC, L, epi, wrot)
        if only is None or len(only) > 4:
            cur = {}

            def epig(ps, pb, g, tt, cur=cur):
                if tt == 0:
                    cur["s"] = stg.next()
                s, sb = cur["s"]
                t.op("act", lambda e: e.activation(s[:, tt * 512:(tt + 1) * 512], ps[:, :], AF.Sigmoid),
                     reads=[pb], writes=[sb])
                if tt == L // 512 - 1:
                    t.dma("sp", c.d_gT[0][g * 128:(g + 1) * 128, :], s[:, :], reads=[sb],
                          writes=[c.d_gT[1]])

            gemm_fm(c, w[:, C_G:C_G + 3 * D], 3 * D, hT, hT_buf, KC, L, epig, wrot)
        tm = [(C_HFF, 2048, c.d_hf), (C_HI, 1024, c.d_hi), (C_AV, 128, c.d_av)]
        if only is not None:
            tm = tm[only[2]:only[3]]
        for c0, n, (dst, dbuf) in tm:
            def epi2(ps, pb, tt, cc0, cw, dst=dst, dbuf=dbuf):
                s, sb = stg_tm.next()
                evac(c, s[:, 0:cw], ps[:, 0:cw], [pb], [sb])
                t.dma("sp", dst[tt * 128:(tt + 1) * 128, cc0:cc0 + cw], s[:, 0:cw],
                      reads=[sb], writes=[dbuf])

            gemm_tm(c, w[:, c0:c0 + n], n, hT, hT_buf, KC, L, epi2, wrot)
        t.barrier()


def host_consts():
    cs = {}
    cs["ident"] = np.eye(128, dtype=np.float32)
    M1, M2, M3, M4, M5 = hg_masks()
    cs["hg_M1"], cs["hg_M2"], cs["hg_M3"], cs["hg_M4"], cs["hg_M5"] = M1, M2, M3, M4, M5
    cs.update(hy_consts())
    cs.update(att_consts())
    cs.update(moe_consts())
    return cs


def build(layer_list=(0, 1), stop=None, dbg=(), only=None):
    nc = bass.Bass("TRN2", target_bir_lowering=False)
    c = Ctx()
    c.nc = nc
    import os as _os
    c.moe_nexp = int(_os.environ.get("MOE_NEXP", N_EXP))
    c.moe_stop = _os.environ.get("MOE_STOP", "")
    c.moe_dense = _os.environ.get("MOE_DENSE", "0") == "1"
    c.moe_epi = int(_os.environ.get("MOE_EPI", 6))
    INPUT_NAMES = []

    def ein(name, shape):
        INPUT_NAMES.append(name)
        return nc.dram_tensor(name, list(shape), F32, kind="ExternalInput").ap()
    c.x = ein("x", [L, D])
    c.ln_in_g = ein("ln_in_g", [D]); c.ln_in_b = ein("ln_in_b", [D])
    c.w_in = ein("w_in", [DEPTH, D, IN_COLS])
    c.hg_lb = ein("hg_lower_bound", [DEPTH, 2048])
    c.hg_norm_g = ein("hg_norm_g", [DEPTH, 1024])
    c.hy_conv_w = ein("hy_conv_w", [DEPTH, 3, 3072]); c.hy_conv_b = ein("hy_conv_b", [DEPTH, 3072])
    c.hy_w1 = ein("hy_filt_w1", [DEPTH, 33, 64]); c.hy_b1 = ein("hy_filt_b1", [DEPTH, 64])
    c.hy_w2 = ein("hy_filt_w2", [DEPTH, 2, 64, 64]); c.hy_b2 = ein("hy_filt_b2", [DEPTH, 2, 64])
    c.hy_freq = ein("hy_filt_freq", [DEPTH, 64]); c.hy_w3 = ein("hy_filt_w3", [DEPTH, 64, 2048])
    c.hy_skip = ein("hy_skip", [DEPTH, 1024])
    c.att_sink = ein("att_sink", [DEPTH, 16]); c.rel_bias = ein("rel_bias", [32, 16])
    c.w_branch = ein("w_branch", [DEPTH, 3, 1024, D]); c.w_out = ein("w_out", [DEPTH, D, D])
    c.ln_mix_g = ein("ln_mix_g", [DEPTH, D]); c.ln_mix_b = ein("ln_mix_b", [DEPTH, D])
    c.router_w = ein("router_w", [DEPTH, D, N_EXP]); c.router_b = ein("router_b", [DEPTH, N_EXP])
    c.w_gate_up = [ein("w_gate_up%d" % l, [c.moe_nexp, 16, 128, KC * 256]) if l in layer_list else None for l in range(DEPTH)]
    c.w_down = [ein("w_down%d" % l, [c.moe_nexp, 8, 128, KC * 256]) if l in layer_list else None for l in range(DEPTH)]
    c.b_gate_up = ein("b_gate_up", [DEPTH, N_EXP, 2 * DFF]); c.b_down = ein("b_down", [DEPTH, N_EXP, D])
    c.ln_moe_g = ein("ln_moe_g", [DEPTH, D]); c.ln_moe_b = ein("ln_moe_b", [DEPTH, D])
    consts = host_consts()
    global LAST_INPUT_NAMES
    c.k = {k: ein("k_" + k, v.shape) for k, v in consts.items()}
    LAST_INPUT_NAMES = set(INPUT_NAMES)
    c.out = nc.dram_tensor("out", [L, D], F32, kind="ExternalOutput").ap()
    c.out_buf = Buf()

    def scratch(name, shape, dt=F32):
        kind = "ExternalOutput" if name in dbg else "Internal"
        return (nc.dram_tensor(name, list(shape), dt, kind=kind).ap(), Buf(name))

    c.d_h = scratch("d_h", [L, D]); c.d_h1 = scratch("d_h1", [L, D])
    c.d_gT = scratch("d_gT", [3 * D, L])
    c.d_hqT = scratch("d_hqT", [1024, L]); c.d_hffT = scratch("d_hffT", [1024, L])
    c.d_hfbT = scratch("d_hfbT", [1024, L]); c.d_hogT = scratch("d_hogT", [1024, L])
    c.d_hyT = scratch("d_hyT", [3072, L]); c.d_aqT = scratch("d_aqT", [1024, L])
    c.d_akT = scratch("d_akT", [128, L])
    c.d_hf = scratch("d_hf", [L, 2048]); c.d_hi = scratch("d_hi", [L, 1024])
    c.d_av = scratch("d_av", [L, 128])
    c.d_ohgT = scratch("d_ohgT", [1024, L], BF16)
    c.d_ohyT = scratch("d_ohyT", [1024, L], BF16)
    c.d_oatT = scratch("d_oatT", [1024, L], BF16)
    c.d_mix = scratch("d_mix", [L, D])
    c.d_xT = scratch("d_xT", [D, L], BF16); c.d_ffn = scratch("d_ffn", [L, D])
    c.d_gate = scratch("d_gate", [L, N_EXP]); c.d_gateT = scratch("d_gateT", [N_EXP, L])
    c.d_xtm = scratch("d_xtm", [L, D], BF16)
    c.d_yT = scratch("d_yT", [D, L], BF16)
    c.d_Kr = scratch("d_Kr", [L, 1024]); c.d_Ki = scratch("d_Ki", [L, 1024])
    c.d_Yr = scratch("d_Yr", [L, 1024]); c.d_Yi = scratch("d_Yi", [L, 1024])
    c.d_uT = scratch("d_uT", [1024, L]); c.d_x0c = scratch("d_x0c", [1024, L])

    with ExitStack() as st:
        global LAST_TRK
        c.t = t = LAST_TRK = Trk(nc, st)
        S = lambda name, shape, dt=F32: st.enter_context(nc.sbuf_tensor(_u(name), shape, dt))
        banks = [st.enter_context(nc.psum_tensor("ps%d" % i, [128, 512], F32)) for i in range(8)]
        c.psum = Rot(banks[0:6], excl=True)
        c.psacc = Rot(banks[6:8], excl=True)
        c.ident = S("ident", [128, 128]); c.ident_buf = Buf()
        t.dma("sp", c.ident[:, :], c.k["ident"][:, :], writes=[c.ident_buf])
        c.rowtmp = Rot([S("rowtmp%d" % i, [128, 128]) for i in range(2)])
        c.cbuf = Buf("consts")
        c.eps_ln = S("eps_ln", [128, 1])
        t.op("pool", lambda e: e.memset(c.eps_ln[:, :], float(LN_EPS)), writes=[c.cbuf])

        def finish():
            for k in list(t.cnt):
                if isinstance(k, tuple) and t.cnt[k] > 0:
                    t._wait("sp", {k: t.cnt[k]})
            for e in ("pe", "dve", "act", "pool"):
                if t.cnt[e] > 0:
                    t._wait("sp", {e: t.cnt[e]})

        pending = dict(src=(c.x, Buf("x")), res=None, g=c.ln_in_g, b=c.ln_in_b)
        for layer in layer_list:
            phase_ln(c, pending["src"], pending["res"], pending["g"], pending["b"], c.d_h, c.d_xT)
            if stop == "ln0":
                finish(); return nc
            phase_proj(c, layer, only)
            if stop == "proj":
                finish(); return nc
            if "nohgrn" not in dbg:
                phase_hgrn(c, layer)
            if stop == "hgrn":
                finish(); return nc
            if "nohyena" not in dbg:
                phase_hyena(c, layer)
            if stop == "hyena":
                finish(); return nc
            if "noattn" not in dbg:
                phase_attn(c, layer)
            if stop == "attn":
                finish(); return nc
            phase_merge(c, layer)
            if stop == "merge":
                finish(); return nc
            phase_ln(c, c.d_mix, c.d_h, c.ln_mix_g[layer], c.ln_mix_b[layer], c.d_h1, c.d_xT, router=layer)
            if stop == "ln1":
                finish(); return nc
            (phase_moe if c.moe_dense else phase_moe_sparse)(c, layer)
            if stop == "moe":
                finish(); return nc
            pending = dict(src=c.d_ffn, res=c.d_h1, g=c.ln_moe_g[layer], b=c.ln_moe_b[layer])
        phase_ln(c, pending["src"], pending["res"], pending["g"], pending["b"], None, None, final_out=c.out)
        finish()
    return nc


def load_cols(c, dst, dst_buf, vec_ap, n, col0=0):
    t = c.t
    rows, rb = c.rowtmp.next()
    t.dma("sp", rows[0:n, :], vec_ap.rearrange("(j p) -> j p", p=128), writes=[rb])
    ps, pb = c.psum.next()
    t.op("pe", lambda e: e.transpose(ps[:, 0:n], rows[0:n, :], c.ident[0:n, 0:n]),
         reads=[rb, c.ident_buf], writes=[pb])
    t.op("dve", lambda e: e.tensor_copy(dst[:, col0:col0 + n], ps[:, 0:n]), reads=[pb],
         writes=[dst_buf])


def hg_masks():
    s = np.arange(128)
    same = (s[:, None] // 32) == (s[None, :] // 32)
    M1 = (same & (s[:, None] <= s[None, :])).astype(np.float32)
    M3 = (same & (s[:, None] > s[None, :])).astype(np.float32)
    M5 = (s[:, None] // 32 == np.arange(4)[None, :]).astype(np.float32)
    return M1, M1.T.copy(), M3, M3.T.copy(), M5


def phase_hgrn(c, layer):
    t, nc = c.t, c.nc
    V = lambda fn, r=(), w=(): t.op("dve", fn, reads=r, writes=w)
    A = lambda fn, r=(), w=(): t.op("act", fn, reads=r, writes=w)
    G = lambda fn, r=(), w=(): t.op("pool", fn, reads=r, writes=w)
    PE = lambda fn, r=(), w=(): t.op("pe", fn, reads=r, writes=w)
    QS = 128.0 ** -0.5
    with ExitStack() as st:
        S = lambda name, shape, dt=F32: st.enter_context(nc.sbuf_tensor(_u(name), shape, dt))
        kb = Buf("hgk")
        Ms = []
        for nm in ("M1", "M2", "M3", "M4"):
            m = S("hg_" + nm, [128, 128])
            t.dma("sp", m[:, :], c.k["hg_" + nm][:, :], writes=[kb])
            Ms.append(m)
        M1, M2, M3, M4 = Ms
        M5 = S("hg_M5", [128, 4])
        t.dma("sp", M5[:, :], c.k["hg_M5"][:, :], writes=[kb])
        ones = S("hg_ones", [128, 128])
        G(lambda e: e.memset(ones[:, :], 1.0), w=[kb])
        eps = S("hg_eps", [128, 1])
        G(lambda e: e.memset(eps[:, :], 1e-6), w=[kb])
        lbT = S("hg_lbT", [128, 16]); omlT = S("hg_omlT", [128, 16]); nomlT = S("hg_nomlT", [128, 16])
        lbtm = S("hg_lbtm", [128, 2048]); omltm = S("hg_omltm", [128, 2048])
        gT = S("hg_gT", [128, 8])
        lbb = Buf("lb")
        load_cols(c, gT, lbb, c.hg_norm_g[layer], 8)
        if layer == 0:
            G(lambda e: e.memset(lbT[:, :], 0.0), w=[lbb])
            G(lambda e: e.memset(lbtm[:, :], 0.0), w=[lbb])
        else:
            x0T = S("hg_x0T", [128, 16])
            load_cols(c, x0T, lbb, c.hg_lb[0], 16)
            load_cols(c, lbT, lbb, c.hg_lb[1], 16)
            V(lambda e: e.tensor_tensor(lbT[:, :], lbT[:, :], x0T[:, :], op=ALU.subtract), [lbb], [lbb])
            A(lambda e: e.activation(lbT[:, :], lbT[:, :], AF.Sigmoid), [lbb], [lbb])
            t.dma("sp", omltm[:, :], c.hg_lb[0].partition_broadcast(128), writes=[lbb])
            t.dma("sp", lbtm[:, :], c.hg_lb[1].partition_broadcast(128), writes=[lbb])
            G(lambda e: e.tensor_tensor(lbtm[:, :], lbtm[:, :], omltm[:, :], op=ALU.subtract), [lbb], [lbb])
            A(lambda e: e.activation(lbtm[:, :], lbtm[:, :], AF.Sigmoid), [lbb], [lbb])
        V(lambda e: e.tensor_scalar(omlT[:, :], lbT[:, :], -1.0, 1.0, op0=ALU.mult, op1=ALU.add), [lbb], [lbb])
        V(lambda e: e.tensor_scalar(nomlT[:, :], omlT[:, :], -1.0, None, op0=ALU.mult), [lbb], [lbb])
        V(lambda e: e.tensor_scalar(omltm[:, :], lbtm[:, :], -1.0, 1.0, op0=ALU.mult, op1=ALU.add), [lbb], [lbb])

        ztm = [S("hg_ztm%d" % d, [128, NT, 128]) for d in range(2)]; ztm_b = [Buf() for _ in range(2)]
        lgf = [S("hg_lgf%d" % d, [128, NT, 128]) for d in range(2)]; lgf_b = [Buf() for _ in range(2)]
        vt = S("hg_v", [128, NT, 128]); vt_b = Buf()
        qs = S("hg_qs", [128, L]); qs_b = Buf()
        qh = [S("hg_qh%d" % d, [128, L]) for d in range(2)]; qh_b = [Buf() for _ in range(2)]
        kt = [S("hg_kt%d" % d, [128, L]) for d in range(2)]; kt_b = [Buf() for _ in range(2)]
        og = S("hg_og", [128, L]); og_b = Buf()
        Dd = [S("hg_D%d" % d, [128, 64]) for d in range(2)]; Dd_b = [Buf() for _ in range(2)]
        St = [S("hg_S%d" % d, [128, 64, 128]) for d in range(2)]; St_b = [Buf() for _ in range(2)]
        ex = Rot([S("hg_ex%d" % i, [128, 512]) for i in range(3)])
        scm = Rot([S("hg_scm%d" % i, [128, 128]) for i in range(4)])
        vmr = Rot([S("hg_vm%d" % i, [128, 4, 128]) for i in range(2)])
        sq = Rot([S("hg_sq%d" % i, [128, 512]) for i in range(2)])
        rs = Rot([S("hg_rs%d" % i, [128, 512]) for i in range(2)])
        ost = Rot([S("hg_o%d" % i, [128, 512]) for i in range(2)])
        ostb = Rot([S("hg_ob%d" % i, [128, 512], BF16) for i in range(2)])
        Mpre = [M1, M2]
        Mex = [M3, M4]

        for hd in range(8):
            hs = slice(hd * 128, (hd + 1) * 128)
            for d in range(2):
                t.dma("sp", ztm[d][:, :, :],
                      c.d_hf[0][:, d * 1024 + hd * 128:d * 1024 + (hd + 1) * 128].rearrange(
                          "(n p) k -> p n k", p=128), reads=[c.d_hf[1]], writes=[ztm_b[d]])
            t.dma("sp", vt[:, :, :], c.d_hi[0][:, hs].rearrange("(n p) k -> p n k", p=128),
                  reads=[c.d_hi[1]], writes=[vt_b])
            t.dma("sp", qs[:, :], c.d_hqT[0][hs, :], reads=[c.d_hqT[1]], writes=[qs_b])
            t.dma("sp", kt[0][:, :], c.d_hffT[0][hs, :], reads=[c.d_hffT[1]], writes=[kt_b[0]])
            t.dma("sp", kt[1][:, :], c.d_hfbT[0][hs, :], reads=[c.d_hfbT[1]], writes=[kt_b[1]])
            t.dma("sp", og[:, :], c.d_hogT[0][hs, :], reads=[c.d_hogT[1]], writes=[og_b])
            for d in range(2):
                z = ztm[d]; zb = ztm_b[d]; lg = lgf[d]; lb_ = lgf_b[d]
                col = d * 1024 + hd * 128
                oml_bc = omltm[:, col:col + 128].unsqueeze(1).to_broadcast([128, NT, 128])
                lb_bc = lbtm[:, col:col + 128].unsqueeze(1).to_broadcast([128, NT, 128])
                A(lambda e: e.activation(z[:, :, :], z[:, :, :], AF.Sigmoid), [zb], [zb])
                G(lambda e: e.tensor_tensor(lg[:, :, :], z[:, :, :], oml_bc, op=ALU.mult), [zb, lbb], [lb_])
                G(lambda e: e.tensor_tensor(lg[:, :, :], lg[:, :, :], lb_bc, op=ALU.add), [lb_, lbb], [lb_])
                V(lambda e: e.tensor_scalar(lg[:, :, :], lg[:, :, :], 1e-30, None, op0=ALU.max), [lb_], [lb_])
                V(lambda e: e.tensor_scalar(z[:, :, :], z[:, :, :], -1.0, 1.0, op0=ALU.mult, op1=ALU.add), [zb], [zb])
                G(lambda e: e.tensor_tensor(z[:, :, :], z[:, :, :], oml_bc, op=ALU.mult), [zb, lbb], [zb])
            for d in range(2):
                lg = lgf[d]; lb_ = lgf_b[d]
                A(lambda e: e.activation(lg[:, :, :], lg[:, :, :], AF.Ln), [lb_], [lb_])
            A(lambda e: e.activation(qs[:, :], qs[:, :], AF.Silu), [qs_b], [qs_b])
            A(lambda e: e.activation(og[:, :], og[:, :], AF.Silu), [og_b], [og_b])
            for d in range(2):
                j = d * 8 + hd
                A(lambda e: e.activation(kt[d][:, :], kt[d][:, :], AF.Sigmoid), [kt_b[d]], [kt_b[d]])
                V(lambda e: e.tensor_scalar(kt[d][:, :], kt[d][:, :], nomlT[:, j:j + 1], omlT[:, j:j + 1],
                                            op0=ALU.mult, op1=ALU.add), [kt_b[d], lbb], [kt_b[d]])
            for d in range(2):
                lg = lgf[d]; lb_ = lgf_b[d]; z = ztm[d]; zb = ztm_b[d]
                ps, pb = c.psum.next()
                for n in range(NT):
                    PE(lambda e, n=n: e.matmul(ps[:, n * 4:(n + 1) * 4], lg[:, n, :], M5[:, :],
                                               start=True, stop=True), [lb_, kb], [pb])
                A(lambda e: e.activation(Dd[d][:, :], ps[:, 0:64], AF.Exp), [pb], [Dd_b[d]])
                for g4 in range(4):
                    ps, pb = c.psum.next()
                    for jn in range(4):
                        n = g4 * 4 + jn
                        PE(lambda e, n=n, jn=jn: e.matmul(ps[:, jn * 128:(jn + 1) * 128], Mex[d][:, :],
                                                          lg[:, n, :], start=True, stop=True),
                           [lb_, kb], [pb])
                    x_, xb_ = ex.next()
                    A(lambda e: e.activation(x_[:, :], ps[:, :], AF.Exp), [pb], [xb_])
                    V(lambda e, g4=g4: e.tensor_tensor(
                        z[:, g4 * 4:(g4 + 1) * 4, :], z[:, g4 * 4:(g4 + 1) * 4, :],
                        x_[:, :].rearrange("p (a b) -> p a b", a=4), op=ALU.mult), [zb, xb_], [zb])
                for g4 in range(4):
                    ps, pb = c.psum.next()
                    for jn in range(4):
                        n = g4 * 4 + jn
                        PE(lambda e, n=n, jn=jn: e.matmul(ps[:, jn * 128:(jn + 1) * 128], lg[:, n, :],
                                                          Mpre[d][:, :], start=True, stop=True),
                           [lb_, kb], [pb])
                    cs = slice(g4 * 512, (g4 + 1) * 512)
                    x1, xb1 = ex.next()
                    A(lambda e: e.activation(x1[:, :], ps[:, :], AF.Exp), [pb], [xb1])
                    V(lambda e, cs=cs: e.scalar_tensor_tensor(
                        out=qh[d][:, cs], in0=x1[:, :], scalar=float(QS), in1=qs[:, cs],
                        op0=ALU.mult, op1=ALU.mult), [xb1, qs_b], [qh_b[d]])
                    x2, xb2 = ex.next()
                    A(lambda e: e.activation(x2[:, :], ps[:, :], AF.Exp, scale=-1.0), [pb], [xb2])
                    V(lambda e, cs=cs: e.tensor_tensor(kt[d][:, cs], kt[d][:, cs], x2[:, :], op=ALU.mult),
                      [xb2, kt_b[d]], [kt_b[d]])
            for d in range(2):
                z = ztm[d]; zb = ztm_b[d]
                tiles = range(NT) if d == 0 else range(NT - 1, -1, -1)
                first = True
                for n in tiles:
                    vm, vmb = vmr.next()
                    G(lambda e, n=n: e.tensor_tensor(
                        vm[:, :, :], vt[:, n:n + 1, :].to_broadcast([128, 4, 128]),
                        M5[:, :].unsqueeze(2).to_broadcast([128, 4, 128]), op=ALU.mult),
                      [vt_b, kb], [vmb])
                    ps, pb = c.psum.next()
                    PE(lambda e, n=n: e.matmul(ps[:, :], z[:, n, :],
                                               vm[:, :, :].rearrange("p a b -> p (a b)"),
                                               start=True, stop=True), [zb, vmb], [pb])
                    chunks = range(4) if d == 0 else range(3, -1, -1)
                    for jc in chunks:
                        j = n * 4 + jc
                        if first:
                            V(lambda e, j=j: e.memset(St[d][:, j, :], 0.0), [], [St_b[d]])
                            first = False
                        jn_ = j + 1 if d == 0 else j - 1
                        if jn_ < 0 or jn_ > 63:
                            continue
                        V(lambda e, j=j, jn_=jn_, jc=jc: e.scalar_tensor_tensor(
                            out=St[d][:, jn_, :], in0=St[d][:, j, :], scalar=Dd[d][:, j:j + 1],
                            in1=ps[:, jc * 128:(jc + 1) * 128], op0=ALU.mult, op1=ALU.add),
                          [St_b[d], Dd_b[d], pb], [St_b[d]])
            def scores(n):
                res = []
                for d in range(2):
                    ps, pb = c.psum.next()
                    ts = slice(n * 128, (n + 1) * 128)
                    PE(lambda e, d=d, ts=ts: e.matmul(ps[:, 0:128], kt[d][:, ts], qh[d][:, ts],
                                                      start=True, stop=True),
                       [kt_b[d], qh_b[d]], [pb])
                    sm, smb = scm.next()
                    V(lambda e, d=d: e.tensor_tensor(sm[:, :], ps[:, 0:128], Mpre[d][:, :], op=ALU.mult),
                      [pb, kb], [smb])
                    res.append((sm, smb))
                return res

            nxt = scores(0)
            for g4 in range(4):
                pso, pbo = c.psacc.next()
                for jn in range(4):
                    n = g4 * 4 + jn
                    cur = nxt
                    if n + 1 < NT:
                        nxt = scores(n + 1)
                    oc = slice(jn * 128, (jn + 1) * 128)
                    PE(lambda e, n=n, oc=oc: e.matmul(pso[:, oc], vt[:, n, :], cur[0][0][:, :],
                                                      start=True, stop=False), [vt_b, cur[0][1]], [pbo])
                    PE(lambda e, n=n, oc=oc: e.matmul(pso[:, oc], vt[:, n, :], cur[1][0][:, :],
                                                      start=False, stop=False), [vt_b, cur[1][1]], [pbo])
                    for d in range(2):
                        for jc in range(4):
                            j = n * 4 + jc
                            last = (d == 1 and jc == 3)
                            PE(lambda e, d=d, j=j, jc=jc, last=last, jn=jn: e.matmul(
                                pso[:, jn * 128 + jc * 32:jn * 128 + (jc + 1) * 32], St[d][:, j, :],
                                qh[d][:, j * 32:(j + 1) * 32], start=False, stop=last),
                               [St_b[d], qh_b[d]], [pbo])
                cs = slice(g4 * 512, (g4 + 1) * 512)
                s_, sb_ = sq.next()
                A(lambda e: e.activation(s_[:, :], pso[:, :], AF.Square), [pbo], [sb_])
                ps2, pb2 = c.psum.next()
                PE(lambda e: e.matmul(ps2[:, :], ones[:, :], s_[:, :], start=True, stop=True),
                   [sb_, kb], [pb2])
                r_, rb_ = rs.next()
                A(lambda e: e.activation(r_[:, :], ps2[:, :], AF.Sqrt, bias=eps[:, :], scale=1.0 / 128.0),
                  [pb2, kb], [rb_])
                V(lambda e: e.reciprocal(r_[:, :], r_[:, :]), [rb_], [rb_])
                o_, ob_ = ost.next()
                V(lambda e: e.tensor_tensor(o_[:, :], pso[:, :], r_[:, :], op=ALU.mult), [pbo, rb_], [ob_])
                o2_, ob2_ = ostb.next()
                V(lambda e, cs=cs: e.scalar_tensor_tensor(
                    out=o2_[:, :], in0=o_[:, :], scalar=gT[:, hd:hd + 1], in1=og[:, cs],
                    op0=ALU.mult, op1=ALU.mult), [ob_, lbb, og_b], [ob2_])
                t.dma("sp", c.d_ohgT[0][hs, cs], o2_[:, :], reads=[ob2_], writes=[c.d_ohgT[1]])
        t.barrier()


def hy_consts():
    N = 2 * L
    t = np.arange(L, dtype=np.float64)
    ang = 2.0 * np.pi * np.outer(t, t) / N
    A = np.cos(ang)
    B = -np.sin(ang)
    B[:, 0] = (-1.0) ** t
    tl = np.linspace(0.0, 1.0, L, dtype=np.float32)[:, None]
    w = (2.0 * np.float32(math.pi) * np.arange(L, dtype=np.float32)[:, None] / np.float32(L)).astype(np.float32)
    f = np.linspace(1e-4, 15, 16, dtype=np.float32)[None]
    z = np.concatenate([tl, np.cos(f * w), -np.sin(f * w)], axis=-1).astype(np.float32)
    max_decay = math.log(1e-2) / 0.3
    min_decay = math.log(1e-2) / 1.5
    deltas = np.linspace(min_decay, max_decay, 1024, dtype=np.float32)
    win = np.exp(-tl * np.abs(deltas)[None, :]).astype(np.float32)
    return dict(hy_A=A.astype(np.float32), hy_B=B.astype(np.float32),
                hy_BT=np.ascontiguousarray(B.T).astype(np.float32),
                hy_zT=np.ascontiguousarray(z.T), hy_win=win)


def phase_hyena(c, layer):
    t, nc = c.t, c.nc
    V = lambda fn, r=(), w=(): t.op("dve", fn, reads=r, writes=w)
    A = lambda fn, r=(), w=(): t.op("act", fn, reads=r, writes=w)
    G = lambda fn, r=(), w=(): t.op("pool", fn, reads=r, writes=w)
    PE = lambda fn, r=(), w=(): t.op("pe", fn, reads=r, writes=w)
    PI = math.pi
    kA, kB, kBT = c.k["hy_A"], c.k["hy_B"], c.k["hy_BT"]
    with ExitStack() as st0:
        S0 = lambda name, shape, dt=F32: st0.enter_context(nc.sbuf_tensor(_u(name), shape, dt))
        with ExitStack() as st:
            S = lambda name, shape, dt=F32: st.enter_context(nc.sbuf_tensor(_u(name), shape, dt))
            cb = Buf("hyc")
            P = S("hy_P", [128, NT, 1024]); Pb = Buf()
            Q = S("hy_Q", [128, NT, 1024]); Qb = Buf()
            st_in = ExitStack()
            S = lambda name, shape, dt=F32: st_in.enter_context(nc.sbuf_tensor(_u(name), shape, dt))
            zT = S("hy_zT", [33, L]); t.dma("sp", zT[:, :], c.k["hy_zT"][:, :], writes=[cb])
            w1 = S("hy_w1", [33, 64]); t.dma("sp", w1[:, :], c.hy_w1[layer], writes=[cb])
            w2 = S("hy_w2", [64, 2, 64])
            t.dma("sp", w2[:, :, :], c.hy_w2[layer].rearrange("j k m -> k j m"), writes=[cb])
            w3 = S("hy_w3", [64, 2048]); t.dma("sp", w3[:, :], c.hy_w3[layer], writes=[cb])
            fr = S("hy_fr", [64, 4])
            bb = S("hy_bb", [64, 3])
            t.dma("sp", fr[:, 0:1], c.hy_freq[layer].rearrange("(p o) -> p o", o=1), writes=[cb])
            t.dma("sp", bb[:, 0:1], c.hy_b1[layer].rearrange("(p o) -> p o", o=1), writes=[cb])
            for j in range(2):
                t.dma("sp", bb[:, 1 + j:2 + j], c.hy_b2[layer][j].rearrange("(p o) -> p o", o=1), writes=[cb])
            V(lambda e: e.tensor_scalar(fr[:, 1:4], bb[:, 0:3], fr[:, 0:1], None, op0=ALU.mult), [cb], [cb])
            frs = S("hy_frs", [64, 4])
            V(lambda e: e.tensor_scalar(frs[:, 0:1], fr[:, 0:1], 1.0 / (2.0 * PI), None, op0=ALU.mult), [cb], [cb])
            V(lambda e: e.tensor_scalar(frs[:, 1:4], fr[:, 1:4], 1.0 / (2.0 * PI), 8.5, op0=ALU.mult, op1=ALU.add), [cb], [cb])
            qi = S("hy_qi", [64, 512], mybir.dt.int32); qib = Buf()
            qf = S("hy_qf", [64, 512]); qfb = Buf()
            negpi = S("hy_negpi", [64, 1])
            G(lambda e: e.memset(negpi[:, :], -PI), w=[cb])
            hA = S("hy_hA", [64, L]); hAb = Buf()
            hB = S("hy_hB", [64, L]); hBb = Buf()

            def sin_layer(lhsT, src, srcb, dst, dstb, li):
                for tt in range(4):
                    cs = slice(tt * 512, (tt + 1) * 512)
                    ps, pb = c.psum.next()
                    kk = lhsT.shape[0]
                    PE(lambda e: e.matmul(ps[0:64, :], lhsT, src[0:kk, cs], start=True, stop=True),
                       [cb, srcb], [pb])
                    V(lambda e: e.tensor_scalar(dst[:, cs], ps[0:64, :], frs[:, 0:1], frs[:, li:li + 1],
                                                op0=ALU.mult, op1=ALU.add), [pb, cb], [dstb])
                    V(lambda e: e.tensor_copy(qi[:, :], dst[:, cs]), [dstb], [qib])
                    V(lambda e: e.tensor_copy(qf[:, :], qi[:, :]), [qib], [qfb])
                    V(lambda e: e.tensor_tensor(dst[:, cs], dst[:, cs], qf[:, :], op=ALU.subtract), [dstb, qfb], [dstb])
                    V(lambda e: e.scalar_tensor_tensor(out=dst[:, cs], in0=dst[:, cs], scalar=0.0, in1=dst[:, cs],
                                                       op0=ALU.is_lt, op1=ALU.add), [dstb], [dstb])
                    A(lambda e: e.activation(dst[:, cs], dst[:, cs], AF.Sin, bias=negpi[:, :], scale=2.0 * PI),
                      [dstb, cb], [dstb])

            sin_layer(w1[:, :], zT, cb, hA, hAb, 1)
            sin_layer(w2[:, 0, :], hA, hAb, hB, hBb, 2)
            sin_layer(w2[:, 1, :], hB, hBb, hA, hAb, 3)
            winr = Rot([S("hy_win%d" % i, [128, 1024]) for i in range(2)])
            for n in range(NT):
                wn, wnb = winr.next()
                t.dma("sp", wn[:, :], c.k["hy_win"][n * 128:(n + 1) * 128, :], writes=[wnb])
                for q4 in range(4):
                    ps, pb = c.psum.next()
                    PE(lambda e: e.matmul(ps[:, :], hA[:, n * 128:(n + 1) * 128],
                                          w3[:, q4 * 512:(q4 + 1) * 512], start=True, stop=True),
                       [hAb, cb], [pb])
                    dst, dstb = (P, Pb) if q4 < 2 else (Q, Qb)
                    cc = slice((q4 % 2) * 512, (q4 % 2 + 1) * 512)
                    V(lambda e: e.tensor_tensor(dst[:, n, cc], ps[:, :], wn[:, cc], op=ALU.mult),
                      [pb, wnb], [dstb])
            G(lambda e: e.memset(Q[0:1, 0, :], 0.0), [], [Qb])
            t.barrier()
            st_in.close()
            S = lambda name, shape, dt=F32: st.enter_context(nc.sbuf_tensor(_u(name), shape, dt))
            tabA = Rot([S("hy_tA%d" % i, [128, 16, 128]) for i in range(2)])
            tabB = Rot([S("hy_tB%d" % i, [128, 16, 128]) for i in range(2)])
            stg = Rot([S("hy_stg%d" % i, [128, 512]) for i in range(4)])
            for n in range(NT):
                eng = V if n % 2 == 0 else G
                eng(lambda e: e.tensor_tensor(P[:, n, :], P[:, n, :], Q[:, n, :], op=ALU.subtract), [Pb, Qb], [Pb])
                V(lambda e: e.scalar_tensor_tensor(out=Q[:, n, :], in0=Q[:, n, :], scalar=2.0, in1=P[:, n, :],
                                                     op0=ALU.mult, op1=ALU.add), [Pb, Qb], [Qb])
            for fc in range(16):
                ta, tab_ = tabA.next(); tb, tbb_ = tabB.next()
                t.dma("sp", ta[:, :, :], kA[:, fc * 128:(fc + 1) * 128].rearrange("(k p) f -> p k f", p=128), writes=[tab_])
                t.dma("sp", tb[:, :, :], kB[:, fc * 128:(fc + 1) * 128].rearrange("(k p) f -> p k f", p=128), writes=[tbb_])
                for hh in range(2):
                    cc = slice(hh * 512, (hh + 1) * 512)
                    ps, pb = c.psum.next()
                    for k in range(16):
                        PE(lambda e: e.matmul(ps[:, :], ta[:, k, :], Q[:, k, cc], start=(k == 0), stop=(k == 15)),
                           [tab_, Qb], [pb])
                    s1, s1b = stg.next()
                    A(lambda e: e.copy(s1[:, :], ps[:, :]), [pb], [s1b])
                    t.dma("sp", c.d_Kr[0][fc * 128:(fc + 1) * 128, cc], s1[:, :], reads=[s1b], writes=[c.d_Kr[1]])
                    ps, pb = c.psum.next()
                    for k in range(16):
                        PE(lambda e: e.matmul(ps[:, :], tb[:, k, :], P[:, k, cc], start=(k == 0), stop=(k == 15)),
                           [tbb_, Pb], [pb])
                    s2, s2b = stg.next()
                    V(lambda e: e.tensor_copy(s2[:, :], ps[:, :]), [pb], [s2b])
                    if fc == 0:
                        ps3, pb3 = c.psum.next()
                        for k in range(16):
                            PE(lambda e: e.matmul(ps3[:, :], tb[:, k, :], Q[:, k, cc], start=(k == 0), stop=(k == 15)),
                               [tbb_, Qb], [pb3])
                        V(lambda e: e.tensor_copy(s2[0:1, :], ps3[0:1, :]), [pb3, s2b], [s2b])
                    t.dma("sp", c.d_Ki[0][fc * 128:(fc + 1) * 128, cc], s2[:, :], reads=[s2b], writes=[c.d_Ki[1]])
            t.barrier()
        with ExitStack() as st:
            S = lambda name, shape, dt=F32: st.enter_context(nc.sbuf_tensor(_u(name), shape, dt))
            cb = Buf("hyc2")
            cw = S("hy_cw", [128, 3, 24])
            for k in range(3):
                load_cols(c, cw[:, k, :], cb, c.hy_conv_w[layer][k], 24)
            cbias = S("hy_cb", [128, 24]); load_cols(c, cbias, cb, c.hy_conv_b[layer], 24)
            skip = S("hy_skip", [128, 8]); load_cols(c, skip, cb, c.hy_skip[layer], 8)
            utm = S("hy_utm", [128, NT, 1024]); utmb = Buf()
            tabA = Rot([S("hy_tA%d" % i, [128, 16, 128]) for i in range(2)])
            tabB = Rot([S("hy_tB%d" % i, [128, 16, 128]) for i in range(2)])
            stg = Rot([S("hy_stg%d" % i, [128, 512]) for i in range(4)])
            raw = Rot([S("hy_raw%d" % i, [128, L]) for i in range(2)])
            cv = Rot([S("hy_cv%d" % i, [128, L]) for i in range(3)])

            def conv(chunk):
                r, rb = raw.next()
                t.dma("sp", r[:, :], c.d_hyT[0][chunk * 128:(chunk + 1) * 128, :], reads=[c.d_hyT[1]], writes=[rb])
                y, yb = cv.next()
                V(lambda e: e.tensor_scalar(y[:, :], r[:, :], cw[:, 1, chunk:chunk + 1], cbias[:, chunk:chunk + 1],
                                            op0=ALU.mult, op1=ALU.add), [rb, cb], [yb])
                V(lambda e: e.scalar_tensor_tensor(out=y[:, 1:L], in0=r[:, 0:L - 1], scalar=cw[:, 0, chunk:chunk + 1],
                                                   in1=y[:, 1:L], op0=ALU.mult, op1=ALU.add), [rb, cb, yb], [yb])
                V(lambda e: e.scalar_tensor_tensor(out=y[:, 0:L - 1], in0=r[:, 1:L], scalar=cw[:, 2, chunk:chunk + 1],
                                                   in1=y[:, 0:L - 1], op0=ALU.mult, op1=ALU.add), [rb, cb, yb], [yb])
                return y, yb

            for ch in range(8):
                x0, x0b = conv(ch)
                t.dma("sp", c.d_x0c[0][ch * 128:(ch + 1) * 128, :], x0[:, :], reads=[x0b], writes=[c.d_x0c[1]])
                x1, x1b = conv(8 + ch)
                vv, vvb = conv(16 + ch)
                G(lambda e: e.tensor_tensor(x1[:, :], x1[:, :], vv[:, :], op=ALU.mult), [x1b, vvb], [x1b])
                t.dma("sp", c.d_uT[0][ch * 128:(ch + 1) * 128, :], x1[:, :], reads=[x1b], writes=[c.d_uT[1]])
                for g4 in range(4):
                    ps, pb = c.psum.next()
                    for jn in range(4):
                        n = g4 * 4 + jn
                        PE(lambda e: e.transpose(ps[:, jn * 128:(jn + 1) * 128], x1[:, n * 128:(n + 1) * 128],
                                                 c.ident[:, :]), [x1b, c.ident_buf], [pb])
                    evac(c, utm[:, g4 * 4:(g4 + 1) * 4, ch * 128:(ch + 1) * 128],
                         ps[:, :].rearrange("p (a b) -> p a b", a=4), [pb], [utmb])
            kr = Rot([S("hy_kr%d" % i, [128, 1024]) for i in range(2)])
            ki = Rot([S("hy_ki%d" % i, [128, 1024]) for i in range(2)])
            tmp = Rot([S("hy_tmp%d" % i, [128, 512]) for i in range(4)])
            for fc in range(16):
                ta, tab_ = tabA.next(); tb, tbb_ = tabB.next()
                t.dma("sp", ta[:, :, :], kA[:, fc * 128:(fc + 1) * 128].rearrange("(k p) f -> p k f", p=128), writes=[tab_])
                t.dma("sp", tb[:, :, :], kB[:, fc * 128:(fc + 1) * 128].rearrange("(k p) f -> p k f", p=128), writes=[tbb_])
                krt, krb = kr.next(); kit, kib = ki.next()
                t.dma("sp", krt[:, :], c.d_Kr[0][fc * 128:(fc + 1) * 128, :], reads=[c.d_Kr[1]], writes=[krb])
                t.dma("sp", kit[:, :], c.d_Ki[0][fc * 128:(fc + 1) * 128, :], reads=[c.d_Ki[1]], writes=[kib])
                for hh in range(2):
                    cc = slice(hh * 512, (hh + 1) * 512)
                    psr, pbr = c.psum.next()
                    for k in range(16):
                        PE(lambda e: e.matmul(psr[:, :], ta[:, k, :], utm[:, k, cc], start=(k == 0), stop=(k == 15)),
                           [tab_, utmb], [pbr])
                    psi, pbi = c.psum.next()
                    for k in range(16):
                        PE(lambda e: e.matmul(psi[:, :], tb[:, k, :], utm[:, k, cc], start=(k == 0), stop=(k == 15)),
                           [tbb_, utmb], [pbi])
                    ur, urb = tmp.next(); ui, uib = tmp.next()
                    A(lambda e: e.copy(ur[:, :], psr[:, :]), [pbr], [urb])
                    A(lambda e: e.copy(ui[:, :], psi[:, :]), [pbi], [uib])
                    yr, yrb = stg.next(); yi, yib = stg.next()
                    t1, t1b = tmp.next(); t2, t2b = tmp.next()
                    V(lambda e: e.tensor_tensor(yr[:, :], ur[:, :], krt[:, cc], op=ALU.mult), [urb, krb], [yrb])
                    G(lambda e: e.tensor_tensor(t1[:, :], ui[:, :], kit[:, cc], op=ALU.mult), [uib, kib], [t1b])
                    V(lambda e: e.tensor_tensor(yi[:, :], ur[:, :], kit[:, cc], op=ALU.mult), [urb, kib], [yib])
                    G(lambda e: e.tensor_tensor(t2[:, :], ui[:, :], krt[:, cc], op=ALU.mult), [uib, krb], [t2b])
                    V(lambda e: e.tensor_tensor(yr[:, :], yr[:, :], t1[:, :], op=ALU.subtract), [yrb, t1b], [yrb])
                    V(lambda e: e.tensor_tensor(yi[:, :], yi[:, :], t2[:, :], op=ALU.add), [yib, t2b], [yib])
                    if fc == 0:
                        V(lambda e: e.scalar_tensor_tensor(out=yr[0:1, :], in0=ur[0:1, :], scalar=0.5, in1=krt[0:1, cc],
                                                           op0=ALU.mult, op1=ALU.mult), [urb, krb, yrb], [yrb])
                        V(lambda e: e.scalar_tensor_tensor(out=yi[0:1, :], in0=ui[0:1, :], scalar=0.5, in1=kit[0:1, cc],
                                                           op0=ALU.mult, op1=ALU.mult), [uib, kib, yib], [yib])
                    t.dma("sp", c.d_Yr[0][fc * 128:(fc + 1) * 128, cc], yr[:, :], reads=[yrb], writes=[c.d_Yr[1]])
                    t.dma("sp", c.d_Yi[0][fc * 128:(fc + 1) * 128, cc], yi[:, :], reads=[yib], writes=[c.d_Yi[1]])
            t.barrier()
        with ExitStack() as st:
            S = lambda name, shape, dt=F32: st.enter_context(nc.sbuf_tensor(_u(name), shape, dt))
            cb = Buf("hyc3")
            skip = S("hy_skip2", [128, 8]); load_cols(c, skip, cb, c.hy_skip[layer], 8)
            itA = Rot([S("hy_itA%d" % i, [128, 16, 512]) for i in range(2)])
            itB = Rot([S("hy_itB%d" % i, [128, 16, 512]) for i in range(2)])
            yrr = Rot([S("hy_yr%d" % i, [128, 16, 128]) for i in range(2)])
            yir = Rot([S("hy_yi%d" % i, [128, 16, 128]) for i in range(2)])
            usr = Rot([S("hy_us%d" % i, [128, L]) for i in range(2)])
            x0r = Rot([S("hy_x0%d" % i, [128, L]) for i in range(2)])
            outr = Rot([S("hy_out%d" % i, [128, 512]) for i in range(3)])
            outb = Rot([S("hy_outb%d" % i, [128, 512], BF16) for i in range(3)])
            for ch in range(8):
                rows = slice(ch * 128, (ch + 1) * 128)
                yrt, yrb = yrr.next(); yit, yib = yir.next()
                t.dma("sp", yrt[:, :, :], c.d_Yr[0][:, rows].rearrange("(k p) m -> p k m", p=128), reads=[c.d_Yr[1]], writes=[yrb])
                t.dma("sp", yit[:, :, :], c.d_Yi[0][:, rows].rearrange("(k p) m -> p k m", p=128), reads=[c.d_Yi[1]], writes=[yib])
                us, usb = usr.next(); x0, x0b = x0r.next()
                t.dma("sp", us[:, :], c.d_uT[0][rows, :], reads=[c.d_uT[1]], writes=[usb])
                t.dma("sp", x0[:, :], c.d_x0c[0][rows, :], reads=[c.d_x0c[1]], writes=[x0b])
                G(lambda e: e.tensor_scalar(us[:, :], us[:, :], skip[:, ch:ch + 1], None, op0=ALU.mult), [usb, cb], [usb])
                for tt in range(4):
                    cs = slice(tt * 512, (tt + 1) * 512)
                    ia, iab = itA.next(); ib, ibb = itB.next()
                    t.dma("sp", ia[:, :, :], kA[:, cs].rearrange("(k p) q -> p k q", p=128), writes=[iab])
                    t.dma("sp", ib[:, :, :], kBT[:, cs].rearrange("(k p) q -> p k q", p=128), writes=[ibb])
                    ps, pb = c.psum.next()
                    for k in range(16):
                        PE(lambda e: e.matmul(ps[:, :], yrt[:, k, :], ia[:, k, :], start=(k == 0), stop=False),
                           [yrb, iab], [pb])
                    for k in range(16):
                        PE(lambda e: e.matmul(ps[:, :], yit[:, k, :], ib[:, k, :], start=False, stop=(k == 15)),
                           [yib, ibb], [pb])
                    o_, ob_ = outr.next()
                    V(lambda e: e.scalar_tensor_tensor(out=o_[:, :], in0=ps[:, :], scalar=2.0 / (2 * L), in1=us[:, cs],
                                                       op0=ALU.mult, op1=ALU.add), [pb, usb], [ob_])
                    o2_, ob2_ = outb.next()
                    G(lambda e: e.tensor_tensor(o2_[:, :], o_[:, :], x0[:, cs], op=ALU.mult), [ob_, x0b], [ob2_])
                    t.dma("sp", c.d_ohyT[0][rows, cs], o2_[:, :], reads=[ob2_], writes=[c.d_ohyT[1]])
            t.barrier()


def att_consts():
    W = 128
    kofs = np.arange(3 * W)[None, :] - W
    rel = kofs - np.arange(W)[:, None]
    half, max_exact = 16, 8
    bucket = (rel > 0).astype(np.int32) * half
    n = np.abs(rel)
    n_safe = np.maximum(n, 1).astype(np.float32)
    large = max_exact + (np.log(n_safe / np.float32(max_exact)) / np.float32(math.log(128 / max_exact))
                         * np.float32(half - max_exact)).astype(np.int32)
    large = np.clip(large, 0, half - 1)
    bucket = bucket + np.where(n < max_exact, n, large)
    onehot = (bucket[None, :, :] == np.arange(32)[:, None, None]).astype(np.float32)
    maskadd = np.where(np.abs(rel) <= 128, 0.0, -1e30).astype(np.float32)
    return dict(at_onehot=onehot, at_mask=maskadd)


def phase_attn(c, layer):
    t, nc = c.t, c.nc
    V = lambda fn, r=(), w=(): t.op("dve", fn, reads=r, writes=w)
    A = lambda fn, r=(), w=(): t.op("act", fn, reads=r, writes=w)
    G = lambda fn, r=(), w=(): t.op("pool", fn, reads=r, writes=w)
    PE = lambda fn, r=(), w=(): t.op("pe", fn, reads=r, writes=w)
    with ExitStack() as st:
        S = lambda name, shape, dt=F32: st.enter_context(nc.sbuf_tensor(_u(name), shape, dt))
        cb = Buf("atc")
        biasM = S("at_bias", [128, 16, 384])
        rbb = S("at_rbb", [128, 512])
        t.dma("sp", rbb[:, :], c.rel_bias.rearrange("b h -> (b h)").partition_broadcast(128), writes=[cb])
        sinkb = S("at_sink", [128, 16])
        t.dma("sp", sinkb[:, :], c.att_sink[layer].partition_broadcast(128), writes=[cb])
        mk = S("at_mk", [128, 384])
        t.dma("sp", mk[:, :], c.k["at_mask"][:, :], writes=[cb])
        V(lambda e: e.tensor_copy(biasM[:, :, :], mk[:, :].unsqueeze(1).to_broadcast([128, 16, 384])), [cb], [cb])
        ohr = Rot([S("at_oh%d" % i, [128, 384]) for i in range(2)])
        for b in range(32):
            oh, ohb = ohr.next()
            t.dma("sp", oh[:, :], c.k["at_onehot"][b], writes=[ohb])
            for h in range(16):
                V(lambda e: e.scalar_tensor_tensor(out=biasM[:, h, :], in0=oh[:, :], scalar=rbb[:, b * 16 + h:b * 16 + h + 1],
                                                   in1=biasM[:, h, :], op0=ALU.mult, op1=ALU.add), [ohb, cb], [cb])
        kd = []
        for g in range(2):
            k_ = S("at_kd%d" % g, [128, L])
            for half in range(2):
                t.dma("sp", k_[half * 64:(half + 1) * 64, :], c.d_akT[0][g * 64:(g + 1) * 64, :],
                      reads=[c.d_akT[1]], writes=[cb])
            kd.append(k_)
        vx = {}
        for g in range(2):
            for hh in range(2):
                v_ = S("at_v%d%d" % (g, hh), [128, NT, 128])
                G(lambda e: e.memset(v_[:, :, :], 0.0), [], [cb])
                t.dma("sp", v_[:, :, hh * 64:(hh + 1) * 64],
                      c.d_av[0][:, g * 64:(g + 1) * 64].rearrange("(n p) d -> p n d", p=128),
                      reads=[c.d_av[1]], writes=[cb])
                vx[(g, hh)] = v_
        qr = Rot([S("at_q%d" % i, [128, L]) for i in range(2)])
        otr = Rot([S("at_o%d" % i, [128, L], BF16) for i in range(2)])
        sr = Rot([S("at_s%d" % i, [128, 384]) for i in range(3)])
        pr = Rot([S("at_p%d" % i, [128, 384]) for i in range(3)])
        ptr = Rot([S("at_pt%d" % i, [128, 384]) for i in range(3)])
        smr = Rot([S("at_sm%d" % i, [128, 8]) for i in range(4)])
        for j in range(8):
            q, qb = qr.next()
            t.dma("sp", q[:, :], c.d_aqT[0][j * 128:(j + 1) * 128, :], reads=[c.d_aqT[1]], writes=[qb])
            ot, otb = otr.next()
            for n in range(NT):
                klo = max(0, (n - 1) * 128); khi = min(L, (n + 2) * 128)
                bo = klo - (n - 1) * 128; wdt = khi - klo
                nkb = wdt // 128
                oacc, oab = c.psacc.next()
                for hh in range(2):
                    h = 2 * j + hh; g = h // 8; po = hh * 64
                    ps, pb = c.psum.next()
                    PE(lambda e: e.matmul(ps[:, 0:wdt], q[po:po + 64, n * 128:(n + 1) * 128],
                                          kd[g][po:po + 64, klo:khi], start=True, stop=True), [qb, cb], [pb])
                    s_, sb_ = sr.next()
                    V(lambda e: e.scalar_tensor_tensor(out=s_[:, 0:wdt], in0=ps[:, 0:wdt], scalar=0.125,
                                                       in1=biasM[:, h, bo:bo + wdt], op0=ALU.mult, op1=ALU.add),
                      [pb, cb], [sb_])
                    sm, smb = smr.next()
                    V(lambda e: e.reduce_max(sm[:, 0:1], s_[:, 0:wdt], axis=AX.X), [sb_], [smb])
                    V(lambda e: e.tensor_tensor(sm[:, 0:1], sm[:, 0:1], sinkb[:, h:h + 1], op=ALU.max), [smb, cb], [smb])
                    V(lambda e: e.tensor_scalar(sm[:, 1:2], sm[:, 0:1], -1.0, None, op0=ALU.mult), [smb], [smb])
                    p_, pb_ = pr.next()
                    A(lambda e: e.activation(p_[:, 0:wdt], s_[:, 0:wdt], AF.Exp, bias=sm[:, 1:2], scale=1.0,
                                             accum_out=sm[:, 2:3]), [sb_, smb], [pb_, smb])
                    A(lambda e: e.activation(sm[:, 3:4], sinkb[:, h:h + 1], AF.Exp, bias=sm[:, 1:2], scale=1.0),
                      [smb, cb], [smb])
                    V(lambda e: e.tensor_tensor(sm[:, 4:5], sm[:, 2:3], sm[:, 3:4], op=ALU.add), [smb], [smb])
                    V(lambda e: e.reciprocal(sm[:, 5:6], sm[:, 4:5]), [smb], [smb])
                    G(lambda e: e.tensor_scalar(p_[:, 0:wdt], p_[:, 0:wdt], sm[:, 5:6], None, op0=ALU.mult),
                      [pb_, smb], [pb_])
                    ps2, pb2 = c.psum.next()
                    for kb in range(nkb):
                        PE(lambda e: e.transpose(ps2[:, kb * 128:(kb + 1) * 128], p_[:, kb * 128:(kb + 1) * 128],
                                                 c.ident[:, :]), [pb_, c.ident_buf], [pb2])
                    pt, ptb = ptr.next()
                    A(lambda e: e.copy(pt[:, 0:wdt], ps2[:, 0:wdt]), [pb2], [ptb])
                    for kb in range(nkb):
                        kt_ = klo // 128 + kb
                        PE(lambda e: e.matmul(oacc[:, 0:128], vx[(g, hh)][:, kt_, :], pt[:, kb * 128:(kb + 1) * 128],
                                              start=(hh == 0 and kb == 0), stop=(hh == 1 and kb == nkb - 1)),
                           [cb, ptb], [oab])
                evac(c, ot[:, n * 128:(n + 1) * 128], oacc[:, 0:128], [oab], [otb])
            t.dma("sp", c.d_oatT[0][j * 128:(j + 1) * 128, :], ot[:, :], reads=[otb], writes=[c.d_oatT[1]])
        t.barrier()


def phase_merge(c, layer):
    t, nc = c.t, c.nc
    V = lambda fn, r=(), w=(): t.op("dve", fn, reads=r, writes=w)
    G = lambda fn, r=(), w=(): t.op("pool", fn, reads=r, writes=w)
    PE = lambda fn, r=(), w=(): t.op("pe", fn, reads=r, writes=w)
    with ExitStack() as st:
        S = lambda name, shape, dt=F32: st.enter_context(nc.sbuf_tensor(_u(name), shape, dt))
        otr = Rot([S("mg_o%d" % i, [128, 8, L], BF16) for i in range(2)])
        acc = S("mg_acc", [128, 4, L]); accb = Buf()
        accbf = Rot([S("mg_accb%d" % i, [128, L], BF16) for i in range(2)])
        gr = Rot([S("mg_g%d" % i, [128, L]) for i in range(2)])
        wrot = WPool(c, S, "mg_w", 8, 512)
        tmpr = Rot([S("mg_t%d" % i, [128, 512]) for i in range(3)])
        srcs = (c.d_ohgT, c.d_ohyT, c.d_oatT)
        for blk in range(4):
            for n in range(3):
                wt, wb = wrot.load(c.w_branch[layer][n][:, blk * 512:(blk + 1) * 512], 8, 512)
                o_, ob_ = otr.next()
                t.dma("sp", o_[:, :, :], srcs[n][0].rearrange("(k p) q -> p k q", p=128),
                      reads=[srcs[n][1]], writes=[ob_])
                for cg in range(4):
                    dg = blk * 4 + cg
                    g_, gb_ = gr.next()
                    t.dma("sp", g_[:, :], c.d_gT[0][n * D + dg * 128:n * D + (dg + 1) * 128, :],
                          reads=[c.d_gT[1]], writes=[gb_])
                    for tt in range(4):
                        cs = slice(tt * 512, (tt + 1) * 512)
                        ps, pb = c.psum.next()
                        for kc in range(8):
                            PE(lambda e: e.matmul(ps[:, :], wt[:, kc, cg * 128:(cg + 1) * 128], o_[:, kc, cs],
                                                  start=(kc == 0), stop=(kc == 7)), [wb, ob_], [pb])
                        if n == 0:
                            V(lambda e: e.tensor_tensor(acc[:, cg, cs], ps[:, :], g_[:, cs], op=ALU.mult),
                              [pb, gb_], [accb])
                        else:
                            tm_, tmb_ = tmpr.next()
                            V(lambda e: e.tensor_tensor(tm_[:, :], ps[:, :], g_[:, cs], op=ALU.mult),
                              [pb, gb_], [tmb_])
                            G(lambda e: e.tensor_tensor(acc[:, cg, cs], acc[:, cg, cs], tm_[:, :], op=ALU.add),
                              [accb, tmb_], [accb])
                    if n == 2:
                        ab_, abb_ = accbf.next()
                        t.op("act", lambda e: e.copy(ab_[:, :], acc[:, cg, :]), reads=[accb], writes=[abb_])
                        t.dma("sp", c.d_yT[0][dg * 128:(dg + 1) * 128, :], ab_[:, :], reads=[abb_],
                              writes=[c.d_yT[1]])
        t.barrier()
    with ExitStack() as st:
        S = lambda name, shape, dt=F32: st.enter_context(nc.sbuf_tensor(_u(name), shape, dt))
        yT = S("mg_yT", [128, KC, L], BF16); yTb = Buf()
        t.dma("sp", yT[:, :, :], c.d_yT[0].rearrange("(k p) q -> p k q", p=128), reads=[c.d_yT[1]], writes=[yTb])
        wrot = WPool(c, S, "mg_wo", KC, 512)
        stg_tm = Rot([S("mg_s%d" % i, [128, 512]) for i in range(3)])

        def epi2(ps, pb, tt, cc0, cw):
            s, sb = stg_tm.next()
            evac(c, s[:, 0:cw], ps[:, 0:cw], [pb], [sb])
            t.dma("sp", c.d_mix[0][tt * 128:(tt + 1) * 128, cc0:cc0 + cw], s[:, 0:cw],
                  reads=[sb], writes=[c.d_mix[1]])

        gemm_tm(c, c.w_out[layer], D, yT, yTb, KC, L, epi2, wrot)
        t.barrier()


SIG7 = 1.0 / (1.0 + math.exp(-1.702 * 7.0))


def phase_moe(c, layer):
    t, nc = c.t, c.nc
    V = lambda fn, r=(), w=(): t.op("dve", fn, reads=r, writes=w)
    A = lambda fn, r=(), w=(): t.op("act", fn, reads=r, writes=w)
    G = lambda fn, r=(), w=(): t.op("pool", fn, reads=r, writes=w)
    PE = lambda fn, r=(), w=(): t.op("pe", fn, reads=r, writes=w)
    HT = L // 2
    NTH = HT // 128
    with ExitStack() as st:
        S = lambda name, shape, dt=F32: st.enter_context(nc.sbuf_tensor(_u(name), shape, dt))
        gs_ = Rot([S("me_g%d" % i, [128, 512]) for i in range(2)])
        sg_ = Rot([S("me_s%d" % i, [128, 512]) for i in range(2)])
        us_ = Rot([S("me_u%d" % i, [128, 512]) for i in range(2)])
        xh = S("me_x", [128, KC, HT], BF16); xhb = Buf()
        yacc = S("me_y", [128, NTH, D]); yab = Buf()
        act2 = S("me_a", [128, KC * HT], BF16); actb = Buf()
        actT = act2[:, :].rearrange("p (k q) -> p k q", k=KC)
        bd = S("me_bd", [N_EXP, D]); bdb = Buf()
        gateT = S("me_gT", [N_EXP, HT]); gateTb = Buf()
        wstage = Rot([S("me_ws%d" % i, [128, KC, 256]) for i in range(2)])
        wbf = Rot([S("me_wb%d" % i, [128, KC, 256], BF16) for i in range(2)])
        gate = S("me_gate", [128, NTH, N_EXP]); gateb = Buf()
        c7 = S("me_c7", [128, 1])
        G(lambda e: e.memset(c7[:, :], 7.0), [], [gateb])
        bgu = Rot([S("me_bgu%d" % i, [128, 32]) for i in range(2)])
        bgs = Rot([S("me_bgs%d" % i, [128, 16]) for i in range(2)])
        items = []
        for ex in range(c.moe_nexp):
            items += [(ex, "gu", fc) for fc in range(16)]
            items += [(ex, "dn", db) for db in range(8)]
        import os as _os2
        items = items[:int(_os2.environ.get("MOE_ITEMS", len(items)))]

        def load_item(it):
            ex, kind, j = it
            stg, stgb = wstage.next()
            if kind == "gu":
                for two in range(2):
                    src = c.w_gate_up[layer][ex][:, two * DFF + j * 128:two * DFF + (j + 1) * 128].rearrange(
                        "(kc p) c -> p kc c", p=128)
                    t.dma("sp", stg[:, :, two * 128:(two + 1) * 128], src, writes=[stgb])
            else:
                src = c.w_down[layer][ex][:, j * 256:(j + 1) * 256].rearrange("(kc p) c -> p kc c", p=128)
                t.dma("sp", stg[:, :, :], src, writes=[stgb])
            bf, bfb = wbf.next()
            G(lambda e: e.tensor_copy(bf[:, :, :], stg[:, :, :]), [stgb], [bfb])
            return bf, bfb

        for hf in range(2):
            t0 = hf * HT
            t.dma("sp", xh[:, :, :], c.d_xT[0][:, t0:t0 + HT].rearrange("(k p) q -> p k q", p=128),
                  reads=[c.d_xT[1]], writes=[xhb])
            t.dma("sp", gate[:, :, :], c.d_gate[0][t0:t0 + HT, :].rearrange("(n p) e -> p n e", p=128),
                  reads=[c.d_gate[1]], writes=[gateb])
            t.dma("sp", gateT[:, :], c.d_gateT[0][:, t0:t0 + HT], reads=[c.d_gateT[1]], writes=[gateTb])
            t.dma("sp", bd[:, :], c.b_down[layer], writes=[bdb])
            import os as _os
            _lv = int(_os.environ.get("MOE_INIT", 9))
            if _lv == 0:
                t.barrier(); return
            for tt in range(NTH if _lv in (2, 9) else 1):
                for cb4 in range(4):
                    ps, pb = c.psum.next()
                    PE(lambda e: e.matmul(ps[:, :], gateT[:, tt * 128:(tt + 1) * 128], bd[:, cb4 * 512:(cb4 + 1) * 512],
                                          start=True, stop=True), [gateTb, bdb], [pb])
                    if _lv != 3:
                        evac(c, yacc[:, tt, cb4 * 512:(cb4 + 1) * 512], ps[:, :], [pb], [yab])
            if _lv in (3, 4):
                t.barrier(); return
            nxt = load_item(items[0])
            bg = bgb = bs = bsb = None
            for ii, (ex, kind, j) in enumerate(items):
                wt, wtb = nxt
                if ii + 1 < len(items):
                    nxt = load_item(items[ii + 1])
                if kind == "gu":
                    fc = j
                    if fc == 0:
                        bg, bgb = bgu.next()
                        load_cols(c, bg, bgb, c.b_gate_up[layer][ex], 32)
                        bs, bsb = bgs.next()
                        V(lambda e: e.tensor_tensor(bs[:, :], bg[:, 0:16], c7[:, 0:1].to_broadcast([128, 16]), op=ALU.mult),
                          [bgb, gateb], [bsb])
                        V(lambda e: e.tensor_scalar(bs[:, :], bs[:, :], 1.702 / 7.0, None, op0=ALU.mult), [bsb], [bsb])
                    wv = wt[:, :, :].rearrange("p k (two f) -> p k two f", two=2)
                    for tt in range(HT // 512):
                        cs = slice(tt * 512, (tt + 1) * 512)
                        psg, pbg = c.psum.next()
                        for kc in range(KC):
                            PE(lambda e: e.matmul(psg[:, :], wv[:, kc, 0, :], xh[:, kc, cs],
                                                  start=(kc == 0), stop=(kc == KC - 1)), [wtb, xhb], [pbg])
                        psu, pbu = c.psum.next()
                        for kc in range(KC):
                            PE(lambda e: e.matmul(psu[:, :], wv[:, kc, 1, :], xh[:, kc, cs],
                                                  start=(kc == 0), stop=(kc == KC - 1)), [wtb, xhb], [pbu])
                        g1, g1b = gs_.next(); s1, s1b = sg_.next(); u1, u1b = us_.next()
                        A(lambda e: e.activation(s1[:, :], psg[:, :], AF.Sigmoid, bias=bs[:, fc:fc + 1], scale=1.702),
                          [pbg, bsb], [s1b])
                        if c.moe_epi == 1:
                            continue
                        if c.moe_epi == 24:
                            V(lambda e: e.tensor_copy(g1[:, :], psu[:, :]), [pbu], [g1b])
                            continue
                        if c.moe_epi == 25:
                            A(lambda e: e.copy(g1[:, :], psu[:, :]), [pbu], [g1b])
                            continue
                        if c.moe_epi == 26:
                            V(lambda e: e.tensor_copy(g1[:, :], psg[:, :]), [pbg, s1b], [g1b])
                            continue
                        if c.moe_epi == 21:
                            V(lambda e: e.tensor_copy(g1[:, :], psg[:, :]), [pbg], [g1b])
                            continue
                        if c.moe_epi == 7:
                            A(lambda e: e.activation(g1[:, :], psg[:, :], AF.Identity, bias=bg[:, fc:fc + 1], scale=1.0),
                              [pbg, bgb], [g1b])
                            A(lambda e: e.activation(u1[:, :], psu[:, :], AF.Identity, bias=bg[:, 16 + fc:17 + fc], scale=1.0),
                              [pbu, bgb], [u1b])
                            V(lambda e: e.tensor_scalar(g1[:, :], g1[:, :], 7.0, None, op0=ALU.min), [g1b], [g1b])
                            V(lambda e: e.tensor_scalar(u1[:, :], u1[:, :], 7.0, None, op0=ALU.min), [u1b], [u1b])
                        else:
                            V(lambda e: e.tensor_scalar(g1[:, :], psg[:, :], bg[:, fc:fc + 1], c7[:, 0:1], op0=ALU.add, op1=ALU.min),
                              [pbg, bgb, gateb], [g1b])
                            V(lambda e: e.tensor_scalar(u1[:, :], psu[:, :], bg[:, 16 + fc:17 + fc], c7[:, 0:1], op0=ALU.add, op1=ALU.min),
                              [pbu, bgb, gateb], [u1b])
                        V(lambda e: e.scalar_tensor_tensor(out=g1[:, :], in0=s1[:, :], scalar=float(SIG7), in1=g1[:, :],
                                                           op0=ALU.min, op1=ALU.mult), [s1b, g1b], [g1b])
                        V(lambda e: e.tensor_scalar(u1[:, :], u1[:, :], -7.0, 1.0, op0=ALU.max, op1=ALU.add), [u1b], [u1b])
                        V(lambda e: e.tensor_tensor(actT[:, fc, cs], g1[:, :], u1[:, :], op=ALU.mult), [g1b, u1b], [actb])
                else:
                    db = j
                    for tt in range(NTH):
                        ps, pb = c.psum.next()
                        for kc in range(KC):
                            PE(lambda e: e.matmul(ps[:, 0:256], actT[:, kc, tt * 128:(tt + 1) * 128], wt[:, kc, :],
                                                  start=(kc == 0), stop=(kc == KC - 1)), [actb, wtb], [pb])
                        V(lambda e: e.scalar_tensor_tensor(
                            out=yacc[:, tt, db * 256:(db + 1) * 256], in0=ps[:, 0:256], scalar=gate[:, tt, ex:ex + 1],
                            in1=yacc[:, tt, db * 256:(db + 1) * 256], op0=ALU.mult, op1=ALU.add),
                          [pb, gateb, yab], [yab])
            t.dma("sp", c.d_ffn[0][t0:t0 + HT, :].rearrange("(n p) d -> p n d", p=128), yacc[:, :, :],
                  reads=[yab], writes=[c.d_ffn[1]])
        t.barrier()


def relayout_gu(w):
    e = w.shape[0]
    v = w.reshape(e, KC, 128, 2, 16, 128)
    v = v.transpose(0, 4, 2, 1, 3, 5)
    return np.ascontiguousarray(v).reshape(e, 16, 128, KC * 256)


def relayout_dn(w):
    e = w.shape[0]
    v = w.reshape(e, KC, 128, 8, 256)
    v = v.transpose(0, 3, 2, 1, 4)
    return np.ascontiguousarray(v).reshape(e, 8, 128, KC * 256)


def kernel(**inputs):
    n = 8
    nc = build()
    consts = host_consts()
    shared = {}
    for name in LAST_INPUT_NAMES:
        if name == "x":
            continue
        if name.startswith("k_"):
            shared[name] = consts[name[2:]]
        elif name.startswith("w_gate_up"):
            shared[name] = relayout_gu(np.asarray(inputs["w_gate_up"], dtype=np.float32)[int(name[-1])])
        elif name.startswith("w_down"):
            shared[name] = relayout_dn(np.asarray(inputs["w_down"], dtype=np.float32)[int(name[-1])])
        else:
            shared[name] = np.ascontiguousarray(np.asarray(inputs[name], dtype=np.float32))
    x = np.asarray(inputs["x"], dtype=np.float32)
    in_maps = []
    for b in range(n):
        m = dict(shared)
        m["x"] = np.ascontiguousarray(x[b])
        in_maps.append(m)
    res = run_bass_kernel_spmd(nc, in_maps, core_ids=list(range(n)))
    return np.stack([np.asarray(res.results[b]["out"], dtype=np.float32) for b in range(n)], axis=0)


CAP = 256


def moe_consts():
    s = np.arange(128)
    ustrict = (s[:, None] < s[None, :]).astype(np.float32)
    sele = np.zeros((N_EXP, N_EXP, 128), np.float32)
    for e in range(N_EXP):
        sele[e, e, :] = 1.0
    iota_c = np.tile(np.arange(CAP, dtype=np.float32)[None, :], (128, 1))
    pidx = (np.arange(128, dtype=np.float32)[:, None] + 128.0 * np.arange(CAP // 128, dtype=np.float32)[None, :])
    return dict(me_ustrict=ustrict, me_sele=sele, me_iota=iota_c, me_pidx=np.ascontiguousarray(pidx))


def phase_moe_sparse(c, layer):
    t, nc = c.t, c.nc
    V = lambda fn, r=(), w=(): t.op("dve", fn, reads=r, writes=w)
    A = lambda fn, r=(), w=(): t.op("act", fn, reads=r, writes=w)
    G = lambda fn, r=(), w=(): t.op("pool", fn, reads=r, writes=w)
    PE = lambda fn, r=(), w=(): t.op("pe", fn, reads=r, writes=w)
    HT = L // 2
    NTH = HT // 128
    NCC = CAP // 128
    with ExitStack() as st:
        S = lambda name, shape, dt=F32: st.enter_context(nc.sbuf_tensor(_u(name), shape, dt))
        gs_ = Rot([S("ms_g%d" % i, [128, CAP]) for i in range(2)])
        sg_ = Rot([S("ms_s%d" % i, [128, CAP]) for i in range(2)])
        us_ = Rot([S("ms_u%d" % i, [128, CAP]) for i in range(2)])
        xtm = S("ms_x", [128, NTH, D], BF16); xtmb = Buf()
        yacc = S("ms_y", [128, NTH, D]); yab = Buf()
        xeT = S("ms_xe", [128, KC, CAP], BF16); xeb = Buf()
        actT = S("ms_a", [128, KC, CAP], BF16); actb = Buf()
        ye = S("ms_ye", [128, NCC, D], BF16); yeb = Buf()
        selr = Rot([S("ms_sel%d" % i, [128, NTH, CAP], BF16) for i in range(2)])
        seltr = Rot([S("ms_selT%d" % i, [128, NCC, HT], BF16) for i in range(2)])
        wstage = Rot([S("ms_ws%d" % i, [128, KC, 256]) for i in range(2)])
        wbf = Rot([S("ms_wb%d" % i, [128, KC, 256], BF16) for i in range(2)])
        wbf_x = {id(b): (Buf(), Buf()) for b in wbf.bufs}
        gate = S("ms_gate", [128, NTH, N_EXP]); gateb = Buf()
        rk = S("ms_rk", [128, NTH, N_EXP]); rkb = Buf()
        msk = S("ms_msk", [128, NTH, N_EXP]); mskb = Buf()
        rkT = S("ms_rkT", [N_EXP, HT]); rkTb = Buf()
        cb = Buf("msc")
        iota_c = S("ms_iota", [128, CAP]); t.dma("sp", iota_c[:, :], c.k["me_iota"][:, :], writes=[cb])
        pidx = S("ms_pidx", [128, NCC]); t.dma("sp", pidx[:, :], c.k["me_pidx"][:, :], writes=[cb])
        ustr = S("ms_us", [128, 128]); t.dma("sp", ustr[:, :], c.k["me_ustrict"][:, :], writes=[cb])
        ones = S("ms_ones", [128, 128]); G(lambda e: e.memset(ones[:, :], 1.0), [], [cb])
        c7 = S("ms_c7", [128, 1]); G(lambda e: e.memset(c7[:, :], 7.0), [], [cb])
        seler = Rot([S("ms_sele%d" % i, [N_EXP, 128]) for i in range(2)])
        bgu = Rot([S("ms_bgu%d" % i, [128, 32]) for i in range(2)])
        bgs = Rot([S("ms_bgs%d" % i, [128, 16]) for i in range(2)])
        items = []
        for ex in range(c.moe_nexp):
            items += [(ex, "gu", fc) for fc in range(16)]
            items += [(ex, "dn", db) for db in range(8)]

        def load_item(it):
            ex, kind, j = it
            stg, stgb = wstage.next()
            src = (c.w_gate_up if kind == "gu" else c.w_down)[layer][ex, j]
            t.dma("sp", stg[:, :, :].rearrange("p k c -> p (k c)"), src, writes=[stgb])
            bf, bfb = wbf.next()
            b2, b3 = wbf_x[id(bfb)]
            A(lambda e: e.copy(bf[:, 0:9, :], stg[:, 0:9, :]), [stgb], [bfb])
            V(lambda e: e.tensor_copy(bf[:, 9:14, :], stg[:, 9:14, :]), [stgb], [b2])
            G(lambda e: e.tensor_copy(bf[:, 14:16, :], stg[:, 14:16, :]), [stgb], [b3])
            return bf, [bfb, b2, b3]

        for hf in range(2):
            t0 = hf * HT
            t.dma("sp", xtm[:, :, :], c.d_xtm[0][t0:t0 + HT, :].rearrange("(n p) d -> p n d", p=128),
                  reads=[c.d_xtm[1]], writes=[xtmb])
            t.dma("sp", gate[:, :, :], c.d_gate[0][t0:t0 + HT, :].rearrange("(n p) e -> p n e", p=128),
                  reads=[c.d_gate[1]], writes=[gateb])
            (sa, sab), (sb_, sbb) = wstage.next(), wstage.next()
            bd = sa[0:N_EXP, 0:8, :].rearrange("p k c -> p (k c)")
            gateT = sb_[0:N_EXP, 0:4, :].rearrange("p k c -> p (k c)")
            t.dma("sp", gateT, c.d_gateT[0][:, t0:t0 + HT], reads=[c.d_gateT[1]], writes=[sbb])
            t.dma("sp", bd, c.b_down[layer], writes=[sab])
            for tt in range(NTH):
                for cb4 in range(4):
                    ps, pb = c.psum.next()
                    PE(lambda e: e.matmul(ps[:, :], gateT[:, tt * 128:(tt + 1) * 128], bd[:, cb4 * 512:(cb4 + 1) * 512],
                                          start=True, stop=True), [sab, sbb], [pb])
                    evac(c, yacc[:, tt, cb4 * 512:(cb4 + 1) * 512], ps[:, :], [pb], [yab])
            V(lambda e: e.tensor_scalar(msk[:, :, :], gate[:, :, :], 0.0, None, op0=ALU.is_gt), [gateb], [mskb])
            for n in range(NTH):
                ps, pb = c.psum.next()
                for m in range(n):
                    PE(lambda e: e.matmul(ps[:, 0:N_EXP], ones[:, :], msk[:, m, :], start=(m == 0), stop=False),
                       [cb, mskb], [pb])
                PE(lambda e: e.matmul(ps[:, 0:N_EXP], ustr[:, :], msk[:, n, :], start=(n == 0), stop=True),
                   [cb, mskb], [pb])
                V(lambda e: e.tensor_tensor(rk[:, n, :], ps[:, 0:N_EXP], msk[:, n, :], op=ALU.mult), [pb, mskb], [rkb])
                V(lambda e: e.tensor_tensor(rk[:, n, :], rk[:, n, :], msk[:, n, :], op=ALU.add), [rkb, mskb], [rkb])
                V(lambda e: e.tensor_scalar(rk[:, n, :], rk[:, n, :], -1.0, None, op0=ALU.add), [rkb], [rkb])
                ps2, pb2 = c.psum.next()
                PE(lambda e: e.transpose(ps2[0:N_EXP, 0:128], rk[:, n, :], c.ident[:, :]), [rkb, c.ident_buf], [pb2])
                A(lambda e: e.copy(rkT[:, n * 128:(n + 1) * 128], ps2[0:N_EXP, 0:128]), [pb2], [rkTb])
            nxt = load_item(items[0])
            bg = bgb = bs = bsb = None
            sel = selb = selT = selTb = None
            for ii, (ex, kind, j) in enumerate(items):
                wt, wtb = nxt
                if ii + 1 < len(items):
                    nxt = load_item(items[ii + 1])
                if kind == "gu":
                    fc = j
                    if fc == 0:
                        bg, bgb = bgu.next()
                        load_cols(c, bg, bgb, c.b_gate_up[layer][ex], 32)
                        bs, bsb = bgs.next()
                        V(lambda e: e.tensor_scalar(bs[:, :], bg[:, 0:16], 1.702, None, op0=ALU.mult), [bgb], [bsb])
                        sel, selb = selr.next()
                        for n in range(NTH):
                            V(lambda e: e.tensor_scalar(sel[:, n, :], iota_c[:, :], rk[:, n, ex:ex + 1], None,
                                                        op0=ALU.is_equal), [cb, rkb], [selb])
                        se, seb = seler.next()
                        t.dma("sp", se[:, :], c.k["me_sele"][ex], writes=[seb])
                        selT, selTb = seltr.next()
                        for th in range(HT // 512):
                            psb, pbb = c.psum.next()
                            PE(lambda e: e.matmul(psb[:, :], se[:, :], rkT[:, th * 512:(th + 1) * 512], start=True, stop=True),
                               [seb, rkTb], [pbb])
                            for cc in range(NCC):
                                V(lambda e: e.tensor_scalar(selT[:, cc, th * 512:(th + 1) * 512], psb[:, :], pidx[:, cc:cc + 1],
                                                            None, op0=ALU.is_equal), [pbb, cb], [selTb])
                        for kc in range(KC):
                            psx, pbx = c.psum.next()
                            for n in range(NTH):
                                PE(lambda e: e.matmul(psx[:, 0:CAP], xtm[:, n, kc * 128:(kc + 1) * 128], sel[:, n, :],
                                                      start=(n == 0), stop=(n == NTH - 1)), [xtmb, selb], [pbx])
                            evac(c, xeT[:, kc, :], psx[:, 0:CAP], [pbx], [xeb])
                    psg, pbg = c.psum.next()
                    for kc in range(KC):
                        PE(lambda e: e.matmul(psg[:, 0:CAP], wt[:, kc, 0:128], xeT[:, kc, :],
                                              start=(kc == 0), stop=(kc == KC - 1)), wtb + [xeb], [pbg])
                    psu, pbu = c.psum.next()
                    for kc in range(KC):
                        PE(lambda e: e.matmul(psu[:, 0:CAP], wt[:, kc, 128:256], xeT[:, kc, :],
                                              start=(kc == 0), stop=(kc == KC - 1)), wtb + [xeb], [pbu])
                    g1, g1b = gs_.next(); s1, s1b = sg_.next(); u1, u1b = us_.next()
                    A(lambda e: e.activation(s1[:, :], psg[:, 0:CAP], AF.Sigmoid, bias=bs[:, fc:fc + 1], scale=1.702),
                      [pbg, bsb], [s1b])
                    V(lambda e: e.tensor_scalar(g1[:, :], psg[:, 0:CAP], bg[:, fc:fc + 1], c7[:, 0:1], op0=ALU.add, op1=ALU.min),
                      [pbg, bgb, cb], [g1b])
                    V(lambda e: e.tensor_scalar(u1[:, :], psu[:, 0:CAP], bg[:, 16 + fc:17 + fc], c7[:, 0:1], op0=ALU.add, op1=ALU.min),
                      [pbu, bgb, cb], [u1b])
                    V(lambda e: e.scalar_tensor_tensor(out=g1[:, :], in0=s1[:, :], scalar=float(SIG7), in1=g1[:, :],
                                                       op0=ALU.min, op1=ALU.mult), [s1b, g1b], [g1b])
                    V(lambda e: e.tensor_scalar(u1[:, :], u1[:, :], -7.0, 1.0, op0=ALU.max, op1=ALU.add), [u1b], [u1b])
                    V(lambda e: e.tensor_tensor(actT[:, fc, :], g1[:, :], u1[:, :], op=ALU.mult), [g1b, u1b], [actb])
                else:
                    db = j
                    for cc in range(NCC):
                        ps, pb = c.psum.next()
                        for kc in range(KC):
                            PE(lambda e: e.matmul(ps[:, 0:256], actT[:, kc, cc * 128:(cc + 1) * 128], wt[:, kc, :],
                                                  start=(kc == 0), stop=(kc == KC - 1)), [actb] + wtb, [pb])
                        A(lambda e: e.copy(ye[:, cc, db * 256:(db + 1) * 256], ps[:, 0:256]), [pb], [yeb])
                    if db == 7:
                        for n in range(NTH):
                            for b4 in range(4):
                                ps, pb = c.psum.next()
                                for cc in range(NCC):
                                    PE(lambda e: e.matmul(ps[:, :], selT[:, cc, n * 128:(n + 1) * 128],
                                                          ye[:, cc, b4 * 512:(b4 + 1) * 512],
                                                          start=(cc == 0), stop=(cc == NCC - 1)), [selTb, yeb], [pb])
                                V(lambda e: e.scalar_tensor_tensor(
                                    out=yacc[:, n, b4 * 512:(b4 + 1) * 512], in0=ps[:, :], scalar=gate[:, n, ex:ex + 1],
                                    in1=yacc[:, n, b4 * 512:(b4 + 1) * 512], op0=ALU.mult, op1=ALU.add),
                                  [pb, gateb, yab], [yab])
            t.dma("sp", c.d_ffn[0][t0:t0 + HT, :].rearrange("(n p) d -> p n d", p=128), yacc[:, :, :],
                  reads=[yab], writes=[c.d_ffn[1]])
        t.barrier()
```

```python
import math
from contextlib import ExitStack
import numpy as np
import concourse.bass as bass
import concourse.mybir as mybir
from concourse.bass_utils import run_bass_kernel_spmd

F32 = mybir.dt.float32
BF16 = mybir.dt.bfloat16
AF = mybir.ActivationFunctionType
ALU = mybir.AluOpType
AX = mybir.AxisListType

D = 2048
L = 2048
DEPTH = 2
IN_COLS = 15616
NT = L // 128
KC = D // 128
LN_EPS = 1e-5
ALPHA = (2 * DEPTH) ** 0.25
N_EXP = 32
DFF = 2048

C_HQ, C_HFF, C_HFB, C_HI, C_HOG, C_HY, C_AQ, C_AK, C_AV, C_G = (
    0, 1024, 2048, 3072, 4096, 5120, 8192, 9216, 9344, 9472)


class Buf:
    __slots__ = ("name", "w", "r", "excl")

    def __init__(self, name="", excl=False):
        self.name = name
        self.w = None
        self.r = {}
        self.excl = excl


class Trk:
    NDS = 24

    def __init__(self, nc, stack):
        self.nc = nc
        self.E = {"pe": nc.tensor, "dve": nc.vector, "act": nc.scalar, "pool": nc.gpsimd,
                  "sp": nc.sync}
        self.sem = {}
        self.cnt = {}
        for e in ("pe", "dve", "act", "pool"):
            self.sem[e] = stack.enter_context(nc.semaphore("s_" + e))
            self.cnt[e] = 0
        for i in range(self.NDS):
            self.sem[("d", i)] = stack.enter_context(nc.semaphore("d%d" % i))
            self.cnt[("d", i)] = 0
        self.seen = {e: {} for e in self.E}
        self.dnext = 0
        self.n_ins = 0
        self.n_wait = 0

    def _need(self, reads, writes, e=None):
        need = {}
        for b in reads:
            if b.w is not None:
                k, v = b.w
                if need.get(k, 0) < v:
                    need[k] = v
            if b.excl:
                for k, v in b.r.items():
                    if k != e and need.get(k, 0) < v:
                        need[k] = v
        for b in writes:
            if b.w is not None:
                k, v = b.w
                if need.get(k, 0) < v:
                    need[k] = v
            for k, v in b.r.items():
                if need.get(k, 0) < v:
                    need[k] = v
        return need

    def _wait(self, e, need):
        seen = self.seen[e]
        for k, v in need.items():
            if k == e and e == "pe":
                continue
            if seen.get(k, 0) >= v:
                continue
            self.E[e].wait_ge(self.sem[k], v)
            seen[k] = v
            self.n_wait += 1

    def _mark(self, ev, reads, writes):
        k, v = ev
        for b in reads:
            if b.r.get(k, 0) < v:
                b.r[k] = v
        for b in writes:
            b.w = ev
            b.r = {}

    def op(self, e, fn, reads=(), writes=()):
        self._wait(e, self._need(reads, writes, e))
        self.cnt[e] += 1
        ins = fn(self.E[e])
        ins.then_inc(self.sem[e], 1)
        self._mark((e, self.cnt[e]), reads, writes)
        self.n_ins += 1
        return ins

    def dma(self, q, out, in_, reads=(), writes=()):
        need = self._need(reads, writes)
        i = self.dnext
        self.dnext = (self.dnext + 1) % self.NDS
        k = ("d", i)
        if self.cnt[k] > 0:
            need[k] = max(need.get(k, 0), self.cnt[k])
        self._wait(q, need)
        ins = self.E[q].dma_start(out=out, in_=in_)
        self.cnt[k] += 16
        ins.then_inc(self.sem[k], 16)
        self._mark((k, self.cnt[k]), reads, writes)
        self.n_ins += 1
        return ins

    def barrier(self):
        need = {k: v for k, v in self.cnt.items() if v > 0}
        for e in self.E:
            self._wait(e, dict(need))

    def drain(self, e, bufs):
        self._wait(e, self._need((), bufs))


class Rot:
    def __init__(self, tiles, excl=False):
        self.tiles = tiles
        self.bufs = [Buf(excl=excl) for _ in tiles]
        self.i = 0

    def next(self):
        i = self.i
        self.i = (i + 1) % len(self.tiles)
        return self.tiles[i], self.bufs[i]


class Ctx:
    pass


_uc = [0]


def _u(name):
    _uc[0] += 1
    return "%s_%d" % (name, _uc[0])


LAST_INPUT_NAMES = set()
LAST_TRK = None


class WPool:
    def __init__(self, c, S, name, kc, ncols, n_stage=2, n_bf=2):
        self.c = c
        self.stage = Rot([S("%s_st%d" % (name, i), [128, kc, ncols], F32) for i in range(n_stage)])
        self.bf = Rot([S("%s_bf%d" % (name, i), [128, kc, ncols], BF16) for i in range(n_bf)])

    def load(self, w_ap, kc, ncols):
        t = self.c.t
        st, stb = self.stage.next()
        t.dma("sp", st[:, 0:kc, 0:ncols], w_ap.rearrange("(kc p) c -> p kc c", p=128), writes=[stb])
        bf, bfb = self.bf.next()
        t.op("pool", lambda e: e.tensor_copy(bf[:, 0:kc, 0:ncols], st[:, 0:kc, 0:ncols]), reads=[stb], writes=[bfb])
        return bf, bfb


def gemm_fm(c, w_ap, n_cols, xT, xT_buf, k_chunks, n_tok, epilogue, wp, blk=512):
    t = c.t
    blocks = [(c0, min(blk, n_cols - c0)) for c0 in range(0, n_cols, blk)]
    nxt = wp.load(w_ap[:, blocks[0][0]:blocks[0][0] + blocks[0][1]], k_chunks, blocks[0][1])
    for bi, (c0, cw) in enumerate(blocks):
        wt, wb = nxt
        if bi + 1 < len(blocks):
            n0, nw = blocks[bi + 1]
            nxt = wp.load(w_ap[:, n0:n0 + nw], k_chunks, nw)
        for cg in range(cw // 128):
            for tt in range(n_tok // 512):
                ps, pb = c.psum.next()
                for kc in range(k_chunks):
                    t.op("pe", lambda e, kc=kc: e.matmul(
                        ps[:, :], wt[:, kc, cg * 128:(cg + 1) * 128],
                        xT[:, kc, tt * 512:(tt + 1) * 512],
                        start=(kc == 0), stop=(kc == k_chunks - 1)),
                        reads=[wb, xT_buf], writes=[pb])
                epilogue(ps, pb, (c0 // 128) + cg, tt)


def gemm_tm(c, w_ap, n_cols, xT, xT_buf, k_chunks, n_tok, epilogue, wp, blk=512):
    t = c.t
    blocks = [(c0, min(blk, n_cols - c0)) for c0 in range(0, n_cols, blk)]
    nxt = wp.load(w_ap[:, blocks[0][0]:blocks[0][0] + blocks[0][1]], k_chunks, blocks[0][1])
    for bi, (c0, cw) in enumerate(blocks):
        wt, wb = nxt
        if bi + 1 < len(blocks):
            n0, nw = blocks[bi + 1]
            nxt = wp.load(w_ap[:, n0:n0 + nw], k_chunks, nw)
        for tt in range(n_tok // 128):
            ps, pb = c.psum.next()
            for kc in range(k_chunks):
                t.op("pe", lambda e, kc=kc: e.matmul(
                    ps[:, 0:cw], xT[:, kc, tt * 128:(tt + 1) * 128], wt[:, kc, 0:cw],
                    start=(kc == 0), stop=(kc == k_chunks - 1)),
                    reads=[wb, xT_buf], writes=[pb])
            epilogue(ps, pb, tt, c0, cw)


_evac_flip = [0]


def evac(c, out_ap, in_ap, reads, writes):
    _evac_flip[0] ^= 1
    if _evac_flip[0]:
        c.t.op("act", lambda e: e.copy(out_ap, in_ap), reads=reads, writes=writes)
    else:
        c.t.op("dve", lambda e: e.tensor_copy(out_ap, in_ap), reads=reads, writes=writes)


def phase_ln(c, src, res, g_ap, b_ap, h_out, xT_out, final_out=None, router=None):
    t, nc = c.t, c.nc
    V = lambda fn, r=(), w=(): t.op("dve", fn, reads=r, writes=w)
    A = lambda fn, r=(), w=(): t.op("act", fn, reads=r, writes=w)
    PE = lambda fn, r=(), w=(): t.op("pe", fn, reads=r, writes=w)
    src_ap, src_buf = src
    res_ap, res_buf = res if res is not None else (None, None)
    h_out_ap, h_out_buf = h_out if h_out is not None else (None, None)
    with ExitStack() as st:
        S = lambda name, shape, dt=F32: st.enter_context(nc.sbuf_tensor(_u(name), shape, dt))
        gt = S("ln_g", [128, D]); gb_ = Buf()
        bt = S("ln_b", [128, D]); bb_ = Buf()
        t.dma("sp", gt[:, :], g_ap.partition_broadcast(128), writes=[gb_])
        t.dma("sp", bt[:, :], b_ap.partition_broadcast(128), writes=[bb_])
        xin = Rot([S("ln_x%d" % i, [128, D]) for i in range(2)])
        rin = Rot([S("ln_r%d" % i, [128, D]) for i in range(2)])
        yo = Rot([S("ln_y%d" % i, [128, D]) for i in range(2)])
        xts = Rot([S("ln_xt%d" % i, [128, 4, 128]) for i in range(5)])
        xtb = Rot([S("ln_xb%d" % i, [128, 4, 128], BF16) for i in range(3)])
        stats = S("ln_st", [128, 4 * 6]); sb_ = Buf()
        mv = S("ln_mv", [128, 2]); mvb = Buf()
        rstd = S("ln_rstd", [128, 1]); rsb = Buf()
        if router is not None:
            rw = S("ln_rw", [128, KC, N_EXP]); rwb = Buf()
            t.dma("sp", rw[:, :, :], c.router_w[router].rearrange("(k p) e -> p k e", p=128), writes=[rwb])
            rbias = S("ln_rb", [128, N_EXP])
            t.dma("sp", rbias[:, :], c.router_b[router].partition_broadcast(128), writes=[rwb])
            lg = Rot([S("ln_lg%d" % i, [128, N_EXP]) for i in range(2)])
            ex_ = Rot([S("ln_ex%d" % i, [128, N_EXP]) for i in range(2)])
            m8 = Rot([S("ln_m8%d" % i, [128, 12]) for i in range(2)])
            gT_ = Rot([S("ln_gT%d" % i, [N_EXP, 128]) for i in range(2)])
            ybf = Rot([S("ln_ybf%d" % i, [128, D], BF16) for i in range(2)])
        for tt in range(NT):
            rows = slice(tt * 128, (tt + 1) * 128)
            x, xb = xin.next()
            t.dma("sp", x[:, :], src_ap[rows, :], reads=[src_buf], writes=[xb])
            if res_ap is not None:
                r, rb = rin.next()
                t.dma("sp", r[:, :], res_ap[rows, :], reads=[res_buf], writes=[rb])
                V(lambda e: e.scalar_tensor_tensor(out=x[:, :], in0=r[:, :], scalar=float(ALPHA), in1=x[:, :],
                                                   op0=ALU.mult, op1=ALU.add), [rb, xb], [xb])
            for j in range(4):
                V(lambda e: e.bn_stats(stats[:, j * 6:(j + 1) * 6], x[:, j * 512:(j + 1) * 512]), [xb], [sb_])
            V(lambda e: e.bn_aggr(mv[:, :], stats[:, :]), [sb_], [mvb])
            A(lambda e: e.activation(rstd[:, :], mv[:, 1:2], AF.Sqrt, bias=c.eps_ln[:, :], scale=1.0),
              [mvb, c.cbuf], [rsb])
            V(lambda e: e.reciprocal(rstd[:, :], rstd[:, :]), [rsb], [rsb])
            y, yb = yo.next()
            V(lambda e: e.tensor_scalar(y[:, :], x[:, :], mv[:, 0:1], rstd[:, 0:1], op0=ALU.subtract, op1=ALU.mult),
              [xb, mvb, rsb], [yb])
            t.op("pool", lambda e: e.tensor_tensor(y[:, :], y[:, :], gt[:, :], op=ALU.mult), reads=[yb, gb_], writes=[yb])
            t.op("pool", lambda e: e.tensor_tensor(y[:, :], y[:, :], bt[:, :], op=ALU.add), reads=[yb, bb_], writes=[yb])
            if h_out_ap is not None:
                t.dma("sp", h_out_ap[rows, :], y[:, :], reads=[yb], writes=[h_out_buf])
            if router is not None:
                yb16, yb16b = ybf.next()
                A(lambda e: e.copy(yb16[:, :], y[:, :]), [yb], [yb16b])
                t.dma("sp", c.d_xtm[0][rows, :], yb16[:, :], reads=[yb16b], writes=[c.d_xtm[1]])
            if final_out is not None:
                t.dma("sp", final_out[rows, :], y[:, :], reads=[yb], writes=[c.out_buf])
            if xT_out is not None:
                if router is not None:
                    pl, plb = c.psacc.next()
                for g4 in range(KC // 4):
                    ps, pb = c.psum.next()
                    for j in range(4):
                        fc = g4 * 4 + j
                        PE(lambda e: e.transpose(ps[:, j * 128:(j + 1) * 128], y[:, fc * 128:(fc + 1) * 128],
                                                 c.ident[:, :]), [yb, c.ident_buf], [pb])
                    xb_, xbb_ = xtb.next()
                    A(lambda e: e.copy(xb_[:, :, :], ps[:, :].rearrange("p (j q) -> p j q", j=4)), [pb], [xbb_])
                    t.dma("sp", xT_out[0][g4 * 512:(g4 + 1) * 512, rows].rearrange("(j p) q -> p j q", p=128),
                          xb_[:, :, :], reads=[xbb_], writes=[xT_out[1]])
                    if router is not None:
                        xs, xsb = xts.next()
                        V(lambda e: e.tensor_copy(xs[:, :, :], ps[:, :].rearrange("p (j q) -> p j q", j=4)), [pb], [xsb])
                    if router is not None:
                        for j in range(4):
                            fc = g4 * 4 + j
                            PE(lambda e: e.matmul(pl[:, 0:N_EXP], xs[:, j, :], rw[:, fc, :],
                                                  start=(fc == 0), stop=(fc == KC - 1)), [xsb, rwb], [plb])
                if router is not None:
                    l_, lb_ = lg.next()
                    V(lambda e: e.tensor_tensor(l_[:, :], pl[:, 0:N_EXP], rbias[:, :], op=ALU.add), [plb, rwb], [lb_])
                    m_, mb_ = m8.next()
                    V(lambda e: e.max(m_[:, 0:8], l_[:, :]), [lb_], [mb_])
                    V(lambda e: e.tensor_scalar(m_[:, 8:9], m_[:, 0:1], -1.0, None, op0=ALU.mult), [mb_], [mb_])
                    e_, eb_ = ex_.next()
                    A(lambda e: e.activation(e_[:, :], l_[:, :], AF.Exp, bias=m_[:, 8:9], scale=1.0), [lb_, mb_], [eb_])
                    V(lambda e: e.scalar_tensor_tensor(out=e_[:, :], in0=l_[:, :], scalar=m_[:, 3:4], in1=e_[:, :],
                                                       op0=ALU.is_ge, op1=ALU.mult), [lb_, mb_, eb_], [eb_])
                    V(lambda e: e.reduce_sum(m_[:, 9:10], e_[:, :], axis=AX.X), [eb_], [mb_])
                    V(lambda e: e.reciprocal(m_[:, 10:11], m_[:, 9:10]), [mb_], [mb_])
                    V(lambda e: e.tensor_scalar(e_[:, :], e_[:, :], m_[:, 10:11], None, op0=ALU.mult), [eb_, mb_], [eb_])
                    t.dma("sp", c.d_gate[0][rows, :], e_[:, :], reads=[eb_], writes=[c.d_gate[1]])
                    ps, pb = c.psum.next()
                    PE(lambda e: e.transpose(ps[0:N_EXP, 0:128], e_[:, :], c.ident[:, :]), [eb_, c.ident_buf], [pb])
                    g_, gb2 = gT_.next()
                    A(lambda e: e.copy(g_[:, :], ps[0:N_EXP, 0:128]), [pb], [gb2])
                    t.dma("sp", c.d_gateT[0][:, rows], g_[:, :], reads=[gb2], writes=[c.d_gateT[1]])
        t.barrier()


def phase_proj(c, layer, only=None):
    t, nc = c.t, c.nc
    w = c.w_in[layer]
    with ExitStack() as st:
        S = lambda name, shape, dt=F32: st.enter_context(nc.sbuf_tensor(_u(name), shape, dt))
        hT = S("hT", [128, KC, L], BF16); hT_buf = Buf("hT")
        t.dma("sp", hT[:, :, :], c.d_xT[0].rearrange("(k p) q -> p k q", p=128), reads=[c.d_xT[1]], writes=[hT_buf])
        wrot = WPool(c, S, "pj_w", KC, 512)
        stg = Rot([S("pj_s%d" % i, [128, L]) for i in range(2)])
        stg_tm = Rot([S("pj_t%d" % i, [128, 512]) for i in range(3)])
        fm = [(C_HQ, 1024, c.d_hqT), (C_HFF, 1024, c.d_hffT), (C_HFB, 1024, c.d_hfbT),
              (C_HOG, 1024, c.d_hogT), (C_HY, 3072, c.d_hyT), (C_AQ, 1024, c.d_aqT),
              (C_AK, 128, c.d_akT)]
        if only is not None:
            fm = fm[only[0]:only[1]]
        for c0, n, (dst, dbuf) in fm:
            cur = {}

            def epi(ps, pb, g, tt, dst=dst, dbuf=dbuf, cur=cur):
                if tt == 0:
                    cur["s"] = stg.next()
                s, sb = cur["s"]
                evac(c, s[:, tt * 512:(tt + 1) * 512], ps[:, :], [pb], [sb])
                if tt == L // 512 - 1:
                    t.dma("sp", dst[g * 128:(g + 1) * 128, :], s[:, :], reads=[sb], writes=[dbuf])

            gemm_fm(c, w[:, c0:c0 + n], n, hT, hT_buf, KC, L, epi, wrot)
        if only is None or len(only) > 4:
            cur = {}

            def epig(ps, pb, g, tt, cur=cur):
                if tt == 0:
                    cur["s"] = stg.next()
                s, sb = cur["s"]
                t.op("act", lambda e: e.activation(s[:, tt * 512:(tt + 1) * 512], ps[:, :], AF.Sigmoid),
                     reads=[pb], writes=[sb])
                if tt == L // 512 - 1:
                    t.dma("sp", c.d_gT[0][g * 128:(g + 1) * 128, :], s[:, :], reads=[sb],
                          writes=[c.d_gT[1]])

            gemm_fm(c, w[:, C_G:C_G + 3 * D], 3 * D, hT, hT_buf, KC, L, epig, wrot)
        tm = [(C_HFF, 2048, c.d_hf), (C_HI, 1024, c.d_hi), (C_AV, 128, c.d_av)]
        if only is not None:
            tm = tm[only[2]:only[3]]
        for c0, n, (dst, dbuf) in tm:
            def epi2(ps, pb, tt, cc0, cw, dst=dst, dbuf=dbuf):
                s, sb = stg_tm.next()
                evac(c, s[:, 0:cw], ps[:, 0:cw], [pb], [sb])
                t.dma("sp", dst[tt * 128:(tt + 1) * 128, cc0:cc0 + cw], s[:, 0:cw],
                      reads=[sb], writes=[dbuf])

            gemm_tm(c, w[:, c0:c0 + n], n, hT, hT_buf, KC, L, epi2, wrot)
        t.barrier()


def host_consts():
    cs = {}
    cs["ident"] = np.eye(128, dtype=np.float32)
    M1, M2, M3, M4, M5 = hg_masks()
    cs["hg_M1"], cs["hg_M2"], cs["hg_M3"], cs["hg_M4"], cs["hg_M5"] = M1, M2, M3, M4, M5
    cs.update(hy_consts())
    cs.update(att_consts())
    cs.update(moe_consts())
    return cs


def build(layer_list=(0, 1), stop=None, dbg=(), only=None):
    nc = bass.Bass("TRN2", target_bir_lowering=False)
    c = Ctx()
    c.nc = nc
    import os as _os
    c.moe_nexp = int(_os.environ.get("MOE_NEXP", N_EXP))
    c.moe_stop = _os.environ.get("MOE_STOP", "")
    c.moe_dense = _os.environ.get("MOE_DENSE", "0") == "1"
    c.moe_epi = int(_os.environ.get("MOE_EPI", 6))
    INPUT_NAMES = []

    def ein(name, shape):
        INPUT_NAMES.append(name)
        return nc.dram_tensor(name, list(shape), F32, kind="ExternalInput").ap()
    c.x = ein("x", [L, D])
    c.ln_in_g = ein("ln_in_g", [D]); c.ln_in_b = ein("ln_in_b", [D])
    c.w_in = ein("w_in", [DEPTH, D, IN_COLS])
    c.hg_lb = ein("hg_lower_bound", [DEPTH, 2048])
    c.hg_norm_g = ein("hg_norm_g", [DEPTH, 1024])
    c.hy_conv_w = ein("hy_conv_w", [DEPTH, 3, 3072]); c.hy_conv_b = ein("hy_conv_b", [DEPTH, 3072])
    c.hy_w1 = ein("hy_filt_w1", [DEPTH, 33, 64]); c.hy_b1 = ein("hy_filt_b1", [DEPTH, 64])
    c.hy_w2 = ein("hy_filt_w2", [DEPTH, 2, 64, 64]); c.hy_b2 = ein("hy_filt_b2", [DEPTH, 2, 64])
    c.hy_freq = ein("hy_filt_freq", [DEPTH, 64]); c.hy_w3 = ein("hy_filt_w3", [DEPTH, 64, 2048])
    c.hy_skip = ein("hy_skip", [DEPTH, 1024])
    c.att_sink = ein("att_sink", [DEPTH, 16]); c.rel_bias = ein("rel_bias", [32, 16])
    c.w_branch = ein("w_branch", [DEPTH, 3, 1024, D]); c.w_out = ein("w_out", [DEPTH, D, D])
    c.ln_mix_g = ein("ln_mix_g", [DEPTH, D]); c.ln_mix_b = ein("ln_mix_b", [DEPTH, D])
    c.router_w = ein("router_w", [DEPTH, D, N_EXP]); c.router_b = ein("router_b", [DEPTH, N_EXP])
    c.w_gate_up = [ein("w_gate_up%d" % l, [c.moe_nexp, 16, 128, KC * 256]) if l in layer_list else None for l in range(DEPTH)]
    c.w_down = [ein("w_down%d" % l, [c.moe_nexp, 8, 128, KC * 256]) if l in layer_list else None for l in range(DEPTH)]
    c.b_gate_up = ein("b_gate_up", [DEPTH, N_EXP, 2 * DFF]); c.b_down = ein("b_down", [DEPTH, N_EXP, D])
    c.ln_moe_g = ein("ln_moe_g", [DEPTH, D]); c.ln_moe_b = ein("ln_moe_b", [DEPTH, D])
    consts = host_consts()
    global LAST_INPUT_NAMES
    c.k = {k: ein("k_" + k, v.shape) for k, v in consts.items()}
    LAST_INPUT_NAMES = set(INPUT_NAMES)
    c.out = nc.dram_tensor("out", [L, D], F32, kind="ExternalOutput").ap()
    c.out_buf = Buf()

    def scratch(name, shape, dt=F32):
        kind = "ExternalOutput" if name in dbg else "Internal"
        return (nc.dram_tensor(name, list(shape), dt, kind=kind).ap(), Buf(name))

    c.d_h = scratch("d_h", [L, D]); c.d_h1 = scratch("d_h1", [L, D])
    c.d_gT = scratch("d_gT", [3 * D, L])
    c.d_hqT = scratch("d_hqT", [1024, L]); c.d_hffT = scratch("d_hffT", [1024, L])
    c.d_hfbT = scratch("d_hfbT", [1024, L]); c.d_hogT = scratch("d_hogT", [1024, L])
    c.d_hyT = scratch("d_hyT", [3072, L]); c.d_aqT = scratch("d_aqT", [1024, L])
    c.d_akT = scratch("d_akT", [128, L])
    c.d_hf = scratch("d_hf", [L, 2048]); c.d_hi = scratch("d_hi", [L, 1024])
    c.d_av = scratch("d_av", [L, 128])
    c.d_ohgT = scratch("d_ohgT", [1024, L], BF16)
    c.d_ohyT = scratch("d_ohyT", [1024, L], BF16)
    c.d_oatT = scratch("d_oatT", [1024, L], BF16)
    c.d_mix = scratch("d_mix", [L, D])
    c.d_xT = scratch("d_xT", [D, L], BF16); c.d_ffn = scratch("d_ffn", [L, D])
    c.d_gate = scratch("d_gate", [L, N_EXP]); c.d_gateT = scratch("d_gateT", [N_EXP, L])
    c.d_xtm = scratch("d_xtm", [L, D], BF16)
    c.d_yT = scratch("d_yT", [D, L], BF16)
    c.d_Kr = scratch("d_Kr", [L, 1024]); c.d_Ki = scratch("d_Ki", [L, 1024])
    c.d_Yr = scratch("d_Yr", [L, 1024]); c.d_Yi = scratch("d_Yi", [L, 1024])
    c.d_uT = scratch("d_uT", [1024, L]); c.d_x0c = scratch("d_x0c", [1024, L])

    with ExitStack() as st:
        global LAST_TRK
        c.t = t = LAST_TRK = Trk(nc, st)
        S = lambda name, shape, dt=F32: st.enter_context(nc.sbuf_tensor(_u(name), shape, dt))
        banks = [st.enter_context(nc.psum_tensor("ps%d" % i, [128, 512], F32)) for i in range(8)]
        c.psum = Rot(banks[0:6], excl=True)
        c.psacc = Rot(banks[6:8], excl=True)
        c.ident = S("ident", [128, 128]); c.ident_buf = Buf()
        t.dma("sp", c.ident[:, :], c.k["ident"][:, :], writes=[c.ident_buf])
        c.rowtmp = Rot([S("rowtmp%d" % i, [128, 128]) for i in range(2)])
        c.cbuf = Buf("consts")
        c.eps_ln = S("eps_ln", [128, 1])
        t.op("pool", lambda e: e.memset(c.eps_ln[:, :], float(LN_EPS)), writes=[c.cbuf])

        def finish():
            for k in list(t.cnt):
                if isinstance(k, tuple) and t.cnt[k] > 0:
                    t._wait("sp", {k: t.cnt[k]})
            for e in ("pe", "dve", "act", "pool"):
                if t.cnt[e] > 0:
                    t._wait("sp", {e: t.cnt[e]})

        pending = dict(src=(c.x, Buf("x")), res=None, g=c.ln_in_g, b=c.ln_in_b)
        for layer in layer_list:
            phase_ln(c, pending["src"], pending["res"], pending["g"], pending["b"], c.d_h, c.d_xT)
            if stop == "ln0":
                finish(); return nc
            phase_proj(c, layer, only)
            if stop == "proj":
                finish(); return nc
            if "nohgrn" not in dbg:
                phase_hgrn(c, layer)
            if stop == "hgrn":
                finish(); return nc
            if "nohyena" not in dbg:
                phase_hyena(c, layer)
            if stop == "hyena":
                finish(); return nc
            if "noattn" not in dbg:
                phase_attn(c, layer)
            if stop == "attn":
                finish(); return nc
            phase_merge(c, layer)
            if stop == "merge":
                finish(); return nc
            phase_ln(c, c.d_mix, c.d_h, c.ln_mix_g[layer], c.ln_mix_b[layer], c.d_h1, c.d_xT, router=layer)
            if stop == "ln1":
                finish(); return nc
            (phase_moe if c.moe_dense else phase_moe_sparse)(c, layer)
            if stop == "moe":
                finish(); return nc
            pending = dict(src=c.d_ffn, res=c.d_h1, g=c.ln_moe_g[layer], b=c.ln_moe_b[layer])
        phase_ln(c, pending["src"], pending["res"], pending["g"], pending["b"], None, None, final_out=c.out)
        finish()
    return nc


def load_cols(c, dst, dst_buf, vec_ap, n, col0=0):
    t = c.t
    rows, rb = c.rowtmp.next()
    t.dma("sp", rows[0:n, :], vec_ap.rearrange("(j p) -> j p", p=128), writes=[rb])
    ps, pb = c.psum.next()
    t.op("pe", lambda e: e.transpose(ps[:, 0:n], rows[0:n, :], c.ident[0:n, 0:n]),
         reads=[rb, c.ident_buf], writes=[pb])
    t.op("dve", lambda e: e.tensor_copy(dst[:, col0:col0 + n], ps[:, 0:n]), reads=[pb],
         writes=[dst_buf])


def hg_masks():
    s = np.arange(128)
    same = (s[:, None] // 32) == (s[None, :] // 32)
    M1 = (same & (s[:, None] <= s[None, :])).astype(np.float32)
    M3 = (same & (s[:, None] > s[None, :])).astype(np.float32)
    M5 = (s[:, None] // 32 == np.arange(4)[None, :]).astype(np.float32)
    return M1, M1.T.copy(), M3, M3.T.copy(), M5


def phase_hgrn(c, layer):
    t, nc = c.t, c.nc
    V = lambda fn, r=(), w=(): t.op("dve", fn, reads=r, writes=w)
    A = lambda fn, r=(), w=(): t.op("act", fn, reads=r, writes=w)
    G = lambda fn, r=(), w=(): t.op("pool", fn, reads=r, writes=w)
    PE = lambda fn, r=(), w=(): t.op("pe", fn, reads=r, writes=w)
    QS = 128.0 ** -0.5
    with ExitStack() as st:
        S = lambda name, shape, dt=F32: st.enter_context(nc.sbuf_tensor(_u(name), shape, dt))
        kb = Buf("hgk")
        Ms = []
        for nm in ("M1", "M2", "M3", "M4"):
            m = S("hg_" + nm, [128, 128])
            t.dma("sp", m[:, :], c.k["hg_" + nm][:, :], writes=[kb])
            Ms.append(m)
        M1, M2, M3, M4 = Ms
        M5 = S("hg_M5", [128, 4])
        t.dma("sp", M5[:, :], c.k["hg_M5"][:, :], writes=[kb])
        ones = S("hg_ones", [128, 128])
        G(lambda e: e.memset(ones[:, :], 1.0), w=[kb])
        eps = S("hg_eps", [128, 1])
        G(lambda e: e.memset(eps[:, :], 1e-6), w=[kb])
        lbT = S("hg_lbT", [128, 16]); omlT = S("hg_omlT", [128, 16]); nomlT = S("hg_nomlT", [128, 16])
        lbtm = S("hg_lbtm", [128, 2048]); omltm = S("hg_omltm", [128, 2048])
        gT = S("hg_gT", [128, 8])
        lbb = Buf("lb")
        load_cols(c, gT, lbb, c.hg_norm_g[layer], 8)
        if layer == 0:
            G(lambda e: e.memset(lbT[:, :], 0.0), w=[lbb])
            G(lambda e: e.memset(lbtm[:, :], 0.0), w=[lbb])
        else:
            x0T = S("hg_x0T", [128, 16])
            load_cols(c, x0T, lbb, c.hg_lb[0], 16)
            load_cols(c, lbT, lbb, c.hg_lb[1], 16)
            V(lambda e: e.tensor_tensor(lbT[:, :], lbT[:, :], x0T[:, :], op=ALU.subtract), [lbb], [lbb])
            A(lambda e: e.activation(lbT[:, :], lbT[:, :], AF.Sigmoid), [lbb], [lbb])
            t.dma("sp", omltm[:, :], c.hg_lb[0].partition_broadcast(128), writes=[lbb])
            t.dma("sp", lbtm[:, :], c.hg_lb[1].partition_broadcast(128), writes=[lbb])
            G(lambda e: e.tensor_tensor(lbtm[:, :], lbtm[:, :], omltm[:, :], op=ALU.subtract), [lbb], [lbb])
            A(lambda e: e.activation(lbtm[:, :], lbtm[:, :], AF.Sigmoid), [lbb], [lbb])
        V(lambda e: e.tensor_scalar(omlT[:, :], lbT[:, :], -1.0, 1.0, op0=ALU.mult, op1=ALU.add), [lbb], [lbb])
        V(lambda e: e.tensor_scalar(nomlT[:, :], omlT[:, :], -1.0, None, op0=ALU.mult), [lbb], [lbb])
        V(lambda e: e.tensor_scalar(omltm[:, :], lbtm[:, :], -1.0, 1.0, op0=ALU.mult, op1=ALU.add), [lbb], [lbb])

        ztm = [S("hg_ztm%d" % d, [128, NT, 128]) for d in range(2)]; ztm_b = [Buf() for _ in range(2)]
        lgf = [S("hg_lgf%d" % d, [128, NT, 128]) for d in range(2)]; lgf_b = [Buf() for _ in range(2)]
        vt = S("hg_v", [128, NT, 128]); vt_b = Buf()
        qs = S("hg_qs", [128, L]); qs_b = Buf()
        qh = [S("hg_qh%d" % d, [128, L]) for d in range(2)]; qh_b = [Buf() for _ in range(2)]
        kt = [S("hg_kt%d" % d, [128, L]) for d in range(2)]; kt_b = [Buf() for _ in range(2)]
        og = S("hg_og", [128, L]); og_b = Buf()
        Dd = [S("hg_D%d" % d, [128, 64]) for d in range(2)]; Dd_b = [Buf() for _ in range(2)]
        St = [S("hg_S%d" % d, [128, 64, 128]) for d in range(2)]; St_b = [Buf() for _ in range(2)]
        ex = Rot([S("hg_ex%d" % i, [128, 512]) for i in range(3)])
        scm = Rot([S("hg_scm%d" % i, [128, 128]) for i in range(4)])
        vmr = Rot([S("hg_vm%d" % i, [128, 4, 128]) for i in range(2)])
        sq = Rot([S("hg_sq%d" % i, [128, 512]) for i in range(2)])
        rs = Rot([S("hg_rs%d" % i, [128, 512]) for i in range(2)])
        ost = Rot([S("hg_o%d" % i, [128, 512]) for i in range(2)])
        ostb = Rot([S("hg_ob%d" % i, [128, 512], BF16) for i in range(2)])
        Mpre = [M1, M2]
        Mex = [M3, M4]

        for hd in range(8):
            hs = slice(hd * 128, (hd + 1) * 128)
            for d in range(2):
                t.dma("sp", ztm[d][:, :, :],
                      c.d_hf[0][:, d * 1024 + hd * 128:d * 1024 + (hd + 1) * 128].rearrange(
                          "(n p) k -> p n k", p=128), reads=[c.d_hf[1]], writes=[ztm_b[d]])
            t.dma("sp", vt[:, :, :], c.d_hi[0][:, hs].rearrange("(n p) k -> p n k", p=128),
                  reads=[c.d_hi[1]], writes=[vt_b])
            t.dma("sp", qs[:, :], c.d_hqT[0][hs, :], reads=[c.d_hqT[1]], writes=[qs_b])
            t.dma("sp", kt[0][:, :], c.d_hffT[0][hs, :], reads=[c.d_hffT[1]], writes=[kt_b[0]])
            t.dma("sp", kt[1][:, :], c.d_hfbT[0][hs, :], reads=[c.d_hfbT[1]], writes=[kt_b[1]])
            t.dma("sp", og[:, :], c.d_hogT[0][hs, :], reads=[c.d_hogT[1]], writes=[og_b])
            for d in range(2):
                z = ztm[d]; zb = ztm_b[d]; lg = lgf[d]; lb_ = lgf_b[d]
                col = d * 1024 + hd * 128
                oml_bc = omltm[:, col:col + 128].unsqueeze(1).to_broadcast([128, NT, 128])
                lb_bc = lbtm[:, col:col + 128].unsqueeze(1).to_broadcast([128, NT, 128])
                A(lambda e: e.activation(z[:, :, :], z[:, :, :], AF.Sigmoid), [zb], [zb])
                G(lambda e: e.tensor_tensor(lg[:, :, :], z[:, :, :], oml_bc, op=ALU.mult), [zb, lbb], [lb_])
                G(lambda e: e.tensor_tensor(lg[:, :, :], lg[:, :, :], lb_bc, op=ALU.add), [lb_, lbb], [lb_])
                V(lambda e: e.tensor_scalar(lg[:, :, :], lg[:, :, :], 1e-30, None, op0=ALU.max), [lb_], [lb_])
                V(lambda e: e.tensor_scalar(z[:, :, :], z[:, :, :], -1.0, 1.0, op0=ALU.mult, op1=ALU.add), [zb], [zb])
                G(lambda e: e.tensor_tensor(z[:, :, :], z[:, :, :], oml_bc, op=ALU.mult), [zb, lbb], [zb])
            for d in range(2):
                lg = lgf[d]; lb_ = lgf_b[d]
                A(lambda e: e.activation(lg[:, :, :], lg[:, :, :], AF.Ln), [lb_], [lb_])
            A(lambda e: e.activation(qs[:, :], qs[:, :], AF.Silu), [qs_b], [qs_b])
            A(lambda e: e.activation(og[:, :], og[:, :], AF.Silu), [og_b], [og_b])
            for d in range(2):
                j = d * 8 + hd
                A(lambda e: e.activation(kt[d][:, :], kt[d][:, :], AF.Sigmoid), [kt_b[d]], [kt_b[d]])
                V(lambda e: e.tensor_scalar(kt[d][:, :], kt[d][:, :], nomlT[:, j:j + 1], omlT[:, j:j + 1],
                                            op0=ALU.mult, op1=ALU.add), [kt_b[d], lbb], [kt_b[d]])
            for d in range(2):
                lg = lgf[d]; lb_ = lgf_b[d]; z = ztm[d]; zb = ztm_b[d]
                ps, pb = c.psum.next()
                for n in range(NT):
                    PE(lambda e, n=n: e.matmul(ps[:, n * 4:(n + 1) * 4], lg[:, n, :], M5[:, :],
                                               start=True, stop=True), [lb_, kb], [pb])
                A(lambda e: e.activation(Dd[d][:, :], ps[:, 0:64], AF.Exp), [pb], [Dd_b[d]])
                for g4 in range(4):
                    ps, pb = c.psum.next()
                    for jn in range(4):
                        n = g4 * 4 + jn
                        PE(lambda e, n=n, jn=jn: e.matmul(ps[:, jn * 128:(jn + 1) * 128], Mex[d][:, :],
                                                          lg[:, n, :], start=True, stop=True),
                           [lb_, kb], [pb])
                    x_, xb_ = ex.next()
                    A(lambda e: e.activation(x_[:, :], ps[:, :], AF.Exp), [pb], [xb_])
                    V(lambda e, g4=g4: e.tensor_tensor(
                        z[:, g4 * 4:(g4 + 1) * 4, :], z[:, g4 * 4:(g4 + 1) * 4, :],
                        x_[:, :].rearrange("p (a b) -> p a b", a=4), op=ALU.mult), [zb, xb_], [zb])
                for g4 in range(4):
                    ps, pb = c.psum.next()
                    for jn in range(4):
                        n = g4 * 4 + jn
                        PE(lambda e, n=n, jn=jn: e.matmul(ps[:, jn * 128:(jn + 1) * 128], lg[:, n, :],
                                                          Mpre[d][:, :], start=True, stop=True),
                           [lb_, kb], [pb])
                    cs = slice(g4 * 512, (g4 + 1) * 512)
                    x1, xb1 = ex.next()
                    A(lambda e: e.activation(x1[:, :], ps[:, :], AF.Exp), [pb], [xb1])
                    V(lambda e, cs=cs: e.scalar_tensor_tensor(
                        out=qh[d][:, cs], in0=x1[:, :], scalar=float(QS), in1=qs[:, cs],
                        op0=ALU.mult, op1=ALU.mult), [xb1, qs_b], [qh_b[d]])
                    x2, xb2 = ex.next()
                    A(lambda e: e.activation(x2[:, :], ps[:, :], AF.Exp, scale=-1.0), [pb], [xb2])
                    V(lambda e, cs=cs: e.tensor_tensor(kt[d][:, cs], kt[d][:, cs], x2[:, :], op=ALU.mult),
                      [xb2, kt_b[d]], [kt_b[d]])
            for d in range(2):
                z = ztm[d]; zb = ztm_b[d]
                tiles = range(NT) if d == 0 else range(NT - 1, -1, -1)
                first = True
                for n in tiles:
                    vm, vmb = vmr.next()
                    G(lambda e, n=n: e.tensor_tensor(
                        vm[:, :, :], vt[:, n:n + 1, :].to_broadcast([128, 4, 128]),
                        M5[:, :].unsqueeze(2).to_broadcast([128, 4, 128]), op=ALU.mult),
                      [vt_b, kb], [vmb])
                    ps, pb = c.psum.next()
                    PE(lambda e, n=n: e.matmul(ps[:, :], z[:, n, :],
                                               vm[:, :, :].rearrange("p a b -> p (a b)"),
                                               start=True, stop=True), [zb, vmb], [pb])
                    chunks = range(4) if d == 0 else range(3, -1, -1)
                    for jc in chunks:
                        j = n * 4 + jc
                        if first:
                            V(lambda e, j=j: e.memset(St[d][:, j, :], 0.0), [], [St_b[d]])
                            first = False
                        jn_ = j + 1 if d == 0 else j - 1
                        if jn_ < 0 or jn_ > 63:
                            continue
                        V(lambda e, j=j, jn_=jn_, jc=jc: e.scalar_tensor_tensor(
                            out=St[d][:, jn_, :], in0=St[d][:, j, :], scalar=Dd[d][:, j:j + 1],
                            in1=ps[:, jc * 128:(jc + 1) * 128], op0=ALU.mult, op1=ALU.add),
                          [St_b[d], Dd_b[d], pb], [St_b[d]])
            def scores(n):
                res = []
                for d in range(2):
                    ps, pb = c.psum.next()
                    ts = slice(n * 128, (n + 1) * 128)
                    PE(lambda e, d=d, ts=ts: e.matmul(ps[:, 0:128], kt[d][:, ts], qh[d][:, ts],
                                                      start=True, stop=True),
                       [kt_b[d], qh_b[d]], [pb])
                    sm, smb = scm.next()
                    V(lambda e, d=d: e.tensor_tensor(sm[:, :], ps[:, 0:128], Mpre[d][:, :], op=ALU.mult),
                      [pb, kb], [smb])
                    res.append((sm, smb))
                return res

            nxt = scores(0)
            for g4 in range(4):
                pso, pbo = c.psacc.next()
                for jn in range(4):
                    n = g4 * 4 + jn
                    cur = nxt
                    if n + 1 < NT:
                        nxt = scores(n + 1)
                    oc = slice(jn * 128, (jn + 1) * 128)
                    PE(lambda e, n=n, oc=oc: e.matmul(pso[:, oc], vt[:, n, :], cur[0][0][:, :],
                                                      start=True, stop=False), [vt_b, cur[0][1]], [pbo])
                    PE(lambda e, n=n, oc=oc: e.matmul(pso[:, oc], vt[:, n, :], cur[1][0][:, :],
                                                      start=False, stop=False), [vt_b, cur[1][1]], [pbo])
                    for d in range(2):
                        for jc in range(4):
                            j = n * 4 + jc
                            last = (d == 1 and jc == 3)
                            PE(lambda e, d=d, j=j, jc=jc, last=last, jn=jn: e.matmul(
                                pso[:, jn * 128 + jc * 32:jn * 128 + (jc + 1) * 32], St[d][:, j, :],
                                qh[d][:, j * 32:(j + 1) * 32], start=False, stop=last),
                               [St_b[d], qh_b[d]], [pbo])
                cs = slice(g4 * 512, (g4 + 1) * 512)
                s_, sb_ = sq.next()
                A(lambda e: e.activation(s_[:, :], pso[:, :], AF.Square), [pbo], [sb_])
                ps2, pb2 = c.psum.next()
                PE(lambda e: e.matmul(ps2[:, :], ones[:, :], s_[:, :], start=True, stop=True),
                   [sb_, kb], [pb2])
                r_, rb_ = rs.next()
                A(lambda e: e.activation(r_[:, :], ps2[:, :], AF.Sqrt, bias=eps[:, :], scale=1.0 / 128.0),
                  [pb2, kb], [rb_])
                V(lambda e: e.reciprocal(r_[:, :], r_[:, :]), [rb_], [rb_])
                o_, ob_ = ost.next()
                V(lambda e: e.tensor_tensor(o_[:, :], pso[:, :], r_[:, :], op=ALU.mult), [pbo, rb_], [ob_])
                o2_, ob2_ = ostb.next()
                V(lambda e, cs=cs: e.scalar_tensor_tensor(
                    out=o2_[:, :], in0=o_[:, :], scalar=gT[:, hd:hd + 1], in1=og[:, cs],
                    op0=ALU.mult, op1=ALU.mult), [ob_, lbb, og_b], [ob2_])
                t.dma("sp", c.d_ohgT[0][hs, cs], o2_[:, :], reads=[ob2_], writes=[c.d_ohgT[1]])
        t.barrier()


def hy_consts():
    N = 2 * L
    t = np.arange(L, dtype=np.float64)
    ang = 2.0 * np.pi * np.outer(t, t) / N
    A = np.cos(ang)
    B = -np.sin(ang)
    B[:, 0] = (-1.0) ** t
    tl = np.linspace(0.0, 1.0, L, dtype=np.float32)[:, None]
    w = (2.0 * np.float32(math.pi) * np.arange(L, dtype=np.float32)[:, None] / np.float32(L)).astype(np.float32)
    f = np.linspace(1e-4, 15, 16, dtype=np.float32)[None]
    z = np.concatenate([tl, np.cos(f * w), -np.sin(f * w)], axis=-1).astype(np.float32)
    max_decay = math.log(1e-2) / 0.3
    min_decay = math.log(1e-2) / 1.5
    deltas = np.linspace(min_decay, max_decay, 1024, dtype=np.float32)
    win = np.exp(-tl * np.abs(deltas)[None, :]).astype(np.float32)
    return dict(hy_A=A.astype(np.float32), hy_B=B.astype(np.float32),
                hy_BT=np.ascontiguousarray(B.T).astype(np.float32),
                hy_zT=np.ascontiguousarray(z.T), hy_win=win)


def phase_hyena(c, layer):
    t, nc = c.t, c.nc
    V = lambda fn, r=(), w=(): t.op("dve", fn, reads=r, writes=w)
    A = lambda fn, r=(), w=(): t.op("act", fn, reads=r, writes=w)
    G = lambda fn, r=(), w=(): t.op("pool", fn, reads=r, writes=w)
    PE = lambda fn, r=(), w=(): t.op("pe", fn, reads=r, writes=w)
    PI = math.pi
    kA, kB, kBT = c.k["hy_A"], c.k["hy_B"], c.k["hy_BT"]
    with ExitStack() as st0:
        S0 = lambda name, shape, dt=F32: st0.enter_context(nc.sbuf_tensor(_u(name), shape, dt))
        with ExitStack() as st:
            S = lambda name, shape, dt=F32: st.enter_context(nc.sbuf_tensor(_u(name), shape, dt))
            cb = Buf("hyc")
            P = S("hy_P", [128, NT, 1024]); Pb = Buf()
            Q = S("hy_Q", [128, NT, 1024]); Qb = Buf()
            st_in = ExitStack()
            S = lambda name, shape, dt=F32: st_in.enter_context(nc.sbuf_tensor(_u(name), shape, dt))
            zT = S("hy_zT", [33, L]); t.dma("sp", zT[:, :], c.k["hy_zT"][:, :], writes=[cb])
            w1 = S("hy_w1", [33, 64]); t.dma("sp", w1[:, :], c.hy_w1[layer], writes=[cb])
            w2 = S("hy_w2", [64, 2, 64])
            t.dma("sp", w2[:, :, :], c.hy_w2[layer].rearrange("j k m -> k j m"), writes=[cb])
            w3 = S("hy_w3", [64, 2048]); t.dma("sp", w3[:, :], c.hy_w3[layer], writes=[cb])
            fr = S("hy_fr", [64, 4])
            bb = S("hy_bb", [64, 3])
            t.dma("sp", fr[:, 0:1], c.hy_freq[layer].rearrange("(p o) -> p o", o=1), writes=[cb])
            t.dma("sp", bb[:, 0:1], c.hy_b1[layer].rearrange("(p o) -> p o", o=1), writes=[cb])
            for j in range(2):
                t.dma("sp", bb[:, 1 + j:2 + j], c.hy_b2[layer][j].rearrange("(p o) -> p o", o=1), writes=[cb])
            V(lambda e: e.tensor_scalar(fr[:, 1:4], bb[:, 0:3], fr[:, 0:1], None, op0=ALU.mult), [cb], [cb])
            frs = S("hy_frs", [64, 4])
            V(lambda e: e.tensor_scalar(frs[:, 0:1], fr[:, 0:1], 1.0 / (2.0 * PI), None, op0=ALU.mult), [cb], [cb])
            V(lambda e: e.tensor_scalar(frs[:, 1:4], fr[:, 1:4], 1.0 / (2.0 * PI), 8.5, op0=ALU.mult, op1=ALU.add), [cb], [cb])
            qi = S("hy_qi", [64, 512], mybir.dt.int32); qib = Buf()
            qf = S("hy_qf", [64, 512]); qfb = Buf()
            negpi = S("hy_negpi", [64, 1])
            G(lambda e: e.memset(negpi[:, :], -PI), w=[cb])
            hA = S("hy_hA", [64, L]); hAb = Buf()
            hB = S("hy_hB", [64, L]); hBb = Buf()

            def sin_layer(lhsT, src, srcb, dst, dstb, li):
                for tt in range(4):
                    cs = slice(tt * 512, (tt + 1) * 512)
                    ps, pb = c.psum.next()
                    kk = lhsT.shape[0]
                    PE(lambda e: e.matmul(ps[0:64, :], lhsT, src[0:kk, cs], start=True, stop=True),
                       [cb, srcb], [pb])
                    V(lambda e: e.tensor_scalar(dst[:, cs], ps[0:64, :], frs[:, 0:1], frs[:, li:li + 1],
                                                op0=ALU.mult, op1=ALU.add), [pb, cb], [dstb])
                    V(lambda e: e.tensor_copy(qi[:, :], dst[:, cs]), [dstb], [qib])
                    V(lambda e: e.tensor_copy(qf[:, :], qi[:, :]), [qib], [qfb])
                    V(lambda e: e.tensor_tensor(dst[:, cs], dst[:, cs], qf[:, :], op=ALU.subtract), [dstb, qfb], [dstb])
                    V(lambda e: e.scalar_tensor_tensor(out=dst[:, cs], in0=dst[:, cs], scalar=0.0, in1=dst[:, cs],
                                                       op0=ALU.is_lt, op1=ALU.add), [dstb], [dstb])
                    A(lambda e: e.activation(dst[:, cs], dst[:, cs], AF.Sin, bias=negpi[:, :], scale=2.0 * PI),
                      [dstb, cb], [dstb])

            sin_layer(w1[:, :], zT, cb, hA, hAb, 1)
            sin_layer(w2[:, 0, :], hA, hAb, hB, hBb, 2)
            sin_layer(w2[:, 1, :], hB, hBb, hA, hAb, 3)
            winr = Rot([S("hy_win%d" % i, [128, 1024]) for i in range(2)])
            for n in range(NT):
                wn, wnb = winr.next()
                t.dma("sp", wn[:, :], c.k["hy_win"][n * 128:(n + 1) * 128, :], writes=[wnb])
                for q4 in range(4):
                    ps, pb = c.psum.next()
                    PE(lambda e: e.matmul(ps[:, :], hA[:, n * 128:(n + 1) * 128],
                                          w3[:, q4 * 512:(q4 + 1) * 512], start=True, stop=True),
                       [hAb, cb], [pb])
                    dst, dstb = (P, Pb) if q4 < 2 else (Q, Qb)
                    cc = slice((q4 % 2) * 512, (q4 % 2 + 1) * 512)
                    V(lambda e: e.tensor_tensor(dst[:, n, cc], ps[:, :], wn[:, cc], op=ALU.mult),
                      [pb, wnb], [dstb])
            G(lambda e: e.memset(Q[0:1, 0, :], 0.0), [], [Qb])
            t.barrier()
            st_in.close()
            S = lambda name, shape, dt=F32: st.enter_context(nc.sbuf_tensor(_u(name), shape, dt))
            tabA = Rot([S("hy_tA%d" % i, [128, 16, 128]) for i in range(2)])
            tabB = Rot([S("hy_tB%d" % i, [128, 16, 128]) for i in range(2)])
            stg = Rot([S("hy_stg%d" % i, [128, 512]) for i in range(4)])
            for n in range(NT):
                eng = V if n % 2 == 0 else G
                eng(lambda e: e.tensor_tensor(P[:, n, :], P[:, n, :], Q[:, n, :], op=ALU.subtract), [Pb, Qb], [Pb])
                V(lambda e: e.scalar_tensor_tensor(out=Q[:, n, :], in0=Q[:, n, :], scalar=2.0, in1=P[:, n, :],
                                                     op0=ALU.mult, op1=ALU.add), [Pb, Qb], [Qb])
            for fc in range(16):
                ta, tab_ = tabA.next(); tb, tbb_ = tabB.next()
                t.dma("sp", ta[:, :, :], kA[:, fc * 128:(fc + 1) * 128].rearrange("(k p) f -> p k f", p=128), writes=[tab_])
                t.dma("sp", tb[:, :, :], kB[:, fc * 128:(fc + 1) * 128].rearrange("(k p) f -> p k f", p=128), writes=[tbb_])
                for hh in range(2):
                    cc = slice(hh * 512, (hh + 1) * 512)
                    ps, pb = c.psum.next()
                    for k in range(16):
                        PE(lambda e: e.matmul(ps[:, :], ta[:, k, :], Q[:, k, cc], start=(k == 0), stop=(k == 15)),
                           [tab_, Qb], [pb])
                    s1, s1b = stg.next()
                    A(lambda e: e.copy(s1[:, :], ps[:, :]), [pb], [s1b])
                    t.dma("sp", c.d_Kr[0][fc * 128:(fc + 1) * 128, cc], s1[:, :], reads=[s1b], writes=[c.d_Kr[1]])
                    ps, pb = c.psum.next()
                    for k in range(16):
                        PE(lambda e: e.matmul(ps[:, :], tb[:, k, :], P[:, k, cc], start=(k == 0), stop=(k == 15)),
                           [tbb_, Pb], [pb])
                    s2, s2b = stg.next()
                    V(lambda e: e.tensor_copy(s2[:, :], ps[:, :]), [pb], [s2b])
                    if fc == 0:
                        ps3, pb3 = c.psum.next()
                        for k in range(16):
                            PE(lambda e: e.matmul(ps3[:, :], tb[:, k, :], Q[:, k, cc], start=(k == 0), stop=(k == 15)),
                               [tbb_, Qb], [pb3])
                        V(lambda e: e.tensor_copy(s2[0:1, :], ps3[0:1, :]), [pb3, s2b], [s2b])
                    t.dma("sp", c.d_Ki[0][fc * 128:(fc + 1) * 128, cc], s2[:, :], reads=[s2b], writes=[c.d_Ki[1]])
            t.barrier()
        with ExitStack() as st:
            S = lambda name, shape, dt=F32: st.enter_context(nc.sbuf_tensor(_u(name), shape, dt))
            cb = Buf("hyc2")
            cw = S("hy_cw", [128, 3, 24])
            for k in range(3):
                load_cols(c, cw[:, k, :], cb, c.hy_conv_w[layer][k], 24)
            cbias = S("hy_cb", [128, 24]); load_cols(c, cbias, cb, c.hy_conv_b[layer], 24)
            skip = S("hy_skip", [128, 8]); load_cols(c, skip, cb, c.hy_skip[layer], 8)
            utm = S("hy_utm", [128, NT, 1024]); utmb = Buf()
            tabA = Rot([S("hy_tA%d" % i, [128, 16, 128]) for i in range(2)])
            tabB = Rot([S("hy_tB%d" % i, [128, 16, 128]) for i in range(2)])
            stg = Rot([S("hy_stg%d" % i, [128, 512]) for i in range(4)])
            raw = Rot([S("hy_raw%d" % i, [128, L]) for i in range(2)])
            cv = Rot([S("hy_cv%d" % i, [128, L]) for i in range(3)])

            def conv(chunk):
                r, rb = raw.next()
                t.dma("sp", r[:, :], c.d_hyT[0][chunk * 128:(chunk + 1) * 128, :], reads=[c.d_hyT[1]], writes=[rb])
                y, yb = cv.next()
                V(lambda e: e.tensor_scalar(y[:, :], r[:, :], cw[:, 1, chunk:chunk + 1], cbias[:, chunk:chunk + 1],
                                            op0=ALU.mult, op1=ALU.add), [rb, cb], [yb])
                V(lambda e: e.scalar_tensor_tensor(out=y[:, 1:L], in0=r[:, 0:L - 1], scalar=cw[:, 0, chunk:chunk + 1],
                                                   in1=y[:, 1:L], op0=ALU.mult, op1=ALU.add), [rb, cb, yb], [yb])
                V(lambda e: e.scalar_tensor_tensor(out=y[:, 0:L - 1], in0=r[:, 1:L], scalar=cw[:, 2, chunk:chunk + 1],
                                                   in1=y[:, 0:L - 1], op0=ALU.mult, op1=ALU.add), [rb, cb, yb], [yb])
                return y, yb

            for ch in range(8):
                x0, x0b = conv(ch)
                t.dma("sp", c.d_x0c[0][ch * 128:(ch + 1) * 128, :], x0[:, :], reads=[x0b], writes=[c.d_x0c[1]])
                x1, x1b = conv(8 + ch)
                vv, vvb = conv(16 + ch)
                G(lambda e: e.tensor_tensor(x1[:, :], x1[:, :], vv[:, :], op=ALU.mult), [x1b, vvb], [x1b])
                t.dma("sp", c.d_uT[0][ch * 128:(ch + 1) * 128, :], x1[:, :], reads=[x1b], writes=[c.d_uT[1]])
                for g4 in range(4):
                    ps, pb = c.psum.next()
                    for jn in range(4):
                        n = g4 * 4 + jn
                        PE(lambda e: e.transpose(ps[:, jn * 128:(jn + 1) * 128], x1[:, n * 128:(n + 1) * 128],
                                                 c.ident[:, :]), [x1b, c.ident_buf], [pb])
                    evac(c, utm[:, g4 * 4:(g4 + 1) * 4, ch * 128:(ch + 1) * 128],
                         ps[:, :].rearrange("p (a b) -> p a b", a=4), [pb], [utmb])
            kr = Rot([S("hy_kr%d" % i, [128, 1024]) for i in range(2)])
            ki = Rot([S("hy_ki%d" % i, [128, 1024]) for i in range(2)])
            tmp = Rot([S("hy_tmp%d" % i, [128, 512]) for i in range(4)])
            for fc in range(16):
                ta, tab_ = tabA.next(); tb, tbb_ = tabB.next()
                t.dma("sp", ta[:, :, :], kA[:, fc * 128:(fc + 1) * 128].rearrange("(k p) f -> p k f", p=128), writes=[tab_])
                t.dma("sp", tb[:, :, :], kB[:, fc * 128:(fc + 1) * 128].rearrange("(k p) f -> p k f", p=128), writes=[tbb_])
                krt, krb = kr.next(); kit, kib = ki.next()
                t.dma("sp", krt[:, :], c.d_Kr[0][fc * 128:(fc + 1) * 128, :], reads=[c.d_Kr[1]], writes=[krb])
                t.dma("sp", kit[:, :], c.d_Ki[0][fc * 128:(fc + 1) * 128, :], reads=[c.d_Ki[1]], writes=[kib])
                for hh in range(2):
                    cc = slice(hh * 512, (hh + 1) * 512)
                    psr, pbr = c.psum.next()
                    for k in range(16):
                        PE(lambda e: e.matmul(psr[:, :], ta[:, k, :], utm[:, k, cc], start=(k == 0), stop=(k == 15)),
                           [tab_, utmb], [pbr])
                    psi, pbi = c.psum.next()
                    for k in range(16):
                        PE(lambda e: e.matmul(psi[:, :], tb[:, k, :], utm[:, k, cc], start=(k == 0), stop=(k == 15)),
                           [tbb_, utmb], [pbi])
                    ur, urb = tmp.next(); ui, uib = tmp.next()
                    A(lambda e: e.copy(ur[:, :], psr[:, :]), [pbr], [urb])
                    A(lambda e: e.copy(ui[:, :], psi[:, :]), [pbi], [uib])
                    yr, yrb = stg.next(); yi, yib = stg.next()
                    t1, t1b = tmp.next(); t2, t2b = tmp.next()
                    V(lambda e: e.tensor_tensor(yr[:, :], ur[:, :], krt[:, cc], op=ALU.mult), [urb, krb], [yrb])
                    G(lambda e: e.tensor_tensor(t1[:, :], ui[:, :], kit[:, cc], op=ALU.mult), [uib, kib], [t1b])
                    V(lambda e: e.tensor_tensor(yi[:, :], ur[:, :], kit[:, cc], op=ALU.mult), [urb, kib], [yib])
                    G(lambda e: e.tensor_tensor(t2[:, :], ui[:, :], krt[:, cc], op=ALU.mult), [uib, krb], [t2b])
                    V(lambda e: e.tensor_tensor(yr[:, :], yr[:, :], t1[:, :], op=ALU.subtract), [yrb, t1b], [yrb])
                    V(lambda e: e.tensor_tensor(yi[:, :], yi[:, :], t2[:, :], op=ALU.add), [yib, t2b], [yib])
                    if fc == 0:
                        V(lambda e: e.scalar_tensor_tensor(out=yr[0:1, :], in0=ur[0:1, :], scalar=0.5, in1=krt[0:1, cc],
                                                           op0=ALU.mult, op1=ALU.mult), [urb, krb, yrb], [yrb])
                        V(lambda e: e.scalar_tensor_tensor(out=yi[0:1, :], in0=ui[0:1, :], scalar=0.5, in1=kit[0:1, cc],
                                                           op0=ALU.mult, op1=ALU.mult), [uib, kib, yib], [yib])
                    t.dma("sp", c.d_Yr[0][fc * 128:(fc + 1) * 128, cc], yr[:, :], reads=[yrb], writes=[c.d_Yr[1]])
                    t.dma("sp", c.d_Yi[0][fc * 128:(fc + 1) * 128, cc], yi[:, :], reads=[yib], writes=[c.d_Yi[1]])
            t.barrier()
        with ExitStack() as st:
            S = lambda name, shape, dt=F32: st.enter_context(nc.sbuf_tensor(_u(name), shape, dt))
            cb = Buf("hyc3")
            skip = S("hy_skip2", [128, 8]); load_cols(c, skip, cb, c.hy_skip[layer], 8)
            itA = Rot([S("hy_itA%d" % i, [128, 16, 512]) for i in range(2)])
            itB = Rot([S("hy_itB%d" % i, [128, 16, 512]) for i in range(2)])
            yrr = Rot([S("hy_yr%d" % i, [128, 16, 128]) for i in range(2)])
            yir = Rot([S("hy_yi%d" % i, [128, 16, 128]) for i in range(2)])
            usr = Rot([S("hy_us%d" % i, [128, L]) for i in range(2)])
            x0r = Rot([S("hy_x0%d" % i, [128, L]) for i in range(2)])
            outr = Rot([S("hy_out%d" % i, [128, 512]) for i in range(3)])
            outb = Rot([S("hy_outb%d" % i, [128, 512], BF16) for i in range(3)])
            for ch in range(8):
                rows = slice(ch * 128, (ch + 1) * 128)
                yrt, yrb = yrr.next(); yit, yib = yir.next()
                t.dma("sp", yrt[:, :, :], c.d_Yr[0][:, rows].rearrange("(k p) m -> p k m", p=128), reads=[c.d_Yr[1]], writes=[yrb])
                t.dma("sp", yit[:, :, :], c.d_Yi[0][:, rows].rearrange("(k p) m -> p k m", p=128), reads=[c.d_Yi[1]], writes=[yib])
                us, usb = usr.next(); x0, x0b = x0r.next()
                t.dma("sp", us[:, :], c.d_uT[0][rows, :], reads=[c.d_uT[1]], writes=[usb])
                t.dma("sp", x0[:, :], c.d_x0c[0][rows, :], reads=[c.d_x0c[1]], writes=[x0b])
                G(lambda e: e.tensor_scalar(us[:, :], us[:, :], skip[:, ch:ch + 1], None, op0=ALU.mult), [usb, cb], [usb])
                for tt in range(4):
                    cs = slice(tt * 512, (tt + 1) * 512)
                    ia, iab = itA.next(); ib, ibb = itB.next()
                    t.dma("sp", ia[:, :, :], kA[:, cs].rearrange("(k p) q -> p k q", p=128), writes=[iab])
                    t.dma("sp", ib[:, :, :], kBT[:, cs].rearrange("(k p) q -> p k q", p=128), writes=[ibb])
                    ps, pb = c.psum.next()
                    for k in range(16):
                        PE(lambda e: e.matmul(ps[:, :], yrt[:, k, :], ia[:, k, :], start=(k == 0), stop=False),
                           [yrb, iab], [pb])
                    for k in range(16):
                        PE(lambda e: e.matmul(ps[:, :], yit[:, k, :], ib[:, k, :], start=False, stop=(k == 15)),
                           [yib, ibb], [pb])
                    o_, ob_ = outr.next()
                    V(lambda e: e.scalar_tensor_tensor(out=o_[:, :], in0=ps[:, :], scalar=2.0 / (2 * L), in1=us[:, cs],
                                                       op0=ALU.mult, op1=ALU.add), [pb, usb], [ob_])
                    o2_, ob2_ = outb.next()
                    G(lambda e: e.tensor_tensor(o2_[:, :], o_[:, :], x0[:, cs], op=ALU.mult), [ob_, x0b], [ob2_])
                    t.dma("sp", c.d_ohyT[0][rows, cs], o2_[:, :], reads=[ob2_], writes=[c.d_ohyT[1]])
            t.barrier()


def att_consts():
    W = 128
    kofs = np.arange(3 * W)[None, :] - W
    rel = kofs - np.arange(W)[:, None]
    half, max_exact = 16, 8
    bucket = (rel > 0).astype(np.int32) * half
    n = np.abs(rel)
    n_safe = np.maximum(n, 1).astype(np.float32)
    large = max_exact + (np.log(n_safe / np.float32(max_exact)) / np.float32(math.log(128 / max_exact))
                         * np.float32(half - max_exact)).astype(np.int32)
    large = np.clip(large, 0, half - 1)
    bucket = bucket + np.where(n < max_exact, n, large)
    onehot = (bucket[None, :, :] == np.arange(32)[:, None, None]).astype(np.float32)
    maskadd = np.where(np.abs(rel) <= 128, 0.0, -1e30).astype(np.float32)
    return dict(at_onehot=onehot, at_mask=maskadd)


def phase_attn(c, layer):
    t, nc = c.t, c.nc
    V = lambda fn, r=(), w=(): t.op("dve", fn, reads=r, writes=w)
    A = lambda fn, r=(), w=(): t.op("act", fn, reads=r, writes=w)
    G = lambda fn, r=(), w=(): t.op("pool", fn, reads=r, writes=w)
    PE = lambda fn, r=(), w=(): t.op("pe", fn, reads=r, writes=w)
    with ExitStack() as st:
        S = lambda name, shape, dt=F32: st.enter_context(nc.sbuf_tensor(_u(name), shape, dt))
        cb = Buf("atc")
        biasM = S("at_bias", [128, 16, 384])
        rbb = S("at_rbb", [128, 512])
        t.dma("sp", rbb[:, :], c.rel_bias.rearrange("b h -> (b h)").partition_broadcast(128), writes=[cb])
        sinkb = S("at_sink", [128, 16])
        t.dma("sp", sinkb[:, :], c.att_sink[layer].partition_broadcast(128), writes=[cb])
        mk = S("at_mk", [128, 384])
        t.dma("sp", mk[:, :], c.k["at_mask"][:, :], writes=[cb])
        V(lambda e: e.tensor_copy(biasM[:, :, :], mk[:, :].unsqueeze(1).to_broadcast([128, 16, 384])), [cb], [cb])
        ohr = Rot([S("at_oh%d" % i, [128, 384]) for i in range(2)])
        for b in range(32):
            oh, ohb = ohr.next()
            t.dma("sp", oh[:, :], c.k["at_onehot"][b], writes=[ohb])
            for h in range(16):
                V(lambda e: e.scalar_tensor_tensor(out=biasM[:, h, :], in0=oh[:, :], scalar=rbb[:, b * 16 + h:b * 16 + h + 1],
                                                   in1=biasM[:, h, :], op0=ALU.mult, op1=ALU.add), [ohb, cb], [cb])
        kd = []
        for g in range(2):
            k_ = S("at_kd%d" % g, [128, L])
            for half in range(2):
                t.dma("sp", k_[half * 64:(half + 1) * 64, :], c.d_akT[0][g * 64:(g + 1) * 64, :],
                      reads=[c.d_akT[1]], writes=[cb])
            kd.append(k_)
        vx = {}
        for g in range(2):
            for hh in range(2):
                v_ = S("at_v%d%d" % (g, hh), [128, NT, 128])
                G(lambda e: e.memset(v_[:, :, :], 0.0), [], [cb])
                t.dma("sp", v_[:, :, hh * 64:(hh + 1) * 64],
                      c.d_av[0][:, g * 64:(g + 1) * 64].rearrange("(n p) d -> p n d", p=128),
                      reads=[c.d_av[1]], writes=[cb])
                vx[(g, hh)] = v_
        qr = Rot([S("at_q%d" % i, [128, L]) for i in range(2)])
        otr = Rot([S("at_o%d" % i, [128, L], BF16) for i in range(2)])
        sr = Rot([S("at_s%d" % i, [128, 384]) for i in range(3)])
        pr = Rot([S("at_p%d" % i, [128, 384]) for i in range(3)])
        ptr = Rot([S("at_pt%d" % i, [128, 384]) for i in range(3)])
        smr = Rot([S("at_sm%d" % i, [128, 8]) for i in range(4)])
        for j in range(8):
            q, qb = qr.next()
            t.dma("sp", q[:, :], c.d_aqT[0][j * 128:(j + 1) * 128, :], reads=[c.d_aqT[1]], writes=[qb])
            ot, otb = otr.next()
            for n in range(NT):
                klo = max(0, (n - 1) * 128); khi = min(L, (n + 2) * 128)
                bo = klo - (n - 1) * 128; wdt = khi - klo
                nkb = wdt // 128
                oacc, oab = c.psacc.next()
                for hh in range(2):
                    h = 2 * j + hh; g = h // 8; po = hh * 64
                    ps, pb = c.psum.next()
                    PE(lambda e: e.matmul(ps[:, 0:wdt], q[po:po + 64, n * 128:(n + 1) * 128],
                                          kd[g][po:po + 64, klo:khi], start=True, stop=True), [qb, cb], [pb])
                    s_, sb_ = sr.next()
                    V(lambda e: e.scalar_tensor_tensor(out=s_[:, 0:wdt], in0=ps[:, 0:wdt], scalar=0.125,
                                                       in1=biasM[:, h, bo:bo + wdt], op0=ALU.mult, op1=ALU.add),
                      [pb, cb], [sb_])
                    sm, smb = smr.next()
                    V(lambda e: e.reduce_max(sm[:, 0:1], s_[:, 0:wdt], axis=AX.X), [sb_], [smb])
                    V(lambda e: e.tensor_tensor(sm[:, 0:1], sm[:, 0:1], sinkb[:, h:h + 1], op=ALU.max), [smb, cb], [smb])
                    V(lambda e: e.tensor_scalar(sm[:, 1:2], sm[:, 0:1], -1.0, None, op0=ALU.mult), [smb], [smb])
                    p_, pb_ = pr.next()
                    A(lambda e: e.activation(p_[:, 0:wdt], s_[:, 0:wdt], AF.Exp, bias=sm[:, 1:2], scale=1.0,
                                             accum_out=sm[:, 2:3]), [sb_, smb], [pb_, smb])
                    A(lambda e: e.activation(sm[:, 3:4], sinkb[:, h:h + 1], AF.Exp, bias=sm[:, 1:2], scale=1.0),
                      [smb, cb], [smb])
                    V(lambda e: e.tensor_tensor(sm[:, 4:5], sm[:, 2:3], sm[:, 3:4], op=ALU.add), [smb], [smb])
                    V(lambda e: e.reciprocal(sm[:, 5:6], sm[:, 4:5]), [smb], [smb])
                    V(lambda e: e.tensor_scalar(p_[:, 0:wdt], p_[:, 0:wdt], sm[:, 5:6], None, op0=ALU.mult),
                      [pb_, smb], [pb_])
                    ps2, pb2 = c.psum.next()
                    for kb in range(nkb):
                        PE(lambda e: e.transpose(ps2[:, kb * 128:(kb + 1) * 128], p_[:, kb * 128:(kb + 1) * 128],
                                                 c.ident[:, :]), [pb_, c.ident_buf], [pb2])
                    pt, ptb = ptr.next()
                    A(lambda e: e.copy(pt[:, 0:wdt], ps2[:, 0:wdt]), [pb2], [ptb])
                    for kb in range(nkb):
                        kt_ = klo // 128 + kb
                        PE(lambda e: e.matmul(oacc[:, 0:128], vx[(g, hh)][:, kt_, :], pt[:, kb * 128:(kb + 1) * 128],
                                              start=(hh == 0 and kb == 0), stop=(hh == 1 and kb == nkb - 1)),
                           [cb, ptb], [oab])
                evac(c, ot[:, n * 128:(n + 1) * 128], oacc[:, 0:128], [oab], [otb])
            t.dma("sp", c.d_oatT[0][j * 128:(j + 1) * 128, :], ot[:, :], reads=[otb], writes=[c.d_oatT[1]])
        t.barrier()


def phase_merge(c, layer):
    t, nc = c.t, c.nc
    V = lambda fn, r=(), w=(): t.op("dve", fn, reads=r, writes=w)
    G = lambda fn, r=(), w=(): t.op("pool", fn, reads=r, writes=w)
    PE = lambda fn, r=(), w=(): t.op("pe", fn, reads=r, writes=w)
    with ExitStack() as st:
        S = lambda name, shape, dt=F32: st.enter_context(nc.sbuf_tensor(_u(name), shape, dt))
        otr = Rot([S("mg_o%d" % i, [128, 8, L], BF16) for i in range(2)])
        acc = S("mg_acc", [128, 4, L]); accb = Buf()
        accbf = Rot([S("mg_accb%d" % i, [128, L], BF16) for i in range(2)])
        gr = Rot([S("mg_g%d" % i, [128, L]) for i in range(2)])
        wrot = WPool(c, S, "mg_w", 8, 512)
        tmpr = Rot([S("mg_t%d" % i, [128, 512]) for i in range(3)])
        srcs = (c.d_ohgT, c.d_ohyT, c.d_oatT)
        for blk in range(4):
            for n in range(3):
                wt, wb = wrot.load(c.w_branch[layer][n][:, blk * 512:(blk + 1) * 512], 8, 512)
                o_, ob_ = otr.next()
                t.dma("sp", o_[:, :, :], srcs[n][0].rearrange("(k p) q -> p k q", p=128),
                      reads=[srcs[n][1]], writes=[ob_])
                for cg in range(4):
                    dg = blk * 4 + cg
                    g_, gb_ = gr.next()
                    t.dma("sp", g_[:, :], c.d_gT[0][n * D + dg * 128:n * D + (dg + 1) * 128, :],
                          reads=[c.d_gT[1]], writes=[gb_])
                    for tt in range(4):
                        cs = slice(tt * 512, (tt + 1) * 512)
                        ps, pb = c.psum.next()
                        for kc in range(8):
                            PE(lambda e: e.matmul(ps[:, :], wt[:, kc, cg * 128:(cg + 1) * 128], o_[:, kc, cs],
                                                  start=(kc == 0), stop=(kc == 7)), [wb, ob_], [pb])
                        if n == 0:
                            V(lambda e: e.tensor_tensor(acc[:, cg, cs], ps[:, :], g_[:, cs], op=ALU.mult),
                              [pb, gb_], [accb])
                        else:
                            tm_, tmb_ = tmpr.next()
                            V(lambda e: e.tensor_tensor(tm_[:, :], ps[:, :], g_[:, cs], op=ALU.mult),
                              [pb, gb_], [tmb_])
                            G(lambda e: e.tensor_tensor(acc[:, cg, cs], acc[:, cg, cs], tm_[:, :], op=ALU.add),
                              [accb, tmb_], [accb])
                    if n == 2:
                        ab_, abb_ = accbf.next()
                        t.op("act", lambda e: e.copy(ab_[:, :], acc[:, cg, :]), reads=[accb], writes=[abb_])
                        t.dma("sp", c.d_yT[0][dg * 128:(dg + 1) * 128, :], ab_[:, :], reads=[abb_],
                              writes=[c.d_yT[1]])
        t.barrier()
    with ExitStack() as st:
        S = lambda name, shape, dt=F32: st.enter_context(nc.sbuf_tensor(_u(name), shape, dt))
        yT = S("mg_yT", [128, KC, L], BF16); yTb = Buf()
        t.dma("sp", yT[:, :, :], c.d_yT[0].rearrange("(k p) q -> p k q", p=128), reads=[c.d_yT[1]], writes=[yTb])
        wrot = WPool(c, S, "mg_wo", KC, 512)
        stg_tm = Rot([S("mg_s%d" % i, [128, 512]) for i in range(3)])

        def epi2(ps, pb, tt, cc0, cw):
            s, sb = stg_tm.next()
            evac(c, s[:, 0:cw], ps[:, 0:cw], [pb], [sb])
            t.dma("sp", c.d_mix[0][tt * 128:(tt + 1) * 128, cc0:cc0 + cw], s[:, 0:cw],
                  reads=[sb], writes=[c.d_mix[1]])

        gemm_tm(c, c.w_out[layer], D, yT, yTb, KC, L, epi2, wrot)
        t.barrier()


SIG7 = 1.0 / (1.0 + math.exp(-1.702 * 7.0))


def phase_moe(c, layer):
    t, nc = c.t, c.nc
    V = lambda fn, r=(), w=(): t.op("dve", fn, reads=r, writes=w)
    A = lambda fn, r=(), w=(): t.op("act", fn, reads=r, writes=w)
    G = lambda fn, r=(), w=(): t.op("pool", fn, reads=r, writes=w)
    PE = lambda fn, r=(), w=(): t.op("pe", fn, reads=r, writes=w)
    HT = L // 2
    NTH = HT // 128
    with ExitStack() as st:
        S = lambda name, shape, dt=F32: st.enter_context(nc.sbuf_tensor(_u(name), shape, dt))
        gs_ = Rot([S("me_g%d" % i, [128, 512]) for i in range(2)])
        sg_ = Rot([S("me_s%d" % i, [128, 512]) for i in range(2)])
        us_ = Rot([S("me_u%d" % i, [128, 512]) for i in range(2)])
        xh = S("me_x", [128, KC, HT], BF16); xhb = Buf()
        yacc = S("me_y", [128, NTH, D]); yab = Buf()
        act2 = S("me_a", [128, KC * HT], BF16); actb = Buf()
        actT = act2[:, :].rearrange("p (k q) -> p k q", k=KC)
        bd = S("me_bd", [N_EXP, D]); bdb = Buf()
        gateT = S("me_gT", [N_EXP, HT]); gateTb = Buf()
        wstage = Rot([S("me_ws%d" % i, [128, KC, 256]) for i in range(2)])
        wbf = Rot([S("me_wb%d" % i, [128, KC, 256], BF16) for i in range(2)])
        gate = S("me_gate", [128, NTH, N_EXP]); gateb = Buf()
        c7 = S("me_c7", [128, 1])
        G(lambda e: e.memset(c7[:, :], 7.0), [], [gateb])
        bgu = Rot([S("me_bgu%d" % i, [128, 32]) for i in range(2)])
        bgs = Rot([S("me_bgs%d" % i, [128, 16]) for i in range(2)])
        items = []
        for ex in range(c.moe_nexp):
            items += [(ex, "gu", fc) for fc in range(16)]
            items += [(ex, "dn", db) for db in range(8)]
        import os as _os2
        items = items[:int(_os2.environ.get("MOE_ITEMS", len(items)))]

        def load_item(it):
            ex, kind, j = it
            stg, stgb = wstage.next()
            if kind == "gu":
                for two in range(2):
                    src = c.w_gate_up[layer][ex][:, two * DFF + j * 128:two * DFF + (j + 1) * 128].rearrange(
                        "(kc p) c -> p kc c", p=128)
                    t.dma("sp", stg[:, :, two * 128:(two + 1) * 128], src, writes=[stgb])
            else:
                src = c.w_down[layer][ex][:, j * 256:(j + 1) * 256].rearrange("(kc p) c -> p kc c", p=128)
                t.dma("sp", stg[:, :, :], src, writes=[stgb])
            bf, bfb = wbf.next()
            G(lambda e: e.tensor_copy(bf[:, :, :], stg[:, :, :]), [stgb], [bfb])
            return bf, bfb

        for hf in range(2):
            t0 = hf * HT
            t.dma("sp", xh[:, :, :], c.d_xT[0][:, t0:t0 + HT].rearrange("(k p) q -> p k q", p=128),
                  reads=[c.d_xT[1]], writes=[xhb])
            t.dma("sp", gate[:, :, :], c.d_gate[0][t0:t0 + HT, :].rearrange("(n p) e -> p n e", p=128),
                  reads=[c.d_gate[1]], writes=[gateb])
            t.dma("sp", gateT[:, :], c.d_gateT[0][:, t0:t0 + HT], reads=[c.d_gateT[1]], writes=[gateTb])
            t.dma("sp", bd[:, :], c.b_down[layer], writes=[bdb])
            import os as _os
            _lv = int(_os.environ.get("MOE_INIT", 9))
            if _lv == 0:
                t.barrier(); return
            for tt in range(NTH if _lv in (2, 9) else 1):
                for cb4 in range(4):
                    ps, pb = c.psum.next()
                    PE(lambda e: e.matmul(ps[:, :], gateT[:, tt * 128:(tt + 1) * 128], bd[:, cb4 * 512:(cb4 + 1) * 512],
                                          start=True, stop=True), [gateTb, bdb], [pb])
                    if _lv != 3:
                        evac(c, yacc[:, tt, cb4 * 512:(cb4 + 1) * 512], ps[:, :], [pb], [yab])
            if _lv in (3, 4):
                t.barrier(); return
            nxt = load_item(items[0])
            bg = bgb = bs = bsb = None
            for ii, (ex, kind, j) in enumerate(items):
                wt, wtb = nxt
                if ii + 1 < len(items):
                    nxt = load_item(items[ii + 1])
                if kind == "gu":
                    fc = j
                    if fc == 0:
                        bg, bgb = bgu.next()
                        load_cols(c, bg, bgb, c.b_gate_up[layer][ex], 32)
                        bs, bsb = bgs.next()
                        V(lambda e: e.tensor_tensor(bs[:, :], bg[:, 0:16], c7[:, 0:1].to_broadcast([128, 16]), op=ALU.mult),
                          [bgb, gateb], [bsb])
                        V(lambda e: e.tensor_scalar(bs[:, :], bs[:, :], 1.702 / 7.0, None, op0=ALU.mult), [bsb], [bsb])
                    wv = wt[:, :, :].rearrange("p k (two f) -> p k two f", two=2)
                    for tt in range(HT // 512):
                        cs = slice(tt * 512, (tt + 1) * 512)
                        psg, pbg = c.psum.next()
                        for kc in range(KC):
                            PE(lambda e: e.matmul(psg[:, :], wv[:, kc, 0, :], xh[:, kc, cs],
                                                  start=(kc == 0), stop=(kc == KC - 1)), [wtb, xhb], [pbg])
                        psu, pbu = c.psum.next()
                        for kc in range(KC):
                            PE(lambda e: e.matmul(psu[:, :], wv[:, kc, 1, :], xh[:, kc, cs],
                                                  start=(kc == 0), stop=(kc == KC - 1)), [wtb, xhb], [pbu])
                        g1, g1b = gs_.next(); s1, s1b = sg_.next(); u1, u1b = us_.next()
                        A(lambda e: e.activation(s1[:, :], psg[:, :], AF.Sigmoid, bias=bs[:, fc:fc + 1], scale=1.702),
                          [pbg, bsb], [s1b])
                        if c.moe_epi == 1:
                            continue
                        if c.moe_epi == 24:
                            V(lambda e: e.tensor_copy(g1[:, :], psu[:, :]), [pbu], [g1b])
                            continue
                        if c.moe_epi == 25:
                            A(lambda e: e.copy(g1[:, :], psu[:, :]), [pbu], [g1b])
                            continue
                        if c.moe_epi == 26:
                            V(lambda e: e.tensor_copy(g1[:, :], psg[:, :]), [pbg, s1b], [g1b])
                            continue
                        if c.moe_epi == 21:
                            V(lambda e: e.tensor_copy(g1[:, :], psg[:, :]), [pbg], [g1b])
                            continue
                        if c.moe_epi == 7:
                            A(lambda e: e.activation(g1[:, :], psg[:, :], AF.Identity, bias=bg[:, fc:fc + 1], scale=1.0),
                              [pbg, bgb], [g1b])
                            A(lambda e: e.activation(u1[:, :], psu[:, :], AF.Identity, bias=bg[:, 16 + fc:17 + fc], scale=1.0),
                              [pbu, bgb], [u1b])
                            V(lambda e: e.tensor_scalar(g1[:, :], g1[:, :], 7.0, None, op0=ALU.min), [g1b], [g1b])
                            V(lambda e: e.tensor_scalar(u1[:, :], u1[:, :], 7.0, None, op0=ALU.min), [u1b], [u1b])
                        else:
                            V(lambda e: e.tensor_scalar(g1[:, :], psg[:, :], bg[:, fc:fc + 1], c7[:, 0:1], op0=ALU.add, op1=ALU.min),
                              [pbg, bgb, gateb], [g1b])
                            V(lambda e: e.tensor_scalar(u1[:, :], psu[:, :], bg[:, 16 + fc:17 + fc], c7[:, 0:1], op0=ALU.add, op1=ALU.min),
                              [pbu, bgb, gateb], [u1b])
                        V(lambda e: e.scalar_tensor_tensor(out=g1[:, :], in0=s1[:, :], scalar=float(SIG7), in1=g1[:, :],
                                                           op0=ALU.min, op1=ALU.mult), [s1b, g1b], [g1b])
                        V(lambda e: e.tensor_scalar(u1[:, :], u1[:, :], -7.0, 1.0, op0=ALU.max, op1=ALU.add), [u1b], [u1b])
                        V(lambda e: e.tensor_tensor(actT[:, fc, cs], g1[:, :], u1[:, :], op=ALU.mult), [g1b, u1b], [actb])
                else:
                    db = j
                    for tt in range(NTH):
                        ps, pb = c.psum.next()
                        for kc in range(KC):
                            PE(lambda e: e.matmul(ps[:, 0:256], actT[:, kc, tt * 128:(tt + 1) * 128], wt[:, kc, :],
                                                  start=(kc == 0), stop=(kc == KC - 1)), [actb, wtb], [pb])
                        V(lambda e: e.scalar_tensor_tensor(
                            out=yacc[:, tt, db * 256:(db + 1) * 256], in0=ps[:, 0:256], scalar=gate[:, tt, ex:ex + 1],
                            in1=yacc[:, tt, db * 256:(db + 1) * 256], op0=ALU.mult, op1=ALU.add),
                          [pb, gateb, yab], [yab])
            t.dma("sp", c.d_ffn[0][t0:t0 + HT, :].rearrange("(n p) d -> p n d", p=128), yacc[:, :, :],
                  reads=[yab], writes=[c.d_ffn[1]])
        t.barrier()


def relayout_gu(w):
    e = w.shape[0]
    v = w.reshape(e, KC, 128, 2, 16, 128)
    v = v.transpose(0, 4, 2, 1, 3, 5)
    return np.ascontiguousarray(v).reshape(e, 16, 128, KC * 256)


def relayout_dn(w):
    e = w.shape[0]
    v = w.reshape(e, KC, 128, 8, 256)
    v = v.transpose(0, 3, 2, 1, 4)
    return np.ascontiguousarray(v).reshape(e, 8, 128, KC * 256)


def kernel(**inputs):
    n = 8
    nc = build()
    consts = host_consts()
    shared = {}
    for name in LAST_INPUT_NAMES:
        if name == "x":
            continue
        if name.startswith("k_"):
            shared[name] = consts[name[2:]]
        elif name.startswith("w_gate_up"):
            shared[name] = relayout_gu(np.asarray(inputs["w_gate_up"], dtype=np.float32)[int(name[-1])])
        elif name.startswith("w_down"):
            shared[name] = relayout_dn(np.asarray(inputs["w_down"], dtype=np.float32)[int(name[-1])])
        else:
            shared[name] = np.ascontiguousarray(np.asarray(inputs[name], dtype=np.float32))
    x = np.asarray(inputs["x"], dtype=np.float32)
    in_maps = []
    for b in range(n):
        m = dict(shared)
        m["x"] = np.ascontiguousarray(x[b])
        in_maps.append(m)
    res = run_bass_kernel_spmd(nc, in_maps, core_ids=list(range(n)))
    return np.stack([np.asarray(res.results[b]["out"], dtype=np.float32) for b in range(n)], axis=0)


CAP = 256


def moe_consts():
    s = np.arange(128)
    ustrict = (s[:, None] < s[None, :]).astype(np.float32)
    sele = np.zeros((N_EXP, N_EXP, 128), np.float32)
    for e in range(N_EXP):
        sele[e, e, :] = 1.0
    iota_c = np.tile(np.arange(CAP, dtype=np.float32)[None, :], (128, 1))
    pidx = (np.arange(128, dtype=np.float32)[:, None] + 128.0 * np.arange(CAP // 128, dtype=np.float32)[None, :])
    return dict(me_ustrict=ustrict, me_sele=sele, me_iota=iota_c, me_pidx=np.ascontiguousarray(pidx))


def phase_moe_sparse(c, layer):
    t, nc = c.t, c.nc
    V = lambda fn, r=(), w=(): t.op("dve", fn, reads=r, writes=w)
    A = lambda fn, r=(), w=(): t.op("act", fn, reads=r, writes=w)
    G = lambda fn, r=(), w=(): t.op("pool", fn, reads=r, writes=w)
    PE = lambda fn, r=(), w=(): t.op("pe", fn, reads=r, writes=w)
    HT = L // 2
    NTH = HT // 128
    NCC = CAP // 128
    with ExitStack() as st:
        S = lambda name, shape, dt=F32: st.enter_context(nc.sbuf_tensor(_u(name), shape, dt))
        gs_ = Rot([S("ms_g%d" % i, [128, CAP]) for i in range(2)])
        sg_ = Rot([S("ms_s%d" % i, [128, CAP]) for i in range(2)])
        us_ = Rot([S("ms_u%d" % i, [128, CAP]) for i in range(2)])
        xtm = S("ms_x", [128, NTH, D], BF16); xtmb = Buf()
        yacc = S("ms_y", [128, NTH, D]); yab = Buf()
        xeT = S("ms_xe", [128, KC, CAP], BF16); xeb = Buf()
        actT = S("ms_a", [128, KC, CAP], BF16); actb = Buf()
        ye = S("ms_ye", [128, NCC, D], BF16); yeb = Buf()
        selr = Rot([S("ms_sel%d" % i, [128, NTH, CAP], BF16) for i in range(2)])
        seltr = Rot([S("ms_selT%d" % i, [128, NCC, HT], BF16) for i in range(2)])
        wstage = Rot([S("ms_ws%d" % i, [128, KC, 256]) for i in range(2)])
        wbf = Rot([S("ms_wb%d" % i, [128, KC, 256], BF16) for i in range(2)])
        wbf_x = {id(b): (Buf(), Buf()) for b in wbf.bufs}
        gate = S("ms_gate", [128, NTH, N_EXP]); gateb = Buf()
        rk = S("ms_rk", [128, NTH, N_EXP]); rkb = Buf()
        msk = S("ms_msk", [128, NTH, N_EXP]); mskb = Buf()
        rkT = S("ms_rkT", [N_EXP, HT]); rkTb = Buf()
        cb = Buf("msc")
        iota_c = S("ms_iota", [128, CAP]); t.dma("sp", iota_c[:, :], c.k["me_iota"][:, :], writes=[cb])
        pidx = S("ms_pidx", [128, NCC]); t.dma("sp", pidx[:, :], c.k["me_pidx"][:, :], writes=[cb])
        ustr = S("ms_us", [128, 128]); t.dma("sp", ustr[:, :], c.k["me_ustrict"][:, :], writes=[cb])
        ones = S("ms_ones", [128, 128]); G(lambda e: e.memset(ones[:, :], 1.0), [], [cb])
        c7 = S("ms_c7", [128, 1]); G(lambda e: e.memset(c7[:, :], 7.0), [], [cb])
        seler = Rot([S("ms_sele%d" % i, [N_EXP, 128]) for i in range(2)])
        bgu = Rot([S("ms_bgu%d" % i, [128, 32]) for i in range(2)])
        bgs = Rot([S("ms_bgs%d" % i, [128, 16]) for i in range(2)])
        items = []
        for ex in range(c.moe_nexp):
            items += [(ex, "gu", fc) for fc in range(16)]
            items += [(ex, "dn", db) for db in range(8)]

        def load_item(it):
            ex, kind, j = it
            stg, stgb = wstage.next()
            src = (c.w_gate_up if kind == "gu" else c.w_down)[layer][ex, j]
            t.dma("sp", stg[:, :, :].rearrange("p k c -> p (k c)"), src, writes=[stgb])
            bf, bfb = wbf.next()
            b2, b3 = wbf_x[id(bfb)]
            A(lambda e: e.copy(bf[:, 0:9, :], stg[:, 0:9, :]), [stgb], [bfb])
            V(lambda e: e.tensor_copy(bf[:, 9:14, :], stg[:, 9:14, :]), [stgb], [b2])
            G(lambda e: e.tensor_copy(bf[:, 14:16, :], stg[:, 14:16, :]), [stgb], [b3])
            return bf, [bfb, b2, b3]

        for hf in range(2):
            t0 = hf * HT
            t.dma("sp", xtm[:, :, :], c.d_xtm[0][t0:t0 + HT, :].rearrange("(n p) d -> p n d", p=128),
                  reads=[c.d_xtm[1]], writes=[xtmb])
            t.dma("sp", gate[:, :, :], c.d_gate[0][t0:t0 + HT, :].rearrange("(n p) e -> p n e", p=128),
                  reads=[c.d_gate[1]], writes=[gateb])
            (sa, sab), (sb_, sbb) = wstage.next(), wstage.next()
            bd = sa[0:N_EXP, 0:8, :].rearrange("p k c -> p (k c)")
            gateT = sb_[0:N_EXP, 0:4, :].rearrange("p k c -> p (k c)")
            t.dma("sp", gateT, c.d_gateT[0][:, t0:t0 + HT], reads=[c.d_gateT[1]], writes=[sbb])
            t.dma("sp", bd, c.b_down[layer], writes=[sab])
            for tt in range(NTH):
                for cb4 in range(4):
                    ps, pb = c.psum.next()
                    PE(lambda e: e.matmul(ps[:, :], gateT[:, tt * 128:(tt + 1) * 128], bd[:, cb4 * 512:(cb4 + 1) * 512],
                                          start=True, stop=True), [sab, sbb], [pb])
                    evac(c, yacc[:, tt, cb4 * 512:(cb4 + 1) * 512], ps[:, :], [pb], [yab])
            V(lambda e: e.tensor_scalar(msk[:, :, :], gate[:, :, :], 0.0, None, op0=ALU.is_gt), [gateb], [mskb])
            for n in range(NTH):
                ps, pb = c.psum.next()
                for m in range(n):
                    PE(lambda e: e.matmul(ps[:, 0:N_EXP], ones[:, :], msk[:, m, :], start=(m == 0), stop=False),
                       [cb, mskb], [pb])
                PE(lambda e: e.matmul(ps[:, 0:N_EXP], ustr[:, :], msk[:, n, :], start=(n == 0), stop=True),
                   [cb, mskb], [pb])
                V(lambda e: e.tensor_tensor(rk[:, n, :], ps[:, 0:N_EXP], msk[:, n, :], op=ALU.mult), [pb, mskb], [rkb])
                V(lambda e: e.tensor_tensor(rk[:, n, :], rk[:, n, :], msk[:, n, :], op=ALU.add), [rkb, mskb], [rkb])
                V(lambda e: e.tensor_scalar(rk[:, n, :], rk[:, n, :], -1.0, None, op0=ALU.add), [rkb], [rkb])
                ps2, pb2 = c.psum.next()
                PE(lambda e: e.transpose(ps2[0:N_EXP, 0:128], rk[:, n, :], c.ident[:, :]), [rkb, c.ident_buf], [pb2])
                A(lambda e: e.copy(rkT[:, n * 128:(n + 1) * 128], ps2[0:N_EXP, 0:128]), [pb2], [rkTb])
            nxt = load_item(items[0])
            bg = bgb = bs = bsb = None
            sel = selb = selT = selTb = None
            for ii, (ex, kind, j) in enumerate(items):
                wt, wtb = nxt
                if ii + 1 < len(items):
                    nxt = load_item(items[ii + 1])
                if kind == "gu":
                    fc = j
                    if fc == 0:
                        bg, bgb = bgu.next()
                        load_cols(c, bg, bgb, c.b_gate_up[layer][ex], 32)
                        bs, bsb = bgs.next()
                        V(lambda e: e.tensor_scalar(bs[:, :], bg[:, 0:16], 1.702, None, op0=ALU.mult), [bgb], [bsb])
                        sel, selb = selr.next()
                        for n in range(NTH):
                            V(lambda e: e.tensor_scalar(sel[:, n, :], iota_c[:, :], rk[:, n, ex:ex + 1], None,
                                                        op0=ALU.is_equal), [cb, rkb], [selb])
                        se, seb = seler.next()
                        t.dma("sp", se[:, :], c.k["me_sele"][ex], writes=[seb])
                        selT, selTb = seltr.next()
                        for th in range(HT // 512):
                            psb, pbb = c.psum.next()
                            PE(lambda e: e.matmul(psb[:, :], se[:, :], rkT[:, th * 512:(th + 1) * 512], start=True, stop=True),
                               [seb, rkTb], [pbb])
                            for cc in range(NCC):
                                V(lambda e: e.tensor_scalar(selT[:, cc, th * 512:(th + 1) * 512], psb[:, :], pidx[:, cc:cc + 1],
                                                            None, op0=ALU.is_equal), [pbb, cb], [selTb])
                        for kc in range(KC):
                            psx, pbx = c.psum.next()
                            for n in range(NTH):
                                PE(lambda e: e.matmul(psx[:, 0:CAP], xtm[:, n, kc * 128:(kc + 1) * 128], sel[:, n, :],
                                                      start=(n == 0), stop=(n == NTH - 1)), [xtmb, selb], [pbx])
                            evac(c, xeT[:, kc, :], psx[:, 0:CAP], [pbx], [xeb])
                    psg, pbg = c.psum.next()
                    for kc in range(KC):
                        PE(lambda e: e.matmul(psg[:, 0:CAP], wt[:, kc, 0:128], xeT[:, kc, :],
                                              start=(kc == 0), stop=(kc == KC - 1)), wtb + [xeb], [pbg])
                    psu, pbu = c.psum.next()
                    for kc in range(KC):
                        PE(lambda e: e.matmul(psu[:, 0:CAP], wt[:, kc, 128:256], xeT[:, kc, :],
                                              start=(kc == 0), stop=(kc == KC - 1)), wtb + [xeb], [pbu])
                    g1, g1b = gs_.next(); s1, s1b = sg_.next(); u1, u1b = us_.next()
                    A(lambda e: e.activation(s1[:, :], psg[:, 0:CAP], AF.Sigmoid, bias=bs[:, fc:fc + 1], scale=1.702),
                      [pbg, bsb], [s1b])
                    V(lambda e: e.tensor_scalar(g1[:, :], psg[:, 0:CAP], bg[:, fc:fc + 1], c7[:, 0:1], op0=ALU.add, op1=ALU.min),
                      [pbg, bgb, cb], [g1b])
                    V(lambda e: e.tensor_scalar(u1[:, :], psu[:, 0:CAP], bg[:, 16 + fc:17 + fc], c7[:, 0:1], op0=ALU.add, op1=ALU.min),
                      [pbu, bgb, cb], [u1b])
                    V(lambda e: e.scalar_tensor_tensor(out=g1[:, :], in0=s1[:, :], scalar=float(SIG7), in1=g1[:, :],
                                                       op0=ALU.min, op1=ALU.mult), [s1b, g1b], [g1b])
                    V(lambda e: e.tensor_scalar(u1[:, :], u1[:, :], -7.0, 1.0, op0=ALU.max, op1=ALU.add), [u1b], [u1b])
                    V(lambda e: e.tensor_tensor(actT[:, fc, :], g1[:, :], u1[:, :], op=ALU.mult), [g1b, u1b], [actb])
                else:
                    db = j
                    for cc in range(NCC):
                        ps, pb = c.psum.next()
                        for kc in range(KC):
                            PE(lambda e: e.matmul(ps[:, 0:256], actT[:, kc, cc * 128:(cc + 1) * 128], wt[:, kc, :],
                                                  start=(kc == 0), stop=(kc == KC - 1)), [actb] + wtb, [pb])
                        A(lambda e: e.copy(ye[:, cc, db * 256:(db + 1) * 256], ps[:, 0:256]), [pb], [yeb])
                    if db == 7:
                        for n in range(NTH):
                            for b4 in range(4):
                                ps, pb = c.psum.next()
                                for cc in range(NCC):
                                    PE(lambda e: e.matmul(ps[:, :], selT[:, cc, n * 128:(n + 1) * 128],
                                                          ye[:, cc, b4 * 512:(b4 + 1) * 512],
                                                          start=(cc == 0), stop=(cc == NCC - 1)), [selTb, yeb], [pb])
                                V(lambda e: e.scalar_tensor_tensor(
                                    out=yacc[:, n, b4 * 512:(b4 + 1) * 512], in0=ps[:, :], scalar=gate[:, n, ex:ex + 1],
                                    in1=yacc[:, n, b4 * 512:(b4 + 1) * 512], op0=ALU.mult, op1=ALU.add),
                                  [pb, gateb, yab], [yab])
            t.dma("sp", c.d_ffn[0][t0:t0 + HT, :].rearrange("(n p) d -> p n d", p=128), yacc[:, :, :],
                  reads=[yab], writes=[c.d_ffn[1]])
        t.barrier()
```
